# Optimizing a Trainium2 kernel written in Bass

```python
import jax
import jax.numpy as jnp
from jax import lax
import numpy as np

D_MODEL = 1024
BATCH = 8
SEQ = 2048
DEPTH = 1

HEAD_DIM = 64
NSA_HEADS = 8
NSA_KV_GROUPS = 2
NSA_CMP_BLOCK = 32
NSA_CMP_STRIDE = 16
NSA_CMP_HIDDEN = 128
NSA_SEL_BLOCK = 64
NSA_SEL_TOPK = 16
NSA_WINDOW = 512
WIN_QBLOCK = 128
NSA_QUERY_CHUNK = 32
MOBA_HEADS = 8
MOBA_BLOCK = 256
MOBA_TOPK = 3
MOBA_QUERY_CHUNK = 16
D_FF = 2816
PLE_DIM = 256
RMS_EPS = 1e-6
NEG_INF = -1e30
TINY = 1e-30
FORCE_SCORE = 1e9

NSA_WIDTH = NSA_HEADS * HEAD_DIM
NSA_KV_WIDTH = NSA_KV_GROUPS * HEAD_DIM
MOBA_WIDTH = MOBA_HEADS * HEAD_DIM
IN_SPLITS = (NSA_WIDTH,) + (NSA_KV_WIDTH,) * 6 + (3 * NSA_HEADS,) + (MOBA_WIDTH,) * 3 + (D_MODEL, D_MODEL)
IN_WIDTH = sum(IN_SPLITS)

kernel_name = "nsa_moba_macaron_hybrid"


def _rmsnorm(x, g):
    xf = x.astype(jnp.float32)
    y = xf * lax.rsqrt(jnp.mean(xf * xf, axis=-1, keepdims=True) + RMS_EPS)
    return (y * g.astype(jnp.float32)).astype(x.dtype)


def _swiglu(x, w1, w3, w2):
    return (jax.nn.silu(x @ w1) * (x @ w3)) @ w2


def _softmax_stats(s, mask):
    s = jnp.where(mask, s.astype(jnp.float32), NEG_INF)
    m = jnp.max(s, axis=-1, keepdims=True)
    e = jnp.where(mask, jnp.exp(s - m), 0.0)
    return e, m, jnp.sum(e, axis=-1, keepdims=True)


def _masked_softmax(s, mask):
    e, _, l = _softmax_stats(s, mask)
    return e / jnp.maximum(l, TINY)


def _alibi_slopes():
    n = NSA_HEADS + MOBA_HEADS
    slopes = jnp.exp2(-8.0 * (jnp.arange(n, dtype=jnp.float32) + 1.0) / n)
    return slopes[0::2], slopes[1::2]


def _nsa_attention(q, k_cmp, v_cmp, k_slc, v_slc, k_win, v_win, gate_logits,
                   cmp_pos_k, cmp_w1_k, cmp_w2_k, cmp_pos_v, cmp_w1_v, cmp_w2_v, slopes):
    B, S = q.shape[0], q.shape[1]
    G, Hg, hd = NSA_KV_GROUPS, NSA_HEADS // NSA_KV_GROUPS, HEAD_DIM
    scale = hd ** -0.5
    qg = q.reshape(B, S, G, Hg, hd).transpose(0, 2, 3, 1, 4)
    k_cmp, v_cmp, k_slc, v_slc, k_win, v_win = [t.transpose(0, 2, 1, 3) for t in
                                                (k_cmp, v_cmp, k_slc, v_slc, k_win, v_win)]
    sl = slopes.reshape(G, Hg)
    t_pos = jnp.arange(S)

    n_c = (S - NSA_CMP_BLOCK) // NSA_CMP_STRIDE + 1
    blk_idx = np.arange(n_c)[:, None] * NSA_CMP_STRIDE + np.arange(NSA_CMP_BLOCK)[None, :]

    def compress(t, pos, w1, w2):
        blocks = t[:, :, blk_idx] + pos
        flat = blocks.reshape(B, G, n_c, NSA_CMP_BLOCK * hd)
        return jax.nn.gelu(flat @ w1) @ w2

    kc = compress(k_cmp, cmp_pos_k, cmp_w1_k, cmp_w2_k)
    vc = compress(v_cmp, cmp_pos_v, cmp_w1_v, cmp_w2_v)
    c_end = jnp.arange(n_c) * NSA_CMP_STRIDE + NSA_CMP_BLOCK - 1
    dist_c = t_pos[:, None] - c_end[None, :]
    s_c = (jnp.einsum('bghtd,bgcd->bghtc', qg, kc).astype(jnp.float32) * scale
           - sl[:, :, None, None] * dist_c.astype(jnp.float32))
    p_c = _masked_softmax(s_c, dist_c >= 0)
    o_cmp = jnp.einsum('bghtc,bgcd->bghtd', p_c.astype(vc.dtype), vc)

    n_sel = S // NSA_SEL_BLOCK
    c_start_np = np.arange(n_c) * NSA_CMP_STRIDE
    c_end_np = c_start_np + NSA_CMP_BLOCK - 1
    s_start_np = np.arange(n_sel) * NSA_SEL_BLOCK
    s_end_np = s_start_np + NSA_SEL_BLOCK - 1
    overlap = ((c_start_np[:, None] <= s_end_np[None, :]) &
               (c_end_np[:, None] >= s_start_np[None, :])).astype(np.float32)
    imp = jnp.einsum('bghtc,cj->bgtj', p_c, jnp.asarray(overlap))
    blk = jnp.arange(n_sel)[None, :]
    cur = (t_pos // NSA_SEL_BLOCK)[:, None]
    causal = blk * NSA_SEL_BLOCK <= t_pos[:, None]
    forced = (blk == 0) | (blk == cur) | (blk == cur - 1)
    imp = jnp.where(causal, jnp.where(forced, FORCE_SCORE, imp), NEG_INF)
    k_top = min(NSA_SEL_TOPK, n_sel)
    _, sel_idx = lax.top_k(imp, k_top)
    k_blk = k_slc.reshape(B, G, n_sel, NSA_SEL_BLOCK, hd)
    v_blk = v_slc.reshape(B, G, n_sel, NSA_SEL_BLOCK, hd)
    C = NSA_QUERY_CHUNK
    nq = S // C
    q_ch = qg.reshape(B, G, Hg, nq, C, hd).transpose(3, 0, 1, 2, 4, 5)
    i_ch = sel_idx.reshape(B, G, nq, C, k_top).transpose(2, 0, 1, 3, 4)
    t_ch = t_pos.reshape(nq, C)
    b_ix = jnp.arange(B)[:, None, None, None]
    g_ix = jnp.arange(G)[None, :, None, None]
    n_keys = k_top * NSA_SEL_BLOCK

    def sel_chunk(args):
        qc, ic, tc = args
        kg = k_blk[b_ix, g_ix, ic].reshape(B, G, C, n_keys, hd)
        vg = v_blk[b_ix, g_ix, ic].reshape(B, G, C, n_keys, hd)
        kpos = (ic[..., None] * NSA_SEL_BLOCK + jnp.arange(NSA_SEL_BLOCK)).reshape(B, G, C, n_keys)
        dist = tc[:, None] - kpos
        s = (jnp.einsum('bghcd,bgcnd->bghcn', qc, kg).astype(jnp.float32) * scale
             - sl[None, :, :, None, None] * dist.astype(jnp.float32)[:, :, None])
        p = _masked_softmax(s, (dist >= 0)[:, :, None])
        return jnp.einsum('bghcn,bgcnd->bghcd', p.astype(vg.dtype), vg)

    o_sel = lax.map(sel_chunk, (q_ch, i_ch, t_ch))
    o_sel = o_sel.transpose(1, 2, 3, 0, 4, 5).reshape(B, G, Hg, S, hd)

    WB = WIN_QBLOCK
    nwb = S // WB
    nw = NSA_WINDOW // WB

    def windows(t):
        tp = jnp.pad(t, ((0, 0), (0, 0), (nw * WB, 0), (0, 0))).reshape(B, G, nwb + nw, WB, hd)
        return jnp.concatenate([tp[:, :, i:i + nwb] for i in range(nw + 1)], axis=3)

    kw = windows(k_win).transpose(2, 0, 1, 3, 4)
    vw = windows(v_win).transpose(2, 0, 1, 3, 4)
    q_blocks = qg.reshape(B, G, Hg, nwb, WB, hd).transpose(3, 0, 1, 2, 4, 5)
    qpos = jnp.arange(nwb)[:, None] * WB + jnp.arange(WB)[None, :]
    kpos_w = jnp.arange(nwb)[:, None] * WB - nw * WB + jnp.arange((nw + 1) * WB)[None, :]

    def win_block(args):
        qb, kb, vb, qp, kp = args
        dist = qp[:, None] - kp[None, :]
        mask = (dist >= 0) & (dist < NSA_WINDOW) & (kp[None, :] >= 0)
        s = (jnp.einsum('bghqd,bgkd->bghqk', qb, kb).astype(jnp.float32) * scale
             - sl[None, :, :, None, None] * dist.astype(jnp.float32))
        p = _masked_softmax(s, mask)
        return jnp.einsum('bghqk,bgkd->bghqd', p.astype(vb.dtype), vb)

    o_win = lax.map(win_block, (q_blocks, kw, vw, qpos, kpos_w))
    o_win = o_win.transpose(1, 2, 3, 0, 4, 5).reshape(B, G, Hg, S, hd)

    g = jax.nn.sigmoid(gate_logits.astype(jnp.float32)).transpose(0, 2, 3, 1, 4)
    o = g[..., 0:1] * o_cmp + g[..., 1:2] * o_sel + g[..., 2:3] * o_win
    return o.transpose(0, 3, 1, 2, 4).reshape(B, S, NSA_WIDTH).astype(q.dtype)


def _moba_attention(q, k, v, slopes):
    B, S, H, hd = q.shape
    L = MOBA_BLOCK
    nb = -(-S // L)
    S_pad = nb * L
    scale = hd ** -0.5
    q, k, v = [jnp.pad(t.transpose(0, 2, 1, 3), ((0, 0), (0, 0), (0, S_pad - S), (0, 0)))
               for t in (q, k, v)]
    t_pos = jnp.arange(S_pad)
    k_blk = k.reshape(B, H, nb, L, hd)
    v_blk = v.reshape(B, H, nb, L, hd)
    q_blk = q.reshape(B, H, nb, L, hd)

    rel = jnp.arange(L)[:, None] - jnp.arange(L)[None, :]
    s_own = (jnp.einsum('bhnqd,bhnkd->bhnqk', q_blk, k_blk).astype(jnp.float32) * scale
             - slopes[None, :, None, None, None] * rel.astype(jnp.float32))
    e_own, m_own, l_own = _softmax_stats(s_own, rel >= 0)
    o_own = jnp.einsum('bhnqk,bhnkd->bhnqd', e_own, v_blk.astype(jnp.float32)).reshape(B, H, S_pad, hd)
    m_own = m_own.reshape(B, H, S_pad, 1)
    l_own = l_own.reshape(B, H, S_pad, 1)

    k_sel = min(MOBA_TOPK, nb - 1)
    if k_sel > 0:
        k_mean = jnp.mean(k_blk.astype(jnp.float32), axis=3)
        gs = jnp.einsum('bhtd,bhnd->bhtn', q.astype(jnp.float32), k_mean)
        cur = t_pos // L
        past = jnp.arange(nb)[None, :] < cur[:, None]
        gs = jnp.where(past, gs, NEG_INF)
        _, idx = lax.top_k(gs, k_sel)
        valid = idx < cur[:, None]
        C = MOBA_QUERY_CHUNK
        nq = S_pad // C
        q_ch = q.reshape(B, H, nq, C, hd).transpose(2, 0, 1, 3, 4)
        i_ch = idx.reshape(B, H, nq, C, k_sel).transpose(2, 0, 1, 3, 4)
        v_ch = valid.reshape(B, H, nq, C, k_sel).transpose(2, 0, 1, 3, 4)
        t_ch = t_pos.reshape(nq, C)
        b_ix = jnp.arange(B)[:, None, None, None]
        h_ix = jnp.arange(H)[None, :, None, None]
        n_keys = k_sel * L

        def sel_chunk(args):
            qc, ic, vc, tc = args
            kg = k_blk[b_ix, h_ix, ic].reshape(B, H, C, n_keys, hd)
            vg = v_blk[b_ix, h_ix, ic].reshape(B, H, C, n_keys, hd)
            kpos = (ic[..., None] * L + jnp.arange(L)).reshape(B, H, C, n_keys)
            mask = jnp.broadcast_to(vc[..., None], (B, H, C, k_sel, L)).reshape(B, H, C, n_keys)
            dist = (tc[:, None] - kpos).astype(jnp.float32)
            s = (jnp.einsum('bhcd,bhcnd->bhcn', qc, kg).astype(jnp.float32) * scale
                 - slopes[None, :, None, None] * dist)
            e, m, l = _softmax_stats(s, mask)
            return jnp.einsum('bhcn,bhcnd->bhcd', e, vg.astype(jnp.float32)), m, l

        o_s, m_s, l_s = lax.map(sel_chunk, (q_ch, i_ch, v_ch, t_ch))
        o_s, m_s, l_s = [a.transpose(1, 2, 0, 3, 4).reshape(B, H, S_pad, a.shape[-1]) for a in (o_s, m_s, l_s)]
        m_tot = jnp.maximum(m_own, m_s)
        a_own = jnp.exp(m_own - m_tot)
        a_s = jnp.exp(m_s - m_tot)
        o = (o_own * a_own + o_s * a_s) / (l_own * a_own + l_s * a_s)
    else:
        o = o_own / l_own
    return o[:, :, :S].transpose(0, 2, 1, 3).reshape(B, S, H * hd).astype(k.dtype)


def _layer(h, p_i, ffn1_norm, ffn1_w1, ffn1_w3, ffn1_w2, mix_norm, w_in,
           cmp_pos_k, cmp_w1_k, cmp_w2_k, cmp_pos_v, cmp_w1_v, cmp_w2_v,
           w_up_nsa, w_up_moba, w_out, ffn2_norm, ffn2_w1, ffn2_w3, ffn2_w2,
           ple_norm, w_ple_gate, w_ple):
    B, S = h.shape[0], h.shape[1]
    h = h + 0.5 * _swiglu(_rmsnorm(h, ffn1_norm), ffn1_w1, ffn1_w3, ffn1_w2)
    u = _rmsnorm(h, mix_norm)
    z = u @ w_in
    offs = np.cumsum((0,) + IN_SPLITS)
    parts = [z[..., int(offs[i]):int(offs[i + 1])] for i in range(len(IN_SPLITS))]
    q_n = parts[0].reshape(B, S, NSA_HEADS, HEAD_DIM)
    kv_n = [t.reshape(B, S, NSA_KV_GROUPS, HEAD_DIM) for t in parts[1:7]]
    g_n = parts[7].reshape(B, S, NSA_KV_GROUPS, NSA_HEADS // NSA_KV_GROUPS, 3)
    q_m, k_m, v_m = [t.reshape(B, S, MOBA_HEADS, HEAD_DIM) for t in parts[8:11]]
    gate_a, gate_b = parts[11], parts[12]
    slopes_n, slopes_m = _alibi_slopes()
    y_n = _nsa_attention(q_n, kv_n[0], kv_n[1], kv_n[2], kv_n[3], kv_n[4], kv_n[5], g_n,
                         cmp_pos_k, cmp_w1_k, cmp_w2_k, cmp_pos_v, cmp_w1_v, cmp_w2_v, slopes_n) @ w_up_nsa
    y_m = _moba_attention(q_m, k_m, v_m, slopes_m) @ w_up_moba
    mixed = jax.nn.sigmoid(gate_a) * y_n + jax.nn.sigmoid(gate_b) * y_m
    h = h + mixed @ w_out
    h = h + 0.5 * _swiglu(_rmsnorm(h, ffn2_norm), ffn2_w1, ffn2_w3, ffn2_w2)
    gate_p = jax.nn.sigmoid(_rmsnorm(h, ple_norm) @ w_ple_gate)
    h = h + gate_p * (p_i @ w_ple).astype(h.dtype)
    return h


def setup_inputs(seed: int = 0) -> dict:
    key = jax.random.key(seed)
    ks = iter(jax.random.split(key, 40))

    def nrm(shape, scale):
        return jax.random.normal(next(ks), shape, jnp.float32) * scale

    def gain(shape):
        return 1.0 + nrm(shape, 0.02)

    L, D, F = DEPTH, D_MODEL, D_FF
    cmp_in = NSA_CMP_BLOCK * HEAD_DIM
    return {
        "x": nrm((BATCH, SEQ, D), 1.0),
        "p": nrm((L, BATCH, SEQ, PLE_DIM), 1.0),
        "ffn1_norm": gain((L, D)),
        "ffn1_w1": nrm((L, D, F), D ** -0.5),
        "ffn1_w3": nrm((L, D, F), D ** -0.5),
        "ffn1_w2": nrm((L, F, D), F ** -0.5),
        "mix_norm": gain((L, D)),
        "w_in": nrm((L, D, IN_WIDTH), D ** -0.5),
        "cmp_pos_k": nrm((L, NSA_CMP_BLOCK, HEAD_DIM), 0.1),
        "cmp_w1_k": nrm((L, cmp_in, NSA_CMP_HIDDEN), cmp_in ** -0.5),
        "cmp_w2_k": nrm((L, NSA_CMP_HIDDEN, HEAD_DIM), NSA_CMP_HIDDEN ** -0.5),
        "cmp_pos_v": nrm((L, NSA_CMP_BLOCK, HEAD_DIM), 0.1),
        "cmp_w1_v": nrm((L, cmp_in, NSA_CMP_HIDDEN), cmp_in ** -0.5),
        "cmp_w2_v": nrm((L, NSA_CMP_HIDDEN, HEAD_DIM), NSA_CMP_HIDDEN ** -0.5),
        "w_up_nsa": nrm((L, NSA_WIDTH, D), NSA_WIDTH ** -0.5),
        "w_up_moba": nrm((L, MOBA_WIDTH, D), MOBA_WIDTH ** -0.5),
        "w_out": nrm((L, D, D), D ** -0.5),
        "ffn2_norm": gain((L, D)),
        "ffn2_w1": nrm((L, D, F), D ** -0.5),
        "ffn2_w3": nrm((L, D, F), D ** -0.5),
        "ffn2_w2": nrm((L, F, D), F ** -0.5),
        "ple_norm": gain((L, D)),
        "w_ple_gate": nrm((L, D, D), D ** -0.5),
        "w_ple": nrm((L, PLE_DIM, D), PLE_DIM ** -0.5),
        "final_norm": gain((D,)),
    }


def reference(x, p, ffn1_norm, ffn1_w1, ffn1_w3, ffn1_w2, mix_norm, w_in,
              cmp_pos_k, cmp_w1_k, cmp_w2_k, cmp_pos_v, cmp_w1_v, cmp_w2_v,
              w_up_nsa, w_up_moba, w_out, ffn2_norm, ffn2_w1, ffn2_w3, ffn2_w2,
              ple_norm, w_ple_gate, w_ple, final_norm):
    h = x
    for i in range(DEPTH):
        h = _layer(h, p[i], ffn1_norm[i], ffn1_w1[i], ffn1_w3[i], ffn1_w2[i], mix_norm[i], w_in[i],
                   cmp_pos_k[i], cmp_w1_k[i], cmp_w2_k[i], cmp_pos_v[i], cmp_w1_v[i], cmp_w2_v[i],
                   w_up_nsa[i], w_up_moba[i], w_out[i], ffn2_norm[i], ffn2_w1[i], ffn2_w3[i], ffn2_w2[i],
                   ple_norm[i], w_ple_gate[i], w_ple[i])
    return _rmsnorm(h, final_norm)
```

```python
import os
import numpy as np
import ml_dtypes
import concourse.bass as bass
import concourse.mybir as mybir
from concourse.bass_utils import run_bass_kernel_spmd
from contextlib import ExitStack

F32 = mybir.dt.float32
BF16 = mybir.dt.bfloat16
AF = mybir.ActivationFunctionType
ALU = mybir.AluOpType

T = 2048
D = 1024
FF = 2816
MASKV = -240000.0
EPS = 1e-6
TINY = 1e-30
IN_W = 4888


def _slopes():
    s = 2.0 ** (-(np.arange(16) + 1) / 2.0)
    return s[0::2].copy(), s[1::2].copy()


def _make_consts():
    p = np.arange(128)
    f32 = {}
    b16 = {}
    f32["ident"] = np.eye(128)
    b16["ident"] = np.eye(128)
    b16["ones"] = np.ones((128, 128))
    j = np.arange(128)
    b16["triA"] = np.where(j[None, :] >= p[:, None], 0.0, MASKV)
    b16["triB"] = np.where(j[None, :] < p[:, None], 0.0, MASKV)
    tq = np.arange(T)
    cend = 16 * p + 31
    cm = np.where(tq[None, :] >= cend[:, None], 0.0, MASKV)
    cm[127, :] = MASKV
    ov = np.zeros((128, 32))
    for c in range(127):
        for jj in range(32):
            if 16 * c <= 64 * jj + 63 and 16 * c + 31 >= 64 * jj:
                ov[c, jj] = 1.0
    b16["overlap"] = ov
    b16["cmpmask"] = cm
    es = np.zeros((128, 16, 128))
    for pp in range(128):
        jj = pp % 64
        if jj >= 32:
            continue
        for kt in range(16):
            for m_ in range(128):
                if jj == 2 * kt + m_ // 64:
                    es[pp, kt, m_] = 1.0
    b16["esel"] = es.reshape(128, 2048)
    sl_n, sl_m = _slopes()
    sl16 = np.concatenate([sl_n, sl_m])
    d = np.arange(16) - 12
    f32["bw"] = (sl16[None, :, None] * (128 * d[None, None, :] + p[:, None, None] - 256)).reshape(128, 256)
    d2 = np.arange(16) - 15
    f32["bn"] = (sl16[None, :, None] * (128 * d2[None, None, :] + p[:, None, None] - 64)).reshape(128, 256)
    f32["cw"] = (sl_n[None, :, None] * (cend[:, None, None] - (512 * np.arange(4)[None, None, :] + 256))).reshape(128, 32)
    f32["cn"] = (sl_n[None, :, None] * (cend[:, None, None] - (128 * np.arange(16)[None, None, :] + 64))).reshape(128, 128)
    keep = np.zeros((128, 8, 32))
    force = np.zeros((128, 8, 32))
    for tt in range(8, 16):
        for pp in range(128):
            t = tt * 128 + pp
            cur = t // 64
            for jj in range(32):
                if 64 * jj > t:
                    force[pp, tt - 8, jj] = -1e30 * (1.0 + jj / 64.0)
                elif jj == 0:
                    force[pp, tt - 8, jj] = 3e9
                elif jj == cur:
                    force[pp, tt - 8, jj] = 2e9
                elif jj == cur - 1:
                    force[pp, tt - 8, jj] = 1e9
                else:
                    keep[pp, tt - 8, jj] = 1.0
    padneg = np.zeros((128, 8, 8, 8))
    ownhot = np.zeros((128, 8, 8, 8))
    for tt in range(8, 16):
        cur = tt // 2
        for n in range(8):
            if n >= cur:
                padneg[:, tt - 8, :, n] = -1e30
            if n == cur:
                ownhot[:, tt - 8, :, n] = 1.0
    f32["eps"] = np.full((128, 1), EPS)
    rt = []
    for tr in (2, 3):
        a = (tr - 2) * 4
        rt += [keep[:, a:a + 4].reshape(128, 128), force[:, a:a + 4].reshape(128, 128),
               padneg[:, a:a + 4].reshape(128, 256), ownhot[:, a:a + 4].reshape(128, 256)]
    f32["rt"] = np.concatenate(rt, axis=1)
    offs_f = {}
    cols = []
    o = 0
    for k, v in f32.items():
        offs_f[k] = o
        o += v.shape[1]
        cols.append(v.astype(np.float32))
    cf = np.ascontiguousarray(np.concatenate(cols, axis=1))
    offs_b = {}
    cols = []
    o = 0
    for k, v in b16.items():
        offs_b[k] = o
        o += v.shape[1]
        cols.append(v.astype(np.float32))
    cbv = np.ascontiguousarray(np.concatenate(cols, axis=1).astype(ml_dtypes.bfloat16))
    return cf, offs_f, cbv, offs_b


def _box(ap):
    t = ap.tensor
    if t.name.startswith("ps") and len(t.name) == 3:
        return (t.name, 0, 128, 0, 512)
    row = 1
    for s in tuple(t.shape)[1:]:
        row *= int(s)
    off = int(ap.offset)
    p0 = off // row
    f0 = off % row
    apl = ap.ap
    npart = apl[0][1]
    ext = 1
    for st, cnt in apl[1:]:
        ext += (cnt - 1) * abs(st)
    return (t.name, p0, p0 + npart, f0, f0 + ext)


class Sched:
    def __init__(self, nc, ndma=24):
        self.nc = nc
        self.engs = {"pe": nc.tensor, "act": nc.scalar, "dve": nc.vector, "pool": nc.gpsimd, "sp": nc.sync}
        self.semh = {}
        for e in self.engs:
            self.semh[e] = nc.alloc_semaphore("sem_" + e)
        self.cnt = {e: 0 for e in self.engs}
        self.ndma = ndma
        for i in range(ndma):
            self.semh[("d", i)] = nc.alloc_semaphore("dsem%d" % i)
        self.dcnt = [0] * ndma
        self.drr = 0
        self.drr_pool = 0
        self.seen = {e: {} for e in self.engs}
        self.acc = {}
        self.out_tickets = []
        self.nwaits = 0

    def _wait(self, e, sk, val):
        if e == "pe" and sk == "pe":
            return
        if self.seen[e].get(sk, 0) >= val:
            return
        self.engs[e].wait_ge(self.semh[sk], val)
        self.seen[e][sk] = val
        self.nwaits += 1

    def _collect(self, reads, writes):
        waits = {}
        for ap in reads:
            b = _box(ap)
            for ent in self.acc.get(b[0], ()):
                if ent[0] == "w" and ent[3] < b[2] and b[1] < ent[4] and ent[5] < b[4] and b[3] < ent[6]:
                    if waits.get(ent[1], 0) < ent[2]:
                        waits[ent[1]] = ent[2]
        for ap in writes:
            b = _box(ap)
            for ent in self.acc.get(b[0], ()):
                if ent[3] < b[2] and b[1] < ent[4] and ent[5] < b[4] and b[3] < ent[6]:
                    if waits.get(ent[1], 0) < ent[2]:
                        waits[ent[1]] = ent[2]
        return waits

    def _record(self, sk, val, reads, writes):
        for ap in writes:
            b = _box(ap)
            lst = self.acc.setdefault(b[0], [])
            lst[:] = [e for e in lst if not (b[1] <= e[3] and e[4] <= b[2] and b[3] <= e[5] and e[6] <= b[4])]
            lst.append(("w", sk, val, b[1], b[2], b[3], b[4]))
        for ap in reads:
            b = _box(ap)
            lst = self.acc.setdefault(b[0], [])
            lst[:] = [e for e in lst if not (e[0] == "r" and e[1] == sk and b[1] <= e[3] and e[4] <= b[2]
                                             and b[3] <= e[5] and e[6] <= b[4])]
            lst.append(("r", sk, val, b[1], b[2], b[3], b[4]))

    def op(self, e, fn, reads, writes, inc=True):
        assert inc or e == "pe"
        waits = self._collect(reads, writes)
        for sk, val in waits.items():
            self._wait(e, sk, val)
        ins = fn()
        if inc:
            self.cnt[e] += 1
            ins.then_inc(self.semh[e], 1)
            val = self.cnt[e]
        else:
            val = self.cnt[e] + 1
        self._record(e, val, reads, writes)
        return ins

    def dma(self, q, out, in_, reads=(), writes=(), is_out=False, **kw):
        waits = self._collect(reads, writes)
        for sk, val in waits.items():
            self._wait(q, sk, val)
        half = self.ndma // 2
        if q == "pool":
            i = half + (self.drr_pool % half)
            self.drr_pool += 1
        else:
            i = self.drr % half
            self.drr += 1
        sk = ("d", i)
        if self.dcnt[i] > 0:
            self._wait(q, sk, self.dcnt[i])
        ins = self.engs[q].dma_start(out=out, in_=in_, **kw)
        self.dcnt[i] += 16
        ins.then_inc(self.semh[sk], 16)
        self._record(sk, self.dcnt[i], reads, writes)
        if is_out:
            self.out_tickets.append((sk, self.dcnt[i]))

    def barrier(self):
        for e in ("pe", "act", "dve", "pool", "sp"):
            for f in ("pe", "act", "dve", "pool", "sp"):
                if f != e and self.cnt[f] > 0:
                    self._wait(e, f, self.cnt[f])
            for i in range(self.ndma):
                if self.dcnt[i] > 0:
                    self._wait(e, ("d", i), self.dcnt[i])
        self.acc = {}

    def finish(self):
        for sk, val in self.out_tickets:
            self._wait("sp", sk, val)


def build_nc(stage=99):
    nc = bass.Bass("TRN2", target_bir_lowering=False)
    cf_np, OF, cb_np, OB = _make_consts()
    NF = cf_np.shape[1]
    NB = cb_np.shape[1]

    def din(name, shape, dt=F32):
        return nc.dram_tensor(name, list(shape), dt, kind="ExternalInput").ap()

    x_d = din("x", [T, D])
    p_d = din("p", [T, 256])
    wd = {}
    for nm, shp in [("ffn1_w1", [D, FF]), ("ffn1_w3", [D, FF]), ("ffn1_w2", [FF, D]), ("w_in", [D, IN_W]),
                    ("cmp_w1_k", [2048, 128]), ("cmp_w2_k", [128, 64]), ("cmp_w1_v", [2048, 128]),
                    ("cmp_w2_v", [128, 64]), ("pos_kT", [128, 32]), ("pos_vT", [128, 32]),
                    ("w_up_nsa", [512, D]), ("w_up_moba", [512, D]), ("w_out", [D, D]),
                    ("ffn2_w1", [D, FF]), ("ffn2_w3", [D, FF]), ("ffn2_w2", [FF, D]),
                    ("w_ple_gate", [D, D]), ("w_ple", [256, D]), ("gains", [128, 32]), ("gfin", [128, D])]:
        wd[nm] = din(nm, shp)
    cf_d = din("cf32", [128, NF])
    cb_d = din("cbf16", [128, NB], BF16)
    out_d = nc.dram_tensor("out", [T, D], F32, kind="ExternalOutput").ap()

    S = Sched(nc)
    stack = [None]

    def TS(name, shape, dt):
        if stack[0] is None:
            return nc.alloc_sbuf_tensor(name, shape, dt)
        return stack[0].enter_context(nc.sbuf_tensor(name, shape, dt))

    def scoped(fn, *a):
        if PLAN[0]:
            fn(*a)
            return
        S.barrier()
        with ExitStack() as st:
            stack[0] = st
            fn(*a)
            S.barrier()
        stack[0] = None

    hT = TS("hT", [128, 8, T], F32)
    NFS = OF["rt"]
    NBS = OB["cmpmask"]
    cf = TS("cf", [128, NFS], F32)
    cb = TS("cb", [128, NBS], BF16)
    gains = TS("gains_sb", [128, 32], F32)
    slab = [TS("slab%d" % i, [128, 4096], BF16) for i in range(3)]
    ps = [nc.alloc_psum_tensor("ps%d" % i, [128, 512], F32) for i in range(8)]

    identf = cf[:, OF["ident"]:OF["ident"] + 128]
    identb = cb[:, OB["ident"]:OB["ident"] + 128]
    onesb = cb[:, OB["ones"]:OB["ones"] + 128]
    epsc = cf[:, OF["eps"]:OF["eps"] + 1]

    S.dma("sp", cf[:, :], cf_d[:, 0:NFS], writes=[cf[:, :]])
    S.dma("sp", cb[:, :], cb_d[:, 0:NBS], writes=[cb[:, :]])
    S.dma("sp", gains[:, :], wd["gains"][:, :], writes=[gains[:, :]])

    def mm(out, lhsT, rhs, start, stop, inc=None):
        inc = True
        return S.op("pe", lambda: nc.tensor.matmul(out, lhsT, rhs, start=start, stop=stop),
                    [lhsT, rhs], [out], inc=inc)

    def tp(out, in_, ident):
        return S.op("pe", lambda: nc.tensor.transpose(out, in_, ident), [in_, ident], [out])

    def act(out, in_, func, bias=None, scale=None, accum_out=None):
        kw = {}
        rd = [in_]
        wr = [out]
        if bias is not None:
            kw["bias"] = bias
            if not isinstance(bias, (int, float)):
                rd.append(bias)
        if scale is not None:
            kw["scale"] = scale
            if not isinstance(scale, (int, float)):
                rd.append(scale)
        if accum_out is not None:
            kw["accum_out"] = accum_out
            wr.append(accum_out)
        return S.op("act", lambda: nc.scalar.activation(out=out, in_=in_, func=func, **kw), rd, wr)

    def tt(out, in0, in1, op, e="dve"):
        eng = nc.vector if e == "dve" else nc.gpsimd
        return S.op(e, lambda: eng.tensor_tensor(out=out, in0=in0, in1=in1, op=op), [in0, in1], [out])

    def ts(out, in0, s1, s2, op0, op1=None, e="dve"):
        eng = nc.vector if e == "dve" else nc.gpsimd
        rd = [in0]
        if not isinstance(s1, (int, float)):
            rd.append(s1)
        if s2 is not None and not isinstance(s2, (int, float)):
            rd.append(s2)
        if op1 is None:
            return S.op(e, lambda: eng.tensor_scalar(out=out, in0=in0, scalar1=s1, scalar2=None, op0=op0), rd, [out])
        return S.op(e, lambda: eng.tensor_scalar(out=out, in0=in0, scalar1=s1, scalar2=s2, op0=op0, op1=op1),
                    rd, [out])

    def stt(out, in0, scalar, in1, op0, op1):
        rd = [in0, in1]
        if not isinstance(scalar, (int, float)):
            rd.append(scalar)
        return S.op("dve", lambda: nc.vector.scalar_tensor_tensor(out=out, in0=in0, scalar=scalar, in1=in1,
                                                                   op0=op0, op1=op1), rd, [out])

    def cp(out, in_, e="dve"):
        if e == "act":
            return S.op("act", lambda: nc.scalar.copy(out=out, in_=in_), [in_], [out])
        eng = nc.vector if e == "dve" else nc.gpsimd
        return S.op(e, lambda: eng.tensor_copy(out=out, in_=in_), [in_], [out])

    def memset(ap, v, e="dve"):
        eng = nc.vector if e == "dve" else nc.gpsimd
        return S.op(e, lambda: eng.memset(ap, v), [], [ap])

    class RR:
        def __init__(self, items):
            self.items = items
            self.i = 0

        def next(self):
            it = self.items[self.i % len(self.items)]
            self.i += 1
            return it

    class WStream:
        def __init__(self):
            self.jobs = []
            self.issued = 0
            self.k = 0

        def add(self, parts):
            self.jobs.append(parts)

        def _issue(self, k):
            buf = slab[k % 3]
            for (off, shape, src) in self.jobs[k]:
                n = 1
                for s_ in shape:
                    n *= s_
                npart = int(src.shape[0])
                dst = buf[0:npart, off:off + n]
                if len(shape) == 2:
                    dst = dst.rearrange("p (a b) -> p a b", a=shape[0])
                S.dma("pool", dst, src, writes=[buf[0:npart, off:off + n]])

        def get(self):
            k = self.k
            self.k += 1
            while self.issued < min(k + 2, len(self.jobs)):
                self._issue(self.issued)
                self.issued += 1
            return slab[k % 3]

    WS = WStream()
    PLAN = [True]

    def W(parts):
        if PLAN[0]:
            WS.add(parts)
            return None
        return WS.get()

    def trs(tr):
        return slice(tr * 512, (tr + 1) * 512)

    sqb = [TS("sqb%d" % i, [128, 512], BF16) for i in range(2)]
    rstd = TS("rstd", [128, 512], F32)
    sqrr = RR(sqb)
    psM = RR([ps[6], ps[7]])

    def rms_range(tr, gidx, xn3):
        pb = psM.next()
        for c in range(8):
            sq = sqrr.next()
            act(sq[:, :], hT[:, c, trs(tr)], AF.Square)
            mm(pb[:, :], onesb, sq[:, :], c == 0, c == 7)
        act(rstd[:, :], pb[:, :], AF.Ln, bias=epsc, scale=1.0 / D)
        act(rstd[:, :], rstd[:, :], AF.Exp, scale=-0.5)
        for c in range(8):
            stt(xn3[:, c, :], hT[:, c, trs(tr)], gains[:, gidx * 8 + c:gidx * 8 + c + 1], rstd[:, :],
                ALU.mult, ALU.mult)

    def phase_load():
        xtok = [TS("xtok%d" % i, [128, D], F32) for i in range(2)]
        for t_ in range(16):
            xb = xtok[t_ % 2]
            S.dma("sp", xb[:, :], x_d[t_ * 128:(t_ + 1) * 128, :], writes=[xb[:, :]])
            for half in range(2):
                pb = psM.next()
                for c4 in range(4):
                    c = half * 4 + c4
                    tp(pb[:, c4 * 128:(c4 + 1) * 128], xb[:, c * 128:(c + 1) * 128], identf)
                cp(hT[:, half * 4:(half + 1) * 4, t_ * 128:(t_ + 1) * 128],
                   pb[:, :].rearrange("p (c t) -> p c t", c=4), e="act" if half else "dve")

    def phase_ffn(w1n, w3n, w2n, gidx, tag):
        w1 = wd[w1n].rearrange("(c p) f -> p c f", p=128)
        w3 = wd[w3n].rearrange("(c p) f -> p c f", p=128)
        w2 = wd[w2n]
        if not PLAN[0]:
            xnT = TS("xnT" + tag, [128, 8, T], BF16)
            h1 = [TS("h1%s%d" % (tag, i), [128, 2, 512], BF16) for i in range(2)]
            sa = [TS("sa%s%d" % (tag, i), [128, 512], F32) for i in range(2)]
            for tr in range(4):
                rms_range(tr, gidx, xnT[:, :, trs(tr)])
            psAB = RR([ps[0], ps[1], ps[2], ps[3]])
            psY = RR([ps[4], ps[5]])
            sar = RR(sa)
            h1r = RR(h1)
        for s in range(11):
            A = W([(0, [8, 256], w1[:, :, s * 256:(s + 1) * 256]), (2048, [8, 256], w3[:, :, s * 256:(s + 1) * 256])])
            B = W([(0, [2, 1024], w2[s * 256:(s + 1) * 256, :].rearrange("(j p) d -> p j d", p=128))])
            if PLAN[0]:
                continue
            w1s = A[:, 0:2048].rearrange("p (c f) -> p c f", c=8)
            w3s = A[:, 2048:4096].rearrange("p (c f) -> p c f", c=8)
            w2s = B[:, 0:2048].rearrange("p (j d) -> p j d", j=2)
            for tr in range(4):
                hb = h1r.next()
                for j in range(2):
                    pa = psAB.next()
                    pb = psAB.next()
                    for c in range(8):
                        mm(pa[:, :], w1s[:, c, j * 128:(j + 1) * 128], xnT[:, c, trs(tr)], c == 0, c == 7)
                    for c in range(8):
                        mm(pb[:, :], w3s[:, c, j * 128:(j + 1) * 128], xnT[:, c, trs(tr)], c == 0, c == 7)
                    sab = sar.next()
                    act(sab[:, :], pa[:, :], AF.Silu)
                    tt(hb[:, j, :], sab[:, :], pb[:, :], ALU.mult)
                for dc in range(8):
                    py = psY.next()
                    for j in range(2):
                        mm(py[:, :], w2s[:, j, dc * 128:(dc + 1) * 128], hb[:, j, :], j == 0, j == 1)
                    stt(hT[:, dc, trs(tr)], py[:, :], 0.5, hT[:, dc, trs(tr)], ALU.mult, ALU.add)

    def phase_ple():
        wg = wd["w_ple_gate"].rearrange("(c p) f -> p c f", p=128)
        wp = wd["w_ple"].rearrange("(c p) f -> p c f", p=128)
        if not PLAN[0]:
            xnT = TS("xnTple", [128, 8, T], BF16)
            pT = TS("pT", [128, 2, T], BF16)
            ptok = [TS("ptok%d" % i, [128, 256], F32) for i in range(2)]
            sg = [TS("sgple%d" % i, [128, 512], F32) for i in range(2)]
            for t_ in range(16):
                pbuf = ptok[t_ % 2]
                S.dma("sp", pbuf[:, :], p_d[t_ * 128:(t_ + 1) * 128, :], writes=[pbuf[:, :]])
                pb = psM.next()
                for c in range(2):
                    tp(pb[:, c * 128:(c + 1) * 128], pbuf[:, c * 128:(c + 1) * 128], identf)
                cp(pT[:, :, t_ * 128:(t_ + 1) * 128], pb[:, 0:256].rearrange("p (c t) -> p c t", c=2))
            for tr in range(4):
                rms_range(tr, 3, xnT[:, :, trs(tr)])
            psG = RR([ps[0], ps[1], ps[2], ps[3]])
            sgr = RR(sg)
        for dh in range(2):
            A = W([(0, [8, 512], wg[:, :, dh * 512:(dh + 1) * 512])])
            B = W([(0, [2, 512], wp[:, :, dh * 512:(dh + 1) * 512])])
            if PLAN[0]:
                continue
            wgs = A[:, 0:4096].rearrange("p (c f) -> p c f", c=8)
            wps = B[:, 0:1024].rearrange("p (c f) -> p c f", c=2)
            for tr in range(4):
                for d4 in range(4):
                    dc = dh * 4 + d4
                    pg = psG.next()
                    pp = psG.next()
                    for c in range(8):
                        mm(pg[:, :], wgs[:, c, d4 * 128:(d4 + 1) * 128], xnT[:, c, trs(tr)], c == 0, c == 7)
                    for c in range(2):
                        mm(pp[:, :], wps[:, c, d4 * 128:(d4 + 1) * 128], pT[:, c, trs(tr)], c == 0, c == 1)
                    sgb = sgr.next()
                    act(sgb[:, :], pg[:, :], AF.Sigmoid)
                    tt(sgb[:, :], sgb[:, :], pp[:, :], ALU.mult)
                    tt(hT[:, dc, trs(tr)], hT[:, dc, trs(tr)], sgb[:, :], ALU.add)

    def phase_final():
        gfin = TS("gfin_sb", [128, D], F32)
        S.dma("sp", gfin[:, :], wd["gfin"][:, :], writes=[gfin[:, :]])
        otok = [TS("otok%d" % i, [128, D], F32) for i in range(2)]
        junk = TS("junk", [128, 512], F32)
        ssq = TS("ssq", [128, 4], F32)
        psF = RR([ps[0], ps[1], ps[2], ps[3], ps[4], ps[5]])
        for t_ in range(16):
            ob = otok[t_ % 2]
            pbs = [psF.next(), psF.next()]
            for half in range(2):
                for c4 in range(4):
                    c = half * 4 + c4
                    tp(pbs[half][:, c4 * 128:(c4 + 1) * 128], hT[:, c, t_ * 128:(t_ + 1) * 128], identf)
                act(junk[:, :], pbs[half][:, :], AF.Square, accum_out=ssq[:, half:half + 1])
            tt(ssq[:, 2:3], ssq[:, 0:1], ssq[:, 1:2], ALU.add)
            act(ssq[:, 3:4], ssq[:, 2:3], AF.Ln, bias=epsc, scale=1.0 / D)
            act(ssq[:, 3:4], ssq[:, 3:4], AF.Exp, scale=-0.5)
            for half in range(2):
                stt(ob[:, half * 512:(half + 1) * 512], pbs[half][:, :], ssq[:, 3:4],
                    gfin[:, half * 512:(half + 1) * 512], ALU.mult, ALU.mult)
            S.dma("sp", out_d[t_ * 128:(t_ + 1) * 128, :], ob[:, :], reads=[ob[:, :]], is_out=True)

    def phase_mixer():
        win = wd["w_in"].rearrange("(c p) f -> p c f", p=128)
        wupn = wd["w_up_nsa"].rearrange("(c p) f -> p c f", p=128)
        wupm = wd["w_up_moba"].rearrange("(c p) f -> p c f", p=128)
        wout = wd["w_out"].rearrange("(c p) f -> p c f", p=128)
        sl_n, sl_m = _slopes()
        if not PLAN[0]:
            kslcT = TS("kslcT", [128, T], BF16)
            kwinT = TS("kwinT", [128, T], BF16)
            kcmpT = TS("kcmpT", [128, 528], BF16)
            vcmpT = TS("vcmpT", [128, 528], BF16)
            kmT = TS("kmT", [128, 4, T], BF16)
            vslc = TS("vslc", [128, 16, 2, 65], BF16)
            vwin = TS("vwin", [128, 16, 2, 65], BF16)
            vm = TS("vm", [128, 16, 8, 65], BF16)
            gsig = TS("gsig", [128, 16, 24], F32)
            kcT = TS("kcT", [128, 128], BF16)
            vca = TS("vca", [128, 2, 97], BF16)
            kmeanT = TS("kmeanT", [128, 4, 16], BF16)
            kmsum = TS("kmsum", [128, 4, 2], F32)
            w2k = TS("w2k", [128, 128], BF16)
            w2v = TS("w2v", [128, 64], BF16)
            posk = TS("posk", [128, 32], BF16)
            posv = TS("posv", [128, 32], BF16)
            cbias = TS("cbias", [128, 2], F32)
            u_tr = TS("u_tr", [128, 8, 512], BF16)
            qmix = TS("qmix", [128, 8, 512], BF16)

            class _Sub:
                def __init__(self, base, off):
                    self.base, self.off = base, off

                def __getitem__(self, key):
                    a, b, c = key
                    if isinstance(b, int):
                        b = b + self.off
                    else:
                        b = slice((b.start or 0) + self.off, (b.stop if b.stop is not None else 4) + self.off)
                    return self.base[a, b, c]
            qn = _Sub(qmix, 0)
            qm = _Sub(qmix, 4)
            ptl = [TS("ptl%d" % i, [128, 512], BF16) for i in range(3)]
            otk = [TS("otk%d" % i, [128, 4, 4, 64], F32) for i in range(2)]
            onT = TS("onT", [128, 4, 512], BF16)
            omT = TS("omT", [128, 4, 512], BF16)
            mixed = qmix
            sgA = [TS("sgA%d" % i, [128, 512], F32) for i in range(1)]
            sgB = [TS("sgB%d" % i, [128, 512], F32) for i in range(1)]
            mbTn = TS("mbTn", [128, 512], BF16)
            mbTm = TS("mbTm", [128, 512], BF16)
            mbtokN = TS("mbtokN", [128, 128], F32)
            mbtok = TS("mbtok", [128, 128], F32)
            impa = TS("impa", [128, 4, 2, 32], F32)
            smal = TS("smal", [128, 64], F32)
            m8 = TS("m8", [128, 16], F32)
            tk32 = TS("tk32", [128, 64], F32)
            cmsk = TS("cmsk", [128, 512], BF16)
            rtab = TS("rtab", [128, 768], F32)
            esel = TS("esel", [128, 16, 128], BF16)
            S.dma("sp", esel[:, :, :], cb_d[:, OB["esel"]:OB["esel"] + 2048].rearrange("p (k m) -> p k m", k=16),
                  writes=[esel[:, :, :]])
            hid = TS("hid", [128, 4, 32], F32)
            hidb = TS("hidb", [128, 32], BF16)
            hidv = TS("hidv", [128, 2, 128], BF16)
            gtmp = TS("gtmp", [64, 528], BF16)
            gsf = TS("gsf", [128, 8, 16], F32)

            memset(kcT[:, :], 0.0)
            memset(mbtokN[:, :], 0.0)
            memset(gsf[:, :, :], -1.0e30)
            memset(kcmpT[:, 0:16], 0.0)
            memset(vcmpT[:, 0:16], 0.0)
            memset(hidv[:, :, :], 0.0)
            memset(vca[:, :, :], 0.0)
            memset(kmeanT[:, :, :], 0.0)
            memset(vslc[:, :, :, 64:65], 1.0)
            memset(vwin[:, :, :, 64:65], 1.0)
            memset(vm[:, :, :, 64:65], 1.0)
            memset(vca[:, :, 64:65], 1.0)
            for g in range(2):
                cp(vca[:, g, 65:97], cb[:, OB["overlap"]:OB["overlap"] + 32])
            for (w2t, w2n_, pt, pn) in ((w2k, "cmp_w2_k", posk, "pos_kT"), (w2v, "cmp_w2_v", posv, "pos_vT")):
                S.dma("pool", pt[:, :], wd[pn][:, :], writes=[pt[:, :]])
                if w2t is w2k:
                    S.dma("pool", w2t[:, 0:64], wd[w2n_][:, :], writes=[w2t[:, 0:64]])
                    S.dma("pool", w2t[:, 64:128], wd[w2n_][:, :], writes=[w2t[:, 64:128]])
                else:
                    S.dma("pool", w2t[:, :], wd[w2n_][:, :], writes=[w2t[:, :]])

            psS = RR([ps[0], ps[1], ps[2]])
            psO = RR([ps[3], ps[4], ps[5]])
            ptr = RR(ptl)
            otr = RR(otk)
            sgAr = RR(sgA)
            sgBr = RR(sgB)

        def proj_fm(ws, c0, dst, tr, base=None, ev="act"):
            pb = psM.next()
            for c in range(8):
                if base is None:
                    l_ = ws[:, c, c0:c0 + 128]
                else:
                    l_ = base(c)
                mm(pb[:, :], l_, u_tr[:, c, :], c == 0, c == 7)
            cp(dst, pb[:, :], e=ev)

        NTRS = int(os.environ.get("MK_NTR", "4"))
        SUB = int(os.environ.get("MK_SUB", "9"))
        for tr in range(NTRS):
            S0 = W([(0, [8, 512], win[:, :, 0:512])])
            if not PLAN[0]:
                rms_range(tr, 1, u_tr)
                w0 = S0[:, 0:4096].rearrange("p (c f) -> p c f", c=8)
                for i in range(4):
                    proj_fm(w0, i * 128, qn[:, i, :], tr, ev="act" if i % 2 else "dve")
            S1 = W([(0, [8, 512], win[:, :, 512:1024])])
            if not PLAN[0]:
                w1_ = S1[:, 0:4096].rearrange("p (c f) -> p c f", c=8)
                proj_fm(w1_, 0, kcmpT[:, 16:528], tr, ev="act")
                proj_fm(w1_, 128, vcmpT[:, 16:528], tr, ev="dve")
                proj_fm(w1_, 256, kslcT[:, trs(tr)], tr, ev="act")
                for j in range(4):
                    t_ = tr * 4 + j
                    pb = psM.next()
                    for c in range(8):
                        mm(pb[:, 0:128], u_tr[:, c, j * 128:(j + 1) * 128], w1_[:, c, 384:512], c == 0, c == 7)
                    cp(vslc[:, t_, :, 0:64], pb[:, 0:128].rearrange("p (g d) -> p g d", g=2))
            S2 = W([(0, [8, 280], win[:, :, 1024:1304])])
            if not PLAN[0]:
                w2_ = S2[:, 0:2240].rearrange("p (c f) -> p c f", c=8)
                proj_fm(w2_, 0, kwinT[:, trs(tr)], tr, ev="act")
                for j in range(4):
                    t_ = tr * 4 + j
                    pb = psM.next()
                    for c in range(8):
                        mm(pb[:, 0:152], u_tr[:, c, j * 128:(j + 1) * 128], w2_[:, c, 128:280], c == 0, c == 7)
                    cp(vwin[:, t_, :, 0:64], pb[:, 0:128].rearrange("p (g d) -> p g d", g=2))
                    act(gsig[:, t_, :], pb[:, 128:152], AF.Sigmoid)
            S3 = W([(0, [8, 512], win[:, :, 1304:1816])])
            if not PLAN[0]:
                w3_ = S3[:, 0:4096].rearrange("p (c f) -> p c f", c=8)
                for i in range(4):
                    proj_fm(w3_, i * 128, qm[:, i, :], tr, ev="act" if i % 2 else "dve")
            S4 = W([(0, [8, 512], win[:, :, 1816:2328])])
            if not PLAN[0]:
                w4_ = S4[:, 0:4096].rearrange("p (c f) -> p c f", c=8)
                for i in range(4):
                    proj_fm(w4_, i * 128, kmT[:, i, trs(tr)], tr, ev="act" if i % 2 else "dve")
                for i in range(4):
                    S.op("dve", lambda: nc.vector.tensor_reduce(
                        out=kmsum[:, i, :], in_=kmT[:, i, trs(tr)].rearrange("p (n k) -> p n k", n=2),
                        axis=mybir.AxisListType.X, op=ALU.add),
                        [kmT[:, i, trs(tr)]], [kmsum[:, i, :]])
                ts(kmeanT[0:64, :, 2 * tr:2 * tr + 2], kmsum[0:64, :, :], 1.0 / 256.0, None, ALU.mult)
                ts(kmeanT[64:128, :, 8 + 2 * tr:8 + 2 * tr + 2], kmsum[64:128, :, :], 1.0 / 256.0, None, ALU.mult)
            S5 = W([(0, [8, 512], win[:, :, 2328:2840])])
            if not PLAN[0]:
                w5_ = S5[:, 0:4096].rearrange("p (c f) -> p c f", c=8)
                for j in range(4):
                    t_ = tr * 4 + j
                    pb = psM.next()
                    for c in range(8):
                        mm(pb[:, :], u_tr[:, c, j * 128:(j + 1) * 128], w5_[:, c, :], c == 0, c == 7)
                    cp(vm[:, t_, :, 0:64], pb[:, :].rearrange("p (h d) -> p h d", h=8), e="act" if j % 2 else "dve")

            if SUB < 1:
                continue
            c_lo = 0 if tr == 0 else 32 * tr - 1
            c_hi = 32 * tr + 30
            nb = c_hi - c_lo + 1
            for which in range(2):
                wsrc = wd["cmp_w1_k" if which == 0 else "cmp_w1_v"].rearrange("(l d) h -> d l h", d=64)
                CW = W([(q4 * 1024, [8, 128], wsrc[:, q4 * 8:(q4 + 1) * 8, :]) for q4 in range(4)])
                if PLAN[0]:
                    continue
                srcT = kcmpT if which == 0 else vcmpT
                w1t = CW[0:64, 0:4096].rearrange("p (l h) -> p l h", l=32)
                if tr == 0:
                    pt = posk if which == 0 else posv
                    pb = psM.next()
                    for l in range(32):
                        mm(pb[:, 0:1], w1t[:, l, :], pt[0:64, l:l + 1], l == 0, l == 31)
                    cp(cbias[:, which:which + 1], pb[:, 0:1])
                for g in range(2):
                    if g == 1:
                        cp(gtmp[0:64, 0:528], srcT[64:128, 0:528])
                        src_t, src_p = gtmp, 0
                    else:
                        src_t, src_p = srcT, 0
                    pb = psM.next()
                    for l in range(32):
                        col0 = 16 * c_lo + l - 512 * tr + 16
                        rhs_ = bass.AP(src_t, src_p * 528 + col0, [[528, 64], [16, nb]])
                        mm(pb[:, 0:nb], w1t[:, l, :], rhs_, l == 0, l == 31)
                    xx = hid[:, 0, 0:nb]
                    x2 = hid[:, 1, 0:nb]
                    ts(xx, pb[:, 0:nb], cbias[:, which:which + 1], None, ALU.add)
                    tt(x2, xx, xx, ALU.mult)
                    ts(x2, x2, 0.044715, 1.0, ALU.mult, ALU.add)
                    tt(x2, x2, xx, ALU.mult)
                    act(hid[:, 2, 0:nb], x2, AF.Sigmoid, scale=1.5957691216057308)
                    if which == 0:
                        tt(hidb[:, 0:nb], xx, hid[:, 2, 0:nb], ALU.mult)
                        pb2 = psM.next()
                        mm(pb2[:, 0:nb], w2k[:, :], hidb[:, 0:nb], True, True)
                        cp(kcT[64 * g:64 * g + 64, c_lo:c_lo + nb], pb2[64 * g:64 * g + 64, 0:nb])
                    else:
                        tt(hidv[:, g, c_lo:c_lo + nb], xx, hid[:, 2, 0:nb], ALU.mult)
                        pb2 = psM.next()
                        mm(pb2[:, 0:64], hidv[:, g, :], w2v[:, :], True, True)
                        cp(vca[:, g, 0:64], pb2[:, 0:64])
                cp(srcT[:, 0:16], srcT[:, 512:528])
            if not PLAN[0]:
                S.dma("sp", cmsk[:, :], cb_d[:, OB["cmpmask"] + tr * 512:OB["cmpmask"] + (tr + 1) * 512],
                      writes=[cmsk[:, :]])
                if tr >= 2:
                    S.dma("sp", rtab[:, :], cf_d[:, OF["rt"] + (tr - 2) * 768:OF["rt"] + (tr - 1) * 768],
                          writes=[rtab[:, :]])

            if SUB < 2:
                continue
            def attend(kT_ap_fn, q_ap, vfn, tiles, bias_fn, narrow, ops, mask_fn=None, ncols=65):
                npv = [0]
                npv_total = sum((c1 - c0) // 128 for (kt, c0, c1, extra) in tiles)

                def emit_scores(ti):
                    (kt, c0, c1, extra) = tiles[ti]
                    sp_ = psS.next()
                    n_extra = len(extra) + (1 if mask_fn is not None else 0)
                    mm(sp_[:, c0:c1], kT_ap_fn(kt), q_ap[:, c0:c1], True, n_extra == 0)
                    k_ = 0
                    if mask_fn is not None:
                        k_ += 1
                        ml, mr = mask_fn(kt)
                        mm(sp_[:, c0:c1], ml, mr[:, c0:c1], False, k_ == n_extra)
                    for (e0, en, el, er) in extra:
                        k_ += 1
                        mm(sp_[:, e0:e0 + en], el, er, False, k_ == n_extra)
                    return sp_

                nxt = emit_scores(0)
                for ti in range(len(tiles)):
                    (kt, c0, c1, extra) = tiles[ti]
                    sp_ = nxt
                    if ti + 1 < len(tiles):
                        nxt = emit_scores(ti + 1)
                    pt = ptr.next()
                    if narrow:
                        for j in range(c0 // 128, c1 // 128):
                            act(pt[:, j * 128:(j + 1) * 128], sp_[:, j * 128:(j + 1) * 128], AF.Exp,
                                bias=bias_fn(kt, j), scale=0.125)
                    else:
                        act(pt[:, c0:c1], sp_[:, c0:c1], AF.Exp, bias=bias_fn(kt, None), scale=0.125)
                    for j in range(c0 // 128, c1 // 128):
                        npv[0] += 1
                        mm(ops[:, j * 128:j * 128 + ncols], pt[:, j * 128:(j + 1) * 128], vfn(kt),
                           npv[0] == 1, npv[0] == npv_total)

            triA = cb[:, OB["triA"]:OB["triA"] + 128]
            triB = cb[:, OB["triB"]:OB["triB"] + 128]

            def causal_tiles(tr_):
                tl = []
                for kt in range(0, 4 * tr_ + 4):
                    i = kt - 4 * tr_
                    if i < 0:
                        tl.append((kt, 0, 512, []))
                    else:
                        tl.append((kt, 128 * i, 512, [(128 * i, 128, identb, triA)]))
                return tl

            def win_tiles(tr_):
                tl = []
                for kt in range(4 * tr_ - 4, 4 * tr_ + 4):
                    if kt < 0:
                        continue
                    i = kt - 4 * tr_
                    if i < 0:
                        i2 = i + 4
                        tl.append((kt, 0, 128 * (i2 + 1), [(128 * i2, 128, identb, triB)]))
                    else:
                        tl.append((kt, 128 * i, 512, [(128 * i, 128, identb, triA)]))
                return tl

            if not PLAN[0]:
                def bias_head(hidx, narrow):
                    def f(kt, j):
                        if narrow:
                            di = kt - (4 * tr + j) + 15
                            o_ = OF["bn"] + hidx * 16 + di
                        else:
                            di = kt - 4 * tr + 12
                            o_ = OF["bw"] + hidx * 16 + di
                        return cf[:, o_:o_ + 1]
                    return f

                def bias_cmp(h, narrow):
                    def f(kt, j):
                        if narrow:
                            o_ = OF["cn"] + h * 16 + (4 * tr + j)
                        else:
                            o_ = OF["cw"] + h * 4 + tr
                        return cf[:, o_:o_ + 1]
                    return f

                def norm_coef(ob_, gate_ap):
                    ov_ = ob_[:, :].rearrange("p (j c) -> p j c", j=4)
                    ts(smal[:, 0:4], ov_[:, :, 64], TINY, None, ALU.max)
                    S.op("dve", lambda: nc.vector.reciprocal(out=smal[:, 4:8], in_=smal[:, 0:4]),
                         [smal[:, 0:4]], [smal[:, 4:8]])
                    if gate_ap is not None:
                        tt(smal[:, 8:12], smal[:, 4:8], gate_ap, ALU.mult)
                    return ov_

                ots = [otk[0], otk[1]]
            if SUB < 3:
                continue
            if not PLAN[0]:
                for g in range(2):
                    ot = ots[g]
                    for hg in range(4):
                        h = g * 4 + hg
                        ch, bp = h % 4, 64 * (h // 4)
                        nar = bool(sl_n[h] > 0.3)
                        oc = psO.next()
                        q_ap = qn[bp:bp + 64, ch, :]
                        attend(lambda kt: kcT[bp:bp + 64, :], q_ap, lambda kt: vca[:, g, :],
                               [(0, 0, 512, [(0, 512, identb, cmsk[:, :])])], bias_cmp(h, nar), nar, oc, ncols=97)
                        ocv = norm_coef(oc, gsig[:, 4 * tr:4 * tr + 4, h * 3 + 0])
                        for j in range(4):
                            ts(ot[:, j, hg, :], ocv[:, j, 0:64], smal[:, 8 + j:9 + j], None, ALU.mult)
                            if tr >= 2:
                                if hg == 0:
                                    ts(impa[:, j, g, :], ocv[:, j, 65:97], smal[:, 4 + j:5 + j], None, ALU.mult)
                                else:
                                    stt(impa[:, j, g, :], ocv[:, j, 65:97], smal[:, 4 + j:5 + j], impa[:, j, g, :],
                                        ALU.mult, ALU.add)
                if tr >= 2:
                    for j in range(4):
                        kp = rtab[:, j * 32:(j + 1) * 32]
                        fo = rtab[:, 128 + j * 32:128 + (j + 1) * 32]
                        for g in range(2):
                            iv = tk32[:, 0:32]
                            tt(iv, impa[:, j, g, :], kp, ALU.mult)
                            tt(iv, iv, fo, ALU.add)
                            S.op("dve", lambda: nc.vector.max(out=m8[:, 0:8], in_=iv), [iv], [m8[:, 0:8]])
                            S.op("dve", lambda: nc.vector.match_replace(out=tk32[:, 32:64], in_to_replace=m8[:, 0:8],
                                                                         in_values=iv, imm_value=-3.0e38),
                                 [iv, m8[:, 0:8]], [tk32[:, 32:64]])
                            S.op("dve", lambda: nc.vector.max(out=m8[:, 8:16], in_=tk32[:, 32:64]),
                                 [tk32[:, 32:64]], [m8[:, 8:16]])
                            ts(tk32[:, 32:64], iv, m8[:, 15:16], None, ALU.is_ge)
                            ts(mbtokN[:, g * 64:g * 64 + 32], tk32[:, 32:64], -1.0, -MASKV, ALU.add, ALU.mult)
                        pb = psM.next()
                        tp(pb[:, 0:128], mbtokN[:, :], identf)
                        cp(mbTn[:, j * 128:(j + 1) * 128], pb[:, 0:128])
                for g in range(2):
                    ot = ots[g]
                    for hg in range(4):
                        h = g * 4 + hg
                        ch, bp = h % 4, 64 * (h // 4)
                        nar = bool(sl_n[h] > 0.3)
                        q_ap = qn[bp:bp + 64, ch, :]
                        osel = psO.next()
                        mfn = None
                        if tr >= 2:
                            def mfn(kt, g=g):
                                return esel[64 * g:64 * g + 64, kt, :], mbTn[64 * g:64 * g + 64, :]
                        attend(lambda kt: kslcT[bp:bp + 64, kt * 128:(kt + 1) * 128], q_ap,
                               lambda kt: vslc[:, kt, g, :], causal_tiles(tr), bias_head(h, nar), nar, osel,
                               mask_fn=mfn)
                        ov_ = norm_coef(osel, gsig[:, 4 * tr:4 * tr + 4, h * 3 + 1])
                        for j in range(4):
                            stt(ot[:, j, hg, :], ov_[:, j, 0:64], smal[:, 8 + j:9 + j], ot[:, j, hg, :],
                                ALU.mult, ALU.add)
                        owin = psO.next()
                        attend(lambda kt: kwinT[bp:bp + 64, kt * 128:(kt + 1) * 128], q_ap,
                               lambda kt: vwin[:, kt, g, :], win_tiles(tr), bias_head(h, nar), nar, owin)
                        ov_ = norm_coef(owin, gsig[:, 4 * tr:4 * tr + 4, h * 3 + 2])
                        for j in range(4):
                            stt(ot[:, j, hg, :], ov_[:, j, 0:64], smal[:, 8 + j:9 + j], ot[:, j, hg, :],
                                ALU.mult, ALU.add)
                    for i2 in range(2):
                        pb = psM.next()
                        for j in range(4):
                            tp(pb[:, j * 128:(j + 1) * 128], ot[:, j, 2 * i2:2 * i2 + 2, :], identf)
                        cp(onT[:, 2 * g + i2, :], pb[:, :], e="act")

            if SUB < 4:
                continue
            if not PLAN[0]:
                MDBG = int(os.environ.get("MK_MOBA", "3"))
                if tr >= 2 and MDBG >= 1:
                    for j in range(4):
                        pb = psM.next()
                        for ch in range(4):
                            mm(pb[:, ch * 16:(ch + 1) * 16], qm[:, ch, j * 128:(j + 1) * 128],
                               kmeanT[:, ch, :], True, True)
                        GS = int(os.environ.get("MK_GS", "9"))
                        if GS >= 1:
                            tt(gsf[:, :, 0:8], pb[:, 0:64].rearrange("p (h n) -> p h n", h=8),
                               rtab[:, 256 + j * 64:256 + (j + 1) * 64].rearrange("p (h n) -> p h n", h=8), ALU.add)
                        if GS >= 2:
                            for h in range(8):
                                S.op("dve", lambda: nc.vector.max(out=m8[:, 0:8], in_=gsf[:, h, :]),
                                     [gsf[:, h, :]], [m8[:, 0:8]])
                                ts(tk32[:, h * 8:(h + 1) * 8], gsf[:, h, 0:8], m8[:, 2:3], None, ALU.is_ge)
                        if GS >= 3:
                            tt(tk32[:, :], tk32[:, :], rtab[:, 512 + j * 64:512 + (j + 1) * 64], ALU.add)
                            ts(mbtok[:, 0:64], tk32[:, :], -1.0, -MASKV, ALU.add, ALU.mult)
                            ts(mbtok[:, 64:128], tk32[:, :], -1.0, -MASKV, ALU.add, ALU.mult)
                        if GS >= 4:
                            pb2 = psM.next()
                            tp(pb2[:, 0:128], mbtok[:, :], identf)
                            cp(mbTm[:, j * 128:(j + 1) * 128], pb2[:, 0:128])
                for hp in range(4):
                    ot = otr.next()
                    for h2 in range(2):
                        h = hp * 2 + h2
                        ch, bp = hp, 64 * h2
                        nar = bool(sl_m[h] > 0.3)
                        q_ap = qm[bp:bp + 64, ch, :]
                        om = psO.next()
                        mfn = None
                        if tr >= 2 and MDBG >= 2:
                            def mfn(kt, h=h):
                                c_ = 8 * h + kt // 2
                                b_ = 64 * (h % 2)
                                l_ = bass.AP(cb, b_ * NBS + OB["ident"] + b_ + c_, [[NBS, 64], [0, 128]])
                                return l_, mbTm[b_:b_ + 64, :]
                        attend(lambda kt: kmT[bp:bp + 64, ch, kt * 128:(kt + 1) * 128], q_ap,
                               lambda kt: vm[:, kt, h, :], causal_tiles(tr), bias_head(8 + h, nar), nar, om,
                               mask_fn=mfn)
                        ov_ = norm_coef(om, None)
                        for j in range(4):
                            ts(ot[:, j, h2, :], ov_[:, j, 0:64], smal[:, 4 + j:5 + j], None, ALU.mult)
                    pb = psM.next()
                    for j in range(4):
                        tp(pb[:, j * 128:(j + 1) * 128], ot[:, j, 0:2, :], identf)
                    cp(omT[:, hp, :], pb[:, :], e="act")

            if SUB < 5:
                continue
            for dc in range(8):
                c0 = dc * 128
                MJ = W([(0, [8, 128], win[:, :, 2840 + c0:2840 + c0 + 128]),
                        (1024, [8, 128], win[:, :, 3864 + c0:3864 + c0 + 128]),
                        (2048, [4, 128], wupn[:, :, c0:c0 + 128]),
                        (2560, [4, 128], wupm[:, :, c0:c0 + 128])])
                if PLAN[0]:
                    continue
                ga_ = MJ[:, 0:1024].rearrange("p (c f) -> p c f", c=8)
                gb_ = MJ[:, 1024:2048].rearrange("p (c f) -> p c f", c=8)
                un_ = MJ[:, 2048:2560].rearrange("p (c f) -> p c f", c=4)
                um_ = MJ[:, 2560:3072].rearrange("p (c f) -> p c f", c=4)
                pga = psS.next()
                pgb = psS.next()
                pyn = psO.next()
                pym = psO.next()
                for c in range(8):
                    mm(pga[:, :], ga_[:, c, :], u_tr[:, c, :], c == 0, c == 7)
                for c in range(8):
                    mm(pgb[:, :], gb_[:, c, :], u_tr[:, c, :], c == 0, c == 7)
                for c in range(4):
                    mm(pyn[:, :], un_[:, c, :], onT[:, c, :], c == 0, c == 3)
                for c in range(4):
                    mm(pym[:, :], um_[:, c, :], omT[:, c, :], c == 0, c == 3)
                sA = sgAr.next()
                sB = sgBr.next()
                act(sA[:, :], pga[:, :], AF.Sigmoid)
                act(sB[:, :], pgb[:, :], AF.Sigmoid)
                tt(sA[:, :], sA[:, :], pyn[:, :], ALU.mult)
                tt(sB[:, :], sB[:, :], pym[:, :], ALU.mult)
                tt(mixed[:, dc, :], sA[:, :], sB[:, :], ALU.add)
            for dh in range(2):
                WO = W([(0, [8, 512], wout[:, :, dh * 512:(dh + 1) * 512])])
                if PLAN[0]:
                    continue
                wo_ = WO[:, 0:4096].rearrange("p (c f) -> p c f", c=8)
                for d4 in range(4):
                    dc = dh * 4 + d4
                    pb = psM.next()
                    for c in range(8):
                        mm(pb[:, :], wo_[:, c, d4 * 128:(d4 + 1) * 128], mixed[:, c, :], c == 0, c == 7)
                    tt(hT[:, dc, trs(tr)], hT[:, dc, trs(tr)], pb[:, :], ALU.add)

    def run_all():
        if stage >= 1:
            scoped(phase_ffn, "ffn1_w1", "ffn1_w3", "ffn1_w2", 0, "a")
        if stage >= 2:
            scoped(phase_mixer)
        if stage >= 3:
            scoped(phase_ffn, "ffn2_w1", "ffn2_w3", "ffn2_w2", 2, "b")
        if stage >= 4:
            scoped(phase_ple)

    PLAN[0] = True
    run_all()
    PLAN[0] = False
    scoped(phase_load)
    run_all()
    scoped(phase_final)
    S.finish()
    _LAST["S"] = S
    return nc


_NC_CACHE = {}
_LAST = {}


def _prep_inputs(inputs):
    cf_np, _, cb_np, _ = _make_consts()
    f = lambda a: np.ascontiguousarray(np.asarray(a, dtype=np.float32))
    sh = {}
    for nm in ("ffn1_w1", "ffn1_w3", "ffn1_w2", "w_in", "cmp_w1_k", "cmp_w2_k", "cmp_w1_v", "cmp_w2_v",
               "w_up_nsa", "w_up_moba", "w_out", "ffn2_w1", "ffn2_w3", "ffn2_w2", "w_ple_gate", "w_ple"):
        sh[nm] = f(inputs[nm])[0]
    wi = sh["w_in"].copy()
    perm = [0, 4, 1, 5, 2, 6, 3, 7]
    wi[:, 0:512] = sh["w_in"][:, 0:512].reshape(D, 8, 64)[:, perm, :].reshape(D, 512)
    sh["w_in"] = np.ascontiguousarray(wi)
    for nm, src in (("pos_kT", "cmp_pos_k"), ("pos_vT", "cmp_pos_v")):
        pt = f(inputs[src])[0].T
        sh[nm] = np.ascontiguousarray(np.concatenate([pt, pt], axis=0))
    g = np.stack([f(inputs[n])[0] for n in ("ffn1_norm", "mix_norm", "ffn2_norm", "ple_norm")], axis=0)
    sh["gains"] = np.ascontiguousarray(g.reshape(4, 8, 128).transpose(2, 0, 1).reshape(128, 32))
    sh["gfin"] = np.ascontiguousarray(np.broadcast_to(f(inputs["final_norm"])[None, :], (128, D)))
    sh["cf32"] = cf_np
    sh["cbf16"] = cb_np
    return sh


def kernel(**inputs):
    stage = int(os.environ.get("MK_STAGE", "99"))
    ncores = int(os.environ.get("MK_CORES", "8"))
    if stage not in _NC_CACHE:
        _NC_CACHE[stage] = build_nc(stage)
    nc = _NC_CACHE[stage]
    sh = _prep_inputs(inputs)
    x = np.asarray(inputs["x"], dtype=np.float32)
    p = np.asarray(inputs["p"], dtype=np.float32)
    in_maps = []
    for b in range(ncores):
        m = dict(sh)
        m["x"] = np.ascontiguousarray(x[b])
        m["p"] = np.ascontiguousarray(p[0, b])
        in_maps.append(m)
    res = run_bass_kernel_spmd(nc, in_maps, core_ids=list(range(ncores)))
    out = np.stack([np.asarray(r["out"], dtype=np.float32) for r in res.results], axis=0)
    return out
```

```python
import os
import numpy as np
import ml_dtypes
import concourse.bass as bass
import concourse.mybir as mybir
from concourse.bass_utils import run_bass_kernel_spmd
from contextlib import ExitStack

F32 = mybir.dt.float32
BF16 = mybir.dt.bfloat16
AF = mybir.ActivationFunctionType
ALU = mybir.AluOpType

T = 2048
D = 1024
FF = 2816
MASKV = -240000.0
EPS = 1e-6
TINY = 1e-30
IN_W = 4888


def _slopes():
    s = 2.0 ** (-(np.arange(16) + 1) / 2.0)
    return s[0::2].copy(), s[1::2].copy()


def _make_consts():
    p = np.arange(128)
    f32 = {}
    b16 = {}
    f32["ident"] = np.eye(128)
    b16["ident"] = np.eye(128)
    b16["ones"] = np.ones((128, 128))
    j = np.arange(128)
    b16["triA"] = np.where(j[None, :] >= p[:, None], 0.0, MASKV)
    b16["triB"] = np.where(j[None, :] < p[:, None], 0.0, MASKV)
    tq = np.arange(T)
    cend = 16 * p + 31
    cm = np.where(tq[None, :] >= cend[:, None], 0.0, MASKV)
    cm[127, :] = MASKV
    ov = np.zeros((128, 32))
    for c in range(127):
        for jj in range(32):
            if 16 * c <= 64 * jj + 63 and 16 * c + 31 >= 64 * jj:
                ov[c, jj] = 1.0
    b16["overlap"] = ov
    b16["cmpmask"] = cm
    es = np.zeros((128, 16, 128))
    for pp in range(128):
        jj = pp % 64
        if jj >= 32:
            continue
        for kt in range(16):
            for m_ in range(128):
                if jj == 2 * kt + m_ // 64:
                    es[pp, kt, m_] = 1.0
    b16["esel"] = es.reshape(128, 2048)
    sl_n, sl_m = _slopes()
    sl16 = np.concatenate([sl_n, sl_m])
    d = np.arange(16) - 12
    f32["bw"] = (sl16[None, :, None] * (128 * d[None, None, :] + p[:, None, None] - 256)).reshape(128, 256)
    d2 = np.arange(16) - 15
    f32["bn"] = (sl16[None, :, None] * (128 * d2[None, None, :] + p[:, None, None] - 64)).reshape(128, 256)
    f32["cw"] = (sl_n[None, :, None] * (cend[:, None, None] - (512 * np.arange(4)[None, None, :] + 256))).reshape(128, 32)
    f32["cn"] = (sl_n[None, :, None] * (cend[:, None, None] - (128 * np.arange(16)[None, None, :] + 64))).reshape(128, 128)
    keep = np.zeros((128, 8, 32))
    force = np.zeros((128, 8, 32))
    for tt in range(8, 16):
        for pp in range(128):
            t = tt * 128 + pp
            cur = t // 64
            for jj in range(32):
                if 64 * jj > t:
                    force[pp, tt - 8, jj] = -1e30 * (1.0 + jj / 64.0)
                elif jj == 0:
                    force[pp, tt - 8, jj] = 3e9
                elif jj == cur:
                    force[pp, tt - 8, jj] = 2e9
                elif jj == cur - 1:
                    force[pp, tt - 8, jj] = 1e9
                else:
                    keep[pp, tt - 8, jj] = 1.0
    padneg = np.zeros((128, 8, 8, 8))
    ownhot = np.zeros((128, 8, 8, 8))
    for tt in range(8, 16):
        cur = tt // 2
        for n in range(8):
            if n >= cur:
                padneg[:, tt - 8, :, n] = -1e30
            if n == cur:
                ownhot[:, tt - 8, :, n] = 1.0
    f32["eps"] = np.full((128, 1), EPS)
    rt = []
    for tr in (2, 3):
        a = (tr - 2) * 4
        rt += [keep[:, a:a + 4].reshape(128, 128), force[:, a:a + 4].reshape(128, 128),
               padneg[:, a:a + 4].reshape(128, 256), ownhot[:, a:a + 4].reshape(128, 256)]
    f32["rt"] = np.concatenate(rt, axis=1)
    offs_f = {}
    cols = []
    o = 0
    for k, v in f32.items():
        offs_f[k] = o
        o += v.shape[1]
        cols.append(v.astype(np.float32))
    cf = np.ascontiguousarray(np.concatenate(cols, axis=1))
    offs_b = {}
    cols = []
    o = 0
    for k, v in b16.items():
        offs_b[k] = o
        o += v.shape[1]
        cols.append(v.astype(np.float32))
    cbv = np.ascontiguousarray(np.concatenate(cols, axis=1).astype(ml_dtypes.bfloat16))
    return cf, offs_f, cbv, offs_b


def _box(ap):
    t = ap.tensor
    if t.name.startswith("ps") and len(t.name) == 3:
        return (t.name, 0, 128, 0, 512)
    row = 1
    for s in tuple(t.shape)[1:]:
        row *= int(s)
    off = int(ap.offset)
    p0 = off // row
    f0 = off % row
    apl = ap.ap
    npart = apl[0][1]
    ext = 1
    for st, cnt in apl[1:]:
        ext += (cnt - 1) * abs(st)
    return (t.name, p0, p0 + npart, f0, f0 + ext)


class Sched:
    def __init__(self, nc, ndma=24):
        self.nc = nc
        self.engs = {"pe": nc.tensor, "act": nc.scalar, "dve": nc.vector, "pool": nc.gpsimd, "sp": nc.sync}
        self.semh = {}
        for e in self.engs:
            self.semh[e] = nc.alloc_semaphore("sem_" + e)
        self.cnt = {e: 0 for e in self.engs}
        self.ndma = ndma
        for i in range(ndma):
            self.semh[("d", i)] = nc.alloc_semaphore("dsem%d" % i)
        self.dcnt = [0] * ndma
        self.drr = 0
        self.drr_pool = 0
        self.seen = {e: {} for e in self.engs}
        self.acc = {}
        self.out_tickets = []
        self.nwaits = 0

    def _wait(self, e, sk, val):
        if e == "pe" and sk == "pe":
            return
        if self.seen[e].get(sk, 0) >= val:
            return
        self.engs[e].wait_ge(self.semh[sk], val)
        self.seen[e][sk] = val
        self.nwaits += 1

    def _collect(self, reads, writes):
        waits = {}
        for ap in reads:
            b = _box(ap)
            for ent in self.acc.get(b[0], ()):
                if ent[0] == "w" and ent[3] < b[2] and b[1] < ent[4] and ent[5] < b[4] and b[3] < ent[6]:
                    if waits.get(ent[1], 0) < ent[2]:
                        waits[ent[1]] = ent[2]
        for ap in writes:
            b = _box(ap)
            for ent in self.acc.get(b[0], ()):
                if ent[3] < b[2] and b[1] < ent[4] and ent[5] < b[4] and b[3] < ent[6]:
                    if waits.get(ent[1], 0) < ent[2]:
                        waits[ent[1]] = ent[2]
        return waits

    def _record(self, sk, val, reads, writes):
        for ap in writes:
            b = _box(ap)
            lst = self.acc.setdefault(b[0], [])
            lst[:] = [e for e in lst if not (b[1] <= e[3] and e[4] <= b[2] and b[3] <= e[5] and e[6] <= b[4])]
            lst.append(("w", sk, val, b[1], b[2], b[3], b[4]))
        for ap in reads:
            b = _box(ap)
            lst = self.acc.setdefault(b[0], [])
            lst[:] = [e for e in lst if not (e[0] == "r" and e[1] == sk and b[1] <= e[3] and e[4] <= b[2]
                                             and b[3] <= e[5] and e[6] <= b[4])]
            lst.append(("r", sk, val, b[1], b[2], b[3], b[4]))

    def op(self, e, fn, reads, writes, inc=True):
        assert inc or e == "pe"
        waits = self._collect(reads, writes)
        for sk, val in waits.items():
            self._wait(e, sk, val)
        ins = fn()
        if inc:
            self.cnt[e] += 1
            ins.then_inc(self.semh[e], 1)
            val = self.cnt[e]
        else:
            val = self.cnt[e] + 1
        self._record(e, val, reads, writes)
        return ins

    def dma(self, q, out, in_, reads=(), writes=(), is_out=False, **kw):
        waits = self._collect(reads, writes)
        for sk, val in waits.items():
            self._wait(q, sk, val)
        half = self.ndma // 2
        if q == "pool":
            i = half + (self.drr_pool % half)
            self.drr_pool += 1
        else:
            i = self.drr % half
            self.drr += 1
        sk = ("d", i)
        if self.dcnt[i] > 0:
            self._wait(q, sk, self.dcnt[i])
        ins = self.engs[q].dma_start(out=out, in_=in_, **kw)
        self.dcnt[i] += 16
        ins.then_inc(self.semh[sk], 16)
        self._record(sk, self.dcnt[i], reads, writes)
        if is_out:
            self.out_tickets.append((sk, self.dcnt[i]))

    def barrier(self):
        for e in ("pe", "act", "dve", "pool", "sp"):
            for f in ("pe", "act", "dve", "pool", "sp"):
                if f != e and self.cnt[f] > 0:
                    self._wait(e, f, self.cnt[f])
            for i in range(self.ndma):
                if self.dcnt[i] > 0:
                    self._wait(e, ("d", i), self.dcnt[i])
        self.acc = {}

    def finish(self):
        for sk, val in self.out_tickets:
            self._wait("sp", sk, val)


def build_nc(stage=99):
    nc = bass.Bass("TRN2", target_bir_lowering=False)
    cf_np, OF, cb_np, OB = _make_consts()
    NF = cf_np.shape[1]
    NB = cb_np.shape[1]

    def din(name, shape, dt=F32):
        return nc.dram_tensor(name, list(shape), dt, kind="ExternalInput").ap()

    x_d = din("x", [T, D])
    p_d = din("p", [T, 256])
    wd = {}
    for nm, shp in [("ffn1_w1", [D, FF]), ("ffn1_w3", [D, FF]), ("ffn1_w2", [FF, D]), ("w_in", [D, IN_W]),
                    ("cmp_w1_k", [2048, 128]), ("cmp_w2_k", [128, 64]), ("cmp_w1_v", [2048, 128]),
                    ("cmp_w2_v", [128, 64]), ("pos_kT", [128, 32]), ("pos_vT", [128, 32]),
                    ("w_up_nsa", [512, D]), ("w_up_moba", [512, D]), ("w_out", [D, D]),
                    ("ffn2_w1", [D, FF]), ("ffn2_w3", [D, FF]), ("ffn2_w2", [FF, D]),
                    ("w_ple_gate", [D, D]), ("w_ple", [256, D]), ("gains", [128, 32]), ("gfin", [128, D])]:
        wd[nm] = din(nm, shp)
    cf_d = din("cf32", [128, NF])
    cb_d = din("cbf16", [128, NB], BF16)
    out_d = nc.dram_tensor("out", [T, D], F32, kind="ExternalOutput").ap()

    S = Sched(nc)
    stack = [None]

    def TS(name, shape, dt):
        if stack[0] is None:
            return nc.alloc_sbuf_tensor(name, shape, dt)
        return stack[0].enter_context(nc.sbuf_tensor(name, shape, dt))

    def scoped(fn, *a):
        if PLAN[0]:
            fn(*a)
            return
        S.barrier()
        with ExitStack() as st:
            stack[0] = st
            fn(*a)
            S.barrier()
        stack[0] = None

    hT = TS("hT", [128, 8, T], F32)
    NFS = OF["rt"]
    NBS = OB["cmpmask"]
    cf = TS("cf", [128, NFS], F32)
    cb = TS("cb", [128, NBS], BF16)
    gains = TS("gains_sb", [128, 32], F32)
    slab = [TS("slab%d" % i, [128, 4096], BF16) for i in range(3)]
    ps = [nc.alloc_psum_tensor("ps%d" % i, [128, 512], F32) for i in range(8)]

    identf = cf[:, OF["ident"]:OF["ident"] + 128]
    identb = cb[:, OB["ident"]:OB["ident"] + 128]
    onesb = cb[:, OB["ones"]:OB["ones"] + 128]
    epsc = cf[:, OF["eps"]:OF["eps"] + 1]

    S.dma("sp", cf[:, :], cf_d[:, 0:NFS], writes=[cf[:, :]])
    S.dma("sp", cb[:, :], cb_d[:, 0:NBS], writes=[cb[:, :]])
    S.dma("sp", gains[:, :], wd["gains"][:, :], writes=[gains[:, :]])

    def mm(out, lhsT, rhs, start, stop, inc=None):
        inc = True
        return S.op("pe", lambda: nc.tensor.matmul(out, lhsT, rhs, start=start, stop=stop),
                    [lhsT, rhs], [out], inc=inc)

    def tp(out, in_, ident):
        return S.op("pe", lambda: nc.tensor.transpose(out, in_, ident), [in_, ident], [out])

    def act(out, in_, func, bias=None, scale=None, accum_out=None):
        kw = {}
        rd = [in_]
        wr = [out]
        if bias is not None:
            kw["bias"] = bias
            if not isinstance(bias, (int, float)):
                rd.append(bias)
        if scale is not None:
            kw["scale"] = scale
            if not isinstance(scale, (int, float)):
                rd.append(scale)
        if accum_out is not None:
            kw["accum_out"] = accum_out
            wr.append(accum_out)
        return S.op("act", lambda: nc.scalar.activation(out=out, in_=in_, func=func, **kw), rd, wr)

    def tt(out, in0, in1, op, e="dve"):
        eng = nc.vector if e == "dve" else nc.gpsimd
        return S.op(e, lambda: eng.tensor_tensor(out=out, in0=in0, in1=in1, op=op), [in0, in1], [out])

    def ts(out, in0, s1, s2, op0, op1=None, e="dve"):
        eng = nc.vector if e == "dve" else nc.gpsimd
        rd = [in0]
        if not isinstance(s1, (int, float)):
            rd.append(s1)
        if s2 is not None and not isinstance(s2, (int, float)):
            rd.append(s2)
        if op1 is None:
            return S.op(e, lambda: eng.tensor_scalar(out=out, in0=in0, scalar1=s1, scalar2=None, op0=op0), rd, [out])
        return S.op(e, lambda: eng.tensor_scalar(out=out, in0=in0, scalar1=s1, scalar2=s2, op0=op0, op1=op1),
                    rd, [out])

    def stt(out, in0, scalar, in1, op0, op1):
        rd = [in0, in1]
        if not isinstance(scalar, (int, float)):
            rd.append(scalar)
        return S.op("dve", lambda: nc.vector.scalar_tensor_tensor(out=out, in0=in0, scalar=scalar, in1=in1,
                                                                   op0=op0, op1=op1), rd, [out])

    def cp(out, in_, e="dve"):
        if e == "act":
            return S.op("act", lambda: nc.scalar.copy(out=out, in_=in_), [in_], [out])
        eng = nc.vector if e == "dve" else nc.gpsimd
        return S.op(e, lambda: eng.tensor_copy(out=out, in_=in_), [in_], [out])

    def memset(ap, v, e="dve"):
        eng = nc.vector if e == "dve" else nc.gpsimd
        return S.op(e, lambda: eng.memset(ap, v), [], [ap])

    class RR:
        def __init__(self, items):
            self.items = items
            self.i = 0

        def next(self):
            it = self.items[self.i % len(self.items)]
            self.i += 1
            return it

    class WStream:
        def __init__(self):
            self.jobs = []
            self.issued = 0
            self.k = 0

        def add(self, parts):
            self.jobs.append(parts)

        def _issue(self, k):
            buf = slab[k % 3]
            for (off, shape, src) in self.jobs[k]:
                n = 1
                for s_ in shape:
                    n *= s_
                npart = int(src.shape[0])
                dst = buf[0:npart, off:off + n]
                if len(shape) == 2:
                    dst = dst.rearrange("p (a b) -> p a b", a=shape[0])
                S.dma("pool", dst, src, writes=[buf[0:npart, off:off + n]])

        def get(self):
            k = self.k
            self.k += 1
            while self.issued < min(k + 2, len(self.jobs)):
                self._issue(self.issued)
                self.issued += 1
            return slab[k % 3]

    WS = WStream()
    PLAN = [True]

    def W(parts):
        if PLAN[0]:
            WS.add(parts)
            return None
        return WS.get()

    def trs(tr):
        return slice(tr * 512, (tr + 1) * 512)

    sqb = [TS("sqb%d" % i, [128, 512], BF16) for i in range(2)]
    rstd = TS("rstd", [128, 512], F32)
    sqrr = RR(sqb)
    psM = RR([ps[6], ps[7]])

    def rms_range(tr, gidx, xn3):
        pb = psM.next()
        for c in range(8):
            sq = sqrr.next()
            act(sq[:, :], hT[:, c, trs(tr)], AF.Square)
            mm(pb[:, :], onesb, sq[:, :], c == 0, c == 7)
        act(rstd[:, :], pb[:, :], AF.Ln, bias=epsc, scale=1.0 / D)
        act(rstd[:, :], rstd[:, :], AF.Exp, scale=-0.5)
        for c in range(8):
            stt(xn3[:, c, :], hT[:, c, trs(tr)], gains[:, gidx * 8 + c:gidx * 8 + c + 1], rstd[:, :],
                ALU.mult, ALU.mult)

    def phase_load():
        xtok = [TS("xtok%d" % i, [128, D], F32) for i in range(2)]
        for t_ in range(16):
            xb = xtok[t_ % 2]
            S.dma("sp", xb[:, :], x_d[t_ * 128:(t_ + 1) * 128, :], writes=[xb[:, :]])
            for half in range(2):
                pb = psM.next()
                for c4 in range(4):
                    c = half * 4 + c4
                    tp(pb[:, c4 * 128:(c4 + 1) * 128], xb[:, c * 128:(c + 1) * 128], identf)
                cp(hT[:, half * 4:(half + 1) * 4, t_ * 128:(t_ + 1) * 128],
                   pb[:, :].rearrange("p (c t) -> p c t", c=4), e="act" if half else "dve")

    def phase_ffn(w1n, w3n, w2n, gidx, tag):
        w1 = wd[w1n].rearrange("(c p) f -> p c f", p=128)
        w3 = wd[w3n].rearrange("(c p) f -> p c f", p=128)
        w2 = wd[w2n]
        if not PLAN[0]:
            xnT = TS("xnT" + tag, [128, 8, T], BF16)
            h1 = [TS("h1%s%d" % (tag, i), [128, 2, 512], BF16) for i in range(2)]
            sa = [TS("sa%s%d" % (tag, i), [128, 512], F32) for i in range(2)]
            for tr in range(4):
                rms_range(tr, gidx, xnT[:, :, trs(tr)])
            psAB = RR([ps[0], ps[1], ps[2], ps[3]])
            psY = RR([ps[4], ps[5], ps[6], ps[7]])
            sar = RR(sa)
            h1r = RR(h1)
            ytmp = [TS("ytmp%s%d" % (tag, i), [128, 512], F32) for i in range(2)]
            ytr = RR(ytmp)
        for s in range(11):
            A = W([(0, [8, 256], w1[:, :, s * 256:(s + 1) * 256]), (2048, [8, 256], w3[:, :, s * 256:(s + 1) * 256])])
            B = W([(0, [2, 1024], w2[s * 256:(s + 1) * 256, :].rearrange("(j p) d -> p j d", p=128))])
            if PLAN[0]:
                continue
            w1s = A[:, 0:2048].rearrange("p (c f) -> p c f", c=8)
            w3s = A[:, 2048:4096].rearrange("p (c f) -> p c f", c=8)
            w2s = B[:, 0:2048].rearrange("p (j d) -> p j d", j=2)
            for tr in range(4):
                hb = h1r.next()
                for j in range(2):
                    pa = psAB.next()
                    pb = psAB.next()
                    for c in range(8):
                        mm(pa[:, :], w1s[:, c, j * 128:(j + 1) * 128], xnT[:, c, trs(tr)], c == 0, c == 7)
                    for c in range(8):
                        mm(pb[:, :], w3s[:, c, j * 128:(j + 1) * 128], xnT[:, c, trs(tr)], c == 0, c == 7)
                    sab = sar.next()
                    act(sab[:, :], pa[:, :], AF.Silu)
                    tt(hb[:, j, :], sab[:, :], pb[:, :], ALU.mult)
                for dc in range(8):
                    py = psY.next()
                    for j in range(2):
                        mm(py[:, :], w2s[:, j, dc * 128:(dc + 1) * 128], hb[:, j, :], j == 0, j == 1)
                    if False:
                        yb = ytr.next()
                        act(yb[:, :], py[:, :], AF.Copy, scale=0.5)
                        tt(hT[:, dc, trs(tr)], hT[:, dc, trs(tr)], yb[:, :], ALU.add, e="pool")
                    else:
                        stt(hT[:, dc, trs(tr)], py[:, :], 0.5, hT[:, dc, trs(tr)], ALU.mult, ALU.add)

    def phase_ple():
        wg = wd["w_ple_gate"].rearrange("(c p) f -> p c f", p=128)
        wp = wd["w_ple"].rearrange("(c p) f -> p c f", p=128)
        if not PLAN[0]:
            xnT = TS("xnTple", [128, 8, T], BF16)
            pT = TS("pT", [128, 2, T], BF16)
            ptok = [TS("ptok%d" % i, [128, 256], F32) for i in range(2)]
            sg = [TS("sgple%d" % i, [128, 512], F32) for i in range(2)]
            for t_ in range(16):
                pbuf = ptok[t_ % 2]
                S.dma("sp", pbuf[:, :], p_d[t_ * 128:(t_ + 1) * 128, :], writes=[pbuf[:, :]])
                pb = psM.next()
                for c in range(2):
                    tp(pb[:, c * 128:(c + 1) * 128], pbuf[:, c * 128:(c + 1) * 128], identf)
                cp(pT[:, :, t_ * 128:(t_ + 1) * 128], pb[:, 0:256].rearrange("p (c t) -> p c t", c=2))
            for tr in range(4):
                rms_range(tr, 3, xnT[:, :, trs(tr)])
            psG = RR([ps[0], ps[1], ps[2], ps[3]])
            sgr = RR(sg)
        for dh in range(2):
            A = W([(0, [8, 512], wg[:, :, dh * 512:(dh + 1) * 512])])
            B = W([(0, [2, 512], wp[:, :, dh * 512:(dh + 1) * 512])])
            if PLAN[0]:
                continue
            wgs = A[:, 0:4096].rearrange("p (c f) -> p c f", c=8)
            wps = B[:, 0:1024].rearrange("p (c f) -> p c f", c=2)
            for tr in range(4):
                for d4 in range(4):
                    dc = dh * 4 + d4
                    pg = psG.next()
                    pp = psG.next()
                    for c in range(8):
                        mm(pg[:, :], wgs[:, c, d4 * 128:(d4 + 1) * 128], xnT[:, c, trs(tr)], c == 0, c == 7)
                    for c in range(2):
                        mm(pp[:, :], wps[:, c, d4 * 128:(d4 + 1) * 128], pT[:, c, trs(tr)], c == 0, c == 1)
                    sgb = sgr.next()
                    act(sgb[:, :], pg[:, :], AF.Sigmoid)
                    tt(sgb[:, :], sgb[:, :], pp[:, :], ALU.mult)
                    tt(hT[:, dc, trs(tr)], hT[:, dc, trs(tr)], sgb[:, :], ALU.add)

    def phase_final():
        gfin = TS("gfin_sb", [128, D], F32)
        S.dma("sp", gfin[:, :], wd["gfin"][:, :], writes=[gfin[:, :]])
        otok = [TS("otok%d" % i, [128, D], F32) for i in range(2)]
        junk = TS("junk", [128, 512], F32)
        ssq = TS("ssq", [128, 4], F32)
        psF = RR([ps[0], ps[1], ps[2], ps[3], ps[4], ps[5]])
        for t_ in range(16):
            ob = otok[t_ % 2]
            pbs = [psF.next(), psF.next()]
            for half in range(2):
                for c4 in range(4):
                    c = half * 4 + c4
                    tp(pbs[half][:, c4 * 128:(c4 + 1) * 128], hT[:, c, t_ * 128:(t_ + 1) * 128], identf)
                act(junk[:, :], pbs[half][:, :], AF.Square, accum_out=ssq[:, half:half + 1])
            tt(ssq[:, 2:3], ssq[:, 0:1], ssq[:, 1:2], ALU.add)
            act(ssq[:, 3:4], ssq[:, 2:3], AF.Ln, bias=epsc, scale=1.0 / D)
            act(ssq[:, 3:4], ssq[:, 3:4], AF.Exp, scale=-0.5)
            for half in range(2):
                stt(ob[:, half * 512:(half + 1) * 512], pbs[half][:, :], ssq[:, 3:4],
                    gfin[:, half * 512:(half + 1) * 512], ALU.mult, ALU.mult)
            S.dma("sp", out_d[t_ * 128:(t_ + 1) * 128, :], ob[:, :], reads=[ob[:, :]], is_out=True)

    def phase_mixer():
        win = wd["w_in"].rearrange("(c p) f -> p c f", p=128)
        wupn = wd["w_up_nsa"].rearrange("(c p) f -> p c f", p=128)
        wupm = wd["w_up_moba"].rearrange("(c p) f -> p c f", p=128)
        wout = wd["w_out"].rearrange("(c p) f -> p c f", p=128)
        sl_n, sl_m = _slopes()
        if not PLAN[0]:
            kslcT = TS("kslcT", [128, T], BF16)
            kwinT = TS("kwinT", [128, T], BF16)
            kcmpT = TS("kcmpT", [128, 528], BF16)
            vcmpT = TS("vcmpT", [128, 528], BF16)
            kmT = TS("kmT", [128, 4, T], BF16)
            vslc = TS("vslc", [128, 16, 2, 65], BF16)
            vwin = TS("vwin", [128, 16, 2, 65], BF16)
            vm = TS("vm", [128, 16, 8, 65], BF16)
            gsig = TS("gsig", [128, 16, 24], F32)
            kcT = TS("kcT", [128, 128], BF16)
            vca = TS("vca", [128, 2, 97], BF16)
            kmeanT = TS("kmeanT", [128, 4, 16], BF16)
            kmsum = TS("kmsum", [128, 4, 2], F32)
            w2k = TS("w2k", [128, 128], BF16)
            w2v = TS("w2v", [128, 64], BF16)
            posk = TS("posk", [128, 32], BF16)
            posv = TS("posv", [128, 32], BF16)
            cbias = TS("cbias", [128, 2], F32)
            u_tr = TS("u_tr", [128, 8, 512], BF16)
            qmix = TS("qmix", [128, 8, 512], BF16)

            class _Sub:
                def __init__(self, base, off):
                    self.base, self.off = base, off

                def __getitem__(self, key):
                    a, b, c = key
                    if isinstance(b, int):
                        b = b + self.off
                    else:
                        b = slice((b.start or 0) + self.off, (b.stop if b.stop is not None else 4) + self.off)
                    return self.base[a, b, c]
            qn = _Sub(qmix, 0)
            qm = _Sub(qmix, 4)
            ptl = [TS("ptl%d" % i, [128, 512], BF16) for i in range(3)]
            otk = [TS("otk%d" % i, [128, 4, 4, 64], F32) for i in range(2)]
            onT = TS("onT", [128, 4, 512], BF16)
            omT = TS("omT", [128, 4, 512], BF16)
            mixed = qmix
            sgA = [TS("sgA%d" % i, [128, 512], F32) for i in range(1)]
            sgB = [TS("sgB%d" % i, [128, 512], F32) for i in range(1)]
            mbTn = TS("mbTn", [128, 512], BF16)
            mbTm = TS("mbTm", [128, 512], BF16)
            mbtokN = TS("mbtokN", [128, 128], F32)
            mbtok = TS("mbtok", [128, 128], F32)
            impa = TS("impa", [128, 4, 2, 32], F32)
            smal = TS("smal", [128, 64], F32)
            m8 = TS("m8", [128, 16], F32)
            tk32 = TS("tk32", [128, 64], F32)
            cmsk = TS("cmsk", [128, 512], BF16)
            rtab = TS("rtab", [128, 768], F32)
            esel = TS("esel", [128, 16, 128], BF16)
            S.dma("sp", esel[:, :, :], cb_d[:, OB["esel"]:OB["esel"] + 2048].rearrange("p (k m) -> p k m", k=16),
                  writes=[esel[:, :, :]])
            hid = TS("hid", [128, 4, 32], F32)
            hidb = TS("hidb", [128, 32], BF16)
            hidv = TS("hidv", [128, 2, 128], BF16)
            gtmp = TS("gtmp", [64, 528], BF16)
            gsf = TS("gsf", [128, 8, 16], F32)

            memset(kcT[:, :], 0.0)
            memset(mbtokN[:, :], 0.0)
            memset(gsf[:, :, :], -1.0e30)
            memset(kcmpT[:, 0:16], 0.0)
            memset(vcmpT[:, 0:16], 0.0)
            memset(hidv[:, :, :], 0.0)
            memset(vca[:, :, :], 0.0)
            memset(kmeanT[:, :, :], 0.0)
            memset(vslc[:, :, :, 64:65], 1.0)
            memset(vwin[:, :, :, 64:65], 1.0)
            memset(vm[:, :, :, 64:65], 1.0)
            memset(vca[:, :, 64:65], 1.0)
            for g in range(2):
                cp(vca[:, g, 65:97], cb[:, OB["overlap"]:OB["overlap"] + 32])
            for (w2t, w2n_, pt, pn) in ((w2k, "cmp_w2_k", posk, "pos_kT"), (w2v, "cmp_w2_v", posv, "pos_vT")):
                S.dma("pool", pt[:, :], wd[pn][:, :], writes=[pt[:, :]])
                if w2t is w2k:
                    S.dma("pool", w2t[:, 0:64], wd[w2n_][:, :], writes=[w2t[:, 0:64]])
                    S.dma("pool", w2t[:, 64:128], wd[w2n_][:, :], writes=[w2t[:, 64:128]])
                else:
                    S.dma("pool", w2t[:, :], wd[w2n_][:, :], writes=[w2t[:, :]])

            psS = RR([ps[0], ps[1], ps[2], ps[3]])
            psO = RR([ps[4], ps[5]])
            ptr = RR(ptl)
            otr = RR(otk)
            sgAr = RR(sgA)
            sgBr = RR(sgB)

        def proj_fm(ws, c0, dst, tr, base=None, ev="act"):
            pb = psM.next()
            for c in range(8):
                if base is None:
                    l_ = ws[:, c, c0:c0 + 128]
                else:
                    l_ = base(c)
                mm(pb[:, :], l_, u_tr[:, c, :], c == 0, c == 7)
            cp(dst, pb[:, :], e=ev)

        NTRS = int(os.environ.get("MK_NTR", "4"))
        SUB = int(os.environ.get("MK_SUB", "9"))
        for tr in range(NTRS):
            S0 = W([(0, [8, 512], win[:, :, 0:512])])
            if not PLAN[0]:
                rms_range(tr, 1, u_tr)
                w0 = S0[:, 0:4096].rearrange("p (c f) -> p c f", c=8)
                for i in range(4):
                    proj_fm(w0, i * 128, qn[:, i, :], tr, ev="act" if i % 2 else "dve")
            S1 = W([(0, [8, 512], win[:, :, 512:1024])])
            if not PLAN[0]:
                w1_ = S1[:, 0:4096].rearrange("p (c f) -> p c f", c=8)
                proj_fm(w1_, 0, kcmpT[:, 16:528], tr, ev="act")
                proj_fm(w1_, 128, vcmpT[:, 16:528], tr, ev="dve")
                proj_fm(w1_, 256, kslcT[:, trs(tr)], tr, ev="act")
                for j in range(4):
                    t_ = tr * 4 + j
                    pb = psM.next()
                    for c in range(8):
                        mm(pb[:, 0:128], u_tr[:, c, j * 128:(j + 1) * 128], w1_[:, c, 384:512], c == 0, c == 7)
                    cp(vslc[:, t_, :, 0:64], pb[:, 0:128].rearrange("p (g d) -> p g d", g=2))
            S2 = W([(0, [8, 280], win[:, :, 1024:1304])])
            if not PLAN[0]:
                w2_ = S2[:, 0:2240].rearrange("p (c f) -> p c f", c=8)
                proj_fm(w2_, 0, kwinT[:, trs(tr)], tr, ev="act")
                for j in range(4):
                    t_ = tr * 4 + j
                    pb = psM.next()
                    for c in range(8):
                        mm(pb[:, 0:152], u_tr[:, c, j * 128:(j + 1) * 128], w2_[:, c, 128:280], c == 0, c == 7)
                    cp(vwin[:, t_, :, 0:64], pb[:, 0:128].rearrange("p (g d) -> p g d", g=2))
                    act(gsig[:, t_, :], pb[:, 128:152], AF.Sigmoid)
            S3 = W([(0, [8, 512], win[:, :, 1304:1816])])
            if not PLAN[0]:
                w3_ = S3[:, 0:4096].rearrange("p (c f) -> p c f", c=8)
                for i in range(4):
                    proj_fm(w3_, i * 128, qm[:, i, :], tr, ev="act" if i % 2 else "dve")
            S4 = W([(0, [8, 512], win[:, :, 1816:2328])])
            if not PLAN[0]:
                w4_ = S4[:, 0:4096].rearrange("p (c f) -> p c f", c=8)
                for i in range(4):
                    proj_fm(w4_, i * 128, kmT[:, i, trs(tr)], tr, ev="act" if i % 2 else "dve")
                for i in range(4):
                    S.op("dve", lambda: nc.vector.tensor_reduce(
                        out=kmsum[:, i, :], in_=kmT[:, i, trs(tr)].rearrange("p (n k) -> p n k", n=2),
                        axis=mybir.AxisListType.X, op=ALU.add),
                        [kmT[:, i, trs(tr)]], [kmsum[:, i, :]])
                ts(kmeanT[0:64, :, 2 * tr:2 * tr + 2], kmsum[0:64, :, :], 1.0 / 256.0, None, ALU.mult)
                ts(kmeanT[64:128, :, 8 + 2 * tr:8 + 2 * tr + 2], kmsum[64:128, :, :], 1.0 / 256.0, None, ALU.mult)
            S5 = W([(0, [8, 512], win[:, :, 2328:2840])])
            if not PLAN[0]:
                w5_ = S5[:, 0:4096].rearrange("p (c f) -> p c f", c=8)
                for j in range(4):
                    t_ = tr * 4 + j
                    pb = psM.next()
                    for c in range(8):
                        mm(pb[:, :], u_tr[:, c, j * 128:(j + 1) * 128], w5_[:, c, :], c == 0, c == 7)
                    cp(vm[:, t_, :, 0:64], pb[:, :].rearrange("p (h d) -> p h d", h=8), e="act" if j % 2 else "dve")

            if SUB < 1:
                continue
            c_lo = 0 if tr == 0 else 32 * tr - 1
            c_hi = 32 * tr + 30
            nb = c_hi - c_lo + 1
            for which in range(2):
                wsrc = wd["cmp_w1_k" if which == 0 else "cmp_w1_v"].rearrange("(l d) h -> d l h", d=64)
                CW = W([(q4 * 1024, [8, 128], wsrc[:, q4 * 8:(q4 + 1) * 8, :]) for q4 in range(4)])
                if PLAN[0]:
                    continue
                srcT = kcmpT if which == 0 else vcmpT
                w1t = CW[0:64, 0:4096].rearrange("p (l h) -> p l h", l=32)
                if tr == 0:
                    pt = posk if which == 0 else posv
                    pb = psM.next()
                    for l in range(32):
                        mm(pb[:, 0:1], w1t[:, l, :], pt[0:64, l:l + 1], l == 0, l == 31)
                    cp(cbias[:, which:which + 1], pb[:, 0:1])
                for g in range(2):
                    if g == 1:
                        cp(gtmp[0:64, 0:528], srcT[64:128, 0:528])
                        src_t, src_p = gtmp, 0
                    else:
                        src_t, src_p = srcT, 0
                    pb = psM.next()
                    for l in range(32):
                        col0 = 16 * c_lo + l - 512 * tr + 16
                        rhs_ = bass.AP(src_t, src_p * 528 + col0, [[528, 64], [16, nb]])
                        mm(pb[:, 0:nb], w1t[:, l, :], rhs_, l == 0, l == 31)
                    xx = hid[:, 0, 0:nb]
                    x2 = hid[:, 1, 0:nb]
                    ts(xx, pb[:, 0:nb], cbias[:, which:which + 1], None, ALU.add)
                    tt(x2, xx, xx, ALU.mult)
                    ts(x2, x2, 0.044715, 1.0, ALU.mult, ALU.add)
                    tt(x2, x2, xx, ALU.mult)
                    act(hid[:, 2, 0:nb], x2, AF.Sigmoid, scale=1.5957691216057308)
                    if which == 0:
                        tt(hidb[:, 0:nb], xx, hid[:, 2, 0:nb], ALU.mult)
                        pb2 = psM.next()
                        mm(pb2[:, 0:nb], w2k[:, :], hidb[:, 0:nb], True, True)
                        cp(kcT[64 * g:64 * g + 64, c_lo:c_lo + nb], pb2[64 * g:64 * g + 64, 0:nb])
                    else:
                        tt(hidv[:, g, c_lo:c_lo + nb], xx, hid[:, 2, 0:nb], ALU.mult)
                        pb2 = psM.next()
                        mm(pb2[:, 0:64], hidv[:, g, :], w2v[:, :], True, True)
                        cp(vca[:, g, 0:64], pb2[:, 0:64])
                cp(srcT[:, 0:16], srcT[:, 512:528])
            if not PLAN[0]:
                S.dma("sp", cmsk[:, :], cb_d[:, OB["cmpmask"] + tr * 512:OB["cmpmask"] + (tr + 1) * 512],
                      writes=[cmsk[:, :]])
                if tr >= 2:
                    S.dma("sp", rtab[:, :], cf_d[:, OF["rt"] + (tr - 2) * 768:OF["rt"] + (tr - 1) * 768],
                          writes=[rtab[:, :]])

            if SUB < 2:
                continue
            def attend(kT_ap_fn, q_ap, vfn, tiles, bias_fn, narrow, ops, mask_fn=None, ncols=65):
                npv = [0]
                npv_total = sum((c1 - c0) // 128 for (kt, c0, c1, extra) in tiles)

                def emit_scores(ti):
                    (kt, c0, c1, extra) = tiles[ti]
                    sp_ = psS.next()
                    n_extra = len(extra) + (1 if mask_fn is not None else 0)
                    mm(sp_[:, c0:c1], kT_ap_fn(kt), q_ap[:, c0:c1], True, n_extra == 0)
                    k_ = 0
                    if mask_fn is not None:
                        k_ += 1
                        ml, mr = mask_fn(kt)
                        mm(sp_[:, c0:c1], ml, mr[:, c0:c1], False, k_ == n_extra)
                    for (e0, en, el, er) in extra:
                        k_ += 1
                        mm(sp_[:, e0:e0 + en], el, er, False, k_ == n_extra)
                    return sp_

                DEPTH = 2
                pend = [emit_scores(i) for i in range(min(DEPTH, len(tiles)))]
                for ti in range(len(tiles)):
                    (kt, c0, c1, extra) = tiles[ti]
                    sp_ = pend.pop(0)
                    if ti + DEPTH < len(tiles):
                        pend.append(emit_scores(ti + DEPTH))
                    pt = ptr.next()
                    if narrow:
                        for j in range(c0 // 128, c1 // 128):
                            act(pt[:, j * 128:(j + 1) * 128], sp_[:, j * 128:(j + 1) * 128], AF.Exp,
                                bias=bias_fn(kt, j), scale=0.125)
                    else:
                        act(pt[:, c0:c1], sp_[:, c0:c1], AF.Exp, bias=bias_fn(kt, None), scale=0.125)
                    for j in range(c0 // 128, c1 // 128):
                        npv[0] += 1
                        mm(ops[:, j * 128:j * 128 + ncols], pt[:, j * 128:(j + 1) * 128], vfn(kt),
                           npv[0] == 1, npv[0] == npv_total)

            triA = cb[:, OB["triA"]:OB["triA"] + 128]
            triB = cb[:, OB["triB"]:OB["triB"] + 128]

            def causal_tiles(tr_):
                tl = []
                for kt in range(0, 4 * tr_ + 4):
                    i = kt - 4 * tr_
                    if i < 0:
                        tl.append((kt, 0, 512, []))
                    else:
                        tl.append((kt, 128 * i, 512, [(128 * i, 128, identb, triA)]))
                return tl

            def win_tiles(tr_):
                tl = []
                for kt in range(4 * tr_ - 4, 4 * tr_ + 4):
                    if kt < 0:
                        continue
                    i = kt - 4 * tr_
                    if i < 0:
                        i2 = i + 4
                        tl.append((kt, 0, 128 * (i2 + 1), [(128 * i2, 128, identb, triB)]))
                    else:
                        tl.append((kt, 128 * i, 512, [(128 * i, 128, identb, triA)]))
                return tl

            if not PLAN[0]:
                def bias_head(hidx, narrow):
                    def f(kt, j):
                        if narrow:
                            di = kt - (4 * tr + j) + 15
                            o_ = OF["bn"] + hidx * 16 + di
                        else:
                            di = kt - 4 * tr + 12
                            o_ = OF["bw"] + hidx * 16 + di
                        return cf[:, o_:o_ + 1]
                    return f

                def bias_cmp(h, narrow):
                    def f(kt, j):
                        if narrow:
                            o_ = OF["cn"] + h * 16 + (4 * tr + j)
                        else:
                            o_ = OF["cw"] + h * 4 + tr
                        return cf[:, o_:o_ + 1]
                    return f

                def norm_coef(ob_, gate_ap):
                    ov_ = ob_[:, :].rearrange("p (j c) -> p j c", j=4)
                    ts(smal[:, 0:4], ov_[:, :, 64], TINY, None, ALU.max)
                    S.op("dve", lambda: nc.vector.reciprocal(out=smal[:, 4:8], in_=smal[:, 0:4]),
                         [smal[:, 0:4]], [smal[:, 4:8]])
                    if gate_ap is not None:
                        tt(smal[:, 8:12], smal[:, 4:8], gate_ap, ALU.mult)
                    return ov_

                ots = [otk[0], otk[1]]
            if SUB < 3:
                continue
            if not PLAN[0]:
                for g in range(2):
                    ot = ots[g]
                    for hg in range(4):
                        h = g * 4 + hg
                        ch, bp = h % 4, 64 * (h // 4)
                        nar = bool(sl_n[h] > 0.3)
                        oc = psO.next()
                        q_ap = qn[bp:bp + 64, ch, :]
                        attend(lambda kt: kcT[bp:bp + 64, :], q_ap, lambda kt: vca[:, g, :],
                               [(0, 0, 512, [(0, 512, identb, cmsk[:, :])])], bias_cmp(h, nar), nar, oc, ncols=97)
                        ocv = norm_coef(oc, gsig[:, 4 * tr:4 * tr + 4, h * 3 + 0])
                        for j in range(4):
                            ts(ot[:, j, hg, :], ocv[:, j, 0:64], smal[:, 8 + j:9 + j], None, ALU.mult)
                            if tr >= 2:
                                if hg == 0:
                                    ts(impa[:, j, g, :], ocv[:, j, 65:97], smal[:, 4 + j:5 + j], None, ALU.mult)
                                else:
                                    stt(impa[:, j, g, :], ocv[:, j, 65:97], smal[:, 4 + j:5 + j], impa[:, j, g, :],
                                        ALU.mult, ALU.add)
                if tr >= 2:
                    for j in range(4):
                        kp = rtab[:, j * 32:(j + 1) * 32]
                        fo = rtab[:, 128 + j * 32:128 + (j + 1) * 32]
                        for g in range(2):
                            iv = tk32[:, 0:32]
                            tt(iv, impa[:, j, g, :], kp, ALU.mult)
                            tt(iv, iv, fo, ALU.add)
                            S.op("dve", lambda: nc.vector.max(out=m8[:, 0:8], in_=iv), [iv], [m8[:, 0:8]])
                            S.op("dve", lambda: nc.vector.match_replace(out=tk32[:, 32:64], in_to_replace=m8[:, 0:8],
                                                                         in_values=iv, imm_value=-3.0e38),
                                 [iv, m8[:, 0:8]], [tk32[:, 32:64]])
                            S.op("dve", lambda: nc.vector.max(out=m8[:, 8:16], in_=tk32[:, 32:64]),
                                 [tk32[:, 32:64]], [m8[:, 8:16]])
                            ts(tk32[:, 32:64], iv, m8[:, 15:16], None, ALU.is_ge)
                            ts(mbtokN[:, g * 64:g * 64 + 32], tk32[:, 32:64], -1.0, -MASKV, ALU.add, ALU.mult)
                        pb = psM.next()
                        tp(pb[:, 0:128], mbtokN[:, :], identf)
                        cp(mbTn[:, j * 128:(j + 1) * 128], pb[:, 0:128])
                for g in range(2):
                    ot = ots[g]
                    for hg in range(4):
                        h = g * 4 + hg
                        ch, bp = h % 4, 64 * (h // 4)
                        nar = bool(sl_n[h] > 0.3)
                        q_ap = qn[bp:bp + 64, ch, :]
                        osel = psO.next()
                        mfn = None
                        if tr >= 2:
                            def mfn(kt, g=g):
                                return esel[64 * g:64 * g + 64, kt, :], mbTn[64 * g:64 * g + 64, :]
                        attend(lambda kt: kslcT[bp:bp + 64, kt * 128:(kt + 1) * 128], q_ap,
                               lambda kt: vslc[:, kt, g, :], causal_tiles(tr), bias_head(h, nar), nar, osel,
                               mask_fn=mfn)
                        ov_ = norm_coef(osel, gsig[:, 4 * tr:4 * tr + 4, h * 3 + 1])
                        for j in range(4):
                            stt(ot[:, j, hg, :], ov_[:, j, 0:64], smal[:, 8 + j:9 + j], ot[:, j, hg, :],
                                ALU.mult, ALU.add)
                        owin = psO.next()
                        attend(lambda kt: kwinT[bp:bp + 64, kt * 128:(kt + 1) * 128], q_ap,
                               lambda kt: vwin[:, kt, g, :], win_tiles(tr), bias_head(h, nar), nar, owin)
                        ov_ = norm_coef(owin, gsig[:, 4 * tr:4 * tr + 4, h * 3 + 2])
                        for j in range(4):
                            stt(ot[:, j, hg, :], ov_[:, j, 0:64], smal[:, 8 + j:9 + j], ot[:, j, hg, :],
                                ALU.mult, ALU.add)
                    for i2 in range(2):
                        pb = psM.next()
                        for j in range(4):
                            tp(pb[:, j * 128:(j + 1) * 128], ot[:, j, 2 * i2:2 * i2 + 2, :], identf)
                        cp(onT[:, 2 * g + i2, :], pb[:, :], e="act")

            if SUB < 4:
                continue
            if not PLAN[0]:
                MDBG = int(os.environ.get("MK_MOBA", "3"))
                if tr >= 2 and MDBG >= 1:
                    for j in range(4):
                        pb = psM.next()
                        for ch in range(4):
                            mm(pb[:, ch * 16:(ch + 1) * 16], qm[:, ch, j * 128:(j + 1) * 128],
                               kmeanT[:, ch, :], True, True)
                        GS = int(os.environ.get("MK_GS", "9"))
                        if GS >= 1:
                            tt(gsf[:, :, 0:8], pb[:, 0:64].rearrange("p (h n) -> p h n", h=8),
                               rtab[:, 256 + j * 64:256 + (j + 1) * 64].rearrange("p (h n) -> p h n", h=8), ALU.add)
                        if GS >= 2:
                            for h in range(8):
                                S.op("dve", lambda: nc.vector.max(out=m8[:, 0:8], in_=gsf[:, h, :]),
                                     [gsf[:, h, :]], [m8[:, 0:8]])
                                ts(tk32[:, h * 8:(h + 1) * 8], gsf[:, h, 0:8], m8[:, 2:3], None, ALU.is_ge)
                        if GS >= 3:
                            tt(tk32[:, :], tk32[:, :], rtab[:, 512 + j * 64:512 + (j + 1) * 64], ALU.add)
                            ts(mbtok[:, 0:64], tk32[:, :], -1.0, -MASKV, ALU.add, ALU.mult)
                            ts(mbtok[:, 64:128], tk32[:, :], -1.0, -MASKV, ALU.add, ALU.mult)
                        if GS >= 4:
                            pb2 = psM.next()
                            tp(pb2[:, 0:128], mbtok[:, :], identf)
                            cp(mbTm[:, j * 128:(j + 1) * 128], pb2[:, 0:128])
                for hp in range(4):
                    ot = otr.next()
                    for h2 in range(2):
                        h = hp * 2 + h2
                        ch, bp = hp, 64 * h2
                        nar = bool(sl_m[h] > 0.3)
                        q_ap = qm[bp:bp + 64, ch, :]
                        om = psO.next()
                        mfn = None
                        if tr >= 2 and MDBG >= 2:
                            def mfn(kt, h=h):
                                c_ = 8 * h + kt // 2
                                b_ = 64 * (h % 2)
                                l_ = bass.AP(cb, b_ * NBS + OB["ident"] + b_ + c_, [[NBS, 64], [0, 128]])
                                return l_, mbTm[b_:b_ + 64, :]
                        attend(lambda kt: kmT[bp:bp + 64, ch, kt * 128:(kt + 1) * 128], q_ap,
                               lambda kt: vm[:, kt, h, :], causal_tiles(tr), bias_head(8 + h, nar), nar, om,
                               mask_fn=mfn)
                        ov_ = norm_coef(om, None)
                        for j in range(4):
                            ts(ot[:, j, h2, :], ov_[:, j, 0:64], smal[:, 4 + j:5 + j], None, ALU.mult)
                    pb = psM.next()
                    for j in range(4):
                        tp(pb[:, j * 128:(j + 1) * 128], ot[:, j, 0:2, :], identf)
                    cp(omT[:, hp, :], pb[:, :], e="act")

            if SUB < 5:
                continue
            for dc in range(8):
                c0 = dc * 128
                MJ = W([(0, [8, 128], win[:, :, 2840 + c0:2840 + c0 + 128]),
                        (1024, [8, 128], win[:, :, 3864 + c0:3864 + c0 + 128]),
                        (2048, [4, 128], wupn[:, :, c0:c0 + 128]),
                        (2560, [4, 128], wupm[:, :, c0:c0 + 128])])
                if PLAN[0]:
                    continue
                ga_ = MJ[:, 0:1024].rearrange("p (c f) -> p c f", c=8)
                gb_ = MJ[:, 1024:2048].rearrange("p (c f) -> p c f", c=8)
                un_ = MJ[:, 2048:2560].rearrange("p (c f) -> p c f", c=4)
                um_ = MJ[:, 2560:3072].rearrange("p (c f) -> p c f", c=4)
                pga = psS.next()
                pgb = psS.next()
                pyn = psO.next()
                pym = psO.next()
                for c in range(8):
                    mm(pga[:, :], ga_[:, c, :], u_tr[:, c, :], c == 0, c == 7)
                for c in range(8):
                    mm(pgb[:, :], gb_[:, c, :], u_tr[:, c, :], c == 0, c == 7)
                for c in range(4):
                    mm(pyn[:, :], un_[:, c, :], onT[:, c, :], c == 0, c == 3)
                for c in range(4):
                    mm(pym[:, :], um_[:, c, :], omT[:, c, :], c == 0, c == 3)
                sA = sgAr.next()
                sB = sgBr.next()
                act(sA[:, :], pga[:, :], AF.Sigmoid)
                act(sB[:, :], pgb[:, :], AF.Sigmoid)
                tt(sA[:, :], sA[:, :], pyn[:, :], ALU.mult)
                tt(sB[:, :], sB[:, :], pym[:, :], ALU.mult)
                tt(mixed[:, dc, :], sA[:, :], sB[:, :], ALU.add)
            for dh in range(2):
                WO = W([(0, [8, 512], wout[:, :, dh * 512:(dh + 1) * 512])])
                if PLAN[0]:
                    continue
                wo_ = WO[:, 0:4096].rearrange("p (c f) -> p c f", c=8)
                for d4 in range(4):
                    dc = dh * 4 + d4
                    pb = psM.next()
                    for c in range(8):
                        mm(pb[:, :], wo_[:, c, d4 * 128:(d4 + 1) * 128], mixed[:, c, :], c == 0, c == 7)
                    tt(hT[:, dc, trs(tr)], hT[:, dc, trs(tr)], pb[:, :], ALU.add)

    def run_all():
        if stage >= 1:
            scoped(phase_ffn, "ffn1_w1", "ffn1_w3", "ffn1_w2", 0, "a")
        if stage >= 2:
            scoped(phase_mixer)
        if stage >= 3:
            scoped(phase_ffn, "ffn2_w1", "ffn2_w3", "ffn2_w2", 2, "b")
        if stage >= 4:
            scoped(phase_ple)

    PLAN[0] = True
    run_all()
    PLAN[0] = False
    scoped(phase_load)
    run_all()
    scoped(phase_final)
    S.finish()
    _LAST["S"] = S
    return nc


_NC_CACHE = {}
_LAST = {}


def _prep_inputs(inputs):
    cf_np, _, cb_np, _ = _make_consts()
    f = lambda a: np.ascontiguousarray(np.asarray(a, dtype=np.float32))
    sh = {}
    for nm in ("ffn1_w1", "ffn1_w3", "ffn1_w2", "w_in", "cmp_w1_k", "cmp_w2_k", "cmp_w1_v", "cmp_w2_v",
               "w_up_nsa", "w_up_moba", "w_out", "ffn2_w1", "ffn2_w3", "ffn2_w2", "w_ple_gate", "w_ple"):
        sh[nm] = f(inputs[nm])[0]
    wi = sh["w_in"].copy()
    perm = [0, 4, 1, 5, 2, 6, 3, 7]
    wi[:, 0:512] = sh["w_in"][:, 0:512].reshape(D, 8, 64)[:, perm, :].reshape(D, 512)
    sh["w_in"] = np.ascontiguousarray(wi)
    for nm, src in (("pos_kT", "cmp_pos_k"), ("pos_vT", "cmp_pos_v")):
        pt = f(inputs[src])[0].T
        sh[nm] = np.ascontiguousarray(np.concatenate([pt, pt], axis=0))
    g = np.stack([f(inputs[n])[0] for n in ("ffn1_norm", "mix_norm", "ffn2_norm", "ple_norm")], axis=0)
    sh["gains"] = np.ascontiguousarray(g.reshape(4, 8, 128).transpose(2, 0, 1).reshape(128, 32))
    sh["gfin"] = np.ascontiguousarray(np.broadcast_to(f(inputs["final_norm"])[None, :], (128, D)))
    sh["cf32"] = cf_np
    sh["cbf16"] = cb_np
    return sh


def kernel(**inputs):
    stage = int(os.environ.get("MK_STAGE", "99"))
    ncores = int(os.environ.get("MK_CORES", "8"))
    if stage not in _NC_CACHE:
        _NC_CACHE[stage] = build_nc(stage)
    nc = _NC_CACHE[stage]
    sh = _prep_inputs(inputs)
    x = np.asarray(inputs["x"], dtype=np.float32)
    p = np.asarray(inputs["p"], dtype=np.float32)
    in_maps = []
    for b in range(ncores):
        m = dict(sh)
        m["x"] = np.ascontiguousarray(x[b])
        m["p"] = np.ascontiguousarray(p[0, b])
        in_maps.append(m)
    res = run_bass_kernel_spmd(nc, in_maps, core_ids=list(range(ncores)))
    out = np.stack([np.asarray(r["out"], dtype=np.float32) for r in res.results], axis=0)
    return out
```

```python
import os
import numpy as np
import ml_dtypes
import concourse.bass as bass
import concourse.mybir as mybir
from concourse.bass_utils import run_bass_kernel_spmd
from contextlib import ExitStack

F32 = mybir.dt.float32
BF16 = mybir.dt.bfloat16
AF = mybir.ActivationFunctionType
ALU = mybir.AluOpType

T = 2048
D = 1024
FF = 2816
MASKV = -240000.0
EPS = 1e-6
TINY = 1e-30
IN_W = 4888


def _slopes():
    s = 2.0 ** (-(np.arange(16) + 1) / 2.0)
    return s[0::2].copy(), s[1::2].copy()


def _make_consts():
    p = np.arange(128)
    f32 = {}
    b16 = {}
    f32["ident"] = np.eye(128)
    b16["ident"] = np.eye(128)
    b16["ones"] = np.ones((128, 128))
    j = np.arange(128)
    b16["triA"] = np.where(j[None, :] >= p[:, None], 0.0, MASKV)
    b16["triB"] = np.where(j[None, :] < p[:, None], 0.0, MASKV)
    tq = np.arange(T)
    cend = 16 * p + 31
    cm = np.where(tq[None, :] >= cend[:, None], 0.0, MASKV)
    cm[127, :] = MASKV
    ov = np.zeros((128, 32))
    for c in range(127):
        for jj in range(32):
            if 16 * c <= 64 * jj + 63 and 16 * c + 31 >= 64 * jj:
                ov[c, jj] = 1.0
    b16["overlap"] = ov
    b16["cmpmask"] = cm
    es = np.zeros((128, 16, 128))
    for pp in range(128):
        jj = pp % 64
        if jj >= 32:
            continue
        for kt in range(16):
            for m_ in range(128):
                if jj == 2 * kt + m_ // 64:
                    es[pp, kt, m_] = 1.0
    b16["esel"] = es.reshape(128, 2048)
    sl_n, sl_m = _slopes()
    sl16 = np.concatenate([sl_n, sl_m])
    d = np.arange(16) - 12
    f32["bw"] = (sl16[None, :, None] * (128 * d[None, None, :] + p[:, None, None] - 256)).reshape(128, 256)
    d2 = np.arange(16) - 15
    f32["bn"] = (sl16[None, :, None] * (128 * d2[None, None, :] + p[:, None, None] - 64)).reshape(128, 256)
    f32["cw"] = (sl_n[None, :, None] * (cend[:, None, None] - (512 * np.arange(4)[None, None, :] + 256))).reshape(128, 32)
    f32["cn"] = (sl_n[None, :, None] * (cend[:, None, None] - (128 * np.arange(16)[None, None, :] + 64))).reshape(128, 128)
    keep = np.zeros((128, 8, 32))
    force = np.zeros((128, 8, 32))
    for tt in range(8, 16):
        for pp in range(128):
            t = tt * 128 + pp
            cur = t // 64
            for jj in range(32):
                if 64 * jj > t:
                    force[pp, tt - 8, jj] = -1e30 * (1.0 + jj / 64.0)
                elif jj == 0:
                    force[pp, tt - 8, jj] = 3e9
                elif jj == cur:
                    force[pp, tt - 8, jj] = 2e9
                elif jj == cur - 1:
                    force[pp, tt - 8, jj] = 1e9
                else:
                    keep[pp, tt - 8, jj] = 1.0
    padneg = np.zeros((128, 8, 8))
    ownhot = np.zeros((128, 8, 8))
    for tt in range(8, 16):
        cur = tt // 2
        for n in range(8):
            if n >= cur:
                padneg[:, tt - 8, n] = -1e30
            if n == cur:
                ownhot[:, tt - 8, n] = 1.0
    f32["eps"] = np.full((128, 1), EPS)
    rt = []
    for tr in (2, 3):
        a = (tr - 2) * 4
        rt += [keep[:, a:a + 4].reshape(128, 128), force[:, a:a + 4].reshape(128, 128),
               padneg[:, a:a + 4].reshape(128, 32), ownhot[:, a:a + 4].reshape(128, 32)]
    f32["rt"] = np.concatenate(rt, axis=1)
    offs_f = {}
    cols = []
    o = 0
    for k, v in f32.items():
        offs_f[k] = o
        o += v.shape[1]
        cols.append(v.astype(np.float32))
    cf = np.ascontiguousarray(np.concatenate(cols, axis=1))
    offs_b = {}
    cols = []
    o = 0
    for k, v in b16.items():
        offs_b[k] = o
        o += v.shape[1]
        cols.append(v.astype(np.float32))
    cbv = np.ascontiguousarray(np.concatenate(cols, axis=1).astype(ml_dtypes.bfloat16))
    return cf, offs_f, cbv, offs_b


EMBED_WAIT = set(os.environ.get("MK_EMBED", "pe").split(",")) - {""}


def _box(ap):
    t = ap.tensor
    if t.name.startswith("ps") and len(t.name) == 3:
        return (t.name, 0, 128, 0, 512)
    row = 1
    for s in tuple(t.shape)[1:]:
        row *= int(s)
    off = int(ap.offset)
    p0 = off // row
    f0 = off % row
    apl = ap.ap
    npart = apl[0][1]
    ext = 1
    for st, cnt in apl[1:]:
        ext += (cnt - 1) * abs(st)
    return (t.name, p0, p0 + npart, f0, f0 + ext)


class Sched:
    def __init__(self, nc, ndma=24):
        self.nc = nc
        self.engs = {"pe": nc.tensor, "act": nc.scalar, "dve": nc.vector, "pool": nc.gpsimd, "sp": nc.sync}
        self.semh = {}
        for e in self.engs:
            self.semh[e] = nc.alloc_semaphore("sem_" + e)
        self.cnt = {e: 0 for e in self.engs}
        self.ndma = ndma
        for i in range(ndma):
            self.semh[("d", i)] = nc.alloc_semaphore("dsem%d" % i)
        self.dcnt = [0] * ndma
        self.drr = 0
        self.drr_pool = 0
        self.seen = {e: {} for e in self.engs}
        self.acc = {}
        self.out_tickets = []
        self.nwaits = 0
        self.marks = []

    def mark(self, name):
        self.marks.append((name, self.cnt["pe"]))

    def _wait(self, e, sk, val):
        if e == "pe" and sk == "pe":
            return
        if self.seen[e].get(sk, 0) >= val:
            return
        self.engs[e].wait_ge(self.semh[sk], val)
        self.seen[e][sk] = val
        self.nwaits += 1

    def _collect(self, reads, writes):
        waits = {}
        for ap in reads:
            b = _box(ap)
            for ent in self.acc.get(b[0], ()):
                if ent[0] == "w" and ent[3] < b[2] and b[1] < ent[4] and ent[5] < b[4] and b[3] < ent[6]:
                    if waits.get(ent[1], 0) < ent[2]:
                        waits[ent[1]] = ent[2]
        for ap in writes:
            b = _box(ap)
            for ent in self.acc.get(b[0], ()):
                if ent[3] < b[2] and b[1] < ent[4] and ent[5] < b[4] and b[3] < ent[6]:
                    if waits.get(ent[1], 0) < ent[2]:
                        waits[ent[1]] = ent[2]
        return waits

    def _record(self, sk, val, reads, writes):
        for ap in writes:
            b = _box(ap)
            lst = self.acc.setdefault(b[0], [])
            lst[:] = [e for e in lst if not (b[1] <= e[3] and e[4] <= b[2] and b[3] <= e[5] and e[6] <= b[4])]
            lst.append(("w", sk, val, b[1], b[2], b[3], b[4]))
        for ap in reads:
            b = _box(ap)
            lst = self.acc.setdefault(b[0], [])
            lst[:] = [e for e in lst if not (e[0] == "r" and e[1] == sk and b[1] <= e[3] and e[4] <= b[2]
                                             and b[3] <= e[5] and e[6] <= b[4])]
            lst.append(("r", sk, val, b[1], b[2], b[3], b[4]))

    def op(self, e, fn, reads, writes, inc=True):
        assert inc or e == "pe"
        waits = self._collect(reads, writes)
        emb = None
        if EMBED_WAIT and e in EMBED_WAIT:
            need = [(sk, val) for sk, val in waits.items()
                    if not (e == "pe" and sk == "pe") and self.seen[e].get(sk, 0) < val]
            if need:
                emb = need[-1]
                waits = dict(need[:-1])
            else:
                waits = {}
        for sk, val in waits.items():
            self._wait(e, sk, val)
        ins = fn()
        if emb is not None:
            ins._wait_ge(self.semh[emb[0]], emb[1])
            self.seen[e][emb[0]] = emb[1]
        if inc:
            self.cnt[e] += 1
            ins.then_inc(self.semh[e], 1)
            val = self.cnt[e]
        else:
            val = self.cnt[e] + 1
        self._record(e, val, reads, writes)
        return ins

    def dma(self, q, out, in_, reads=(), writes=(), is_out=False, **kw):
        waits = self._collect(reads, writes)
        for sk, val in waits.items():
            self._wait(q, sk, val)
        half = self.ndma // 2
        if q == "pool":
            i = half + (self.drr_pool % half)
            self.drr_pool += 1
        else:
            i = self.drr % half
            self.drr += 1
        sk = ("d", i)
        if self.dcnt[i] > 0:
            self._wait(q, sk, self.dcnt[i])
        ins = self.engs[q].dma_start(out=out, in_=in_, **kw)
        self.dcnt[i] += 16
        ins.then_inc(self.semh[sk], 16)
        self._record(sk, self.dcnt[i], reads, writes)
        if is_out:
            self.out_tickets.append((sk, self.dcnt[i]))

    def barrier(self):
        for e in ("pe", "act", "dve", "pool", "sp"):
            for f in ("pe", "act", "dve", "pool", "sp"):
                if f != e and self.cnt[f] > 0:
                    self._wait(e, f, self.cnt[f])
            for i in range(self.ndma):
                if self.dcnt[i] > 0:
                    self._wait(e, ("d", i), self.dcnt[i])
        self.acc = {}

    def finish(self):
        for sk, val in self.out_tickets:
            self._wait("sp", sk, val)


def build_nc(stage=99):
    nc = bass.Bass("TRN2", target_bir_lowering=False)
    cf_np, OF, cb_np, OB = _make_consts()
    NF = cf_np.shape[1]
    NB = cb_np.shape[1]

    def din(name, shape, dt=F32):
        return nc.dram_tensor(name, list(shape), dt, kind="ExternalInput").ap()

    x_d = din("x", [T, D])
    p_d = din("p", [T, 256])
    wd = {}
    for nm, shp in [("ffn1_w1", [D, FF]), ("ffn1_w3", [D, FF]), ("ffn1_w2", [FF, D]), ("w_in", [D, IN_W]),
                    ("cmp_w1_k", [2048, 128]), ("cmp_w2_k", [128, 64]), ("cmp_w1_v", [2048, 128]),
                    ("cmp_w2_v", [128, 64]), ("pos_kT", [128, 32]), ("pos_vT", [128, 32]),
                    ("w_up_nsa", [512, D]), ("w_up_moba", [512, D]), ("w_out", [D, D]),
                    ("ffn2_w1", [D, FF]), ("ffn2_w3", [D, FF]), ("ffn2_w2", [FF, D]),
                    ("w_ple_gate", [D, D]), ("w_ple", [256, D]), ("gains", [128, 32]), ("gfin", [128, D])]:
        wd[nm] = din(nm, shp)
    cf_d = din("cf32", [128, NF])
    cb_d = din("cbf16", [128, NB], BF16)
    out_d = nc.dram_tensor("out", [T, D], F32, kind="ExternalOutput").ap()

    S = Sched(nc)
    stack = [None]

    def TS(name, shape, dt):
        if stack[0] is None:
            return nc.alloc_sbuf_tensor(name, shape, dt)
        return stack[0].enter_context(nc.sbuf_tensor(name, shape, dt))

    def scoped(fn, *a):
        if PLAN[0]:
            fn(*a)
            return
        S.barrier()
        with ExitStack() as st:
            stack[0] = st
            fn(*a)
            S.barrier()
        stack[0] = None

    hT = TS("hT", [128, 8, T], F32)
    NFS = OF["rt"]
    NBS = OB["cmpmask"]
    cf = TS("cf", [128, NFS], F32)
    cb = TS("cb", [128, NBS], BF16)
    gains = TS("gains_sb", [128, 32], F32)
    slab = [TS("slab%d" % i, [128, 4096], BF16) for i in range(3)]
    ps = [nc.alloc_psum_tensor("ps%d" % i, [128, 512], F32) for i in range(8)]

    identf = cf[:, OF["ident"]:OF["ident"] + 128]
    identb = cb[:, OB["ident"]:OB["ident"] + 128]
    onesb = cb[:, OB["ones"]:OB["ones"] + 128]
    epsc = cf[:, OF["eps"]:OF["eps"] + 1]

    S.dma("sp", cf[:, :], cf_d[:, 0:NFS], writes=[cf[:, :]])
    S.dma("sp", cb[:, :], cb_d[:, 0:NBS], writes=[cb[:, :]])
    S.dma("sp", gains[:, :], wd["gains"][:, :], writes=[gains[:, :]])

    def mm(out, lhsT, rhs, start, stop, inc=None):
        inc = True
        return S.op("pe", lambda: nc.tensor.matmul(out, lhsT, rhs, start=start, stop=stop),
                    [lhsT, rhs], [out], inc=inc)

    def pe_drain():
        if S.cnt["pe"] > 0:
            nc.tensor.wait_ge(S.semh["pe"], S.cnt["pe"])

    def tp(out, in_, ident):
        return S.op("pe", lambda: nc.tensor.transpose(out, in_, ident), [in_, ident], [out])

    def act(out, in_, func, bias=None, scale=None, accum_out=None):
        kw = {}
        rd = [in_]
        wr = [out]
        if bias is not None:
            kw["bias"] = bias
            if not isinstance(bias, (int, float)):
                rd.append(bias)
        if scale is not None:
            kw["scale"] = scale
            if not isinstance(scale, (int, float)):
                rd.append(scale)
        if accum_out is not None:
            kw["accum_out"] = accum_out
            wr.append(accum_out)
        return S.op("act", lambda: nc.scalar.activation(out=out, in_=in_, func=func, **kw), rd, wr)

    def tt(out, in0, in1, op, e="dve"):
        eng = nc.vector if e == "dve" else nc.gpsimd
        return S.op(e, lambda: eng.tensor_tensor(out=out, in0=in0, in1=in1, op=op), [in0, in1], [out])

    def ts(out, in0, s1, s2, op0, op1=None, e="dve"):
        eng = nc.vector if e == "dve" else nc.gpsimd
        rd = [in0]
        if not isinstance(s1, (int, float)):
            rd.append(s1)
        if s2 is not None and not isinstance(s2, (int, float)):
            rd.append(s2)
        if op1 is None:
            return S.op(e, lambda: eng.tensor_scalar(out=out, in0=in0, scalar1=s1, scalar2=None, op0=op0), rd, [out])
        return S.op(e, lambda: eng.tensor_scalar(out=out, in0=in0, scalar1=s1, scalar2=s2, op0=op0, op1=op1),
                    rd, [out])

    def stt(out, in0, scalar, in1, op0, op1):
        rd = [in0, in1]
        if not isinstance(scalar, (int, float)):
            rd.append(scalar)
        return S.op("dve", lambda: nc.vector.scalar_tensor_tensor(out=out, in0=in0, scalar=scalar, in1=in1,
                                                                   op0=op0, op1=op1), rd, [out])

    def cp(out, in_, e="dve"):
        if e == "act":
            return S.op("act", lambda: nc.scalar.copy(out=out, in_=in_), [in_], [out])
        eng = nc.vector if e == "dve" else nc.gpsimd
        return S.op(e, lambda: eng.tensor_copy(out=out, in_=in_), [in_], [out])

    def memset(ap, v, e="dve"):
        eng = nc.vector if e == "dve" else nc.gpsimd
        return S.op(e, lambda: eng.memset(ap, v), [], [ap])

    class RR:
        def __init__(self, items):
            self.items = items
            self.i = 0

        def next(self):
            it = self.items[self.i % len(self.items)]
            self.i += 1
            return it

    class WStream:
        def __init__(self):
            self.jobs = []
            self.issued = 0
            self.k = 0

        def add(self, parts):
            self.jobs.append(parts)

        def _issue(self, k):
            buf = slab[k % 3]
            for (off, shape, src) in self.jobs[k]:
                n = 1
                for s_ in shape:
                    n *= s_
                npart = int(src.shape[0])
                dst = buf[0:npart, off:off + n]
                if len(shape) == 2:
                    dst = dst.rearrange("p (a b) -> p a b", a=shape[0])
                S.dma("pool", dst, src, writes=[buf[0:npart, off:off + n]])

        def get(self):
            k = self.k
            self.k += 1
            while self.issued < min(k + 2, len(self.jobs)):
                self._issue(self.issued)
                self.issued += 1
            return slab[k % 3]

    WS = WStream()
    PLAN = [True]

    def W(parts):
        if PLAN[0]:
            WS.add(parts)
            return None
        return WS.get()

    def trs(tr):
        return slice(tr * 512, (tr + 1) * 512)

    sqb = [TS("sqb%d" % i, [128, 512], BF16) for i in range(2)]
    rstd = TS("rstd", [128, 512], F32)
    sqrr = RR(sqb)
    psM = RR([ps[6], ps[7]])

    def rms_range(tr, gidx, xn3):
        pb = psM.next()
        for c in range(8):
            sq = sqrr.next()
            act(sq[:, :], hT[:, c, trs(tr)], AF.Square)
            mm(pb[:, :], onesb, sq[:, :], c == 0, c == 7)
        act(rstd[:, :], pb[:, :], AF.Ln, bias=epsc, scale=1.0 / D)
        act(rstd[:, :], rstd[:, :], AF.Exp, scale=-0.5)
        for c in range(8):
            stt(xn3[:, c, :], hT[:, c, trs(tr)], gains[:, gidx * 8 + c:gidx * 8 + c + 1], rstd[:, :],
                ALU.mult, ALU.mult)

    def phase_load():
        xtok = [TS("xtok%d" % i, [128, D], F32) for i in range(2)]
        for t_ in range(16):
            xb = xtok[t_ % 2]
            S.dma("sp", xb[:, :], x_d[t_ * 128:(t_ + 1) * 128, :], writes=[xb[:, :]])
            for half in range(2):
                pb = psM.next()
                for c4 in range(4):
                    c = half * 4 + c4
                    tp(pb[:, c4 * 128:(c4 + 1) * 128], xb[:, c * 128:(c + 1) * 128], identf)
                cp(hT[:, half * 4:(half + 1) * 4, t_ * 128:(t_ + 1) * 128],
                   pb[:, :].rearrange("p (c t) -> p c t", c=4), e="act" if half else "dve")

    def phase_ffn(w1n, w3n, w2n, gidx, tag):
        w1 = wd[w1n].rearrange("(c p) f -> p c f", p=128)
        w3 = wd[w3n].rearrange("(c p) f -> p c f", p=128)
        w2 = wd[w2n]
        if not PLAN[0]:
            xnT = TS("xnT" + tag, [128, 8, T], BF16)
            h1 = [TS("h1%s%d" % (tag, i), [128, 2, 512], BF16) for i in range(2)]
            sa = [TS("sa%s%d" % (tag, i), [128, 512], F32) for i in range(2)]
            for tr in range(4):
                rms_range(tr, gidx, xnT[:, :, trs(tr)])
            psAB = RR([ps[0], ps[1], ps[2], ps[3]])
            psY = RR([ps[4], ps[5], ps[6], ps[7]])
            sar = RR(sa)
            h1r = RR(h1)
            ytmp = [TS("ytmp%s%d" % (tag, i), [128, 512], F32) for i in range(2)]
            ytr = RR(ytmp)
        Aj = {}
        Bj = {}

        def getA(s_):
            if s_ not in Aj:
                Aj[s_] = W([(0, [8, 256], w1[:, :, s_ * 256:(s_ + 1) * 256]),
                            (2048, [8, 256], w3[:, :, s_ * 256:(s_ + 1) * 256])])
            return Aj[s_]

        def getB(s_):
            if s_ not in Bj:
                Bj[s_] = W([(0, [2, 1024], w2[s_ * 256:(s_ + 1) * 256, :].rearrange("(j p) d -> p j d", p=128))])
            return Bj[s_]

        def ab(u):
            s_, tr = u
            A = getA(s_)
            if PLAN[0]:
                return None
            w1s = A[:, 0:2048].rearrange("p (c f) -> p c f", c=8)
            w3s = A[:, 2048:4096].rearrange("p (c f) -> p c f", c=8)
            hb = h1r.next()
            for j in range(2):
                pa = psAB.next()
                pb = psAB.next()
                for c in range(8):
                    mm(pa[:, :], w1s[:, c, j * 128:(j + 1) * 128], xnT[:, c, trs(tr)], c == 0, c == 7)
                for c in range(8):
                    mm(pb[:, :], w3s[:, c, j * 128:(j + 1) * 128], xnT[:, c, trs(tr)], c == 0, c == 7)
                sab = sar.next()
                act(sab[:, :], pa[:, :], AF.Silu)
                tt(hb[:, j, :], sab[:, :], pb[:, :], ALU.mult)
            return hb

        def yy(u, hb):
            s_, tr = u
            B = getB(s_)
            if PLAN[0]:
                return
            w2s = B[:, 0:2048].rearrange("p (j d) -> p j d", j=2)
            for dc in range(8):
                py = psY.next()
                for j in range(2):
                    mm(py[:, :], w2s[:, j, dc * 128:(dc + 1) * 128], hb[:, j, :], j == 0, j == 1)
                stt(hT[:, dc, trs(tr)], py[:, :], 0.5, hT[:, dc, trs(tr)], ALU.mult, ALU.add)

        units = [(s_, tr) for s_ in range(11) for tr in range(4)]
        prev = None
        for u in units:
            hb = ab(u)
            if prev is not None:
                yy(prev[0], prev[1])
            prev = (u, hb)
        yy(prev[0], prev[1])

    def phase_ple():
        wg = wd["w_ple_gate"].rearrange("(c p) f -> p c f", p=128)
        wp = wd["w_ple"].rearrange("(c p) f -> p c f", p=128)
        if not PLAN[0]:
            xnT = TS("xnTple", [128, 8, T], BF16)
            pT = TS("pT", [128, 2, T], BF16)
            ptok = [TS("ptok%d" % i, [128, 256], F32) for i in range(2)]
            sg = [TS("sgple%d" % i, [128, 512], F32) for i in range(2)]
            for t_ in range(16):
                pbuf = ptok[t_ % 2]
                S.dma("sp", pbuf[:, :], p_d[t_ * 128:(t_ + 1) * 128, :], writes=[pbuf[:, :]])
                pb = psM.next()
                for c in range(2):
                    tp(pb[:, c * 128:(c + 1) * 128], pbuf[:, c * 128:(c + 1) * 128], identf)
                cp(pT[:, :, t_ * 128:(t_ + 1) * 128], pb[:, 0:256].rearrange("p (c t) -> p c t", c=2))
            for tr in range(4):
                rms_range(tr, 3, xnT[:, :, trs(tr)])
            psG = RR([ps[0], ps[1], ps[2], ps[3]])
            sgr = RR(sg)
        for dh in range(2):
            A = W([(0, [8, 512], wg[:, :, dh * 512:(dh + 1) * 512])])
            B = W([(0, [2, 512], wp[:, :, dh * 512:(dh + 1) * 512])])
            if PLAN[0]:
                continue
            wgs = A[:, 0:4096].rearrange("p (c f) -> p c f", c=8)
            wps = B[:, 0:1024].rearrange("p (c f) -> p c f", c=2)
            for tr in range(4):
                for d4 in range(4):
                    dc = dh * 4 + d4
                    pg = psG.next()
                    pp = psG.next()
                    for c in range(8):
                        mm(pg[:, :], wgs[:, c, d4 * 128:(d4 + 1) * 128], xnT[:, c, trs(tr)], c == 0, c == 7)
                    for c in range(2):
                        mm(pp[:, :], wps[:, c, d4 * 128:(d4 + 1) * 128], pT[:, c, trs(tr)], c == 0, c == 1)
                    sgb = sgr.next()
                    act(sgb[:, :], pg[:, :], AF.Sigmoid)
                    tt(sgb[:, :], sgb[:, :], pp[:, :], ALU.mult)
                    tt(hT[:, dc, trs(tr)], hT[:, dc, trs(tr)], sgb[:, :], ALU.add)

    def phase_final():
        gfin = TS("gfin_sb", [128, D], F32)
        S.dma("sp", gfin[:, :], wd["gfin"][:, :], writes=[gfin[:, :]])
        otok = [TS("otok%d" % i, [128, D], F32) for i in range(2)]
        junk = TS("junk", [128, 512], F32)
        ssq = TS("ssq", [128, 4], F32)
        psF = RR([ps[0], ps[1], ps[2], ps[3], ps[4], ps[5]])
        for t_ in range(16):
            ob = otok[t_ % 2]
            pbs = [psF.next(), psF.next()]
            for half in range(2):
                for c4 in range(4):
                    c = half * 4 + c4
                    tp(pbs[half][:, c4 * 128:(c4 + 1) * 128], hT[:, c, t_ * 128:(t_ + 1) * 128], identf)
                act(junk[:, :], pbs[half][:, :], AF.Square, accum_out=ssq[:, half:half + 1])
            tt(ssq[:, 2:3], ssq[:, 0:1], ssq[:, 1:2], ALU.add)
            act(ssq[:, 3:4], ssq[:, 2:3], AF.Ln, bias=epsc, scale=1.0 / D)
            act(ssq[:, 3:4], ssq[:, 3:4], AF.Exp, scale=-0.5)
            for half in range(2):
                stt(ob[:, half * 512:(half + 1) * 512], pbs[half][:, :], ssq[:, 3:4],
                    gfin[:, half * 512:(half + 1) * 512], ALU.mult, ALU.mult)
            S.dma("sp", out_d[t_ * 128:(t_ + 1) * 128, :], ob[:, :], reads=[ob[:, :]], is_out=True)

    def phase_mixer():
        win = wd["w_in"].rearrange("(c p) f -> p c f", p=128)
        wupn = wd["w_up_nsa"].rearrange("(c p) f -> p c f", p=128)
        wupm = wd["w_up_moba"].rearrange("(c p) f -> p c f", p=128)
        wout = wd["w_out"].rearrange("(c p) f -> p c f", p=128)
        sl_n, sl_m = _slopes()
        if not PLAN[0]:
            kslcT = TS("kslcT", [128, T], BF16)
            kwinT = TS("kwinT", [128, T], BF16)
            kcmpT = TS("kcmpT", [128, 528], BF16)
            vcmpT = TS("vcmpT", [128, 528], BF16)
            kmT = TS("kmT", [128, 4, T], BF16)
            vslc = TS("vslc", [128, 16, 2, 65], BF16)
            vwin = TS("vwin", [128, 16, 2, 65], BF16)
            vm = TS("vm", [128, 16, 8, 65], BF16)
            gsig = TS("gsig", [128, 16, 24], F32)
            kcT = TS("kcT", [128, 128], BF16)
            vca = TS("vca", [128, 2, 97], BF16)
            kmeanT = TS("kmeanT", [128, 4, 16], BF16)
            kmsum = TS("kmsum", [128, 4, 2], F32)
            w2k = TS("w2k", [128, 128], BF16)
            w2v = TS("w2v", [128, 64], BF16)
            posk = TS("posk", [128, 32], BF16)
            posv = TS("posv", [128, 32], BF16)
            cbias = TS("cbias", [128, 2], F32)
            u_tr = TS("u_tr", [128, 8, 512], BF16)
            qmix = TS("qmix", [128, 8, 512], BF16)

            class _Sub:
                def __init__(self, base, off):
                    self.base, self.off = base, off

                def __getitem__(self, key):
                    a, b, c = key
                    if isinstance(b, int):
                        b = b + self.off
                    else:
                        b = slice((b.start or 0) + self.off, (b.stop if b.stop is not None else 4) + self.off)
                    return self.base[a, b, c]
            qn = _Sub(qmix, 0)
            qm = _Sub(qmix, 4)
            ptl = [TS("ptl%d" % i, [128, 512], BF16) for i in range(3)]
            otk = [TS("otk%d" % i, [128, 4, 4, 64], F32) for i in range(2)]
            onT = TS("onT", [128, 4, 512], BF16)
            omT = TS("omT", [128, 4, 512], BF16)
            mixed = qmix
            sgA = [TS("sgA%d" % i, [128, 512], F32) for i in range(1)]
            sgB = [TS("sgB%d" % i, [128, 512], F32) for i in range(1)]
            mbTn = TS("mbTn", [128, 2, 512], BF16)
            qz = [TS("qz%d" % i, [128, 512], BF16) for i in range(4)]
            qzc = [0, 0]
            mbTm = TS("mbTm", [128, 512], BF16)
            mbtokN = TS("mbtokN", [128, 128], F32)
            mbtok = TS("mbtok", [128, 128], F32)
            impa = TS("impa", [128, 4, 2, 32], F32)
            smal = TS("smal", [128, 64], F32)
            m8 = TS("m8", [128, 16], F32)
            tk32 = TS("tk32", [128, 64], F32)
            cmsk = TS("cmsk", [128, 512], BF16)
            rtab = TS("rtab", [128, 320], F32)
            esel = TS("esel", [128, 16, 128], BF16)
            S.dma("sp", esel[:, :, :], cb_d[:, OB["esel"]:OB["esel"] + 2048].rearrange("p (k m) -> p k m", k=16),
                  writes=[esel[:, :, :]])
            hid = TS("hid", [128, 4, 32], F32)
            hidb = TS("hidb", [128, 2, 32], BF16)
            hidv = TS("hidv", [128, 2, 128], BF16)
            gtmp = TS("gtmp", [64, 528], BF16)
            gsf = TS("gsf", [128, 8, 16], F32)

            memset(kcT[:, :], 0.0)
            for i in range(4):
                memset(qz[i][:, :], 0.0)
            memset(mbtokN[:, :], 0.0)
            memset(mbtok[:, :], 0.0)
            memset(gsf[:, :, :], -1.0e30)
            memset(kcmpT[:, 0:16], 0.0)
            memset(vcmpT[:, 0:16], 0.0)
            memset(hidv[:, :, :], 0.0)
            memset(vca[:, :, :], 0.0)
            memset(kmeanT[:, :, :], 0.0)
            memset(vslc[:, :, :, 64:65], 1.0)
            memset(vwin[:, :, :, 64:65], 1.0)
            memset(vm[:, :, :, 64:65], 1.0)
            memset(vca[:, :, 64:65], 1.0)
            for g in range(2):
                cp(vca[:, g, 65:97], cb[:, OB["overlap"]:OB["overlap"] + 32])
            for (w2t, w2n_, pt, pn) in ((w2k, "cmp_w2_k", posk, "pos_kT"), (w2v, "cmp_w2_v", posv, "pos_vT")):
                S.dma("pool", pt[:, :], wd[pn][:, :], writes=[pt[:, :]])
                if w2t is w2k:
                    S.dma("pool", w2t[:, 0:64], wd[w2n_][:, :], writes=[w2t[:, 0:64]])
                    S.dma("pool", w2t[:, 64:128], wd[w2n_][:, :], writes=[w2t[:, 64:128]])
                else:
                    S.dma("pool", w2t[:, :], wd[w2n_][:, :], writes=[w2t[:, :]])

            psS = RR([ps[0], ps[1], ps[2], ps[3]])
            psO = RR([ps[4], ps[5]])
            ptr = RR(ptl)
            otr = RR(otk)
            sgAr = RR(sgA)
            sgBr = RR(sgB)

        def proj_fm(ws, c0, dst, tr, base=None, ev="act"):
            pb = psM.next()
            for c in range(8):
                if base is None:
                    l_ = ws[:, c, c0:c0 + 128]
                else:
                    l_ = base(c)
                mm(pb[:, :], l_, u_tr[:, c, :], c == 0, c == 7)
            cp(dst, pb[:, :], e=ev)

        NTRS = int(os.environ.get("MK_NTR", "4"))
        SUB = int(os.environ.get("MK_SUB", "9"))
        for tr in range(NTRS):
            S0 = W([(0, [8, 512], win[:, :, 0:512])])
            if not PLAN[0]:
                S.mark("tr%d proj" % tr)
                rms_range(tr, 1, u_tr)
                w0 = S0[:, 0:4096].rearrange("p (c f) -> p c f", c=8)
                for i in range(4):
                    proj_fm(w0, i * 128, qn[:, i, :], tr, ev="act" if i % 2 else "dve")
            S1 = W([(0, [8, 512], win[:, :, 512:1024])])
            if not PLAN[0]:
                w1_ = S1[:, 0:4096].rearrange("p (c f) -> p c f", c=8)
                proj_fm(w1_, 0, kcmpT[:, 16:528], tr, ev="act")
                proj_fm(w1_, 128, vcmpT[:, 16:528], tr, ev="dve")
                proj_fm(w1_, 256, kslcT[:, trs(tr)], tr, ev="act")
                for j in range(4):
                    t_ = tr * 4 + j
                    pb = psM.next()
                    for c in range(8):
                        mm(pb[:, 0:128], u_tr[:, c, j * 128:(j + 1) * 128], w1_[:, c, 384:512], c == 0, c == 7)
                    cp(vslc[:, t_, :, 0:64], pb[:, 0:128].rearrange("p (g d) -> p g d", g=2))
            S2 = W([(0, [8, 280], win[:, :, 1024:1304])])
            if not PLAN[0]:
                w2_ = S2[:, 0:2240].rearrange("p (c f) -> p c f", c=8)
                proj_fm(w2_, 0, kwinT[:, trs(tr)], tr, ev="act")
                for j in range(4):
                    t_ = tr * 4 + j
                    pb = psM.next()
                    for c in range(8):
                        mm(pb[:, 0:152], u_tr[:, c, j * 128:(j + 1) * 128], w2_[:, c, 128:280], c == 0, c == 7)
                    cp(vwin[:, t_, :, 0:64], pb[:, 0:128].rearrange("p (g d) -> p g d", g=2))
                    act(gsig[:, t_, :], pb[:, 128:152], AF.Sigmoid)
            S3 = W([(0, [8, 512], win[:, :, 1304:1816])])
            if not PLAN[0]:
                w3_ = S3[:, 0:4096].rearrange("p (c f) -> p c f", c=8)
                for i in range(4):
                    proj_fm(w3_, i * 128, qm[:, i, :], tr, ev="act" if i % 2 else "dve")
            S4 = W([(0, [8, 512], win[:, :, 1816:2328])])
            if not PLAN[0]:
                w4_ = S4[:, 0:4096].rearrange("p (c f) -> p c f", c=8)
                for i in range(4):
                    proj_fm(w4_, i * 128, kmT[:, i, trs(tr)], tr, ev="act" if i % 2 else "dve")
                for i in range(4):
                    S.op("dve", lambda: nc.vector.tensor_reduce(
                        out=kmsum[:, i, :], in_=kmT[:, i, trs(tr)].rearrange("p (n k) -> p n k", n=2),
                        axis=mybir.AxisListType.X, op=ALU.add),
                        [kmT[:, i, trs(tr)]], [kmsum[:, i, :]])
                ts(kmeanT[0:64, :, 2 * tr:2 * tr + 2], kmsum[0:64, :, :], 1.0 / 256.0, None, ALU.mult)
                ts(kmeanT[64:128, :, 8 + 2 * tr:8 + 2 * tr + 2], kmsum[64:128, :, :], 1.0 / 256.0, None, ALU.mult)
            S5 = W([(0, [8, 512], win[:, :, 2328:2840])])
            if not PLAN[0]:
                w5_ = S5[:, 0:4096].rearrange("p (c f) -> p c f", c=8)
                for j in range(4):
                    t_ = tr * 4 + j
                    pb = psM.next()
                    for c in range(8):
                        mm(pb[:, :], u_tr[:, c, j * 128:(j + 1) * 128], w5_[:, c, :], c == 0, c == 7)
                    cp(vm[:, t_, :, 0:64], pb[:, :].rearrange("p (h d) -> p h d", h=8), e="act" if j % 2 else "dve")

            if SUB < 1:
                continue
            if not PLAN[0]:
                S.mark("tr%d compress" % tr)
            c_lo = 0 if tr == 0 else 32 * tr - 1
            c_hi = 32 * tr + 30
            nb = c_hi - c_lo + 1
            if not PLAN[0]:
                pe_drain()
            for which in range(2):
                wsrc = wd["cmp_w1_k" if which == 0 else "cmp_w1_v"].rearrange("(l d) h -> d l h", d=64)
                CW = W([(q4 * 1024, [8, 128], wsrc[:, q4 * 8:(q4 + 1) * 8, :]) for q4 in range(4)])
                if PLAN[0]:
                    continue
                srcT = kcmpT if which == 0 else vcmpT
                w1t = CW[0:64, 0:4096].rearrange("p (l h) -> p l h", l=32)
                if tr == 0:
                    pt = posk if which == 0 else posv
                    pb = psM.next()
                    for l in range(32):
                        mm(pb[:, 0:1], w1t[:, l, :], pt[0:64, l:l + 1], l == 0, l == 31)
                    cp(cbias[:, which:which + 1], pb[:, 0:1])
                for g in range(2):
                    if g == 1:
                        cp(gtmp[0:64, 0:528], srcT[64:128, 0:528])
                        src_t = gtmp
                    else:
                        src_t = srcT
                    pbA = ps[which * 2 + g]
                    for l in range(32):
                        col0 = 16 * c_lo + l - 512 * tr + 16
                        rhs_ = bass.AP(src_t, col0, [[528, 64], [16, nb]])
                        mm(pbA[:, 0:nb], w1t[:, l, :], rhs_, l == 0, l == 31)
                cp(srcT[:, 0:16], srcT[:, 512:528])
            if not PLAN[0]:
                pe_drain()
                for which in range(2):
                    for g in range(2):
                        pbA = ps[which * 2 + g]
                        xx = hid[:, 0, 0:nb]
                        x2 = hid[:, 1, 0:nb]
                        ts(xx, pbA[:, 0:nb], cbias[:, which:which + 1], None, ALU.add)
                        tt(x2, xx, xx, ALU.mult)
                        ts(x2, x2, 0.044715, 1.0, ALU.mult, ALU.add)
                        tt(x2, x2, xx, ALU.mult)
                        act(hid[:, 2, 0:nb], x2, AF.Sigmoid, scale=1.5957691216057308)
                        if which == 0:
                            tt(hidb[:, g, 0:nb], xx, hid[:, 2, 0:nb], ALU.mult)
                        else:
                            tt(hidv[:, g, c_lo:c_lo + nb], xx, hid[:, 2, 0:nb], ALU.mult)
            if not PLAN[0]:
                S.dma("sp", cmsk[:, :], cb_d[:, OB["cmpmask"] + tr * 512:OB["cmpmask"] + (tr + 1) * 512],
                      writes=[cmsk[:, :]])
                if tr >= 2:
                    S.dma("sp", rtab[:, :], cf_d[:, OF["rt"] + (tr - 2) * 320:OF["rt"] + (tr - 1) * 320],
                          writes=[rtab[:, :]])

            if SUB < 2:
                continue
            def attend(kT_ap_fn, q_ap, vfn, tiles, bias_fn, narrow, ops, mask_fn=None, ncols=65):
                npv = [0]
                npv_total = sum((c1 - c0) // 128 for (kt, c0, c1, extra) in tiles)

                def emit_scores(ti):
                    (kt, c0, c1, extra) = tiles[ti]
                    sp_ = psS.next()
                    n_extra = len(extra) + (1 if mask_fn is not None else 0)
                    mm(sp_[:, c0:c1], kT_ap_fn(kt), q_ap[:, c0:c1], True, n_extra == 0)
                    k_ = 0
                    if mask_fn is not None:
                        k_ += 1
                        ml, mr = mask_fn(kt)
                        mm(sp_[:, c0:c1], ml, mr[:, c0:c1], False, k_ == n_extra)
                    for (e0, en, el, er) in extra:
                        k_ += 1
                        mm(sp_[:, e0:e0 + en], el, er, False, k_ == n_extra)
                    return sp_

                DEPTH = 2
                pend = [emit_scores(i) for i in range(min(DEPTH, len(tiles)))]
                for ti in range(len(tiles)):
                    (kt, c0, c1, extra) = tiles[ti]
                    sp_ = pend.pop(0)
                    if ti + DEPTH < len(tiles):
                        pend.append(emit_scores(ti + DEPTH))
                    pt = ptr.next()
                    if narrow:
                        for j in range(c0 // 128, c1 // 128):
                            act(pt[:, j * 128:(j + 1) * 128], sp_[:, j * 128:(j + 1) * 128], AF.Exp,
                                bias=bias_fn(kt, j), scale=0.125)
                    else:
                        act(pt[:, c0:c1], sp_[:, c0:c1], AF.Exp, bias=bias_fn(kt, None), scale=0.125)
                    for j in range(c0 // 128, c1 // 128):
                        npv[0] += 1
                        mm(ops[:, j * 128:j * 128 + ncols], pt[:, j * 128:(j + 1) * 128], vfn(kt),
                           npv[0] == 1, npv[0] == npv_total)

            triA = cb[:, OB["triA"]:OB["triA"] + 128]
            triB = cb[:, OB["triB"]:OB["triB"] + 128]

            def causal_tiles(tr_):
                tl = []
                for kt in range(0, 4 * tr_ + 4):
                    i = kt - 4 * tr_
                    if i < 0:
                        tl.append((kt, 0, 512, []))
                    else:
                        tl.append((kt, 128 * i, 512, [(128 * i, 128, identb, triA)]))
                return tl

            def win_tiles(tr_):
                tl = []
                for kt in range(4 * tr_ - 4, 4 * tr_ + 4):
                    if kt < 0:
                        continue
                    i = kt - 4 * tr_
                    if i < 0:
                        i2 = i + 4
                        tl.append((kt, 0, 128 * (i2 + 1), [(128 * i2, 128, identb, triB)]))
                    else:
                        tl.append((kt, 128 * i, 512, [(128 * i, 128, identb, triA)]))
                return tl

            if not PLAN[0]:
                def bias_head(hidx, narrow):
                    def f(kt, j):
                        if narrow:
                            di = kt - (4 * tr + j) + 15
                            o_ = OF["bn"] + hidx * 16 + di
                        else:
                            di = kt - 4 * tr + 12
                            o_ = OF["bw"] + hidx * 16 + di
                        return cf[:, o_:o_ + 1]
                    return f

                def bias_cmp(h, narrow):
                    def f(kt, j):
                        if narrow:
                            o_ = OF["cn"] + h * 16 + (4 * tr + j)
                        else:
                            o_ = OF["cw"] + h * 4 + tr
                        return cf[:, o_:o_ + 1]
                    return f

                def norm_coef(ob_, gate_ap):
                    ov_ = ob_[:, :].rearrange("p (j c) -> p j c", j=4)
                    ts(smal[:, 0:4], ov_[:, :, 64], TINY, None, ALU.max)
                    S.op("dve", lambda: nc.vector.reciprocal(out=smal[:, 4:8], in_=smal[:, 0:4]),
                         [smal[:, 0:4]], [smal[:, 4:8]])
                    if gate_ap is not None:
                        tt(smal[:, 8:12], smal[:, 4:8], gate_ap, ALU.mult)
                    return ov_

                def qpad(src3, ch, bp, eng):
                    half = bp // 64
                    b = qz[2 * half + (qzc[half] % 2)]
                    qzc[half] += 1
                    cp(b[bp:bp + 64, :], src3[bp:bp + 64, ch, :], e=eng)
                    return b

            if not PLAN[0]:
                S.mark("tr%d moba" % tr)
                MDBG = int(os.environ.get("MK_MOBA", "3"))
                if tr >= 2 and MDBG >= 1:
                    for j in range(4):
                        pb = psM.next()
                        for ch in range(4):
                            mm(pb[:, ch * 16:(ch + 1) * 16], qm[:, ch, j * 128:(j + 1) * 128],
                               kmeanT[:, ch, :], True, True)
                        GS = int(os.environ.get("MK_GS", "9"))
                        if GS >= 1:
                            tt(gsf[:, :, 0:8], pb[:, 0:64].rearrange("p (h n) -> p h n", h=8),
                               bass.AP(rtab, 256 + j * 8, [[320, 128], [0, 8], [1, 8]]), ALU.add)
                        if GS >= 2:
                            for h in range(8):
                                S.op("dve", lambda: nc.vector.max(out=m8[:, 0:8], in_=gsf[:, h, :]),
                                     [gsf[:, h, :]], [m8[:, 0:8]])
                                ts(tk32[:, h * 8:(h + 1) * 8], gsf[:, h, 0:8], m8[:, 2:3], None, ALU.is_ge)
                        if GS >= 3:
                            tt(tk32[:, :].rearrange("p (h n) -> p h n", h=8), tk32[:, :].rearrange("p (h n) -> p h n", h=8),
                               bass.AP(rtab, 288 + j * 8, [[320, 128], [0, 8], [1, 8]]), ALU.add)
                            ts(mbtok[:, 0:64], tk32[:, :], -1.0, -MASKV, ALU.add, ALU.mult)
                        if GS >= 4:
                            pb2 = psM.next()
                            tp(pb2[:, 0:128], mbtok[:, :], identf)
                            cp(mbTm[:, j * 128:(j + 1) * 128], pb2[:, 0:128])
                for hp in range(4):
                    ot = otr.next()
                    for h2 in range(2):
                        h = hp * 2 + h2
                        ch, bp = hp, 64 * h2
                        nar = bool(sl_m[h] > 0.3)
                        q_ap = qpad(qm, ch, bp, "pool")
                        om = psO.next()
                        mfn = None
                        if tr >= 2 and MDBG >= 2:
                            def mfn(kt, h=h):
                                c_ = 8 * h + kt // 2
                                l_ = bass.AP(cb, OB["ident"] + c_, [[NBS, 128], [0, 128]])
                                return l_, mbTm[:, :]
                        attend(lambda kt: kmT[:, ch, kt * 128:(kt + 1) * 128], q_ap,
                               lambda kt: vm[:, kt, h, :], causal_tiles(tr), bias_head(8 + h, nar), nar, om,
                               mask_fn=mfn)
                        ov_ = norm_coef(om, None)
                        for j in range(4):
                            ts(ot[:, j, h2, :], ov_[:, j, 0:64], smal[:, 4 + j:5 + j], None, ALU.mult)
                    pb = psM.next()
                    for j in range(4):
                        tp(pb[:, j * 128:(j + 1) * 128], ot[:, j, 0:2, :], identf)
                    cp(omT[:, hp, :], pb[:, :], e="act")

            if not PLAN[0]:
                for g in range(2):
                    pb2 = psM.next()
                    mm(pb2[:, 0:nb], w2k[:, :], hidb[:, g, 0:nb], True, True)
                    cp(kcT[64 * g:64 * g + 64, c_lo:c_lo + nb], pb2[64 * g:64 * g + 64, 0:nb])
                    pb3 = psM.next()
                    mm(pb3[:, 0:64], hidv[:, g, :], w2v[:, :], True, True)
                    cp(vca[:, g, 0:64], pb3[:, 0:64])
                S.mark("tr%d nsa-cmp" % tr)
                ots = [otk[0], otk[1]]
            if SUB < 3:
                continue
            if not PLAN[0]:
                for g in range(2):
                    ot = ots[g]
                    for hg in range(4):
                        h = g * 4 + hg
                        ch, bp = h % 4, 64 * (h // 4)
                        nar = bool(sl_n[h] > 0.3)
                        oc = psO.next()
                        q_ap = qpad(qn, ch, bp, "pool")
                        attend(lambda kt: kcT[:, :], q_ap, lambda kt: vca[:, g, :],
                               [(0, 0, 512, [(0, 512, identb, cmsk[:, :])])], bias_cmp(h, nar), nar, oc, ncols=97)
                        ocv = norm_coef(oc, gsig[:, 4 * tr:4 * tr + 4, h * 3 + 0])
                        for j in range(4):
                            ts(ot[:, j, hg, :], ocv[:, j, 0:64], smal[:, 8 + j:9 + j], None, ALU.mult)
                            if tr >= 2:
                                if hg == 0:
                                    ts(impa[:, j, g, :], ocv[:, j, 65:97], smal[:, 4 + j:5 + j], None, ALU.mult)
                                else:
                                    stt(impa[:, j, g, :], ocv[:, j, 65:97], smal[:, 4 + j:5 + j], impa[:, j, g, :],
                                        ALU.mult, ALU.add)
                if tr >= 2:
                    for j in range(4):
                        kp = rtab[:, j * 32:(j + 1) * 32]
                        fo = rtab[:, 128 + j * 32:128 + (j + 1) * 32]
                        for g in range(2):
                            iv = tk32[:, 0:32]
                            tt(iv, impa[:, j, g, :], kp, ALU.mult)
                            tt(iv, iv, fo, ALU.add)
                            S.op("dve", lambda: nc.vector.max(out=m8[:, 0:8], in_=iv), [iv], [m8[:, 0:8]])
                            S.op("dve", lambda: nc.vector.match_replace(out=tk32[:, 32:64], in_to_replace=m8[:, 0:8],
                                                                         in_values=iv, imm_value=-3.0e38),
                                 [iv, m8[:, 0:8]], [tk32[:, 32:64]])
                            S.op("dve", lambda: nc.vector.max(out=m8[:, 8:16], in_=tk32[:, 32:64]),
                                 [tk32[:, 32:64]], [m8[:, 8:16]])
                            ts(tk32[:, 32:64], iv, m8[:, 15:16], None, ALU.is_ge)
                            ts(mbtokN[:, g * 64:g * 64 + 32], tk32[:, 32:64], -1.0, -MASKV, ALU.add, ALU.mult)
                            pb = psM.next()
                            tp(pb[:, 0:128], mbtokN[:, :], identf)
                            cp(mbTn[:, g, j * 128:(j + 1) * 128], pb[:, 0:128])
                            memset(mbtokN[:, g * 64:g * 64 + 32], 0.0)
                S.mark("tr%d nsa-selwin" % tr)
                for g in range(2):
                    ot = ots[g]
                    for hg in range(4):
                        h = g * 4 + hg
                        ch, bp = h % 4, 64 * (h // 4)
                        nar = bool(sl_n[h] > 0.3)
                        q_ap = qpad(qn, ch, bp, "pool")
                        osel = psO.next()
                        mfn = None
                        if tr >= 2:
                            def mfn(kt, g=g):
                                return esel[:, kt, :], mbTn[:, g, :]
                        attend(lambda kt: kslcT[:, kt * 128:(kt + 1) * 128], q_ap,
                               lambda kt: vslc[:, kt, g, :], causal_tiles(tr), bias_head(h, nar), nar, osel,
                               mask_fn=mfn)
                        ov_ = norm_coef(osel, gsig[:, 4 * tr:4 * tr + 4, h * 3 + 1])
                        for j in range(4):
                            stt(ot[:, j, hg, :], ov_[:, j, 0:64], smal[:, 8 + j:9 + j], ot[:, j, hg, :],
                                ALU.mult, ALU.add)
                        owin = psO.next()
                        attend(lambda kt: kwinT[:, kt * 128:(kt + 1) * 128], q_ap,
                               lambda kt: vwin[:, kt, g, :], win_tiles(tr), bias_head(h, nar), nar, owin)
                        ov_ = norm_coef(owin, gsig[:, 4 * tr:4 * tr + 4, h * 3 + 2])
                        for j in range(4):
                            stt(ot[:, j, hg, :], ov_[:, j, 0:64], smal[:, 8 + j:9 + j], ot[:, j, hg, :],
                                ALU.mult, ALU.add)
                    for i2 in range(2):
                        pb = psM.next()
                        for j in range(4):
                            tp(pb[:, j * 128:(j + 1) * 128], ot[:, j, 2 * i2:2 * i2 + 2, :], identf)
                        cp(onT[:, 2 * g + i2, :], pb[:, :], e="act")

            if SUB < 5:
                continue
            if not PLAN[0]:
                S.mark("tr%d merge" % tr)
            for dc in range(8):
                c0 = dc * 128
                MJ = W([(0, [8, 128], win[:, :, 2840 + c0:2840 + c0 + 128]),
                        (1024, [8, 128], win[:, :, 3864 + c0:3864 + c0 + 128]),
                        (2048, [4, 128], wupn[:, :, c0:c0 + 128]),
                        (2560, [4, 128], wupm[:, :, c0:c0 + 128])])
                if PLAN[0]:
                    continue
                ga_ = MJ[:, 0:1024].rearrange("p (c f) -> p c f", c=8)
                gb_ = MJ[:, 1024:2048].rearrange("p (c f) -> p c f", c=8)
                un_ = MJ[:, 2048:2560].rearrange("p (c f) -> p c f", c=4)
                um_ = MJ[:, 2560:3072].rearrange("p (c f) -> p c f", c=4)
                pga = psS.next()
                pgb = psS.next()
                pyn = psO.next()
                pym = psO.next()
                for c in range(8):
                    mm(pga[:, :], ga_[:, c, :], u_tr[:, c, :], c == 0, c == 7)
                for c in range(8):
                    mm(pgb[:, :], gb_[:, c, :], u_tr[:, c, :], c == 0, c == 7)
                for c in range(4):
                    mm(pyn[:, :], un_[:, c, :], onT[:, c, :], c == 0, c == 3)
                for c in range(4):
                    mm(pym[:, :], um_[:, c, :], omT[:, c, :], c == 0, c == 3)
                sA = sgAr.next()
                sB = sgBr.next()
                act(sA[:, :], pga[:, :], AF.Sigmoid)
                act(sB[:, :], pgb[:, :], AF.Sigmoid)
                tt(sA[:, :], sA[:, :], pyn[:, :], ALU.mult)
                tt(sB[:, :], sB[:, :], pym[:, :], ALU.mult)
                tt(mixed[:, dc, :], sA[:, :], sB[:, :], ALU.add)
            for dh in range(2):
                WO = W([(0, [8, 512], wout[:, :, dh * 512:(dh + 1) * 512])])
                if PLAN[0]:
                    continue
                wo_ = WO[:, 0:4096].rearrange("p (c f) -> p c f", c=8)
                for d4 in range(4):
                    dc = dh * 4 + d4
                    pb = psM.next()
                    for c in range(8):
                        mm(pb[:, :], wo_[:, c, d4 * 128:(d4 + 1) * 128], mixed[:, c, :], c == 0, c == 7)
                    tt(hT[:, dc, trs(tr)], hT[:, dc, trs(tr)], pb[:, :], ALU.add)

    def run_all():
        if not PLAN[0]:
            S.mark("ffn1")
        if stage >= 1:
            scoped(phase_ffn, "ffn1_w1", "ffn1_w3", "ffn1_w2", 0, "a")
        if stage >= 2:
            scoped(phase_mixer)
        if stage >= 3:
            if not PLAN[0]:
                S.mark("ffn2")
            scoped(phase_ffn, "ffn2_w1", "ffn2_w3", "ffn2_w2", 2, "b")
        if stage >= 4:
            if not PLAN[0]:
                S.mark("ple")
            scoped(phase_ple)

    PLAN[0] = True
    run_all()
    PLAN[0] = False
    scoped(phase_load)
    run_all()
    S.mark("final")
    scoped(phase_final)
    S.mark("end")
    S.finish()
    _LAST["S"] = S
    return nc


_NC_CACHE = {}
_LAST = {}


def _prep_inputs(inputs):
    cf_np, _, cb_np, _ = _make_consts()
    f = lambda a: np.ascontiguousarray(np.asarray(a, dtype=np.float32))
    sh = {}
    for nm in ("ffn1_w1", "ffn1_w3", "ffn1_w2", "w_in", "cmp_w1_k", "cmp_w2_k", "cmp_w1_v", "cmp_w2_v",
               "w_up_nsa", "w_up_moba", "w_out", "ffn2_w1", "ffn2_w3", "ffn2_w2", "w_ple_gate", "w_ple"):
        sh[nm] = f(inputs[nm])[0]
    wi = sh["w_in"].copy()
    perm = [0, 4, 1, 5, 2, 6, 3, 7]
    wi[:, 0:512] = sh["w_in"][:, 0:512].reshape(D, 8, 64)[:, perm, :].reshape(D, 512)
    sh["w_in"] = np.ascontiguousarray(wi)
    for nm, src in (("pos_kT", "cmp_pos_k"), ("pos_vT", "cmp_pos_v")):
        pt = f(inputs[src])[0].T
        sh[nm] = np.ascontiguousarray(np.concatenate([pt, pt], axis=0))
    g = np.stack([f(inputs[n])[0] for n in ("ffn1_norm", "mix_norm", "ffn2_norm", "ple_norm")], axis=0)
    sh["gains"] = np.ascontiguousarray(g.reshape(4, 8, 128).transpose(2, 0, 1).reshape(128, 32))
    sh["gfin"] = np.ascontiguousarray(np.broadcast_to(f(inputs["final_norm"])[None, :], (128, D)))
    sh["cf32"] = cf_np
    sh["cbf16"] = cb_np
    return sh


def kernel(**inputs):
    stage = int(os.environ.get("MK_STAGE", "99"))
    ncores = int(os.environ.get("MK_CORES", "8"))
    if stage not in _NC_CACHE:
        _NC_CACHE[stage] = build_nc(stage)
    nc = _NC_CACHE[stage]
    sh = _prep_inputs(inputs)
    x = np.asarray(inputs["x"], dtype=np.float32)
    p = np.asarray(inputs["p"], dtype=np.float32)
    in_maps = []
    for b in range(ncores):
        m = dict(sh)
        m["x"] = np.ascontiguousarray(x[b])
        m["p"] = np.ascontiguousarray(p[0, b])
        in_maps.append(m)
    res = run_bass_kernel_spmd(nc, in_maps, core_ids=list(range(ncores)))
    out = np.stack([np.asarray(r["out"], dtype=np.float32) for r in res.results], axis=0)
    return out
```

```python
import os
import numpy as np
import ml_dtypes
import concourse.bass as bass
import concourse.mybir as mybir
from concourse.bass_utils import run_bass_kernel_spmd
from contextlib import ExitStack

F32 = mybir.dt.float32
BF16 = mybir.dt.bfloat16
AF = mybir.ActivationFunctionType
ALU = mybir.AluOpType

T = 2048
D = 1024
FF = 2816
MASKV = -240000.0
EPS = 1e-6
TINY = 1e-30
IN_W = 4888


def _slopes():
    s = 2.0 ** (-(np.arange(16) + 1) / 2.0)
    return s[0::2].copy(), s[1::2].copy()


def _make_consts():
    p = np.arange(128)
    f32 = {}
    b16 = {}
    f32["ident"] = np.eye(128)
    b16["ident"] = np.eye(128)
    b16["ones"] = np.ones((128, 128))
    j = np.arange(128)
    b16["triA"] = np.where(j[None, :] >= p[:, None], 0.0, MASKV)
    b16["triB"] = np.where(j[None, :] < p[:, None], 0.0, MASKV)
    tq = np.arange(T)
    cend = 16 * p + 31
    cm = np.where(tq[None, :] >= cend[:, None], 0.0, MASKV)
    cm[127, :] = MASKV
    ov = np.zeros((128, 32))
    for c in range(127):
        for jj in range(32):
            if 16 * c <= 64 * jj + 63 and 16 * c + 31 >= 64 * jj:
                ov[c, jj] = 1.0
    b16["overlap"] = ov
    b16["cmpmask"] = cm
    es = np.zeros((128, 16, 128))
    for pp in range(128):
        jj = pp % 64
        if jj >= 32:
            continue
        for kt in range(16):
            for m_ in range(128):
                if jj == 2 * kt + m_ // 64:
                    es[pp, kt, m_] = 1.0
    b16["esel"] = es.reshape(128, 2048)
    sl_n, sl_m = _slopes()
    sl16 = np.concatenate([sl_n, sl_m])
    d = np.arange(16) - 12
    f32["bw"] = (sl16[None, :, None] * (128 * d[None, None, :] + p[:, None, None] - 256)).reshape(128, 256)
    d2 = np.arange(16) - 15
    f32["bn"] = (sl16[None, :, None] * (128 * d2[None, None, :] + p[:, None, None] - 64)).reshape(128, 256)
    f32["cw"] = (sl_n[None, :, None] * (cend[:, None, None] - (512 * np.arange(4)[None, None, :] + 256))).reshape(128, 32)
    f32["cn"] = (sl_n[None, :, None] * (cend[:, None, None] - (128 * np.arange(16)[None, None, :] + 64))).reshape(128, 128)
    keep = np.zeros((128, 8, 32))
    force = np.zeros((128, 8, 32))
    for tt in range(8, 16):
        for pp in range(128):
            t = tt * 128 + pp
            cur = t // 64
            for jj in range(32):
                if 64 * jj > t:
                    force[pp, tt - 8, jj] = -1e30 * (1.0 + jj / 64.0)
                elif jj == 0:
                    force[pp, tt - 8, jj] = 3e9
                elif jj == cur:
                    force[pp, tt - 8, jj] = 2e9
                elif jj == cur - 1:
                    force[pp, tt - 8, jj] = 1e9
                else:
                    keep[pp, tt - 8, jj] = 1.0
    padneg = np.zeros((128, 8, 8))
    ownhot = np.zeros((128, 8, 8))
    for tt in range(8, 16):
        cur = tt // 2
        for n in range(8):
            if n >= cur:
                padneg[:, tt - 8, n] = -1e30
            if n == cur:
                ownhot[:, tt - 8, n] = 1.0
    f32["eps"] = np.full((128, 1), EPS)
    rt = []
    for tr in (2, 3):
        a = (tr - 2) * 4
        rt += [keep[:, a:a + 4].reshape(128, 128), force[:, a:a + 4].reshape(128, 128),
               padneg[:, a:a + 4].reshape(128, 32), ownhot[:, a:a + 4].reshape(128, 32)]
    f32["rt"] = np.concatenate(rt, axis=1)
    offs_f = {}
    cols = []
    o = 0
    for k, v in f32.items():
        offs_f[k] = o
        o += v.shape[1]
        cols.append(v.astype(np.float32))
    cf = np.ascontiguousarray(np.concatenate(cols, axis=1))
    offs_b = {}
    cols = []
    o = 0
    for k, v in b16.items():
        offs_b[k] = o
        o += v.shape[1]
        cols.append(v.astype(np.float32))
    cbv = np.ascontiguousarray(np.concatenate(cols, axis=1).astype(ml_dtypes.bfloat16))
    return cf, offs_f, cbv, offs_b


EMBED_WAIT = set(os.environ.get("MK_EMBED", "pe").split(",")) - {""}


def _box(ap):
    t = ap.tensor
    if t.name.startswith("ps") and len(t.name) == 3:
        return (t.name, 0, 128, 0, 512)
    row = 1
    for s in tuple(t.shape)[1:]:
        row *= int(s)
    off = int(ap.offset)
    p0 = off // row
    f0 = off % row
    apl = ap.ap
    npart = apl[0][1]
    ext = 1
    for st, cnt in apl[1:]:
        ext += (cnt - 1) * abs(st)
    return (t.name, p0, p0 + npart, f0, f0 + ext)


class Sched:
    def __init__(self, nc, ndma=24):
        self.nc = nc
        self.engs = {"pe": nc.tensor, "act": nc.scalar, "dve": nc.vector, "pool": nc.gpsimd, "sp": nc.sync}
        self.semh = {}
        for e in self.engs:
            self.semh[e] = nc.alloc_semaphore("sem_" + e)
        self.cnt = {e: 0 for e in self.engs}
        self.ndma = ndma
        for i in range(ndma):
            self.semh[("d", i)] = nc.alloc_semaphore("dsem%d" % i)
        self.dcnt = [0] * ndma
        self.drr = 0
        self.drr_pool = 0
        self.seen = {e: {} for e in self.engs}
        self.acc = {}
        self.out_tickets = []
        self.nwaits = 0
        self.marks = []

    def mark(self, name):
        self.marks.append((name, self.cnt["pe"]))

    def _wait(self, e, sk, val):
        if e == "pe" and sk == "pe":
            return
        if self.seen[e].get(sk, 0) >= val:
            return
        self.engs[e].wait_ge(self.semh[sk], val)
        self.seen[e][sk] = val
        self.nwaits += 1

    def _collect(self, reads, writes):
        waits = {}
        for ap in reads:
            b = _box(ap)
            for ent in self.acc.get(b[0], ()):
                if ent[0] == "w" and ent[3] < b[2] and b[1] < ent[4] and ent[5] < b[4] and b[3] < ent[6]:
                    if waits.get(ent[1], 0) < ent[2]:
                        waits[ent[1]] = ent[2]
        for ap in writes:
            b = _box(ap)
            for ent in self.acc.get(b[0], ()):
                if ent[3] < b[2] and b[1] < ent[4] and ent[5] < b[4] and b[3] < ent[6]:
                    if waits.get(ent[1], 0) < ent[2]:
                        waits[ent[1]] = ent[2]
        return waits

    def _record(self, sk, val, reads, writes):
        for ap in writes:
            b = _box(ap)
            lst = self.acc.setdefault(b[0], [])
            lst[:] = [e for e in lst if not (b[1] <= e[3] and e[4] <= b[2] and b[3] <= e[5] and e[6] <= b[4])]
            lst.append(("w", sk, val, b[1], b[2], b[3], b[4]))
        for ap in reads:
            b = _box(ap)
            lst = self.acc.setdefault(b[0], [])
            lst[:] = [e for e in lst if not (e[0] == "r" and e[1] == sk and b[1] <= e[3] and e[4] <= b[2]
                                             and b[3] <= e[5] and e[6] <= b[4])]
            lst.append(("r", sk, val, b[1], b[2], b[3], b[4]))

    def op(self, e, fn, reads, writes, inc=True):
        assert inc or e == "pe"
        waits = self._collect(reads, writes)
        emb = None
        if EMBED_WAIT and e in EMBED_WAIT:
            need = [(sk, val) for sk, val in waits.items()
                    if not (e == "pe" and sk == "pe") and self.seen[e].get(sk, 0) < val]
            if need:
                emb = need[-1]
                waits = dict(need[:-1])
            else:
                waits = {}
        for sk, val in waits.items():
            self._wait(e, sk, val)
        ins = fn()
        if emb is not None:
            ins._wait_ge(self.semh[emb[0]], emb[1])
            self.seen[e][emb[0]] = emb[1]
        if inc:
            self.cnt[e] += 1
            ins.then_inc(self.semh[e], 1)
            val = self.cnt[e]
        else:
            val = self.cnt[e] + 1
        self._record(e, val, reads, writes)
        return ins

    def dma(self, q, out, in_, reads=(), writes=(), is_out=False, **kw):
        waits = self._collect(reads, writes)
        for sk, val in waits.items():
            self._wait(q, sk, val)
        half = self.ndma // 2
        if q == "pool":
            i = half + (self.drr_pool % half)
            self.drr_pool += 1
        else:
            i = self.drr % half
            self.drr += 1
        sk = ("d", i)
        if self.dcnt[i] > 0:
            self._wait(q, sk, self.dcnt[i])
        ins = self.engs[q].dma_start(out=out, in_=in_, **kw)
        self.dcnt[i] += 16
        ins.then_inc(self.semh[sk], 16)
        self._record(sk, self.dcnt[i], reads, writes)
        if is_out:
            self.out_tickets.append((sk, self.dcnt[i]))

    def barrier(self):
        for e in ("pe", "act", "dve", "pool", "sp"):
            for f in ("pe", "act", "dve", "pool", "sp"):
                if f != e and self.cnt[f] > 0:
                    self._wait(e, f, self.cnt[f])
            for i in range(self.ndma):
                if self.dcnt[i] > 0:
                    self._wait(e, ("d", i), self.dcnt[i])
        self.acc = {}

    def finish(self):
        for sk, val in self.out_tickets:
            self._wait("sp", sk, val)


def build_nc(stage=99):
    nc = bass.Bass("TRN2", target_bir_lowering=False)
    cf_np, OF, cb_np, OB = _make_consts()
    NF = cf_np.shape[1]
    NB = cb_np.shape[1]

    def din(name, shape, dt=F32):
        return nc.dram_tensor(name, list(shape), dt, kind="ExternalInput").ap()

    x_d = din("x", [T, D])
    p_d = din("p", [T, 256])
    wd = {}
    for nm, shp in [("ffn1_w1", [D, FF]), ("ffn1_w3", [D, FF]), ("ffn1_w2", [FF, D]), ("w_in", [D, IN_W]),
                    ("cmp_w1_k", [2048, 128]), ("cmp_w2_k", [128, 64]), ("cmp_w1_v", [2048, 128]),
                    ("cmp_w2_v", [128, 64]), ("pos_kT", [128, 32]), ("pos_vT", [128, 32]),
                    ("w_up_nsa", [512, D]), ("w_up_moba", [512, D]), ("w_out", [D, D]),
                    ("ffn2_w1", [D, FF]), ("ffn2_w3", [D, FF]), ("ffn2_w2", [FF, D]),
                    ("w_ple_gate", [D, D]), ("w_ple", [256, D]), ("gains", [128, 32]), ("gfin", [128, D])]:
        wd[nm] = din(nm, shp)
    cf_d = din("cf32", [128, NF])
    cb_d = din("cbf16", [128, NB], BF16)
    out_d = nc.dram_tensor("out", [T, D], F32, kind="ExternalOutput").ap()

    S = Sched(nc)
    stack = [None]

    def TS(name, shape, dt):
        if stack[0] is None:
            return nc.alloc_sbuf_tensor(name, shape, dt)
        return stack[0].enter_context(nc.sbuf_tensor(name, shape, dt))

    def scoped(fn, *a):
        if PLAN[0]:
            fn(*a)
            return
        S.barrier()
        with ExitStack() as st:
            stack[0] = st
            fn(*a)
            S.barrier()
        stack[0] = None

    hT = TS("hT", [128, 8, T], F32)
    NFS = OF["rt"]
    NBS = OB["cmpmask"]
    cf = TS("cf", [128, NFS], F32)
    cb = TS("cb", [128, NBS], BF16)
    gains = TS("gains_sb", [128, 32], F32)
    slab = [TS("slab%d" % i, [128, 4096], BF16) for i in range(3)]
    ps = [nc.alloc_psum_tensor("ps%d" % i, [128, 512], F32) for i in range(8)]

    identf = cf[:, OF["ident"]:OF["ident"] + 128]
    identb = cb[:, OB["ident"]:OB["ident"] + 128]
    onesb = cb[:, OB["ones"]:OB["ones"] + 128]
    epsc = cf[:, OF["eps"]:OF["eps"] + 1]

    S.dma("sp", cf[:, :], cf_d[:, 0:NFS], writes=[cf[:, :]])
    S.dma("sp", cb[:, :], cb_d[:, 0:NBS], writes=[cb[:, :]])
    S.dma("sp", gains[:, :], wd["gains"][:, :], writes=[gains[:, :]])

    def mm(out, lhsT, rhs, start, stop, inc=None):
        inc = True
        return S.op("pe", lambda: nc.tensor.matmul(out, lhsT, rhs, start=start, stop=stop),
                    [lhsT, rhs], [out], inc=inc)

    def pe_drain():
        if S.cnt["pe"] > 0:
            nc.tensor.wait_ge(S.semh["pe"], S.cnt["pe"])

    def tp(out, in_, ident):
        return S.op("pe", lambda: nc.tensor.transpose(out, in_, ident), [in_, ident], [out])

    def act(out, in_, func, bias=None, scale=None, accum_out=None):
        kw = {}
        rd = [in_]
        wr = [out]
        if bias is not None:
            kw["bias"] = bias
            if not isinstance(bias, (int, float)):
                rd.append(bias)
        if scale is not None:
            kw["scale"] = scale
            if not isinstance(scale, (int, float)):
                rd.append(scale)
        if accum_out is not None:
            kw["accum_out"] = accum_out
            wr.append(accum_out)
        return S.op("act", lambda: nc.scalar.activation(out=out, in_=in_, func=func, **kw), rd, wr)

    def tt(out, in0, in1, op, e="dve"):
        eng = nc.vector if e == "dve" else nc.gpsimd
        return S.op(e, lambda: eng.tensor_tensor(out=out, in0=in0, in1=in1, op=op), [in0, in1], [out])

    def ts(out, in0, s1, s2, op0, op1=None, e="dve"):
        eng = nc.vector if e == "dve" else nc.gpsimd
        rd = [in0]
        if not isinstance(s1, (int, float)):
            rd.append(s1)
        if s2 is not None and not isinstance(s2, (int, float)):
            rd.append(s2)
        if op1 is None:
            return S.op(e, lambda: eng.tensor_scalar(out=out, in0=in0, scalar1=s1, scalar2=None, op0=op0), rd, [out])
        return S.op(e, lambda: eng.tensor_scalar(out=out, in0=in0, scalar1=s1, scalar2=s2, op0=op0, op1=op1),
                    rd, [out])

    def stt(out, in0, scalar, in1, op0, op1):
        rd = [in0, in1]
        if not isinstance(scalar, (int, float)):
            rd.append(scalar)
        return S.op("dve", lambda: nc.vector.scalar_tensor_tensor(out=out, in0=in0, scalar=scalar, in1=in1,
                                                                   op0=op0, op1=op1), rd, [out])

    def cp(out, in_, e="dve"):
        if e == "act":
            return S.op("act", lambda: nc.scalar.copy(out=out, in_=in_), [in_], [out])
        eng = nc.vector if e == "dve" else nc.gpsimd
        return S.op(e, lambda: eng.tensor_copy(out=out, in_=in_), [in_], [out])

    def memset(ap, v, e="dve"):
        eng = nc.vector if e == "dve" else nc.gpsimd
        return S.op(e, lambda: eng.memset(ap, v), [], [ap])

    class RR:
        def __init__(self, items):
            self.items = items
            self.i = 0

        def next(self):
            it = self.items[self.i % len(self.items)]
            self.i += 1
            return it

    class WStream:
        def __init__(self):
            self.jobs = []
            self.issued = 0
            self.k = 0

        def add(self, parts):
            self.jobs.append(parts)

        def _issue(self, k):
            buf = slab[k % 3]
            for (off, shape, src) in self.jobs[k]:
                n = 1
                for s_ in shape:
                    n *= s_
                npart = int(src.shape[0])
                dst = buf[0:npart, off:off + n]
                if len(shape) == 2:
                    dst = dst.rearrange("p (a b) -> p a b", a=shape[0])
                S.dma("pool", dst, src, writes=[buf[0:npart, off:off + n]])

        def get(self):
            k = self.k
            self.k += 1
            while self.issued < min(k + 2, len(self.jobs)):
                self._issue(self.issued)
                self.issued += 1
            return slab[k % 3]

    WS = WStream()
    PLAN = [True]

    def W(parts):
        if PLAN[0]:
            WS.add(parts)
            return None
        return WS.get()

    def trs(tr):
        return slice(tr * 512, (tr + 1) * 512)

    sqb = [TS("sqb%d" % i, [128, 512], BF16) for i in range(2)]
    rstd = TS("rstd", [128, 512], F32)
    sqrr = RR(sqb)
    psM = RR([ps[6], ps[7]])

    def rms_range(tr, gidx, xn3):
        pb = psM.next()
        for c in range(8):
            sq = sqrr.next()
            act(sq[:, :], hT[:, c, trs(tr)], AF.Square)
            mm(pb[:, :], onesb, sq[:, :], c == 0, c == 7)
        act(rstd[:, :], pb[:, :], AF.Ln, bias=epsc, scale=1.0 / D)
        act(rstd[:, :], rstd[:, :], AF.Exp, scale=-0.5)
        for c in range(8):
            stt(xn3[:, c, :], hT[:, c, trs(tr)], gains[:, gidx * 8 + c:gidx * 8 + c + 1], rstd[:, :],
                ALU.mult, ALU.mult)

    def phase_load():
        xtok = [TS("xtok%d" % i, [128, D], F32) for i in range(2)]
        for t_ in range(16):
            xb = xtok[t_ % 2]
            S.dma("sp", xb[:, :], x_d[t_ * 128:(t_ + 1) * 128, :], writes=[xb[:, :]])
            for half in range(2):
                pb = psM.next()
                for c4 in range(4):
                    c = half * 4 + c4
                    tp(pb[:, c4 * 128:(c4 + 1) * 128], xb[:, c * 128:(c + 1) * 128], identf)
                cp(hT[:, half * 4:(half + 1) * 4, t_ * 128:(t_ + 1) * 128],
                   pb[:, :].rearrange("p (c t) -> p c t", c=4), e="act" if half else "dve")

    def phase_ffn(w1n, w3n, w2n, gidx, tag):
        w1 = wd[w1n].rearrange("(c p) f -> p c f", p=128)
        w3 = wd[w3n].rearrange("(c p) f -> p c f", p=128)
        w2 = wd[w2n]
        if not PLAN[0]:
            xnT = TS("xnT" + tag, [128, 8, T], BF16)
            h1 = [TS("h1%s%d" % (tag, i), [128, 2, 512], BF16) for i in range(2)]
            sa = [TS("sa%s%d" % (tag, i), [128, 512], F32) for i in range(2)]
            for tr in range(4):
                rms_range(tr, gidx, xnT[:, :, trs(tr)])
            psAB = RR([ps[0], ps[1], ps[2], ps[3]])
            psY = RR([ps[4], ps[5], ps[6], ps[7]])
            sar = RR(sa)
            h1r = RR(h1)
            ytmp = [TS("ytmp%s%d" % (tag, i), [128, 512], F32) for i in range(2)]
            ytr = RR(ytmp)
        Aj = {}
        Bj = {}

        def getA(s_):
            if s_ not in Aj:
                Aj[s_] = W([(0, [8, 256], w1[:, :, s_ * 256:(s_ + 1) * 256]),
                            (2048, [8, 256], w3[:, :, s_ * 256:(s_ + 1) * 256])])
            return Aj[s_]

        def getB(s_):
            if s_ not in Bj:
                Bj[s_] = W([(0, [2, 1024], w2[s_ * 256:(s_ + 1) * 256, :].rearrange("(j p) d -> p j d", p=128))])
            return Bj[s_]

        def ab(u):
            s_, tr = u
            A = getA(s_)
            if PLAN[0]:
                return None
            w1s = A[:, 0:2048].rearrange("p (c f) -> p c f", c=8)
            w3s = A[:, 2048:4096].rearrange("p (c f) -> p c f", c=8)
            hb = h1r.next()
            for j in range(2):
                pa = psAB.next()
                pb = psAB.next()
                for c in range(8):
                    mm(pa[:, :], w1s[:, c, j * 128:(j + 1) * 128], xnT[:, c, trs(tr)], c == 0, c == 7)
                for c in range(8):
                    mm(pb[:, :], w3s[:, c, j * 128:(j + 1) * 128], xnT[:, c, trs(tr)], c == 0, c == 7)
                sab = sar.next()
                act(sab[:, :], pa[:, :], AF.Silu)
                tt(hb[:, j, :], sab[:, :], pb[:, :], ALU.mult)
            return hb

        def yy(u, hb):
            s_, tr = u
            B = getB(s_)
            if PLAN[0]:
                return
            w2s = B[:, 0:2048].rearrange("p (j d) -> p j d", j=2)
            for dc in range(8):
                py = psY.next()
                for j in range(2):
                    mm(py[:, :], w2s[:, j, dc * 128:(dc + 1) * 128], hb[:, j, :], j == 0, j == 1)
                stt(hT[:, dc, trs(tr)], py[:, :], 0.5, hT[:, dc, trs(tr)], ALU.mult, ALU.add)

        units = [(s_, tr) for s_ in range(11) for tr in range(4)]
        prev = None
        for u in units:
            hb = ab(u)
            if prev is not None:
                yy(prev[0], prev[1])
            prev = (u, hb)
        yy(prev[0], prev[1])

    def phase_ple():
        wg = wd["w_ple_gate"].rearrange("(c p) f -> p c f", p=128)
        wp = wd["w_ple"].rearrange("(c p) f -> p c f", p=128)
        if not PLAN[0]:
            xnT = TS("xnTple", [128, 8, T], BF16)
            pT = TS("pT", [128, 2, T], BF16)
            ptok = [TS("ptok%d" % i, [128, 256], F32) for i in range(2)]
            sg = [TS("sgple%d" % i, [128, 512], F32) for i in range(2)]
            for t_ in range(16):
                pbuf = ptok[t_ % 2]
                S.dma("sp", pbuf[:, :], p_d[t_ * 128:(t_ + 1) * 128, :], writes=[pbuf[:, :]])
                pb = psM.next()
                for c in range(2):
                    tp(pb[:, c * 128:(c + 1) * 128], pbuf[:, c * 128:(c + 1) * 128], identf)
                cp(pT[:, :, t_ * 128:(t_ + 1) * 128], pb[:, 0:256].rearrange("p (c t) -> p c t", c=2))
            for tr in range(4):
                rms_range(tr, 3, xnT[:, :, trs(tr)])
            psG = RR([ps[0], ps[1], ps[2], ps[3]])
            sgr = RR(sg)
        for dh in range(2):
            A = W([(0, [8, 512], wg[:, :, dh * 512:(dh + 1) * 512])])
            B = W([(0, [2, 512], wp[:, :, dh * 512:(dh + 1) * 512])])
            if PLAN[0]:
                continue
            wgs = A[:, 0:4096].rearrange("p (c f) -> p c f", c=8)
            wps = B[:, 0:1024].rearrange("p (c f) -> p c f", c=2)
            for tr in range(4):
                for d4 in range(4):
                    dc = dh * 4 + d4
                    pg = psG.next()
                    pp = psG.next()
                    for c in range(8):
                        mm(pg[:, :], wgs[:, c, d4 * 128:(d4 + 1) * 128], xnT[:, c, trs(tr)], c == 0, c == 7)
                    for c in range(2):
                        mm(pp[:, :], wps[:, c, d4 * 128:(d4 + 1) * 128], pT[:, c, trs(tr)], c == 0, c == 1)
                    sgb = sgr.next()
                    act(sgb[:, :], pg[:, :], AF.Sigmoid)
                    tt(sgb[:, :], sgb[:, :], pp[:, :], ALU.mult)
                    tt(hT[:, dc, trs(tr)], hT[:, dc, trs(tr)], sgb[:, :], ALU.add)

    def phase_final():
        gfin = TS("gfin_sb", [128, D], F32)
        S.dma("sp", gfin[:, :], wd["gfin"][:, :], writes=[gfin[:, :]])
        otok = [TS("otok%d" % i, [128, D], F32) for i in range(2)]
        junk = TS("junk", [128, 512], F32)
        ssq = TS("ssq", [128, 4], F32)
        psF = RR([ps[0], ps[1], ps[2], ps[3], ps[4], ps[5]])
        for t_ in range(16):
            ob = otok[t_ % 2]
            pbs = [psF.next(), psF.next()]
            for half in range(2):
                for c4 in range(4):
                    c = half * 4 + c4
                    tp(pbs[half][:, c4 * 128:(c4 + 1) * 128], hT[:, c, t_ * 128:(t_ + 1) * 128], identf)
                act(junk[:, :], pbs[half][:, :], AF.Square, accum_out=ssq[:, half:half + 1])
            tt(ssq[:, 2:3], ssq[:, 0:1], ssq[:, 1:2], ALU.add)
            act(ssq[:, 3:4], ssq[:, 2:3], AF.Ln, bias=epsc, scale=1.0 / D)
            act(ssq[:, 3:4], ssq[:, 3:4], AF.Exp, scale=-0.5)
            for half in range(2):
                stt(ob[:, half * 512:(half + 1) * 512], pbs[half][:, :], ssq[:, 3:4],
                    gfin[:, half * 512:(half + 1) * 512], ALU.mult, ALU.mult)
            S.dma("sp", out_d[t_ * 128:(t_ + 1) * 128, :], ob[:, :], reads=[ob[:, :]], is_out=True)

    def phase_mixer():
        win = wd["w_in"].rearrange("(c p) f -> p c f", p=128)
        wupn = wd["w_up_nsa"].rearrange("(c p) f -> p c f", p=128)
        wupm = wd["w_up_moba"].rearrange("(c p) f -> p c f", p=128)
        wout = wd["w_out"].rearrange("(c p) f -> p c f", p=128)
        sl_n, sl_m = _slopes()
        if not PLAN[0]:
            kslcT = TS("kslcT", [128, T], BF16)
            kwinT = TS("kwinT", [128, T], BF16)
            kcmpT = TS("kcmpT", [128, 528], BF16)
            vcmpT = TS("vcmpT", [128, 528], BF16)
            kmT = TS("kmT", [128, 4, T], BF16)
            vslc = TS("vslc", [128, 16, 2, 65], BF16)
            vwin = TS("vwin", [128, 16, 2, 65], BF16)
            vm = TS("vm", [128, 16, 8, 65], BF16)
            gsig = TS("gsig", [128, 16, 24], F32)
            kcT = TS("kcT", [128, 128], BF16)
            vca = TS("vca", [128, 2, 97], BF16)
            kmeanT = TS("kmeanT", [128, 4, 16], BF16)
            kmsum = TS("kmsum", [128, 4, 2], F32)
            w2k = TS("w2k", [128, 128], BF16)
            w2v = TS("w2v", [128, 64], BF16)
            posk = TS("posk", [128, 32], BF16)
            posv = TS("posv", [128, 32], BF16)
            cbias = TS("cbias", [128, 2], F32)
            u_tr = TS("u_tr", [128, 8, 512], BF16)
            qmix = TS("qmix", [128, 8, 512], BF16)

            class _Sub:
                def __init__(self, base, off):
                    self.base, self.off = base, off

                def __getitem__(self, key):
                    a, b, c = key
                    if isinstance(b, int):
                        b = b + self.off
                    else:
                        b = slice((b.start or 0) + self.off, (b.stop if b.stop is not None else 4) + self.off)
                    return self.base[a, b, c]
            qn = _Sub(qmix, 0)
            qm = _Sub(qmix, 4)
            ptl = [TS("ptl%d" % i, [128, 512], BF16) for i in range(3)]
            otk = [TS("otk%d" % i, [128, 4, 4, 64], F32) for i in range(2)]
            onT = TS("onT", [128, 4, 512], BF16)
            omT = TS("omT", [128, 4, 512], BF16)
            mixed = qmix
            sgA = [TS("sgA%d" % i, [128, 512], F32) for i in range(1)]
            sgB = [TS("sgB%d" % i, [128, 512], F32) for i in range(1)]
            mbTn = TS("mbTn", [128, 2, 512], BF16)
            qz = [TS("qz%d" % i, [128, 512], BF16) for i in range(4)]
            qzc = [0, 0]
            mbTm = TS("mbTm", [128, 512], BF16)
            mbtokN = TS("mbtokN", [128, 128], F32)
            mbtok = TS("mbtok", [128, 128], F32)
            impa = TS("impa", [128, 4, 2, 32], F32)
            smal = TS("smal", [128, 64], F32)
            m8 = TS("m8", [128, 16], F32)
            tk32 = TS("tk32", [128, 64], F32)
            cmsk = TS("cmsk", [128, 512], BF16)
            rtab = TS("rtab", [128, 320], F32)
            esel = TS("esel", [128, 16, 128], BF16)
            S.dma("sp", esel[:, :, :], cb_d[:, OB["esel"]:OB["esel"] + 2048].rearrange("p (k m) -> p k m", k=16),
                  writes=[esel[:, :, :]])
            hid = TS("hid", [128, 4, 32], F32)
            hidb = TS("hidb", [128, 2, 32], BF16)
            hidv = TS("hidv", [128, 2, 128], BF16)
            gtmp = TS("gtmp", [64, 528], BF16)
            gsf = TS("gsf", [128, 8, 16], F32)

            memset(kcT[:, :], 0.0)
            for i in range(4):
                memset(qz[i][:, :], 0.0)
            memset(mbtokN[:, :], 0.0)
            memset(mbtok[:, :], 0.0)
            memset(gsf[:, :, :], -1.0e30)
            memset(kcmpT[:, 0:16], 0.0)
            memset(vcmpT[:, 0:16], 0.0)
            memset(hidv[:, :, :], 0.0)
            memset(vca[:, :, :], 0.0)
            memset(kmeanT[:, :, :], 0.0)
            memset(vslc[:, :, :, 64:65], 1.0)
            memset(vwin[:, :, :, 64:65], 1.0)
            memset(vm[:, :, :, 64:65], 1.0)
            memset(vca[:, :, 64:65], 1.0)
            for g in range(2):
                cp(vca[:, g, 65:97], cb[:, OB["overlap"]:OB["overlap"] + 32])
            for (w2t, w2n_, pt, pn) in ((w2k, "cmp_w2_k", posk, "pos_kT"), (w2v, "cmp_w2_v", posv, "pos_vT")):
                S.dma("pool", pt[:, :], wd[pn][:, :], writes=[pt[:, :]])
                if w2t is w2k:
                    S.dma("pool", w2t[:, 0:64], wd[w2n_][:, :], writes=[w2t[:, 0:64]])
                    S.dma("pool", w2t[:, 64:128], wd[w2n_][:, :], writes=[w2t[:, 64:128]])
                else:
                    S.dma("pool", w2t[:, :], wd[w2n_][:, :], writes=[w2t[:, :]])

            psS = RR([ps[0], ps[1], ps[2], ps[3]])
            psO = RR([ps[4], ps[5]])
            ptr = RR(ptl)
            otr = RR(otk)
            sgAr = RR(sgA)
            sgBr = RR(sgB)

        def proj_fm(ws, c0, dst, tr, base=None, ev="act"):
            pb = psM.next()
            for c in range(8):
                if base is None:
                    l_ = ws[:, c, c0:c0 + 128]
                else:
                    l_ = base(c)
                mm(pb[:, :], l_, u_tr[:, c, :], c == 0, c == 7)
            cp(dst, pb[:, :], e=ev)

        NTRS = int(os.environ.get("MK_NTR", "4"))
        SUB = int(os.environ.get("MK_SUB", "9"))
        for tr in range(NTRS):
            S0 = W([(0, [8, 512], win[:, :, 0:512])])
            if not PLAN[0]:
                S.mark("tr%d proj" % tr)
                rms_range(tr, 1, u_tr)
                w0 = S0[:, 0:4096].rearrange("p (c f) -> p c f", c=8)
                for i in range(4):
                    proj_fm(w0, i * 128, qn[:, i, :], tr, ev="act" if i % 2 else "dve")
            S1 = W([(0, [8, 512], win[:, :, 512:1024])])
            if not PLAN[0]:
                w1_ = S1[:, 0:4096].rearrange("p (c f) -> p c f", c=8)
                proj_fm(w1_, 0, kcmpT[:, 16:528], tr, ev="act")
                proj_fm(w1_, 128, vcmpT[:, 16:528], tr, ev="dve")
                proj_fm(w1_, 256, kslcT[:, trs(tr)], tr, ev="act")
                for j in range(4):
                    t_ = tr * 4 + j
                    pb = psM.next()
                    for c in range(8):
                        mm(pb[:, 0:128], u_tr[:, c, j * 128:(j + 1) * 128], w1_[:, c, 384:512], c == 0, c == 7)
                    cp(vslc[:, t_, :, 0:64], pb[:, 0:128].rearrange("p (g d) -> p g d", g=2))
            S2 = W([(0, [8, 280], win[:, :, 1024:1304])])
            if not PLAN[0]:
                w2_ = S2[:, 0:2240].rearrange("p (c f) -> p c f", c=8)
                proj_fm(w2_, 0, kwinT[:, trs(tr)], tr, ev="act")
                for j in range(4):
                    t_ = tr * 4 + j
                    pb = psM.next()
                    for c in range(8):
                        mm(pb[:, 0:152], u_tr[:, c, j * 128:(j + 1) * 128], w2_[:, c, 128:280], c == 0, c == 7)
                    cp(vwin[:, t_, :, 0:64], pb[:, 0:128].rearrange("p (g d) -> p g d", g=2))
                    act(gsig[:, t_, :], pb[:, 128:152], AF.Sigmoid)
            S3 = W([(0, [8, 512], win[:, :, 1304:1816])])
            if not PLAN[0]:
                w3_ = S3[:, 0:4096].rearrange("p (c f) -> p c f", c=8)
                for i in range(4):
                    proj_fm(w3_, i * 128, qm[:, i, :], tr, ev="act" if i % 2 else "dve")
            S4 = W([(0, [8, 512], win[:, :, 1816:2328])])
            if not PLAN[0]:
                w4_ = S4[:, 0:4096].rearrange("p (c f) -> p c f", c=8)
                for i in range(4):
                    proj_fm(w4_, i * 128, kmT[:, i, trs(tr)], tr, ev="act" if i % 2 else "dve")
                for i in range(4):
                    S.op("dve", lambda: nc.vector.tensor_reduce(
                        out=kmsum[:, i, :], in_=kmT[:, i, trs(tr)].rearrange("p (n k) -> p n k", n=2),
                        axis=mybir.AxisListType.X, op=ALU.add),
                        [kmT[:, i, trs(tr)]], [kmsum[:, i, :]])
                ts(kmeanT[0:64, :, 2 * tr:2 * tr + 2], kmsum[0:64, :, :], 1.0 / 256.0, None, ALU.mult)
                ts(kmeanT[64:128, :, 8 + 2 * tr:8 + 2 * tr + 2], kmsum[64:128, :, :], 1.0 / 256.0, None, ALU.mult)
            S5 = W([(0, [8, 512], win[:, :, 2328:2840])])
            if not PLAN[0]:
                w5_ = S5[:, 0:4096].rearrange("p (c f) -> p c f", c=8)
                for j in range(4):
                    t_ = tr * 4 + j
                    pb = psM.next()
                    for c in range(8):
                        mm(pb[:, :], u_tr[:, c, j * 128:(j + 1) * 128], w5_[:, c, :], c == 0, c == 7)
                    cp(vm[:, t_, :, 0:64], pb[:, :].rearrange("p (h d) -> p h d", h=8), e="act" if j % 2 else "dve")

            if SUB < 1:
                continue
            if not PLAN[0]:
                S.mark("tr%d compress" % tr)
            c_lo = 0 if tr == 0 else 32 * tr - 1
            c_hi = 32 * tr + 30
            nb = c_hi - c_lo + 1
            if not PLAN[0]:
                pe_drain()
            for which in range(2):
                wsrc = wd["cmp_w1_k" if which == 0 else "cmp_w1_v"].rearrange("(l d) h -> d l h", d=64)
                CW = W([(q4 * 1024, [8, 128], wsrc[:, q4 * 8:(q4 + 1) * 8, :]) for q4 in range(4)])
                if PLAN[0]:
                    continue
                srcT = kcmpT if which == 0 else vcmpT
                w1t = CW[0:64, 0:4096].rearrange("p (l h) -> p l h", l=32)
                if tr == 0:
                    pt = posk if which == 0 else posv
                    pb = psM.next()
                    for l in range(32):
                        mm(pb[:, 0:1], w1t[:, l, :], pt[0:64, l:l + 1], l == 0, l == 31)
                    cp(cbias[:, which:which + 1], pb[:, 0:1])
                for g in range(2):
                    if g == 1:
                        cp(gtmp[0:64, 0:528], srcT[64:128, 0:528])
                        src_t = gtmp
                    else:
                        src_t = srcT
                    pbA = ps[which * 2 + g]
                    for l in range(32):
                        col0 = 16 * c_lo + l - 512 * tr + 16
                        rhs_ = bass.AP(src_t, col0, [[528, 64], [16, nb]])
                        mm(pbA[:, 0:nb], w1t[:, l, :], rhs_, l == 0, l == 31)
                cp(srcT[:, 0:16], srcT[:, 512:528])
            if not PLAN[0]:
                pe_drain()
                for which in range(2):
                    for g in range(2):
                        pbA = ps[which * 2 + g]
                        xx = hid[:, 0, 0:nb]
                        x2 = hid[:, 1, 0:nb]
                        ts(xx, pbA[:, 0:nb], cbias[:, which:which + 1], None, ALU.add)
                        tt(x2, xx, xx, ALU.mult)
                        ts(x2, x2, 0.044715, 1.0, ALU.mult, ALU.add)
                        tt(x2, x2, xx, ALU.mult)
                        act(hid[:, 2, 0:nb], x2, AF.Sigmoid, scale=1.5957691216057308)
                        if which == 0:
                            tt(hidb[:, g, 0:nb], xx, hid[:, 2, 0:nb], ALU.mult)
                        else:
                            tt(hidv[:, g, c_lo:c_lo + nb], xx, hid[:, 2, 0:nb], ALU.mult)
            if not PLAN[0]:
                S.dma("sp", cmsk[:, :], cb_d[:, OB["cmpmask"] + tr * 512:OB["cmpmask"] + (tr + 1) * 512],
                      writes=[cmsk[:, :]])
                if tr >= 2:
                    S.dma("sp", rtab[:, :], cf_d[:, OF["rt"] + (tr - 2) * 320:OF["rt"] + (tr - 1) * 320],
                          writes=[rtab[:, :]])

            if SUB < 2:
                continue
            def attend(kT_ap_fn, q_ap, vfn, tiles, bias_fn, narrow, ops, mask_fn=None, ncols=65):
                npv = [0]
                npv_total = sum((c1 - c0) // 128 for (kt, c0, c1, extra) in tiles)

                def emit_scores(ti):
                    (kt, c0, c1, extra) = tiles[ti]
                    sp_ = psS.next()
                    n_extra = len(extra) + (1 if mask_fn is not None else 0)
                    mm(sp_[:, c0:c1], kT_ap_fn(kt), q_ap[:, c0:c1], True, n_extra == 0)
                    k_ = 0
                    if mask_fn is not None:
                        k_ += 1
                        ml, mr = mask_fn(kt)
                        mm(sp_[:, c0:c1], ml, mr[:, c0:c1], False, k_ == n_extra)
                    for (e0, en, el, er) in extra:
                        k_ += 1
                        mm(sp_[:, e0:e0 + en], el, er, False, k_ == n_extra)
                    return sp_

                DEPTH = 2
                pend = [emit_scores(i) for i in range(min(DEPTH, len(tiles)))]
                for ti in range(len(tiles)):
                    (kt, c0, c1, extra) = tiles[ti]
                    sp_ = pend.pop(0)
                    if ti + DEPTH < len(tiles):
                        pend.append(emit_scores(ti + DEPTH))
                    pt = ptr.next()
                    if narrow:
                        for j in range(c0 // 128, c1 // 128):
                            act(pt[:, j * 128:(j + 1) * 128], sp_[:, j * 128:(j + 1) * 128], AF.Exp,
                                bias=bias_fn(kt, j), scale=0.125)
                    else:
                        act(pt[:, c0:c1], sp_[:, c0:c1], AF.Exp, bias=bias_fn(kt, None), scale=0.125)
                    for j in range(c0 // 128, c1 // 128):
                        npv[0] += 1
                        mm(ops[:, j * 128:j * 128 + ncols], pt[:, j * 128:(j + 1) * 128], vfn(kt),
                           npv[0] == 1, npv[0] == npv_total)

            triA = cb[:, OB["triA"]:OB["triA"] + 128]
            triB = cb[:, OB["triB"]:OB["triB"] + 128]

            def _jhi(kt, tr_, dmax):
                return min(3, (dmax + 126) // 128 + kt - 4 * tr_)

            def causal_tiles(tr_, dmax=1 << 30):
                tl = []
                for kt in range(0, 4 * tr_ + 4):
                    i = kt - 4 * tr_
                    j_lo = max(0, i)
                    j_hi = _jhi(kt, tr_, dmax)
                    if j_hi < j_lo:
                        continue
                    extra = [(128 * i, 128, identb, triA)] if i >= 0 else []
                    tl.append((kt, 128 * j_lo, 128 * (j_hi + 1), extra))
                return tl

            def win_tiles(tr_, dmax=1 << 30):
                tl = []
                for kt in range(4 * tr_ - 4, 4 * tr_ + 4):
                    if kt < 0:
                        continue
                    i = kt - 4 * tr_
                    j_hi = _jhi(kt, tr_, dmax)
                    if i < 0:
                        i2 = i + 4
                        jh = min(i2, j_hi)
                        if jh < 0:
                            continue
                        extra = [(128 * i2, 128, identb, triB)] if jh == i2 else []
                        tl.append((kt, 0, 128 * (jh + 1), extra))
                    else:
                        if j_hi < i:
                            continue
                        tl.append((kt, 128 * i, 128 * (j_hi + 1), [(128 * i, 128, identb, triA)]))
                return tl

            def dmax_of(slope):
                return int(np.ceil(64.0 / float(slope)))

            if not PLAN[0]:
                def bias_head(hidx, narrow):
                    def f(kt, j):
                        if narrow:
                            di = kt - (4 * tr + j) + 15
                            o_ = OF["bn"] + hidx * 16 + di
                        else:
                            di = kt - 4 * tr + 12
                            o_ = OF["bw"] + hidx * 16 + di
                        return cf[:, o_:o_ + 1]
                    return f

                def bias_cmp(h, narrow):
                    def f(kt, j):
                        if narrow:
                            o_ = OF["cn"] + h * 16 + (4 * tr + j)
                        else:
                            o_ = OF["cw"] + h * 4 + tr
                        return cf[:, o_:o_ + 1]
                    return f

                def norm_coef(ob_, gate_ap):
                    ov_ = ob_[:, :].rearrange("p (j c) -> p j c", j=4)
                    ts(smal[:, 0:4], ov_[:, :, 64], TINY, None, ALU.max)
                    S.op("dve", lambda: nc.vector.reciprocal(out=smal[:, 4:8], in_=smal[:, 0:4]),
                         [smal[:, 0:4]], [smal[:, 4:8]])
                    if gate_ap is not None:
                        tt(smal[:, 8:12], smal[:, 4:8], gate_ap, ALU.mult)
                    return ov_

                def qpad(src3, ch, bp, eng):
                    half = bp // 64
                    b = qz[2 * half + (qzc[half] % 2)]
                    qzc[half] += 1
                    cp(b[bp:bp + 64, :], src3[bp:bp + 64, ch, :], e=eng)
                    return b

            if not PLAN[0]:
                S.mark("tr%d moba" % tr)
                MDBG = int(os.environ.get("MK_MOBA", "3"))
                if tr >= 2 and MDBG >= 1:
                    for j in range(4):
                        pb = psM.next()
                        for ch in range(4):
                            mm(pb[:, ch * 16:(ch + 1) * 16], qm[:, ch, j * 128:(j + 1) * 128],
                               kmeanT[:, ch, :], True, True)
                        GS = int(os.environ.get("MK_GS", "9"))
                        if GS >= 1:
                            tt(gsf[:, :, 0:8], pb[:, 0:64].rearrange("p (h n) -> p h n", h=8),
                               bass.AP(rtab, 256 + j * 8, [[320, 128], [0, 8], [1, 8]]), ALU.add)
                        if GS >= 2:
                            for h in range(8):
                                S.op("dve", lambda: nc.vector.max(out=m8[:, 0:8], in_=gsf[:, h, :]),
                                     [gsf[:, h, :]], [m8[:, 0:8]])
                                ts(tk32[:, h * 8:(h + 1) * 8], gsf[:, h, 0:8], m8[:, 2:3], None, ALU.is_ge)
                        if GS >= 3:
                            tt(tk32[:, :].rearrange("p (h n) -> p h n", h=8), tk32[:, :].rearrange("p (h n) -> p h n", h=8),
                               bass.AP(rtab, 288 + j * 8, [[320, 128], [0, 8], [1, 8]]), ALU.add)
                            ts(mbtok[:, 0:64], tk32[:, :], -1.0, -MASKV, ALU.add, ALU.mult)
                        if GS >= 4:
                            pb2 = psM.next()
                            tp(pb2[:, 0:128], mbtok[:, :], identf)
                            cp(mbTm[:, j * 128:(j + 1) * 128], pb2[:, 0:128])
                for hp in range(4):
                    ot = otr.next()
                    for h2 in range(2):
                        h = hp * 2 + h2
                        ch, bp = hp, 64 * h2
                        nar = bool(sl_m[h] > 0.3)
                        q_ap = qpad(qm, ch, bp, "pool")
                        om = psO.next()
                        mfn = None
                        if tr >= 2 and MDBG >= 2:
                            def mfn(kt, h=h):
                                c_ = 8 * h + kt // 2
                                l_ = bass.AP(cb, OB["ident"] + c_, [[NBS, 128], [0, 128]])
                                return l_, mbTm[:, :]
                        attend(lambda kt: kmT[:, ch, kt * 128:(kt + 1) * 128], q_ap,
                               lambda kt: vm[:, kt, h, :], causal_tiles(tr, dmax_of(sl_m[h])), bias_head(8 + h, nar), nar, om,
                               mask_fn=mfn)
                        ov_ = norm_coef(om, None)
                        for j in range(4):
                            ts(ot[:, j, h2, :], ov_[:, j, 0:64], smal[:, 4 + j:5 + j], None, ALU.mult)
                    pb = psM.next()
                    for j in range(4):
                        tp(pb[:, j * 128:(j + 1) * 128], ot[:, j, 0:2, :], identf)
                    cp(omT[:, hp, :], pb[:, :], e="act")

            if not PLAN[0]:
                for g in range(2):
                    pb2 = psM.next()
                    mm(pb2[:, 0:nb], w2k[:, :], hidb[:, g, 0:nb], True, True)
                    cp(kcT[64 * g:64 * g + 64, c_lo:c_lo + nb], pb2[64 * g:64 * g + 64, 0:nb])
                    pb3 = psM.next()
                    mm(pb3[:, 0:64], hidv[:, g, :], w2v[:, :], True, True)
                    cp(vca[:, g, 0:64], pb3[:, 0:64])
                S.mark("tr%d nsa-cmp" % tr)
                ots = [otk[0], otk[1]]
            if SUB < 3:
                continue
            if not PLAN[0]:
                for g in range(2):
                    ot = ots[g]
                    for hg in range(4):
                        h = g * 4 + hg
                        ch, bp = h % 4, 64 * (h // 4)
                        nar = bool(sl_n[h] > 0.3)
                        oc = psO.next()
                        q_ap = qpad(qn, ch, bp, "pool")
                        attend(lambda kt: kcT[:, :], q_ap, lambda kt: vca[:, g, :],
                               [(0, 0, 512, [(0, 512, identb, cmsk[:, :])])], bias_cmp(h, nar), nar, oc, ncols=97)
                        ocv = norm_coef(oc, gsig[:, 4 * tr:4 * tr + 4, h * 3 + 0])
                        for j in range(4):
                            ts(ot[:, j, hg, :], ocv[:, j, 0:64], smal[:, 8 + j:9 + j], None, ALU.mult)
                            if tr >= 2:
                                if hg == 0:
                                    ts(impa[:, j, g, :], ocv[:, j, 65:97], smal[:, 4 + j:5 + j], None, ALU.mult)
                                else:
                                    stt(impa[:, j, g, :], ocv[:, j, 65:97], smal[:, 4 + j:5 + j], impa[:, j, g, :],
                                        ALU.mult, ALU.add)
                if tr >= 2:
                    for j in range(4):
                        kp = rtab[:, j * 32:(j + 1) * 32]
                        fo = rtab[:, 128 + j * 32:128 + (j + 1) * 32]
                        for g in range(2):
                            iv = tk32[:, 0:32]
                            tt(iv, impa[:, j, g, :], kp, ALU.mult)
                            tt(iv, iv, fo, ALU.add)
                            S.op("dve", lambda: nc.vector.max(out=m8[:, 0:8], in_=iv), [iv], [m8[:, 0:8]])
                            S.op("dve", lambda: nc.vector.match_replace(out=tk32[:, 32:64], in_to_replace=m8[:, 0:8],
                                                                         in_values=iv, imm_value=-3.0e38),
                                 [iv, m8[:, 0:8]], [tk32[:, 32:64]])
                            S.op("dve", lambda: nc.vector.max(out=m8[:, 8:16], in_=tk32[:, 32:64]),
                                 [tk32[:, 32:64]], [m8[:, 8:16]])
                            ts(tk32[:, 32:64], iv, m8[:, 15:16], None, ALU.is_ge)
                            ts(mbtokN[:, g * 64:g * 64 + 32], tk32[:, 32:64], -1.0, -MASKV, ALU.add, ALU.mult)
                            pb = psM.next()
                            tp(pb[:, 0:128], mbtokN[:, :], identf)
                            cp(mbTn[:, g, j * 128:(j + 1) * 128], pb[:, 0:128])
                            memset(mbtokN[:, g * 64:g * 64 + 32], 0.0)
                S.mark("tr%d nsa-selwin" % tr)
                for g in range(2):
                    ot = ots[g]
                    for hg in range(4):
                        h = g * 4 + hg
                        ch, bp = h % 4, 64 * (h // 4)
                        nar = bool(sl_n[h] > 0.3)
                        q_ap = qpad(qn, ch, bp, "pool")
                        osel = psO.next()
                        mfn = None
                        if tr >= 2:
                            def mfn(kt, g=g):
                                return esel[:, kt, :], mbTn[:, g, :]
                        attend(lambda kt: kslcT[:, kt * 128:(kt + 1) * 128], q_ap,
                               lambda kt: vslc[:, kt, g, :], causal_tiles(tr, dmax_of(sl_n[h])), bias_head(h, nar), nar, osel,
                               mask_fn=mfn)
                        ov_ = norm_coef(osel, gsig[:, 4 * tr:4 * tr + 4, h * 3 + 1])
                        for j in range(4):
                            stt(ot[:, j, hg, :], ov_[:, j, 0:64], smal[:, 8 + j:9 + j], ot[:, j, hg, :],
                                ALU.mult, ALU.add)
                        owin = psO.next()
                        attend(lambda kt: kwinT[:, kt * 128:(kt + 1) * 128], q_ap,
                               lambda kt: vwin[:, kt, g, :], win_tiles(tr, dmax_of(sl_n[h])), bias_head(h, nar), nar, owin)
                        ov_ = norm_coef(owin, gsig[:, 4 * tr:4 * tr + 4, h * 3 + 2])
                        for j in range(4):
                            stt(ot[:, j, hg, :], ov_[:, j, 0:64], smal[:, 8 + j:9 + j], ot[:, j, hg, :],
                                ALU.mult, ALU.add)
                    for i2 in range(2):
                        pb = psM.next()
                        for j in range(4):
                            tp(pb[:, j * 128:(j + 1) * 128], ot[:, j, 2 * i2:2 * i2 + 2, :], identf)
                        cp(onT[:, 2 * g + i2, :], pb[:, :], e="act")

            if SUB < 5:
                continue
            if not PLAN[0]:
                S.mark("tr%d merge" % tr)
            for dc in range(8):
                c0 = dc * 128
                MJ = W([(0, [8, 128], win[:, :, 2840 + c0:2840 + c0 + 128]),
                        (1024, [8, 128], win[:, :, 3864 + c0:3864 + c0 + 128]),
                        (2048, [4, 128], wupn[:, :, c0:c0 + 128]),
                        (2560, [4, 128], wupm[:, :, c0:c0 + 128])])
                if PLAN[0]:
                    continue
                ga_ = MJ[:, 0:1024].rearrange("p (c f) -> p c f", c=8)
                gb_ = MJ[:, 1024:2048].rearrange("p (c f) -> p c f", c=8)
                un_ = MJ[:, 2048:2560].rearrange("p (c f) -> p c f", c=4)
                um_ = MJ[:, 2560:3072].rearrange("p (c f) -> p c f", c=4)
                pga = psS.next()
                pgb = psS.next()
                pyn = psO.next()
                pym = psO.next()
                for c in range(8):
                    mm(pga[:, :], ga_[:, c, :], u_tr[:, c, :], c == 0, c == 7)
                for c in range(8):
                    mm(pgb[:, :], gb_[:, c, :], u_tr[:, c, :], c == 0, c == 7)
                for c in range(4):
                    mm(pyn[:, :], un_[:, c, :], onT[:, c, :], c == 0, c == 3)
                for c in range(4):
                    mm(pym[:, :], um_[:, c, :], omT[:, c, :], c == 0, c == 3)
                sA = sgAr.next()
                sB = sgBr.next()
                act(sA[:, :], pga[:, :], AF.Sigmoid)
                act(sB[:, :], pgb[:, :], AF.Sigmoid)
                tt(sA[:, :], sA[:, :], pyn[:, :], ALU.mult)
                tt(sB[:, :], sB[:, :], pym[:, :], ALU.mult)
                tt(mixed[:, dc, :], sA[:, :], sB[:, :], ALU.add)
            for dh in range(2):
                WO = W([(0, [8, 512], wout[:, :, dh * 512:(dh + 1) * 512])])
                if PLAN[0]:
                    continue
                wo_ = WO[:, 0:4096].rearrange("p (c f) -> p c f", c=8)
                for d4 in range(4):
                    dc = dh * 4 + d4
                    pb = psM.next()
                    for c in range(8):
                        mm(pb[:, :], wo_[:, c, d4 * 128:(d4 + 1) * 128], mixed[:, c, :], c == 0, c == 7)
                    tt(hT[:, dc, trs(tr)], hT[:, dc, trs(tr)], pb[:, :], ALU.add)

    def run_all():
        if not PLAN[0]:
            S.mark("ffn1")
        if stage >= 1:
            scoped(phase_ffn, "ffn1_w1", "ffn1_w3", "ffn1_w2", 0, "a")
        if stage >= 2:
            scoped(phase_mixer)
        if stage >= 3:
            if not PLAN[0]:
                S.mark("ffn2")
            scoped(phase_ffn, "ffn2_w1", "ffn2_w3", "ffn2_w2", 2, "b")
        if stage >= 4:
            if not PLAN[0]:
                S.mark("ple")
            scoped(phase_ple)

    PLAN[0] = True
    run_all()
    PLAN[0] = False
    scoped(phase_load)
    run_all()
    S.mark("final")
    scoped(phase_final)
    S.mark("end")
    S.finish()
    _LAST["S"] = S
    return nc


_NC_CACHE = {}
_LAST = {}


def _prep_inputs(inputs):
    cf_np, _, cb_np, _ = _make_consts()
    f = lambda a: np.ascontiguousarray(np.asarray(a, dtype=np.float32))
    sh = {}
    for nm in ("ffn1_w1", "ffn1_w3", "ffn1_w2", "w_in", "cmp_w1_k", "cmp_w2_k", "cmp_w1_v", "cmp_w2_v",
               "w_up_nsa", "w_up_moba", "w_out", "ffn2_w1", "ffn2_w3", "ffn2_w2", "w_ple_gate", "w_ple"):
        sh[nm] = f(inputs[nm])[0]
    wi = sh["w_in"].copy()
    perm = [0, 4, 1, 5, 2, 6, 3, 7]
    wi[:, 0:512] = sh["w_in"][:, 0:512].reshape(D, 8, 64)[:, perm, :].reshape(D, 512)
    sh["w_in"] = np.ascontiguousarray(wi)
    for nm, src in (("pos_kT", "cmp_pos_k"), ("pos_vT", "cmp_pos_v")):
        pt = f(inputs[src])[0].T
        sh[nm] = np.ascontiguousarray(np.concatenate([pt, pt], axis=0))
    g = np.stack([f(inputs[n])[0] for n in ("ffn1_norm", "mix_norm", "ffn2_norm", "ple_norm")], axis=0)
    sh["gains"] = np.ascontiguousarray(g.reshape(4, 8, 128).transpose(2, 0, 1).reshape(128, 32))
    sh["gfin"] = np.ascontiguousarray(np.broadcast_to(f(inputs["final_norm"])[None, :], (128, D)))
    sh["cf32"] = cf_np
    sh["cbf16"] = cb_np
    return sh


def kernel(**inputs):
    stage = int(os.environ.get("MK_STAGE", "99"))
    ncores = int(os.environ.get("MK_CORES", "8"))
    if stage not in _NC_CACHE:
        _NC_CACHE[stage] = build_nc(stage)
    nc = _NC_CACHE[stage]
    sh = _prep_inputs(inputs)
    x = np.asarray(inputs["x"], dtype=np.float32)
    p = np.asarray(inputs["p"], dtype=np.float32)
    in_maps = []
    for b in range(ncores):
        m = dict(sh)
        m["x"] = np.ascontiguousarray(x[b])
        m["p"] = np.ascontiguousarray(p[0, b])
        in_maps.append(m)
    res = run_bass_kernel_spmd(nc, in_maps, core_ids=list(range(ncores)))
    out = np.stack([np.asarray(r["out"], dtype=np.float32) for r in res.results], axis=0)
    return out
```

```python
import os
import numpy as np
import ml_dtypes
import concourse.bass as bass
import concourse.mybir as mybir
from concourse.bass_utils import run_bass_kernel_spmd
from contextlib import ExitStack

F32 = mybir.dt.float32
BF16 = mybir.dt.bfloat16
AF = mybir.ActivationFunctionType
ALU = mybir.AluOpType

T = 2048
D = 1024
FF = 2816
MASKV = -240000.0
EPS = 1e-6
TINY = 1e-30
IN_W = 4888


def _slopes():
    s = 2.0 ** (-(np.arange(16) + 1) / 2.0)
    return s[0::2].copy(), s[1::2].copy()


def _make_consts():
    p = np.arange(128)
    f32 = {}
    b16 = {}
    f32["ident"] = np.eye(128)
    b16["ident"] = np.eye(128)
    b16["ones"] = np.ones((128, 128))
    j = np.arange(128)
    b16["triA"] = np.where(j[None, :] >= p[:, None], 0.0, MASKV)
    b16["triB"] = np.where(j[None, :] < p[:, None], 0.0, MASKV)
    tq = np.arange(T)
    cend = 16 * p + 31
    cm = np.where(tq[None, :] >= cend[:, None], 0.0, MASKV)
    cm[127, :] = MASKV
    ov = np.zeros((128, 32))
    for c in range(127):
        for jj in range(32):
            if 16 * c <= 64 * jj + 63 and 16 * c + 31 >= 64 * jj:
                ov[c, jj] = 1.0
    b16["overlap"] = ov
    b16["cmpmask"] = cm
    es = np.zeros((128, 16, 128))
    for pp in range(128):
        jj = pp % 64
        if jj >= 32:
            continue
        for kt in range(16):
            for m_ in range(128):
                if jj == 2 * kt + m_ // 64:
                    es[pp, kt, m_] = 1.0
    b16["esel"] = es.reshape(128, 2048)
    sl_n, sl_m = _slopes()
    sl16 = np.concatenate([sl_n, sl_m])
    d = np.arange(16) - 12
    f32["bw"] = (sl16[None, :, None] * (128 * d[None, None, :] + p[:, None, None] - 256)).reshape(128, 256)
    d2 = np.arange(16) - 15
    f32["bn"] = (sl16[None, :, None] * (128 * d2[None, None, :] + p[:, None, None] - 64)).reshape(128, 256)
    f32["cw"] = (sl_n[None, :, None] * (cend[:, None, None] - (512 * np.arange(4)[None, None, :] + 256))).reshape(128, 32)
    f32["cn"] = (sl_n[None, :, None] * (cend[:, None, None] - (128 * np.arange(16)[None, None, :] + 64))).reshape(128, 128)
    keep = np.zeros((128, 8, 32))
    force = np.zeros((128, 8, 32))
    for tt in range(8, 16):
        for pp in range(128):
            t = tt * 128 + pp
            cur = t // 64
            for jj in range(32):
                if 64 * jj > t:
                    force[pp, tt - 8, jj] = -1e30 * (1.0 + jj / 64.0)
                elif jj == 0:
                    force[pp, tt - 8, jj] = 3e9
                elif jj == cur:
                    force[pp, tt - 8, jj] = 2e9
                elif jj == cur - 1:
                    force[pp, tt - 8, jj] = 1e9
                else:
                    keep[pp, tt - 8, jj] = 1.0
    padneg = np.zeros((128, 8, 8))
    ownhot = np.zeros((128, 8, 8))
    for tt in range(8, 16):
        cur = tt // 2
        for n in range(8):
            if n >= cur:
                padneg[:, tt - 8, n] = -1e30
            if n == cur:
                ownhot[:, tt - 8, n] = 1.0
    f32["eps"] = np.full((128, 1), EPS)
    rt = []
    for tr in (2, 3):
        a = (tr - 2) * 4
        rt += [keep[:, a:a + 4].reshape(128, 128), force[:, a:a + 4].reshape(128, 128),
               padneg[:, a:a + 4].reshape(128, 32), ownhot[:, a:a + 4].reshape(128, 32)]
    f32["rt"] = np.concatenate(rt, axis=1)
    offs_f = {}
    cols = []
    o = 0
    for k, v in f32.items():
        offs_f[k] = o
        o += v.shape[1]
        cols.append(v.astype(np.float32))
    cf = np.ascontiguousarray(np.concatenate(cols, axis=1))
    offs_b = {}
    cols = []
    o = 0
    for k, v in b16.items():
        offs_b[k] = o
        o += v.shape[1]
        cols.append(v.astype(np.float32))
    cbv = np.ascontiguousarray(np.concatenate(cols, axis=1).astype(ml_dtypes.bfloat16))
    return cf, offs_f, cbv, offs_b


EMBED_WAIT = set(os.environ.get("MK_EMBED", "pe").split(",")) - {""}


def _box(ap):
    t = ap.tensor
    if t.name.startswith("ps") and len(t.name) == 3:
        return (t.name, 0, 128, 0, 512)
    row = 1
    for s in tuple(t.shape)[1:]:
        row *= int(s)
    off = int(ap.offset)
    p0 = off // row
    f0 = off % row
    apl = ap.ap
    npart = apl[0][1]
    ext = 1
    for st, cnt in apl[1:]:
        ext += (cnt - 1) * abs(st)
    return (t.name, p0, p0 + npart, f0, f0 + ext)


class Sched:
    def __init__(self, nc, ndma=24):
        self.nc = nc
        self.engs = {"pe": nc.tensor, "act": nc.scalar, "dve": nc.vector, "pool": nc.gpsimd, "sp": nc.sync}
        self.semh = {}
        for e in self.engs:
            self.semh[e] = nc.alloc_semaphore("sem_" + e)
        self.cnt = {e: 0 for e in self.engs}
        self.ndma = ndma
        for i in range(ndma):
            self.semh[("d", i)] = nc.alloc_semaphore("dsem%d" % i)
        self.dcnt = [0] * ndma
        self.drr = 0
        self.drr_pool = 0
        self.seen = {e: {} for e in self.engs}
        self.acc = {}
        self.out_tickets = []
        self.nwaits = 0
        self.marks = []

    def mark(self, name):
        self.marks.append((name, self.cnt["pe"]))

    def _wait(self, e, sk, val):
        if e == "pe" and sk == "pe":
            return
        if self.seen[e].get(sk, 0) >= val:
            return
        self.engs[e].wait_ge(self.semh[sk], val)
        self.seen[e][sk] = val
        self.nwaits += 1

    def _collect(self, reads, writes):
        waits = {}
        for ap in reads:
            b = _box(ap)
            for ent in self.acc.get(b[0], ()):
                if ent[0] == "w" and ent[3] < b[2] and b[1] < ent[4] and ent[5] < b[4] and b[3] < ent[6]:
                    if waits.get(ent[1], 0) < ent[2]:
                        waits[ent[1]] = ent[2]
        for ap in writes:
            b = _box(ap)
            for ent in self.acc.get(b[0], ()):
                if ent[3] < b[2] and b[1] < ent[4] and ent[5] < b[4] and b[3] < ent[6]:
                    if waits.get(ent[1], 0) < ent[2]:
                        waits[ent[1]] = ent[2]
        return waits

    def _record(self, sk, val, reads, writes):
        for ap in writes:
            b = _box(ap)
            lst = self.acc.setdefault(b[0], [])
            lst[:] = [e for e in lst if not (b[1] <= e[3] and e[4] <= b[2] and b[3] <= e[5] and e[6] <= b[4])]
            lst.append(("w", sk, val, b[1], b[2], b[3], b[4]))
        for ap in reads:
            b = _box(ap)
            lst = self.acc.setdefault(b[0], [])
            lst[:] = [e for e in lst if not (e[0] == "r" and e[1] == sk and b[1] <= e[3] and e[4] <= b[2]
                                             and b[3] <= e[5] and e[6] <= b[4])]
            lst.append(("r", sk, val, b[1], b[2], b[3], b[4]))

    def op(self, e, fn, reads, writes, inc=True):
        assert inc or e == "pe"
        waits = self._collect(reads, writes)
        emb = None
        if EMBED_WAIT and e in EMBED_WAIT:
            need = [(sk, val) for sk, val in waits.items()
                    if not (e == "pe" and sk == "pe") and self.seen[e].get(sk, 0) < val]
            if need:
                emb = need[-1]
                waits = dict(need[:-1])
            else:
                waits = {}
        for sk, val in waits.items():
            self._wait(e, sk, val)
        ins = fn()
        if emb is not None:
            ins._wait_ge(self.semh[emb[0]], emb[1])
            self.seen[e][emb[0]] = emb[1]
        if inc:
            self.cnt[e] += 1
            ins.then_inc(self.semh[e], 1)
            val = self.cnt[e]
        else:
            val = self.cnt[e] + 1
        self._record(e, val, reads, writes)
        return ins

    def dma(self, q, out, in_, reads=(), writes=(), is_out=False, **kw):
        waits = self._collect(reads, writes)
        for sk, val in waits.items():
            self._wait(q, sk, val)
        half = self.ndma // 2
        if q == "pool":
            i = half + (self.drr_pool % half)
            self.drr_pool += 1
        else:
            i = self.drr % half
            self.drr += 1
        sk = ("d", i)
        if self.dcnt[i] > 0:
            self._wait(q, sk, self.dcnt[i])
        ins = self.engs[q].dma_start(out=out, in_=in_, **kw)
        self.dcnt[i] += 16
        ins.then_inc(self.semh[sk], 16)
        self._record(sk, self.dcnt[i], reads, writes)
        if is_out:
            self.out_tickets.append((sk, self.dcnt[i]))

    def barrier(self):
        for e in ("pe", "act", "dve", "pool", "sp"):
            for f in ("pe", "act", "dve", "pool", "sp"):
                if f != e and self.cnt[f] > 0:
                    self._wait(e, f, self.cnt[f])
            for i in range(self.ndma):
                if self.dcnt[i] > 0:
                    self._wait(e, ("d", i), self.dcnt[i])
        self.acc = {}

    def finish(self):
        for sk, val in self.out_tickets:
            self._wait("sp", sk, val)


def build_nc(stage=99):
    nc = bass.Bass("TRN2", target_bir_lowering=False)
    cf_np, OF, cb_np, OB = _make_consts()
    NF = cf_np.shape[1]
    NB = cb_np.shape[1]

    def din(name, shape, dt=F32):
        return nc.dram_tensor(name, list(shape), dt, kind="ExternalInput").ap()

    x_d = din("x", [T, D])
    p_d = din("p", [T, 256])
    wd = {}
    for nm, shp in [("ffn1_w1", [D, FF]), ("ffn1_w3", [D, FF]), ("ffn1_w2", [FF, D]), ("w_in", [D, IN_W]),
                    ("cmp_w1_k", [2048, 128]), ("cmp_w2_k", [128, 64]), ("cmp_w1_v", [2048, 128]),
                    ("cmp_w2_v", [128, 64]), ("pos_kT", [128, 32]), ("pos_vT", [128, 32]),
                    ("w_up_nsa", [512, D]), ("w_up_moba", [512, D]), ("w_out", [D, D]),
                    ("ffn2_w1", [D, FF]), ("ffn2_w3", [D, FF]), ("ffn2_w2", [FF, D]),
                    ("w_ple_gate", [D, D]), ("w_ple", [256, D]), ("gains", [128, 32]), ("gfin", [128, D])]:
        wd[nm] = din(nm, shp)
    cf_d = din("cf32", [128, NF])
    cb_d = din("cbf16", [128, NB], BF16)
    out_d = nc.dram_tensor("out", [T, D], F32, kind="ExternalOutput").ap()

    S = Sched(nc)
    stack = [None]

    def TS(name, shape, dt):
        if stack[0] is None:
            return nc.alloc_sbuf_tensor(name, shape, dt)
        return stack[0].enter_context(nc.sbuf_tensor(name, shape, dt))

    def scoped(fn, *a):
        if PLAN[0]:
            fn(*a)
            return
        S.barrier()
        with ExitStack() as st:
            stack[0] = st
            fn(*a)
            S.barrier()
        stack[0] = None

    hT = TS("hT", [128, 8, T], F32)
    NFS = OF["rt"]
    NBS = OB["cmpmask"]
    cf = TS("cf", [128, NFS], F32)
    cb = TS("cb", [128, NBS], BF16)
    gains = TS("gains_sb", [128, 32], F32)
    slab = [TS("slab%d" % i, [128, 4096], BF16) for i in range(3)]
    ps = [nc.alloc_psum_tensor("ps%d" % i, [128, 512], F32) for i in range(8)]

    identf = cf[:, OF["ident"]:OF["ident"] + 128]
    identb = cb[:, OB["ident"]:OB["ident"] + 128]
    onesb = cb[:, OB["ones"]:OB["ones"] + 128]
    epsc = cf[:, OF["eps"]:OF["eps"] + 1]

    S.dma("sp", cf[:, :], cf_d[:, 0:NFS], writes=[cf[:, :]])
    S.dma("sp", cb[:, :], cb_d[:, 0:NBS], writes=[cb[:, :]])
    S.dma("sp", gains[:, :], wd["gains"][:, :], writes=[gains[:, :]])

    def mm(out, lhsT, rhs, start, stop, inc=None):
        inc = True
        return S.op("pe", lambda: nc.tensor.matmul(out, lhsT, rhs, start=start, stop=stop),
                    [lhsT, rhs], [out], inc=inc)

    def pe_drain():
        if S.cnt["pe"] > 0:
            nc.tensor.wait_ge(S.semh["pe"], S.cnt["pe"])

    def tp(out, in_, ident):
        return S.op("pe", lambda: nc.tensor.transpose(out, in_, ident), [in_, ident], [out])

    def act(out, in_, func, bias=None, scale=None, accum_out=None):
        kw = {}
        rd = [in_]
        wr = [out]
        if bias is not None:
            kw["bias"] = bias
            if not isinstance(bias, (int, float)):
                rd.append(bias)
        if scale is not None:
            kw["scale"] = scale
            if not isinstance(scale, (int, float)):
                rd.append(scale)
        if accum_out is not None:
            kw["accum_out"] = accum_out
            wr.append(accum_out)
        return S.op("act", lambda: nc.scalar.activation(out=out, in_=in_, func=func, **kw), rd, wr)

    def tt(out, in0, in1, op, e="dve"):
        eng = nc.vector if e == "dve" else nc.gpsimd
        return S.op(e, lambda: eng.tensor_tensor(out=out, in0=in0, in1=in1, op=op), [in0, in1], [out])

    def ts(out, in0, s1, s2, op0, op1=None, e="dve"):
        eng = nc.vector if e == "dve" else nc.gpsimd
        rd = [in0]
        if not isinstance(s1, (int, float)):
            rd.append(s1)
        if s2 is not None and not isinstance(s2, (int, float)):
            rd.append(s2)
        if op1 is None:
            return S.op(e, lambda: eng.tensor_scalar(out=out, in0=in0, scalar1=s1, scalar2=None, op0=op0), rd, [out])
        return S.op(e, lambda: eng.tensor_scalar(out=out, in0=in0, scalar1=s1, scalar2=s2, op0=op0, op1=op1),
                    rd, [out])

    def stt(out, in0, scalar, in1, op0, op1):
        rd = [in0, in1]
        if not isinstance(scalar, (int, float)):
            rd.append(scalar)
        return S.op("dve", lambda: nc.vector.scalar_tensor_tensor(out=out, in0=in0, scalar=scalar, in1=in1,
                                                                   op0=op0, op1=op1), rd, [out])

    def cp(out, in_, e="dve"):
        if e == "act":
            return S.op("act", lambda: nc.scalar.copy(out=out, in_=in_), [in_], [out])
        eng = nc.vector if e == "dve" else nc.gpsimd
        return S.op(e, lambda: eng.tensor_copy(out=out, in_=in_), [in_], [out])

    def memset(ap, v, e="dve"):
        eng = nc.vector if e == "dve" else nc.gpsimd
        return S.op(e, lambda: eng.memset(ap, v), [], [ap])

    class RR:
        def __init__(self, items):
            self.items = items
            self.i = 0

        def next(self):
            it = self.items[self.i % len(self.items)]
            self.i += 1
            return it

    class WStream:
        def __init__(self):
            self.jobs = []
            self.issued = 0
            self.k = 0

        def add(self, parts):
            self.jobs.append(parts)

        def _issue(self, k):
            buf = slab[k % 3]
            for (off, shape, src) in self.jobs[k]:
                n = 1
                for s_ in shape:
                    n *= s_
                npart = int(src.shape[0])
                dst = buf[0:npart, off:off + n]
                if len(shape) == 2:
                    dst = dst.rearrange("p (a b) -> p a b", a=shape[0])
                S.dma("pool", dst, src, writes=[buf[0:npart, off:off + n]])

        def get(self):
            k = self.k
            self.k += 1
            while self.issued < min(k + 2, len(self.jobs)):
                self._issue(self.issued)
                self.issued += 1
            return slab[k % 3]

    WS = WStream()
    PLAN = [True]

    def W(parts):
        if PLAN[0]:
            WS.add(parts)
            return None
        return WS.get()

    def trs(tr):
        return slice(tr * 512, (tr + 1) * 512)

    sqb = [TS("sqb%d" % i, [128, 512], BF16) for i in range(2)]
    rstd = TS("rstd", [128, 512], F32)
    sqrr = RR(sqb)
    psM = RR([ps[6], ps[7]])

    def rms_range(tr, gidx, xn3):
        pb = psM.next()
        for c in range(8):
            sq = sqrr.next()
            act(sq[:, :], hT[:, c, trs(tr)], AF.Square)
            mm(pb[:, :], onesb, sq[:, :], c == 0, c == 7)
        act(rstd[:, :], pb[:, :], AF.Ln, bias=epsc, scale=1.0 / D)
        act(rstd[:, :], rstd[:, :], AF.Exp, scale=-0.5)
        for c in range(8):
            stt(xn3[:, c, :], hT[:, c, trs(tr)], gains[:, gidx * 8 + c:gidx * 8 + c + 1], rstd[:, :],
                ALU.mult, ALU.mult)

    def phase_load():
        xtok = [TS("xtok%d" % i, [128, D], F32) for i in range(2)]
        for t_ in range(16):
            xb = xtok[t_ % 2]
            S.dma("sp", xb[:, :], x_d[t_ * 128:(t_ + 1) * 128, :], writes=[xb[:, :]])
            for half in range(2):
                pb = psM.next()
                for c4 in range(4):
                    c = half * 4 + c4
                    tp(pb[:, c4 * 128:(c4 + 1) * 128], xb[:, c * 128:(c + 1) * 128], identf)
                cp(hT[:, half * 4:(half + 1) * 4, t_ * 128:(t_ + 1) * 128],
                   pb[:, :].rearrange("p (c t) -> p c t", c=4), e="act" if half else "dve")

    def phase_ffn(w1n, w3n, w2n, gidx, tag):
        w1 = wd[w1n].rearrange("(c p) f -> p c f", p=128)
        w3 = wd[w3n].rearrange("(c p) f -> p c f", p=128)
        w2 = wd[w2n]
        if not PLAN[0]:
            xnT = TS("xnT" + tag, [128, 8, T], BF16)
            h1 = [TS("h1%s%d" % (tag, i), [128, 2, 512], BF16) for i in range(2)]
            sa = [TS("sa%s%d" % (tag, i), [128, 512], F32) for i in range(2)]
            for tr in range(4):
                rms_range(tr, gidx, xnT[:, :, trs(tr)])
            psAB = RR([ps[0], ps[1], ps[2], ps[3]])
            psY = RR([ps[4], ps[5], ps[6], ps[7]])
            sar = RR(sa)
            h1r = RR(h1)
            ytmp = [TS("ytmp%s%d" % (tag, i), [128, 512], F32) for i in range(2)]
            ytr = RR(ytmp)
        Aj = {}
        Bj = {}

        def getA(s_):
            if s_ not in Aj:
                Aj[s_] = W([(0, [8, 256], w1[:, :, s_ * 256:(s_ + 1) * 256]),
                            (2048, [8, 256], w3[:, :, s_ * 256:(s_ + 1) * 256])])
            return Aj[s_]

        def getB(s_):
            if s_ not in Bj:
                Bj[s_] = W([(0, [2, 1024], w2[s_ * 256:(s_ + 1) * 256, :].rearrange("(j p) d -> p j d", p=128))])
            return Bj[s_]

        def ab(u):
            s_, tr = u
            A = getA(s_)
            if PLAN[0]:
                return None
            w1s = A[:, 0:2048].rearrange("p (c f) -> p c f", c=8)
            w3s = A[:, 2048:4096].rearrange("p (c f) -> p c f", c=8)
            hb = h1r.next()
            for j in range(2):
                pa = psAB.next()
                pb = psAB.next()
                for c in range(8):
                    mm(pa[:, :], w1s[:, c, j * 128:(j + 1) * 128], xnT[:, c, trs(tr)], c == 0, c == 7)
                for c in range(8):
                    mm(pb[:, :], w3s[:, c, j * 128:(j + 1) * 128], xnT[:, c, trs(tr)], c == 0, c == 7)
                sab = sar.next()
                act(sab[:, :], pa[:, :], AF.Silu)
                tt(hb[:, j, :], sab[:, :], pb[:, :], ALU.mult)
            return hb

        def yy(u, hb):
            s_, tr = u
            B = getB(s_)
            if PLAN[0]:
                return
            w2s = B[:, 0:2048].rearrange("p (j d) -> p j d", j=2)
            for dc in range(8):
                py = psY.next()
                for j in range(2):
                    mm(py[:, :], w2s[:, j, dc * 128:(dc + 1) * 128], hb[:, j, :], j == 0, j == 1)
                stt(hT[:, dc, trs(tr)], py[:, :], 0.5, hT[:, dc, trs(tr)], ALU.mult, ALU.add)

        units = [(s_, tr) for s_ in range(11) for tr in range(4)]
        prev = None
        for u in units:
            hb = ab(u)
            if prev is not None:
                yy(prev[0], prev[1])
            prev = (u, hb)
        yy(prev[0], prev[1])

    def phase_ple():
        wg = wd["w_ple_gate"].rearrange("(c p) f -> p c f", p=128)
        wp = wd["w_ple"].rearrange("(c p) f -> p c f", p=128)
        if not PLAN[0]:
            xnT = TS("xnTple", [128, 8, T], BF16)
            pT = TS("pT", [128, 2, T], BF16)
            ptok = [TS("ptok%d" % i, [128, 256], F32) for i in range(2)]
            sg = [TS("sgple%d" % i, [128, 512], F32) for i in range(2)]
            for t_ in range(16):
                pbuf = ptok[t_ % 2]
                S.dma("sp", pbuf[:, :], p_d[t_ * 128:(t_ + 1) * 128, :], writes=[pbuf[:, :]])
                pb = psM.next()
                for c in range(2):
                    tp(pb[:, c * 128:(c + 1) * 128], pbuf[:, c * 128:(c + 1) * 128], identf)
                cp(pT[:, :, t_ * 128:(t_ + 1) * 128], pb[:, 0:256].rearrange("p (c t) -> p c t", c=2))
            for tr in range(4):
                rms_range(tr, 3, xnT[:, :, trs(tr)])
            psG = RR([ps[0], ps[1], ps[2], ps[3]])
            sgr = RR(sg)
        for dh in range(2):
            A = W([(0, [8, 512], wg[:, :, dh * 512:(dh + 1) * 512])])
            B = W([(0, [2, 512], wp[:, :, dh * 512:(dh + 1) * 512])])
            if PLAN[0]:
                continue
            wgs = A[:, 0:4096].rearrange("p (c f) -> p c f", c=8)
            wps = B[:, 0:1024].rearrange("p (c f) -> p c f", c=2)
            for tr in range(4):
                for d4 in range(4):
                    dc = dh * 4 + d4
                    pg = psG.next()
                    pp = psG.next()
                    for c in range(8):
                        mm(pg[:, :], wgs[:, c, d4 * 128:(d4 + 1) * 128], xnT[:, c, trs(tr)], c == 0, c == 7)
                    for c in range(2):
                        mm(pp[:, :], wps[:, c, d4 * 128:(d4 + 1) * 128], pT[:, c, trs(tr)], c == 0, c == 1)
                    sgb = sgr.next()
                    act(sgb[:, :], pg[:, :], AF.Sigmoid)
                    tt(sgb[:, :], sgb[:, :], pp[:, :], ALU.mult)
                    tt(hT[:, dc, trs(tr)], hT[:, dc, trs(tr)], sgb[:, :], ALU.add)

    def phase_final():
        gfin = TS("gfin_sb", [128, D], F32)
        S.dma("sp", gfin[:, :], wd["gfin"][:, :], writes=[gfin[:, :]])
        otok = [TS("otok%d" % i, [128, D], F32) for i in range(2)]
        junk = TS("junk", [128, 512], F32)
        ssq = TS("ssq", [128, 4], F32)
        psF = RR([ps[0], ps[1], ps[2], ps[3], ps[4], ps[5]])
        for t_ in range(16):
            ob = otok[t_ % 2]
            pbs = [psF.next(), psF.next()]
            for half in range(2):
                for c4 in range(4):
                    c = half * 4 + c4
                    tp(pbs[half][:, c4 * 128:(c4 + 1) * 128], hT[:, c, t_ * 128:(t_ + 1) * 128], identf)
                act(junk[:, :], pbs[half][:, :], AF.Square, accum_out=ssq[:, half:half + 1])
            tt(ssq[:, 2:3], ssq[:, 0:1], ssq[:, 1:2], ALU.add)
            act(ssq[:, 3:4], ssq[:, 2:3], AF.Ln, bias=epsc, scale=1.0 / D)
            act(ssq[:, 3:4], ssq[:, 3:4], AF.Exp, scale=-0.5)
            for half in range(2):
                stt(ob[:, half * 512:(half + 1) * 512], pbs[half][:, :], ssq[:, 3:4],
                    gfin[:, half * 512:(half + 1) * 512], ALU.mult, ALU.mult)
            S.dma("sp", out_d[t_ * 128:(t_ + 1) * 128, :], ob[:, :], reads=[ob[:, :]], is_out=True)

    def phase_mixer():
        win = wd["w_in"].rearrange("(c p) f -> p c f", p=128)
        wupn = wd["w_up_nsa"].rearrange("(c p) f -> p c f", p=128)
        wupm = wd["w_up_moba"].rearrange("(c p) f -> p c f", p=128)
        wout = wd["w_out"].rearrange("(c p) f -> p c f", p=128)
        sl_n, sl_m = _slopes()
        if not PLAN[0]:
            kslcT = TS("kslcT", [128, T], BF16)
            kwinT = TS("kwinT", [128, T], BF16)
            kcmpT = TS("kcmpT", [128, 528], BF16)
            vcmpT = TS("vcmpT", [128, 528], BF16)
            kmT = TS("kmT", [128, 4, T], BF16)
            vslc = TS("vslc", [128, 16, 2, 65], BF16)
            vwin = TS("vwin", [128, 16, 2, 65], BF16)
            vm = TS("vm", [128, 16, 8, 65], BF16)
            gsig = TS("gsig", [128, 16, 24], F32)
            kcT = TS("kcT", [128, 128], BF16)
            vca = TS("vca", [128, 2, 97], BF16)
            kmeanT = TS("kmeanT", [128, 4, 16], BF16)
            kmsum = TS("kmsum", [128, 4, 2], F32)
            w2k = TS("w2k", [128, 128], BF16)
            w2v = TS("w2v", [128, 64], BF16)
            posk = TS("posk", [128, 32], BF16)
            posv = TS("posv", [128, 32], BF16)
            cbias = TS("cbias", [128, 2], F32)
            u_tr = TS("u_tr", [128, 8, 512], BF16)
            qmix = TS("qmix", [128, 8, 512], BF16)

            class _Sub:
                def __init__(self, base, off):
                    self.base, self.off = base, off

                def __getitem__(self, key):
                    a, b, c = key
                    if isinstance(b, int):
                        b = b + self.off
                    else:
                        b = slice((b.start or 0) + self.off, (b.stop if b.stop is not None else 4) + self.off)
                    return self.base[a, b, c]
            qn = _Sub(qmix, 0)
            qm = _Sub(qmix, 4)
            ptl = [TS("ptl%d" % i, [128, 512], BF16) for i in range(3)]
            otk = [TS("otk%d" % i, [128, 4, 4, 64], F32) for i in range(2)]
            onT = TS("onT", [128, 4, 512], BF16)
            omT = TS("omT", [128, 4, 512], BF16)
            mixed = qmix
            sgA = [TS("sgA%d" % i, [128, 512], F32) for i in range(1)]
            sgB = [TS("sgB%d" % i, [128, 512], F32) for i in range(1)]
            mbTn = TS("mbTn", [128, 2, 512], BF16)
            qz = [TS("qz%d" % i, [128, 512], BF16) for i in range(4)]
            qzc = [0, 0]
            mbTm = TS("mbTm", [128, 512], BF16)
            mbtokN = TS("mbtokN", [128, 128], F32)
            mbtok = TS("mbtok", [128, 128], F32)
            impa = TS("impa", [128, 4, 2, 32], F32)
            smal = TS("smal", [128, 64], F32)
            m8 = TS("m8", [128, 16], F32)
            tk32 = TS("tk32", [128, 64], F32)
            cmsk = TS("cmsk", [128, 512], BF16)
            rtab = TS("rtab", [128, 320], F32)
            esel = TS("esel", [128, 16, 128], BF16)
            S.dma("sp", esel[:, :, :], cb_d[:, OB["esel"]:OB["esel"] + 2048].rearrange("p (k m) -> p k m", k=16),
                  writes=[esel[:, :, :]])
            hid = TS("hid", [128, 4, 32], F32)
            hidb = TS("hidb", [128, 2, 32], BF16)
            hidv = TS("hidv", [128, 2, 128], BF16)
            gtmp = TS("gtmp", [64, 528], BF16)
            gsf = TS("gsf", [128, 8, 16], F32)

            memset(kcT[:, :], 0.0)
            for i in range(4):
                memset(qz[i][:, :], 0.0)
            memset(mbtokN[:, :], 0.0)
            memset(mbtok[:, :], 0.0)
            memset(gsf[:, :, :], -1.0e30)
            memset(kcmpT[:, 0:16], 0.0)
            memset(vcmpT[:, 0:16], 0.0)
            memset(hidv[:, :, :], 0.0)
            memset(vca[:, :, :], 0.0)
            memset(kmeanT[:, :, :], 0.0)
            memset(vslc[:, :, :, 64:65], 1.0)
            memset(vwin[:, :, :, 64:65], 1.0)
            memset(vm[:, :, :, 64:65], 1.0)
            memset(vca[:, :, 64:65], 1.0)
            for g in range(2):
                cp(vca[:, g, 65:97], cb[:, OB["overlap"]:OB["overlap"] + 32])
            for (w2t, w2n_, pt, pn) in ((w2k, "cmp_w2_k", posk, "pos_kT"), (w2v, "cmp_w2_v", posv, "pos_vT")):
                S.dma("pool", pt[:, :], wd[pn][:, :], writes=[pt[:, :]])
                if w2t is w2k:
                    S.dma("pool", w2t[:, 0:64], wd[w2n_][:, :], writes=[w2t[:, 0:64]])
                    S.dma("pool", w2t[:, 64:128], wd[w2n_][:, :], writes=[w2t[:, 64:128]])
                else:
                    S.dma("pool", w2t[:, :], wd[w2n_][:, :], writes=[w2t[:, :]])

            psS = RR([ps[0], ps[1], ps[2]])
            psO = RR([ps[3], ps[4], ps[5]])
            ptr = RR(ptl)
            otr = RR(otk)
            sgAr = RR(sgA)
            sgBr = RR(sgB)

        def proj_fm(ws, c0, dst, tr, base=None, ev="act"):
            pb = psM.next()
            for c in range(8):
                if base is None:
                    l_ = ws[:, c, c0:c0 + 128]
                else:
                    l_ = base(c)
                mm(pb[:, :], l_, u_tr[:, c, :], c == 0, c == 7)
            cp(dst, pb[:, :], e=ev)

        NTRS = int(os.environ.get("MK_NTR", "4"))
        SUB = int(os.environ.get("MK_SUB", "9"))
        for tr in range(NTRS):
            S0 = W([(0, [8, 512], win[:, :, 0:512])])
            if not PLAN[0]:
                S.mark("tr%d proj" % tr)
                rms_range(tr, 1, u_tr)
                w0 = S0[:, 0:4096].rearrange("p (c f) -> p c f", c=8)
                for i in range(4):
                    proj_fm(w0, i * 128, qn[:, i, :], tr, ev="act" if i % 2 else "dve")
            S1 = W([(0, [8, 512], win[:, :, 512:1024])])
            if not PLAN[0]:
                w1_ = S1[:, 0:4096].rearrange("p (c f) -> p c f", c=8)
                proj_fm(w1_, 0, kcmpT[:, 16:528], tr, ev="act")
                proj_fm(w1_, 128, vcmpT[:, 16:528], tr, ev="dve")
                proj_fm(w1_, 256, kslcT[:, trs(tr)], tr, ev="act")
                for j in range(4):
                    t_ = tr * 4 + j
                    pb = psM.next()
                    for c in range(8):
                        mm(pb[:, 0:128], u_tr[:, c, j * 128:(j + 1) * 128], w1_[:, c, 384:512], c == 0, c == 7)
                    cp(vslc[:, t_, :, 0:64], pb[:, 0:128].rearrange("p (g d) -> p g d", g=2))
            S2 = W([(0, [8, 280], win[:, :, 1024:1304])])
            if not PLAN[0]:
                w2_ = S2[:, 0:2240].rearrange("p (c f) -> p c f", c=8)
                proj_fm(w2_, 0, kwinT[:, trs(tr)], tr, ev="act")
                for j in range(4):
                    t_ = tr * 4 + j
                    pb = psM.next()
                    for c in range(8):
                        mm(pb[:, 0:152], u_tr[:, c, j * 128:(j + 1) * 128], w2_[:, c, 128:280], c == 0, c == 7)
                    cp(vwin[:, t_, :, 0:64], pb[:, 0:128].rearrange("p (g d) -> p g d", g=2))
                    act(gsig[:, t_, :], pb[:, 128:152], AF.Sigmoid)
            S3 = W([(0, [8, 512], win[:, :, 1304:1816])])
            if not PLAN[0]:
                w3_ = S3[:, 0:4096].rearrange("p (c f) -> p c f", c=8)
                for i in range(4):
                    proj_fm(w3_, i * 128, qm[:, i, :], tr, ev="act" if i % 2 else "dve")
            S4 = W([(0, [8, 512], win[:, :, 1816:2328])])
            if not PLAN[0]:
                w4_ = S4[:, 0:4096].rearrange("p (c f) -> p c f", c=8)
                for i in range(4):
                    proj_fm(w4_, i * 128, kmT[:, i, trs(tr)], tr, ev="act" if i % 2 else "dve")
                for i in range(4):
                    S.op("dve", lambda: nc.vector.tensor_reduce(
                        out=kmsum[:, i, :], in_=kmT[:, i, trs(tr)].rearrange("p (n k) -> p n k", n=2),
                        axis=mybir.AxisListType.X, op=ALU.add),
                        [kmT[:, i, trs(tr)]], [kmsum[:, i, :]])
                ts(kmeanT[0:64, :, 2 * tr:2 * tr + 2], kmsum[0:64, :, :], 1.0 / 256.0, None, ALU.mult)
                ts(kmeanT[64:128, :, 8 + 2 * tr:8 + 2 * tr + 2], kmsum[64:128, :, :], 1.0 / 256.0, None, ALU.mult)
            S5 = W([(0, [8, 512], win[:, :, 2328:2840])])
            if not PLAN[0]:
                w5_ = S5[:, 0:4096].rearrange("p (c f) -> p c f", c=8)
                for j in range(4):
                    t_ = tr * 4 + j
                    pb = psM.next()
                    for c in range(8):
                        mm(pb[:, :], u_tr[:, c, j * 128:(j + 1) * 128], w5_[:, c, :], c == 0, c == 7)
                    cp(vm[:, t_, :, 0:64], pb[:, :].rearrange("p (h d) -> p h d", h=8), e="act" if j % 2 else "dve")

            if SUB < 1:
                continue
            if not PLAN[0]:
                S.mark("tr%d compress" % tr)
            c_lo = 0 if tr == 0 else 32 * tr - 1
            c_hi = 32 * tr + 30
            nb = c_hi - c_lo + 1
            if not PLAN[0]:
                pe_drain()
            for which in range(2):
                wsrc = wd["cmp_w1_k" if which == 0 else "cmp_w1_v"].rearrange("(l d) h -> d l h", d=64)
                CW = W([(q4 * 1024, [8, 128], wsrc[:, q4 * 8:(q4 + 1) * 8, :]) for q4 in range(4)])
                if PLAN[0]:
                    continue
                srcT = kcmpT if which == 0 else vcmpT
                w1t = CW[0:64, 0:4096].rearrange("p (l h) -> p l h", l=32)
                if tr == 0:
                    pt = posk if which == 0 else posv
                    pb = psM.next()
                    for l in range(32):
                        mm(pb[:, 0:1], w1t[:, l, :], pt[0:64, l:l + 1], l == 0, l == 31)
                    cp(cbias[:, which:which + 1], pb[:, 0:1])
                for g in range(2):
                    if g == 1:
                        cp(gtmp[0:64, 0:528], srcT[64:128, 0:528])
                        src_t = gtmp
                    else:
                        src_t = srcT
                    pbA = ps[which * 2 + g]
                    for l in range(32):
                        col0 = 16 * c_lo + l - 512 * tr + 16
                        rhs_ = bass.AP(src_t, col0, [[528, 64], [16, nb]])
                        mm(pbA[:, 0:nb], w1t[:, l, :], rhs_, l == 0, l == 31)
                cp(srcT[:, 0:16], srcT[:, 512:528])
            if not PLAN[0]:
                pe_drain()
                for which in range(2):
                    for g in range(2):
                        pbA = ps[which * 2 + g]
                        xx = hid[:, 0, 0:nb]
                        x2 = hid[:, 1, 0:nb]
                        ts(xx, pbA[:, 0:nb], cbias[:, which:which + 1], None, ALU.add)
                        tt(x2, xx, xx, ALU.mult)
                        ts(x2, x2, 0.044715, 1.0, ALU.mult, ALU.add)
                        tt(x2, x2, xx, ALU.mult)
                        act(hid[:, 2, 0:nb], x2, AF.Sigmoid, scale=1.5957691216057308)
                        if which == 0:
                            tt(hidb[:, g, 0:nb], xx, hid[:, 2, 0:nb], ALU.mult)
                        else:
                            tt(hidv[:, g, c_lo:c_lo + nb], xx, hid[:, 2, 0:nb], ALU.mult)
            if not PLAN[0]:
                S.dma("sp", cmsk[:, :], cb_d[:, OB["cmpmask"] + tr * 512:OB["cmpmask"] + (tr + 1) * 512],
                      writes=[cmsk[:, :]])
                if tr >= 2:
                    S.dma("sp", rtab[:, :], cf_d[:, OF["rt"] + (tr - 2) * 320:OF["rt"] + (tr - 1) * 320],
                          writes=[rtab[:, :]])

            if SUB < 2:
                continue
            def attend_multi(jobs):
                items = []
                for jb in jobs:
                    jb["npv"] = 0
                    jb["npv_total"] = sum((c1 - c0) // 128 for (kt, c0, c1, extra) in jb["tiles"])
                    for t_ in jb["tiles"]:
                        items.append((jb, t_))

                def emit_scores(idx):
                    jb, (kt, c0, c1, extra) = items[idx]
                    if "q" not in jb:
                        jb["q"] = jb["qfn"]()
                        jb["ops"] = psO.next()
                    sp_ = psS.next()
                    mask_fn = jb.get("mask")
                    n_extra = len(extra) + (1 if mask_fn is not None else 0)
                    mm(sp_[:, c0:c1], jb["kT"](kt), jb["q"][:, c0:c1], True, n_extra == 0)
                    k_ = 0
                    if mask_fn is not None:
                        k_ += 1
                        ml, mr = mask_fn(kt)
                        mm(sp_[:, c0:c1], ml, mr[:, c0:c1], False, k_ == n_extra)
                    for (e0, en, el, er) in extra:
                        k_ += 1
                        mm(sp_[:, e0:e0 + en], el, er, False, k_ == n_extra)
                    return sp_

                DEPTH = 2
                pend = [emit_scores(i) for i in range(min(DEPTH, len(items)))]
                for ti in range(len(items)):
                    jb, (kt, c0, c1, extra) = items[ti]
                    sp_ = pend.pop(0)
                    if ti + DEPTH < len(items):
                        pend.append(emit_scores(ti + DEPTH))
                    pt = ptr.next()
                    if jb["narrow"]:
                        for j in range(c0 // 128, c1 // 128):
                            act(pt[:, j * 128:(j + 1) * 128], sp_[:, j * 128:(j + 1) * 128], AF.Exp,
                                bias=jb["bias"](kt, j), scale=0.125)
                    else:
                        act(pt[:, c0:c1], sp_[:, c0:c1], AF.Exp, bias=jb["bias"](kt, None), scale=0.125)
                    ncols = jb.get("ncols", 65)
                    for j in range(c0 // 128, c1 // 128):
                        jb["npv"] += 1
                        mm(jb["ops"][:, j * 128:j * 128 + ncols], pt[:, j * 128:(j + 1) * 128], jb["v"](kt),
                           jb["npv"] == 1, jb["npv"] == jb["npv_total"])
                    if jb["npv"] == jb["npv_total"]:
                        jb["done"](jb["ops"])

            triA = cb[:, OB["triA"]:OB["triA"] + 128]
            triB = cb[:, OB["triB"]:OB["triB"] + 128]

            def _jhi(kt, tr_, dmax):
                return min(3, (dmax + 126) // 128 + kt - 4 * tr_)

            def causal_tiles(tr_, dmax=1 << 30):
                tl = []
                for kt in range(0, 4 * tr_ + 4):
                    i = kt - 4 * tr_
                    j_lo = max(0, i)
                    j_hi = _jhi(kt, tr_, dmax)
                    if j_hi < j_lo:
                        continue
                    extra = [(128 * i, 128, identb, triA)] if i >= 0 else []
                    tl.append((kt, 128 * j_lo, 128 * (j_hi + 1), extra))
                return tl

            def win_tiles(tr_, dmax=1 << 30):
                tl = []
                for kt in range(4 * tr_ - 4, 4 * tr_ + 4):
                    if kt < 0:
                        continue
                    i = kt - 4 * tr_
                    j_hi = _jhi(kt, tr_, dmax)
                    if i < 0:
                        i2 = i + 4
                        jh = min(i2, j_hi)
                        if jh < 0:
                            continue
                        extra = [(128 * i2, 128, identb, triB)] if jh == i2 else []
                        tl.append((kt, 0, 128 * (jh + 1), extra))
                    else:
                        if j_hi < i:
                            continue
                        tl.append((kt, 128 * i, 128 * (j_hi + 1), [(128 * i, 128, identb, triA)]))
                return tl

            def dmax_of(slope):
                return int(np.ceil(64.0 / float(slope)))

            if not PLAN[0]:
                def bias_head(hidx, narrow):
                    def f(kt, j):
                        if narrow:
                            di = kt - (4 * tr + j) + 15
                            o_ = OF["bn"] + hidx * 16 + di
                        else:
                            di = kt - 4 * tr + 12
                            o_ = OF["bw"] + hidx * 16 + di
                        return cf[:, o_:o_ + 1]
                    return f

                def bias_cmp(h, narrow):
                    def f(kt, j):
                        if narrow:
                            o_ = OF["cn"] + h * 16 + (4 * tr + j)
                        else:
                            o_ = OF["cw"] + h * 4 + tr
                        return cf[:, o_:o_ + 1]
                    return f

                def norm_coef(ob_, gate_ap):
                    ov_ = ob_[:, :].rearrange("p (j c) -> p j c", j=4)
                    ts(smal[:, 0:4], ov_[:, :, 64], TINY, None, ALU.max)
                    S.op("dve", lambda: nc.vector.reciprocal(out=smal[:, 4:8], in_=smal[:, 0:4]),
                         [smal[:, 0:4]], [smal[:, 4:8]])
                    if gate_ap is not None:
                        tt(smal[:, 8:12], smal[:, 4:8], gate_ap, ALU.mult)
                    return ov_

                def qpad(src3, ch, bp, eng):
                    half = bp // 64
                    b = qz[2 * half + (qzc[half] % 2)]
                    qzc[half] += 1
                    cp(b[bp:bp + 64, :], src3[bp:bp + 64, ch, :], e=eng)
                    return b

            if not PLAN[0]:
                S.mark("tr%d moba" % tr)
                MDBG = int(os.environ.get("MK_MOBA", "3"))
                if tr >= 2 and MDBG >= 1:
                    for j in range(4):
                        pb = psM.next()
                        for ch in range(4):
                            mm(pb[:, ch * 16:(ch + 1) * 16], qm[:, ch, j * 128:(j + 1) * 128],
                               kmeanT[:, ch, :], True, True)
                        GS = int(os.environ.get("MK_GS", "9"))
                        if GS >= 1:
                            tt(gsf[:, :, 0:8], pb[:, 0:64].rearrange("p (h n) -> p h n", h=8),
                               bass.AP(rtab, 256 + j * 8, [[320, 128], [0, 8], [1, 8]]), ALU.add)
                        if GS >= 2:
                            for h in range(8):
                                S.op("dve", lambda: nc.vector.max(out=m8[:, 0:8], in_=gsf[:, h, :]),
                                     [gsf[:, h, :]], [m8[:, 0:8]])
                                ts(tk32[:, h * 8:(h + 1) * 8], gsf[:, h, 0:8], m8[:, 2:3], None, ALU.is_ge)
                        if GS >= 3:
                            tt(tk32[:, :].rearrange("p (h n) -> p h n", h=8), tk32[:, :].rearrange("p (h n) -> p h n", h=8),
                               bass.AP(rtab, 288 + j * 8, [[320, 128], [0, 8], [1, 8]]), ALU.add)
                            ts(mbtok[:, 0:64], tk32[:, :], -1.0, -MASKV, ALU.add, ALU.mult)
                        if GS >= 4:
                            pb2 = psM.next()
                            tp(pb2[:, 0:128], mbtok[:, :], identf)
                            cp(mbTm[:, j * 128:(j + 1) * 128], pb2[:, 0:128])
                jobs = []
                for hp in range(4):
                    ot = otr.next()
                    for h2 in range(2):
                        h = hp * 2 + h2
                        ch, bp = hp, 64 * h2
                        nar = bool(sl_m[h] > 0.3)
                        mfn = None
                        if tr >= 2 and MDBG >= 2:
                            def mfn(kt, h=h):
                                c_ = 8 * h + kt // 2
                                l_ = bass.AP(cb, OB["ident"] + c_, [[NBS, 128], [0, 128]])
                                return l_, mbTm[:, :]

                        def done(om, ot=ot, h2=h2, hp=hp):
                            ov_ = norm_coef(om, None)
                            for j in range(4):
                                ts(ot[:, j, h2, :], ov_[:, j, 0:64], smal[:, 4 + j:5 + j], None, ALU.mult)
                            if h2 == 1:
                                pb = psM.next()
                                for j in range(4):
                                    tp(pb[:, j * 128:(j + 1) * 128], ot[:, j, 0:2, :], identf)
                                cp(omT[:, hp, :], pb[:, :], e="act")
                        jobs.append(dict(kT=lambda kt, ch=ch: kmT[:, ch, kt * 128:(kt + 1) * 128],
                                         qfn=lambda ch=ch, bp=bp: qpad(qm, ch, bp, "dve"),
                                         v=lambda kt, h=h: vm[:, kt, h, :],
                                         tiles=causal_tiles(tr, dmax_of(sl_m[h])), bias=bias_head(8 + h, nar),
                                         narrow=nar, mask=mfn, done=done))
                attend_multi(jobs)

            if not PLAN[0]:
                for g in range(2):
                    pb2 = psM.next()
                    mm(pb2[:, 0:nb], w2k[:, :], hidb[:, g, 0:nb], True, True)
                    cp(kcT[64 * g:64 * g + 64, c_lo:c_lo + nb], pb2[64 * g:64 * g + 64, 0:nb])
                    pb3 = psM.next()
                    mm(pb3[:, 0:64], hidv[:, g, :], w2v[:, :], True, True)
                    cp(vca[:, g, 0:64], pb3[:, 0:64])
                S.mark("tr%d nsa-cmp" % tr)
                ots = [otk[0], otk[1]]
            if SUB < 3:
                continue
            if not PLAN[0]:
                jobs = []
                for g in range(2):
                    ot = ots[g]
                    for hg in range(4):
                        h = g * 4 + hg
                        ch, bp = h % 4, 64 * (h // 4)
                        nar = bool(sl_n[h] > 0.3)

                        def done(oc, ot=ot, hg=hg, g=g, h=h):
                            ocv = norm_coef(oc, gsig[:, 4 * tr:4 * tr + 4, h * 3 + 0])
                            for j in range(4):
                                ts(ot[:, j, hg, :], ocv[:, j, 0:64], smal[:, 8 + j:9 + j], None, ALU.mult)
                                if tr >= 2:
                                    if hg == 0:
                                        ts(impa[:, j, g, :], ocv[:, j, 65:97], smal[:, 4 + j:5 + j], None, ALU.mult)
                                    else:
                                        stt(impa[:, j, g, :], ocv[:, j, 65:97], smal[:, 4 + j:5 + j],
                                            impa[:, j, g, :], ALU.mult, ALU.add)
                        jobs.append(dict(kT=lambda kt: kcT[:, :],
                                         qfn=lambda ch=ch, bp=bp: qpad(qn, ch, bp, "dve"),
                                         v=lambda kt, g=g: vca[:, g, :],
                                         tiles=[(0, 0, 512, [(0, 512, identb, cmsk[:, :])])],
                                         bias=bias_cmp(h, nar), narrow=nar, mask=None, ncols=97, done=done))
                attend_multi(jobs)
                if tr >= 2:
                    for j in range(4):
                        kp = rtab[:, j * 32:(j + 1) * 32]
                        fo = rtab[:, 128 + j * 32:128 + (j + 1) * 32]
                        for g in range(2):
                            iv = tk32[:, 0:32]
                            tt(iv, impa[:, j, g, :], kp, ALU.mult)
                            tt(iv, iv, fo, ALU.add)
                            S.op("dve", lambda: nc.vector.max(out=m8[:, 0:8], in_=iv), [iv], [m8[:, 0:8]])
                            S.op("dve", lambda: nc.vector.match_replace(out=tk32[:, 32:64], in_to_replace=m8[:, 0:8],
                                                                         in_values=iv, imm_value=-3.0e38),
                                 [iv, m8[:, 0:8]], [tk32[:, 32:64]])
                            S.op("dve", lambda: nc.vector.max(out=m8[:, 8:16], in_=tk32[:, 32:64]),
                                 [tk32[:, 32:64]], [m8[:, 8:16]])
                            ts(tk32[:, 32:64], iv, m8[:, 15:16], None, ALU.is_ge)
                            ts(mbtokN[:, g * 64:g * 64 + 32], tk32[:, 32:64], -1.0, -MASKV, ALU.add, ALU.mult)
                            pb = psM.next()
                            tp(pb[:, 0:128], mbtokN[:, :], identf)
                            cp(mbTn[:, g, j * 128:(j + 1) * 128], pb[:, 0:128])
                            memset(mbtokN[:, g * 64:g * 64 + 32], 0.0)
                S.mark("tr%d nsa-selwin" % tr)
                jobs = []
                for g in range(2):
                    ot = ots[g]
                    for hg in range(4):
                        h = g * 4 + hg
                        ch, bp = h % 4, 64 * (h // 4)
                        nar = bool(sl_n[h] > 0.3)
                        qc = {}

                        def qfn(ch=ch, bp=bp, qc=qc):
                            if "q" not in qc:
                                qc["q"] = qpad(qn, ch, bp, "dve")
                            return qc["q"]
                        mfn = None
                        if tr >= 2:
                            def mfn(kt, g=g):
                                return esel[:, kt, :], mbTn[:, g, :]

                        def done_sel(ob_, ot=ot, hg=hg, h=h):
                            ov_ = norm_coef(ob_, gsig[:, 4 * tr:4 * tr + 4, h * 3 + 1])
                            for j in range(4):
                                stt(ot[:, j, hg, :], ov_[:, j, 0:64], smal[:, 8 + j:9 + j], ot[:, j, hg, :],
                                    ALU.mult, ALU.add)

                        def done_win(ob_, ot=ot, hg=hg, h=h, g=g):
                            ov_ = norm_coef(ob_, gsig[:, 4 * tr:4 * tr + 4, h * 3 + 2])
                            for j in range(4):
                                stt(ot[:, j, hg, :], ov_[:, j, 0:64], smal[:, 8 + j:9 + j], ot[:, j, hg, :],
                                    ALU.mult, ALU.add)
                            if hg == 3:
                                for i2 in range(2):
                                    pb = psM.next()
                                    for j in range(4):
                                        tp(pb[:, j * 128:(j + 1) * 128], ot[:, j, 2 * i2:2 * i2 + 2, :], identf)
                                    cp(onT[:, 2 * g + i2, :], pb[:, :], e="act")
                        jobs.append(dict(kT=lambda kt: kslcT[:, kt * 128:(kt + 1) * 128], qfn=qfn,
                                         v=lambda kt, g=g: vslc[:, kt, g, :],
                                         tiles=causal_tiles(tr, dmax_of(sl_n[h])), bias=bias_head(h, nar),
                                         narrow=nar, mask=mfn, done=done_sel))
                        jobs.append(dict(kT=lambda kt: kwinT[:, kt * 128:(kt + 1) * 128], qfn=qfn,
                                         v=lambda kt, g=g: vwin[:, kt, g, :],
                                         tiles=win_tiles(tr, dmax_of(sl_n[h])), bias=bias_head(h, nar),
                                         narrow=nar, mask=None, done=done_win))
                attend_multi(jobs)

            if SUB < 5:
                continue
            if not PLAN[0]:
                S.mark("tr%d merge" % tr)
            for dc in range(8):
                c0 = dc * 128
                MJ = W([(0, [8, 128], win[:, :, 2840 + c0:2840 + c0 + 128]),
                        (1024, [8, 128], win[:, :, 3864 + c0:3864 + c0 + 128]),
                        (2048, [4, 128], wupn[:, :, c0:c0 + 128]),
                        (2560, [4, 128], wupm[:, :, c0:c0 + 128])])
                if PLAN[0]:
                    continue
                ga_ = MJ[:, 0:1024].rearrange("p (c f) -> p c f", c=8)
                gb_ = MJ[:, 1024:2048].rearrange("p (c f) -> p c f", c=8)
                un_ = MJ[:, 2048:2560].rearrange("p (c f) -> p c f", c=4)
                um_ = MJ[:, 2560:3072].rearrange("p (c f) -> p c f", c=4)
                pga = psS.next()
                pgb = psS.next()
                pyn = psO.next()
                pym = psO.next()
                for c in range(8):
                    mm(pga[:, :], ga_[:, c, :], u_tr[:, c, :], c == 0, c == 7)
                for c in range(8):
                    mm(pgb[:, :], gb_[:, c, :], u_tr[:, c, :], c == 0, c == 7)
                for c in range(4):
                    mm(pyn[:, :], un_[:, c, :], onT[:, c, :], c == 0, c == 3)
                for c in range(4):
                    mm(pym[:, :], um_[:, c, :], omT[:, c, :], c == 0, c == 3)
                sA = sgAr.next()
                sB = sgBr.next()
                act(sA[:, :], pga[:, :], AF.Sigmoid)
                act(sB[:, :], pgb[:, :], AF.Sigmoid)
                tt(sA[:, :], sA[:, :], pyn[:, :], ALU.mult)
                tt(sB[:, :], sB[:, :], pym[:, :], ALU.mult)
                tt(mixed[:, dc, :], sA[:, :], sB[:, :], ALU.add)
            for dh in range(2):
                WO = W([(0, [8, 512], wout[:, :, dh * 512:(dh + 1) * 512])])
                if PLAN[0]:
                    continue
                wo_ = WO[:, 0:4096].rearrange("p (c f) -> p c f", c=8)
                for d4 in range(4):
                    dc = dh * 4 + d4
                    pb = psM.next()
                    for c in range(8):
                        mm(pb[:, :], wo_[:, c, d4 * 128:(d4 + 1) * 128], mixed[:, c, :], c == 0, c == 7)
                    tt(hT[:, dc, trs(tr)], hT[:, dc, trs(tr)], pb[:, :], ALU.add)

    def run_all():
        if not PLAN[0]:
            S.mark("ffn1")
        if stage >= 1:
            scoped(phase_ffn, "ffn1_w1", "ffn1_w3", "ffn1_w2", 0, "a")
        if stage >= 2:
            scoped(phase_mixer)
        if stage >= 3:
            if not PLAN[0]:
                S.mark("ffn2")
            scoped(phase_ffn, "ffn2_w1", "ffn2_w3", "ffn2_w2", 2, "b")
        if stage >= 4:
            if not PLAN[0]:
                S.mark("ple")
            scoped(phase_ple)

    PLAN[0] = True
    run_all()
    PLAN[0] = False
    scoped(phase_load)
    run_all()
    S.mark("final")
    scoped(phase_final)
    S.mark("end")
    S.finish()
    _LAST["S"] = S
    return nc


_NC_CACHE = {}
_LAST = {}


def _prep_inputs(inputs):
    cf_np, _, cb_np, _ = _make_consts()
    f = lambda a: np.ascontiguousarray(np.asarray(a, dtype=np.float32))
    sh = {}
    for nm in ("ffn1_w1", "ffn1_w3", "ffn1_w2", "w_in", "cmp_w1_k", "cmp_w2_k", "cmp_w1_v", "cmp_w2_v",
               "w_up_nsa", "w_up_moba", "w_out", "ffn2_w1", "ffn2_w3", "ffn2_w2", "w_ple_gate", "w_ple"):
        sh[nm] = f(inputs[nm])[0]
    wi = sh["w_in"].copy()
    perm = [0, 4, 1, 5, 2, 6, 3, 7]
    wi[:, 0:512] = sh["w_in"][:, 0:512].reshape(D, 8, 64)[:, perm, :].reshape(D, 512)
    sh["w_in"] = np.ascontiguousarray(wi)
    for nm, src in (("pos_kT", "cmp_pos_k"), ("pos_vT", "cmp_pos_v")):
        pt = f(inputs[src])[0].T
        sh[nm] = np.ascontiguousarray(np.concatenate([pt, pt], axis=0))
    g = np.stack([f(inputs[n])[0] for n in ("ffn1_norm", "mix_norm", "ffn2_norm", "ple_norm")], axis=0)
    sh["gains"] = np.ascontiguousarray(g.reshape(4, 8, 128).transpose(2, 0, 1).reshape(128, 32))
    sh["gfin"] = np.ascontiguousarray(np.broadcast_to(f(inputs["final_norm"])[None, :], (128, D)))
    sh["cf32"] = cf_np
    sh["cbf16"] = cb_np
    return sh


def kernel(**inputs):
    stage = int(os.environ.get("MK_STAGE", "99"))
    ncores = int(os.environ.get("MK_CORES", "8"))
    if stage not in _NC_CACHE:
        _NC_CACHE[stage] = build_nc(stage)
    nc = _NC_CACHE[stage]
    sh = _prep_inputs(inputs)
    x = np.asarray(inputs["x"], dtype=np.float32)
    p = np.asarray(inputs["p"], dtype=np.float32)
    in_maps = []
    for b in range(ncores):
        m = dict(sh)
        m["x"] = np.ascontiguousarray(x[b])
        m["p"] = np.ascontiguousarray(p[0, b])
        in_maps.append(m)
    res = run_bass_kernel_spmd(nc, in_maps, core_ids=list(range(ncores)))
    out = np.stack([np.asarray(r["out"], dtype=np.float32) for r in res.results], axis=0)
    return out
```

```python
import os
import numpy as np
import ml_dtypes
import concourse.bass as bass
import concourse.mybir as mybir
from concourse.bass_utils import run_bass_kernel_spmd
from contextlib import ExitStack

F32 = mybir.dt.float32
BF16 = mybir.dt.bfloat16
AF = mybir.ActivationFunctionType
ALU = mybir.AluOpType

T = 2048
D = 1024
FF = 2816
MASKV = -240000.0
EPS = 1e-6
TINY = 1e-30
IN_W = 4888


def _slopes():
    s = 2.0 ** (-(np.arange(16) + 1) / 2.0)
    return s[0::2].copy(), s[1::2].copy()


def _make_consts():
    p = np.arange(128)
    f32 = {}
    b16 = {}
    f32["ident"] = np.eye(128)
    b16["ident"] = np.eye(128)
    b16["ones"] = np.ones((128, 128))
    j = np.arange(128)
    b16["triA"] = np.where(j[None, :] >= p[:, None], 0.0, MASKV)
    b16["triB"] = np.where(j[None, :] < p[:, None], 0.0, MASKV)
    tq = np.arange(T)
    cend = 16 * p + 31
    cm = np.where(tq[None, :] >= cend[:, None], 0.0, MASKV)
    cm[127, :] = MASKV
    ov = np.zeros((128, 32))
    for c in range(127):
        for jj in range(32):
            if 16 * c <= 64 * jj + 63 and 16 * c + 31 >= 64 * jj:
                ov[c, jj] = 1.0
    b16["overlap"] = ov
    b16["cmpmask"] = cm
    es = np.zeros((128, 16, 128))
    for pp in range(128):
        jj = pp % 64
        if jj >= 32:
            continue
        for kt in range(16):
            for m_ in range(128):
                if jj == 2 * kt + m_ // 64:
                    es[pp, kt, m_] = 1.0
    b16["esel"] = es.reshape(128, 2048)
    sl_n, sl_m = _slopes()
    sl16 = np.concatenate([sl_n, sl_m])
    d = np.arange(16) - 12
    f32["bw"] = (sl16[None, :, None] * (128 * d[None, None, :] + p[:, None, None] - 256)).reshape(128, 256)
    d2 = np.arange(16) - 15
    f32["bn"] = (sl16[None, :, None] * (128 * d2[None, None, :] + p[:, None, None] - 64)).reshape(128, 256)
    f32["cw"] = (sl_n[None, :, None] * (cend[:, None, None] - (512 * np.arange(4)[None, None, :] + 256))).reshape(128, 32)
    f32["cn"] = (sl_n[None, :, None] * (cend[:, None, None] - (128 * np.arange(16)[None, None, :] + 64))).reshape(128, 128)
    keep = np.zeros((128, 8, 32))
    force = np.zeros((128, 8, 32))
    for tt in range(8, 16):
        for pp in range(128):
            t = tt * 128 + pp
            cur = t // 64
            for jj in range(32):
                if 64 * jj > t:
                    force[pp, tt - 8, jj] = -1e30 * (1.0 + jj / 64.0)
                elif jj == 0:
                    force[pp, tt - 8, jj] = 3e9
                elif jj == cur:
                    force[pp, tt - 8, jj] = 2e9
                elif jj == cur - 1:
                    force[pp, tt - 8, jj] = 1e9
                else:
                    keep[pp, tt - 8, jj] = 1.0
    padneg = np.zeros((128, 8, 8))
    ownhot = np.zeros((128, 8, 8))
    for tt in range(8, 16):
        cur = tt // 2
        for n in range(8):
            if n >= cur:
                padneg[:, tt - 8, n] = -1e30
            if n == cur:
                ownhot[:, tt - 8, n] = 1.0
    f32["eps"] = np.full((128, 1), EPS)
    rt = []
    for tr in (2, 3):
        a = (tr - 2) * 4
        rt += [keep[:, a:a + 4].reshape(128, 128), force[:, a:a + 4].reshape(128, 128),
               padneg[:, a:a + 4].reshape(128, 32), ownhot[:, a:a + 4].reshape(128, 32)]
    f32["rt"] = np.concatenate(rt, axis=1)
    offs_f = {}
    cols = []
    o = 0
    for k, v in f32.items():
        offs_f[k] = o
        o += v.shape[1]
        cols.append(v.astype(np.float32))
    cf = np.ascontiguousarray(np.concatenate(cols, axis=1))
    offs_b = {}
    cols = []
    o = 0
    for k, v in b16.items():
        offs_b[k] = o
        o += v.shape[1]
        cols.append(v.astype(np.float32))
    cbv = np.ascontiguousarray(np.concatenate(cols, axis=1).astype(ml_dtypes.bfloat16))
    return cf, offs_f, cbv, offs_b


EMBED_WAIT = set(os.environ.get("MK_EMBED", "pe").split(",")) - {""}


def _box(ap):
    t = ap.tensor
    if t.name.startswith("ps") and len(t.name) == 3:
        return (t.name, 0, 128, 0, 512)
    row = 1
    for s in tuple(t.shape)[1:]:
        row *= int(s)
    off = int(ap.offset)
    p0 = off // row
    f0 = off % row
    apl = ap.ap
    npart = apl[0][1]
    ext = 1
    for st, cnt in apl[1:]:
        ext += (cnt - 1) * abs(st)
    return (t.name, p0, p0 + npart, f0, f0 + ext)


class Sched:
    def __init__(self, nc, ndma=24):
        self.nc = nc
        self.engs = {"pe": nc.tensor, "act": nc.scalar, "dve": nc.vector, "pool": nc.gpsimd, "sp": nc.sync}
        self.semh = {}
        for e in self.engs:
            self.semh[e] = nc.alloc_semaphore("sem_" + e)
        self.cnt = {e: 0 for e in self.engs}
        self.ndma = ndma
        for i in range(ndma):
            self.semh[("d", i)] = nc.alloc_semaphore("dsem%d" % i)
        self.dcnt = [0] * ndma
        self.drr = 0
        self.drr_pool = 0
        self.seen = {e: {} for e in self.engs}
        self.acc = {}
        self.out_tickets = []
        self.nwaits = 0
        self.marks = []

    def mark(self, name):
        self.marks.append((name, self.cnt["pe"]))

    def _wait(self, e, sk, val):
        if e == "pe" and sk == "pe":
            return
        if self.seen[e].get(sk, 0) >= val:
            return
        self.engs[e].wait_ge(self.semh[sk], val)
        self.seen[e][sk] = val
        self.nwaits += 1

    def _collect(self, reads, writes):
        waits = {}
        for ap in reads:
            b = _box(ap)
            for ent in self.acc.get(b[0], ()):
                if ent[0] == "w" and ent[3] < b[2] and b[1] < ent[4] and ent[5] < b[4] and b[3] < ent[6]:
                    if waits.get(ent[1], 0) < ent[2]:
                        waits[ent[1]] = ent[2]
        for ap in writes:
            b = _box(ap)
            for ent in self.acc.get(b[0], ()):
                if ent[3] < b[2] and b[1] < ent[4] and ent[5] < b[4] and b[3] < ent[6]:
                    if waits.get(ent[1], 0) < ent[2]:
                        waits[ent[1]] = ent[2]
        return waits

    def _record(self, sk, val, reads, writes):
        for ap in writes:
            b = _box(ap)
            lst = self.acc.setdefault(b[0], [])
            lst[:] = [e for e in lst if not (b[1] <= e[3] and e[4] <= b[2] and b[3] <= e[5] and e[6] <= b[4])]
            lst.append(("w", sk, val, b[1], b[2], b[3], b[4]))
        for ap in reads:
            b = _box(ap)
            lst = self.acc.setdefault(b[0], [])
            lst[:] = [e for e in lst if not (e[0] == "r" and e[1] == sk and b[1] <= e[3] and e[4] <= b[2]
                                             and b[3] <= e[5] and e[6] <= b[4])]
            lst.append(("r", sk, val, b[1], b[2], b[3], b[4]))

    def op(self, e, fn, reads, writes, inc=True):
        assert inc or e == "pe"
        waits = self._collect(reads, writes)
        emb = None
        if EMBED_WAIT and e in EMBED_WAIT:
            need = [(sk, val) for sk, val in waits.items()
                    if not (e == "pe" and sk == "pe") and self.seen[e].get(sk, 0) < val]
            if need:
                emb = need[-1]
                waits = dict(need[:-1])
            else:
                waits = {}
        for sk, val in waits.items():
            self._wait(e, sk, val)
        ins = fn()
        if emb is not None:
            ins._wait_ge(self.semh[emb[0]], emb[1])
            self.seen[e][emb[0]] = emb[1]
        if inc:
            self.cnt[e] += 1
            ins.then_inc(self.semh[e], 1)
            val = self.cnt[e]
        else:
            val = self.cnt[e] + 1
        self._record(e, val, reads, writes)
        return ins

    def dma(self, q, out, in_, reads=(), writes=(), is_out=False, **kw):
        waits = self._collect(reads, writes)
        for sk, val in waits.items():
            self._wait(q, sk, val)
        half = self.ndma // 2
        if q == "pool":
            i = half + (self.drr_pool % half)
            self.drr_pool += 1
        else:
            i = self.drr % half
            self.drr += 1
        sk = ("d", i)
        if self.dcnt[i] > 0:
            self._wait(q, sk, self.dcnt[i])
        ins = self.engs[q].dma_start(out=out, in_=in_, **kw)
        self.dcnt[i] += 16
        ins.then_inc(self.semh[sk], 16)
        self._record(sk, self.dcnt[i], reads, writes)
        if is_out:
            self.out_tickets.append((sk, self.dcnt[i]))

    def barrier(self):
        for e in ("pe", "act", "dve", "pool", "sp"):
            for f in ("pe", "act", "dve", "pool", "sp"):
                if f != e and self.cnt[f] > 0:
                    self._wait(e, f, self.cnt[f])
            for i in range(self.ndma):
                if self.dcnt[i] > 0:
                    self._wait(e, ("d", i), self.dcnt[i])
        self.acc = {}

    def finish(self):
        for sk, val in self.out_tickets:
            self._wait("sp", sk, val)


def build_nc(stage=99):
    nc = bass.Bass("TRN2", target_bir_lowering=False)
    cf_np, OF, cb_np, OB = _make_consts()
    NF = cf_np.shape[1]
    NB = cb_np.shape[1]

    def din(name, shape, dt=F32):
        return nc.dram_tensor(name, list(shape), dt, kind="ExternalInput").ap()

    x_d = din("x", [T, D])
    p_d = din("p", [T, 256])
    wd = {}
    for nm, shp in [("ffn1_w1", [D, FF]), ("ffn1_w3", [D, FF]), ("ffn1_w2", [FF, D]), ("w_in", [D, IN_W]),
                    ("cmp_w1_k", [2048, 128]), ("cmp_w2_k", [128, 64]), ("cmp_w1_v", [2048, 128]),
                    ("cmp_w2_v", [128, 64]), ("pos_kT", [128, 32]), ("pos_vT", [128, 32]),
                    ("w_up_nsa", [512, D]), ("w_up_moba", [512, D]), ("w_out", [D, D]),
                    ("ffn2_w1", [D, FF]), ("ffn2_w3", [D, FF]), ("ffn2_w2", [FF, D]),
                    ("w_ple_gate", [D, D]), ("w_ple", [256, D]), ("gains", [128, 32]), ("gfin", [128, D])]:
        wd[nm] = din(nm, shp)
    cf_d = din("cf32", [128, NF])
    cb_d = din("cbf16", [128, NB], BF16)
    out_d = nc.dram_tensor("out", [T, D], F32, kind="ExternalOutput").ap()

    S = Sched(nc)
    stack = [None]

    def TS(name, shape, dt):
        if stack[0] is None:
            return nc.alloc_sbuf_tensor(name, shape, dt)
        return stack[0].enter_context(nc.sbuf_tensor(name, shape, dt))

    def scoped(fn, *a):
        if PLAN[0]:
            fn(*a)
            return
        S.barrier()
        with ExitStack() as st:
            stack[0] = st
            fn(*a)
            S.barrier()
        stack[0] = None

    hT = TS("hT", [128, 8, T], F32)
    NFS = OF["rt"]
    NBS = OB["cmpmask"]
    cf = TS("cf", [128, NFS], F32)
    cb = TS("cb", [128, NBS], BF16)
    gains = TS("gains_sb", [128, 32], F32)
    slab = [TS("slab%d" % i, [128, 4096], BF16) for i in range(3)]
    ps = [nc.alloc_psum_tensor("ps%d" % i, [128, 512], F32) for i in range(8)]

    identf = cf[:, OF["ident"]:OF["ident"] + 128]
    identb = cb[:, OB["ident"]:OB["ident"] + 128]
    onesb = cb[:, OB["ones"]:OB["ones"] + 128]
    epsc = cf[:, OF["eps"]:OF["eps"] + 1]

    S.dma("sp", cf[:, :], cf_d[:, 0:NFS], writes=[cf[:, :]])
    S.dma("sp", cb[:, :], cb_d[:, 0:NBS], writes=[cb[:, :]])
    S.dma("sp", gains[:, :], wd["gains"][:, :], writes=[gains[:, :]])

    def mm(out, lhsT, rhs, start, stop, inc=None):
        inc = True
        return S.op("pe", lambda: nc.tensor.matmul(out, lhsT, rhs, start=start, stop=stop),
                    [lhsT, rhs], [out], inc=inc)

    def pe_drain():
        if S.cnt["pe"] > 0:
            nc.tensor.wait_ge(S.semh["pe"], S.cnt["pe"])

    def tp(out, in_, ident):
        return S.op("pe", lambda: nc.tensor.transpose(out, in_, ident), [in_, ident], [out])

    def act(out, in_, func, bias=None, scale=None, accum_out=None):
        kw = {}
        rd = [in_]
        wr = [out]
        if bias is not None:
            kw["bias"] = bias
            if not isinstance(bias, (int, float)):
                rd.append(bias)
        if scale is not None:
            kw["scale"] = scale
            if not isinstance(scale, (int, float)):
                rd.append(scale)
        if accum_out is not None:
            kw["accum_out"] = accum_out
            wr.append(accum_out)
        return S.op("act", lambda: nc.scalar.activation(out=out, in_=in_, func=func, **kw), rd, wr)

    def tt(out, in0, in1, op, e="dve"):
        eng = nc.vector if e == "dve" else nc.gpsimd
        return S.op(e, lambda: eng.tensor_tensor(out=out, in0=in0, in1=in1, op=op), [in0, in1], [out])

    def ts(out, in0, s1, s2, op0, op1=None, e="dve"):
        eng = nc.vector if e == "dve" else nc.gpsimd
        rd = [in0]
        if not isinstance(s1, (int, float)):
            rd.append(s1)
        if s2 is not None and not isinstance(s2, (int, float)):
            rd.append(s2)
        if op1 is None:
            return S.op(e, lambda: eng.tensor_scalar(out=out, in0=in0, scalar1=s1, scalar2=None, op0=op0), rd, [out])
        return S.op(e, lambda: eng.tensor_scalar(out=out, in0=in0, scalar1=s1, scalar2=s2, op0=op0, op1=op1),
                    rd, [out])

    def stt(out, in0, scalar, in1, op0, op1):
        rd = [in0, in1]
        if not isinstance(scalar, (int, float)):
            rd.append(scalar)
        return S.op("dve", lambda: nc.vector.scalar_tensor_tensor(out=out, in0=in0, scalar=scalar, in1=in1,
                                                                   op0=op0, op1=op1), rd, [out])

    def cp(out, in_, e="dve"):
        if e == "act":
            return S.op("act", lambda: nc.scalar.copy(out=out, in_=in_), [in_], [out])
        eng = nc.vector if e == "dve" else nc.gpsimd
        return S.op(e, lambda: eng.tensor_copy(out=out, in_=in_), [in_], [out])

    def memset(ap, v, e="dve"):
        eng = nc.vector if e == "dve" else nc.gpsimd
        return S.op(e, lambda: eng.memset(ap, v), [], [ap])

    class RR:
        def __init__(self, items):
            self.items = items
            self.i = 0

        def next(self):
            it = self.items[self.i % len(self.items)]
            self.i += 1
            return it

    class WStream:
        def __init__(self):
            self.jobs = []
            self.issued = 0
            self.k = 0

        def add(self, parts):
            self.jobs.append(parts)

        def _issue(self, k):
            buf = slab[k % 3]
            for (off, shape, src) in self.jobs[k]:
                n = 1
                for s_ in shape:
                    n *= s_
                npart = int(src.shape[0])
                dst = buf[0:npart, off:off + n]
                if len(shape) == 2:
                    dst = dst.rearrange("p (a b) -> p a b", a=shape[0])
                S.dma("pool", dst, src, writes=[buf[0:npart, off:off + n]])

        def get(self):
            k = self.k
            self.k += 1
            while self.issued < min(k + 2, len(self.jobs)):
                self._issue(self.issued)
                self.issued += 1
            return slab[k % 3]

    WS = WStream()
    PLAN = [True]

    def W(parts):
        if PLAN[0]:
            WS.add(parts)
            return None
        return WS.get()

    def trs(tr):
        return slice(tr * 512, (tr + 1) * 512)

    sqb = [TS("sqb%d" % i, [128, 512], BF16) for i in range(2)]
    rstd = TS("rstd", [128, 512], F32)
    sqrr = RR(sqb)
    psM = RR([ps[6], ps[7]])

    def rms_range(tr, gidx, xn3):
        pb = psM.next()
        for c in range(8):
            sq = sqrr.next()
            act(sq[:, :], hT[:, c, trs(tr)], AF.Square)
            mm(pb[:, :], onesb, sq[:, :], c == 0, c == 7)
        act(rstd[:, :], pb[:, :], AF.Ln, bias=epsc, scale=1.0 / D)
        act(rstd[:, :], rstd[:, :], AF.Exp, scale=-0.5)
        for c in range(8):
            stt(xn3[:, c, :], hT[:, c, trs(tr)], gains[:, gidx * 8 + c:gidx * 8 + c + 1], rstd[:, :],
                ALU.mult, ALU.mult)

    def phase_load():
        xtok = [TS("xtok%d" % i, [128, D], F32) for i in range(2)]
        for t_ in range(16):
            xb = xtok[t_ % 2]
            S.dma("sp", xb[:, :], x_d[t_ * 128:(t_ + 1) * 128, :], writes=[xb[:, :]])
            for half in range(2):
                pb = psM.next()
                for c4 in range(4):
                    c = half * 4 + c4
                    tp(pb[:, c4 * 128:(c4 + 1) * 128], xb[:, c * 128:(c + 1) * 128], identf)
                cp(hT[:, half * 4:(half + 1) * 4, t_ * 128:(t_ + 1) * 128],
                   pb[:, :].rearrange("p (c t) -> p c t", c=4), e="act" if half else "dve")

    def phase_ffn(w1n, w3n, w2n, gidx, tag):
        w1 = wd[w1n].rearrange("(c p) f -> p c f", p=128)
        w3 = wd[w3n].rearrange("(c p) f -> p c f", p=128)
        w2 = wd[w2n]
        if not PLAN[0]:
            xnT = TS("xnT" + tag, [128, 8, T], BF16)
            h1 = [TS("h1%s%d" % (tag, i), [128, 2, 512], BF16) for i in range(2)]
            sa = [TS("sa%s%d" % (tag, i), [128, 512], F32) for i in range(2)]
            for tr in range(4):
                rms_range(tr, gidx, xnT[:, :, trs(tr)])
            psAB = RR([ps[0], ps[1], ps[2], ps[3]])
            psY = RR([ps[4], ps[5], ps[6], ps[7]])
            sar = RR(sa)
            h1r = RR(h1)
            ytmp = [TS("ytmp%s%d" % (tag, i), [128, 512], F32) for i in range(2)]
            ytr = RR(ytmp)
        Aj = {}
        Bj = {}

        def getA(s_):
            if s_ not in Aj:
                Aj[s_] = W([(0, [8, 256], w1[:, :, s_ * 256:(s_ + 1) * 256]),
                            (2048, [8, 256], w3[:, :, s_ * 256:(s_ + 1) * 256])])
            return Aj[s_]

        def getB(s_):
            if s_ not in Bj:
                Bj[s_] = W([(0, [2, 1024], w2[s_ * 256:(s_ + 1) * 256, :].rearrange("(j p) d -> p j d", p=128))])
            return Bj[s_]

        def ab(u):
            s_, tr = u
            A = getA(s_)
            if PLAN[0]:
                return None
            w1s = A[:, 0:2048].rearrange("p (c f) -> p c f", c=8)
            w3s = A[:, 2048:4096].rearrange("p (c f) -> p c f", c=8)
            hb = h1r.next()
            for j in range(2):
                pa = psAB.next()
                pb = psAB.next()
                for c in range(8):
                    mm(pa[:, :], w1s[:, c, j * 128:(j + 1) * 128], xnT[:, c, trs(tr)], c == 0, c == 7)
                for c in range(8):
                    mm(pb[:, :], w3s[:, c, j * 128:(j + 1) * 128], xnT[:, c, trs(tr)], c == 0, c == 7)
                sab = sar.next()
                act(sab[:, :], pa[:, :], AF.Silu)
                tt(hb[:, j, :], sab[:, :], pb[:, :], ALU.mult)
            return hb

        def yy(u, hb):
            s_, tr = u
            B = getB(s_)
            if PLAN[0]:
                return
            w2s = B[:, 0:2048].rearrange("p (j d) -> p j d", j=2)
            for dc in range(8):
                py = psY.next()
                for j in range(2):
                    mm(py[:, :], w2s[:, j, dc * 128:(dc + 1) * 128], hb[:, j, :], j == 0, j == 1)
                stt(hT[:, dc, trs(tr)], py[:, :], 0.5, hT[:, dc, trs(tr)], ALU.mult, ALU.add)

        units = [(s_, tr) for s_ in range(11) for tr in range(4)]
        prev = None
        for u in units:
            hb = ab(u)
            if prev is not None:
                yy(prev[0], prev[1])
            prev = (u, hb)
        yy(prev[0], prev[1])

    def phase_ple():
        wg = wd["w_ple_gate"].rearrange("(c p) f -> p c f", p=128)
        wp = wd["w_ple"].rearrange("(c p) f -> p c f", p=128)
        if not PLAN[0]:
            xnT = TS("xnTple", [128, 8, T], BF16)
            pT = TS("pT", [128, 2, T], BF16)
            ptok = [TS("ptok%d" % i, [128, 256], F32) for i in range(2)]
            sg = [TS("sgple%d" % i, [128, 512], F32) for i in range(2)]
            for t_ in range(16):
                pbuf = ptok[t_ % 2]
                S.dma("sp", pbuf[:, :], p_d[t_ * 128:(t_ + 1) * 128, :], writes=[pbuf[:, :]])
                pb = psM.next()
                for c in range(2):
                    tp(pb[:, c * 128:(c + 1) * 128], pbuf[:, c * 128:(c + 1) * 128], identf)
                cp(pT[:, :, t_ * 128:(t_ + 1) * 128], pb[:, 0:256].rearrange("p (c t) -> p c t", c=2))
            for tr in range(4):
                rms_range(tr, 3, xnT[:, :, trs(tr)])
            psG = RR([ps[0], ps[1], ps[2], ps[3]])
            sgr = RR(sg)
        for dh in range(2):
            A = W([(0, [8, 512], wg[:, :, dh * 512:(dh + 1) * 512])])
            B = W([(0, [2, 512], wp[:, :, dh * 512:(dh + 1) * 512])])
            if PLAN[0]:
                continue
            wgs = A[:, 0:4096].rearrange("p (c f) -> p c f", c=8)
            wps = B[:, 0:1024].rearrange("p (c f) -> p c f", c=2)
            for tr in range(4):
                for d4 in range(4):
                    dc = dh * 4 + d4
                    pg = psG.next()
                    pp = psG.next()
                    for c in range(8):
                        mm(pg[:, :], wgs[:, c, d4 * 128:(d4 + 1) * 128], xnT[:, c, trs(tr)], c == 0, c == 7)
                    for c in range(2):
                        mm(pp[:, :], wps[:, c, d4 * 128:(d4 + 1) * 128], pT[:, c, trs(tr)], c == 0, c == 1)
                    sgb = sgr.next()
                    act(sgb[:, :], pg[:, :], AF.Sigmoid)
                    tt(sgb[:, :], sgb[:, :], pp[:, :], ALU.mult)
                    tt(hT[:, dc, trs(tr)], hT[:, dc, trs(tr)], sgb[:, :], ALU.add)

    def phase_final():
        gfin = TS("gfin_sb", [128, D], F32)
        S.dma("sp", gfin[:, :], wd["gfin"][:, :], writes=[gfin[:, :]])
        otok = [TS("otok%d" % i, [128, D], F32) for i in range(2)]
        junk = TS("junk", [128, 512], F32)
        ssq = TS("ssq", [128, 4], F32)
        psF = RR([ps[0], ps[1], ps[2], ps[3], ps[4], ps[5]])
        for t_ in range(16):
            ob = otok[t_ % 2]
            pbs = [psF.next(), psF.next()]
            for half in range(2):
                for c4 in range(4):
                    c = half * 4 + c4
                    tp(pbs[half][:, c4 * 128:(c4 + 1) * 128], hT[:, c, t_ * 128:(t_ + 1) * 128], identf)
                act(junk[:, :], pbs[half][:, :], AF.Square, accum_out=ssq[:, half:half + 1])
            tt(ssq[:, 2:3], ssq[:, 0:1], ssq[:, 1:2], ALU.add)
            act(ssq[:, 3:4], ssq[:, 2:3], AF.Ln, bias=epsc, scale=1.0 / D)
            act(ssq[:, 3:4], ssq[:, 3:4], AF.Exp, scale=-0.5)
            for half in range(2):
                stt(ob[:, half * 512:(half + 1) * 512], pbs[half][:, :], ssq[:, 3:4],
                    gfin[:, half * 512:(half + 1) * 512], ALU.mult, ALU.mult)
            S.dma("sp", out_d[t_ * 128:(t_ + 1) * 128, :], ob[:, :], reads=[ob[:, :]], is_out=True)

    def phase_mixer():
        win = wd["w_in"].rearrange("(c p) f -> p c f", p=128)
        wupn = wd["w_up_nsa"].rearrange("(c p) f -> p c f", p=128)
        wupm = wd["w_up_moba"].rearrange("(c p) f -> p c f", p=128)
        wout = wd["w_out"].rearrange("(c p) f -> p c f", p=128)
        sl_n, sl_m = _slopes()
        if not PLAN[0]:
            kslcT = TS("kslcT", [128, T], BF16)
            kwinT = TS("kwinT", [128, T], BF16)
            kcmpT = TS("kcmpT", [128, 528], BF16)
            vcmpT = TS("vcmpT", [128, 528], BF16)
            kmT = TS("kmT", [128, 4, T], BF16)
            vslc = TS("vslc", [128, 16, 2, 65], BF16)
            vwin = TS("vwin", [128, 16, 2, 65], BF16)
            vm = TS("vm", [128, 16, 8, 65], BF16)
            gsig = TS("gsig", [128, 16, 24], F32)
            kcT = TS("kcT", [128, 128], BF16)
            vca = TS("vca", [128, 2, 97], BF16)
            kmeanT = TS("kmeanT", [128, 4, 16], BF16)
            kmsum = TS("kmsum", [128, 4, 2], F32)
            w2k = TS("w2k", [128, 128], BF16)
            w2v = TS("w2v", [128, 64], BF16)
            posk = TS("posk", [128, 32], BF16)
            posv = TS("posv", [128, 32], BF16)
            cbias = TS("cbias", [128, 2], F32)
            u_tr = TS("u_tr", [128, 8, 512], BF16)
            qmix = TS("qmix", [128, 8, 512], BF16)

            class _Sub:
                def __init__(self, base, off):
                    self.base, self.off = base, off

                def __getitem__(self, key):
                    a, b, c = key
                    if isinstance(b, int):
                        b = b + self.off
                    else:
                        b = slice((b.start or 0) + self.off, (b.stop if b.stop is not None else 4) + self.off)
                    return self.base[a, b, c]
            qn = _Sub(qmix, 0)
            qm = _Sub(qmix, 4)
            ptl = [TS("ptl%d" % i, [128, 512], BF16) for i in range(3)]
            otk = [TS("otk%d" % i, [128, 4, 4, 64], F32) for i in range(2)]
            omk = [TS("omk%d" % i, [128, 4, 2, 64], F32) for i in range(2)]
            onT = TS("onT", [128, 4, 512], BF16)
            omT = TS("omT", [128, 4, 512], BF16)
            mixed = qmix
            _o0 = otk[0][:, :, :, :].rearrange("p a b c -> p (a b c)")
            sgA = [_o0[:, 0:512]]
            sgB = [_o0[:, 512:1024]]
            mbTn = TS("mbTn", [128, 2, 512], BF16)
            qz = [TS("qz%d" % i, [128, 512], BF16) for i in range(4)]
            qzc = [0, 0]
            mbTm = TS("mbTm", [128, 512], BF16)
            mbtokN = TS("mbtokN", [128, 128], F32)
            mbtok = TS("mbtok", [128, 128], F32)
            impa = TS("impa", [128, 4, 2, 32], F32)
            smal = TS("smal", [128, 64], F32)
            m8 = TS("m8", [128, 16], F32)
            tk32 = TS("tk32", [128, 64], F32)
            cmsk = TS("cmsk", [128, 512], BF16)
            rtab = TS("rtab", [128, 320], F32)
            esel = TS("esel", [128, 16, 128], BF16)
            S.dma("sp", esel[:, :, :], cb_d[:, OB["esel"]:OB["esel"] + 2048].rearrange("p (k m) -> p k m", k=16),
                  writes=[esel[:, :, :]])
            hid = TS("hid", [128, 4, 32], F32)
            hidb = TS("hidb", [128, 2, 32], BF16)
            hidv = TS("hidv", [128, 2, 128], BF16)
            gtmp = TS("gtmp", [64, 528], BF16)
            gsf = TS("gsf", [128, 8, 16], F32)

            memset(kcT[:, :], 0.0)
            for i in range(4):
                memset(qz[i][:, :], 0.0)
            memset(mbtokN[:, :], 0.0)
            memset(mbtok[:, :], 0.0)
            memset(gsf[:, :, :], -1.0e30)
            memset(kcmpT[:, 0:16], 0.0)
            memset(vcmpT[:, 0:16], 0.0)
            memset(hidv[:, :, :], 0.0)
            memset(vca[:, :, :], 0.0)
            memset(kmeanT[:, :, :], 0.0)
            memset(vslc[:, :, :, 64:65], 1.0)
            memset(vwin[:, :, :, 64:65], 1.0)
            memset(vm[:, :, :, 64:65], 1.0)
            memset(vca[:, :, 64:65], 1.0)
            for g in range(2):
                cp(vca[:, g, 65:97], cb[:, OB["overlap"]:OB["overlap"] + 32])
            for (w2t, w2n_, pt, pn) in ((w2k, "cmp_w2_k", posk, "pos_kT"), (w2v, "cmp_w2_v", posv, "pos_vT")):
                S.dma("pool", pt[:, :], wd[pn][:, :], writes=[pt[:, :]])
                if w2t is w2k:
                    S.dma("pool", w2t[:, 0:64], wd[w2n_][:, :], writes=[w2t[:, 0:64]])
                    S.dma("pool", w2t[:, 64:128], wd[w2n_][:, :], writes=[w2t[:, 64:128]])
                else:
                    S.dma("pool", w2t[:, :], wd[w2n_][:, :], writes=[w2t[:, :]])

            psS = RR([ps[0], ps[1], ps[2]])
            psO = RR([ps[3], ps[4], ps[5]])
            ptr = RR(ptl)
            otr = RR(omk)
            sgAr = RR(sgA)
            sgBr = RR(sgB)

        def proj_fm(ws, c0, dst, tr, base=None, ev="act"):
            pb = psM.next()
            for c in range(8):
                if base is None:
                    l_ = ws[:, c, c0:c0 + 128]
                else:
                    l_ = base(c)
                mm(pb[:, :], l_, u_tr[:, c, :], c == 0, c == 7)
            cp(dst, pb[:, :], e=ev)

        NTRS = int(os.environ.get("MK_NTR", "4"))
        SUB = int(os.environ.get("MK_SUB", "9"))
        for tr in range(NTRS):
            S0 = W([(0, [8, 512], win[:, :, 0:512])])
            if not PLAN[0]:
                S.mark("tr%d proj" % tr)
                rms_range(tr, 1, u_tr)
                S.dma("sp", cmsk[:, :], cb_d[:, OB["cmpmask"] + tr * 512:OB["cmpmask"] + (tr + 1) * 512],
                      writes=[cmsk[:, :]])
                if tr >= 2:
                    S.dma("sp", rtab[:, :], cf_d[:, OF["rt"] + (tr - 2) * 320:OF["rt"] + (tr - 1) * 320],
                          writes=[rtab[:, :]])

                w0 = S0[:, 0:4096].rearrange("p (c f) -> p c f", c=8)
                for i in range(4):
                    proj_fm(w0, i * 128, qn[:, i, :], tr, ev="act" if i % 2 else "dve")
            S1 = W([(0, [8, 512], win[:, :, 512:1024])])
            if not PLAN[0]:
                w1_ = S1[:, 0:4096].rearrange("p (c f) -> p c f", c=8)
                proj_fm(w1_, 0, kcmpT[:, 16:528], tr, ev="act")
                proj_fm(w1_, 128, vcmpT[:, 16:528], tr, ev="dve")
                proj_fm(w1_, 256, kslcT[:, trs(tr)], tr, ev="act")
                for j in range(4):
                    t_ = tr * 4 + j
                    pb = psM.next()
                    for c in range(8):
                        mm(pb[:, 0:128], u_tr[:, c, j * 128:(j + 1) * 128], w1_[:, c, 384:512], c == 0, c == 7)
                    cp(vslc[:, t_, :, 0:64], pb[:, 0:128].rearrange("p (g d) -> p g d", g=2))
            S2 = W([(0, [8, 280], win[:, :, 1024:1304])])
            if not PLAN[0]:
                w2_ = S2[:, 0:2240].rearrange("p (c f) -> p c f", c=8)
                proj_fm(w2_, 0, kwinT[:, trs(tr)], tr, ev="act")
                for j in range(4):
                    t_ = tr * 4 + j
                    pb = psM.next()
                    for c in range(8):
                        mm(pb[:, 0:152], u_tr[:, c, j * 128:(j + 1) * 128], w2_[:, c, 128:280], c == 0, c == 7)
                    cp(vwin[:, t_, :, 0:64], pb[:, 0:128].rearrange("p (g d) -> p g d", g=2))
                    act(gsig[:, t_, :], pb[:, 128:152], AF.Sigmoid)
            S3 = W([(0, [8, 512], win[:, :, 1304:1816])])
            if not PLAN[0]:
                w3_ = S3[:, 0:4096].rearrange("p (c f) -> p c f", c=8)
                for i in range(4):
                    proj_fm(w3_, i * 128, qm[:, i, :], tr, ev="act" if i % 2 else "dve")
            S4 = W([(0, [8, 512], win[:, :, 1816:2328])])
            if not PLAN[0]:
                w4_ = S4[:, 0:4096].rearrange("p (c f) -> p c f", c=8)
                for i in range(4):
                    proj_fm(w4_, i * 128, kmT[:, i, trs(tr)], tr, ev="act" if i % 2 else "dve")
                for i in range(4):
                    S.op("dve", lambda: nc.vector.tensor_reduce(
                        out=kmsum[:, i, :], in_=kmT[:, i, trs(tr)].rearrange("p (n k) -> p n k", n=2),
                        axis=mybir.AxisListType.X, op=ALU.add),
                        [kmT[:, i, trs(tr)]], [kmsum[:, i, :]])
                ts(kmeanT[0:64, :, 2 * tr:2 * tr + 2], kmsum[0:64, :, :], 1.0 / 256.0, None, ALU.mult)
                ts(kmeanT[64:128, :, 8 + 2 * tr:8 + 2 * tr + 2], kmsum[64:128, :, :], 1.0 / 256.0, None, ALU.mult)
            if not PLAN[0]:
                MDBG = int(os.environ.get("MK_MOBA", "3"))
                if tr >= 2 and MDBG >= 1:
                    for j in range(4):
                        pb = psM.next()
                        for ch in range(4):
                            mm(pb[:, ch * 16:(ch + 1) * 16], qm[:, ch, j * 128:(j + 1) * 128],
                               kmeanT[:, ch, :], True, True)
                        GS = int(os.environ.get("MK_GS", "9"))
                        if GS >= 1:
                            tt(gsf[:, :, 0:8], pb[:, 0:64].rearrange("p (h n) -> p h n", h=8),
                               bass.AP(rtab, 256 + j * 8, [[320, 128], [0, 8], [1, 8]]), ALU.add)
                        if GS >= 2:
                            for h in range(8):
                                S.op("dve", lambda: nc.vector.max(out=m8[:, 0:8], in_=gsf[:, h, :]),
                                     [gsf[:, h, :]], [m8[:, 0:8]])
                                ts(tk32[:, h * 8:(h + 1) * 8], gsf[:, h, 0:8], m8[:, 2:3], None, ALU.is_ge)
                        if GS >= 3:
                            tt(tk32[:, :].rearrange("p (h n) -> p h n", h=8), tk32[:, :].rearrange("p (h n) -> p h n", h=8),
                               bass.AP(rtab, 288 + j * 8, [[320, 128], [0, 8], [1, 8]]), ALU.add)
                            ts(mbtok[:, 0:64], tk32[:, :], -1.0, -MASKV, ALU.add, ALU.mult)
                        if GS >= 4:
                            pb2 = psM.next()
                            tp(pb2[:, 0:128], mbtok[:, :], identf)
                            cp(mbTm[:, j * 128:(j + 1) * 128], pb2[:, 0:128])
            S5 = W([(0, [8, 512], win[:, :, 2328:2840])])
            if not PLAN[0]:
                w5_ = S5[:, 0:4096].rearrange("p (c f) -> p c f", c=8)
                for j in range(4):
                    t_ = tr * 4 + j
                    pb = psM.next()
                    for c in range(8):
                        mm(pb[:, :], u_tr[:, c, j * 128:(j + 1) * 128], w5_[:, c, :], c == 0, c == 7)
                    cp(vm[:, t_, :, 0:64], pb[:, :].rearrange("p (h d) -> p h d", h=8), e="act" if j % 2 else "dve")

            if SUB < 1:
                continue
            if not PLAN[0]:
                S.mark("tr%d compress" % tr)
            c_lo = 0 if tr == 0 else 32 * tr - 1
            c_hi = 32 * tr + 30
            nb = c_hi - c_lo + 1
            if not PLAN[0]:
                pe_drain()
            for which in range(2):
                wsrc = wd["cmp_w1_k" if which == 0 else "cmp_w1_v"].rearrange("(l d) h -> d l h", d=64)
                CW = W([(q4 * 1024, [8, 128], wsrc[:, q4 * 8:(q4 + 1) * 8, :]) for q4 in range(4)])
                if PLAN[0]:
                    continue
                srcT = kcmpT if which == 0 else vcmpT
                w1t = CW[0:64, 0:4096].rearrange("p (l h) -> p l h", l=32)
                if tr == 0:
                    pt = posk if which == 0 else posv
                    pb = psM.next()
                    for l in range(32):
                        mm(pb[:, 0:1], w1t[:, l, :], pt[0:64, l:l + 1], l == 0, l == 31)
                    cp(cbias[:, which:which + 1], pb[:, 0:1])
                for g in range(2):
                    if g == 1:
                        cp(gtmp[0:64, 0:528], srcT[64:128, 0:528])
                        src_t = gtmp
                    else:
                        src_t = srcT
                    pbA = ps[which * 2 + g]
                    for l in range(32):
                        col0 = 16 * c_lo + l - 512 * tr + 16
                        rhs_ = bass.AP(src_t, col0, [[528, 64], [16, nb]])
                        mm(pbA[:, 0:nb], w1t[:, l, :], rhs_, l == 0, l == 31)
                cp(srcT[:, 0:16], srcT[:, 512:528])
            if not PLAN[0]:
                pe_drain()
                for which in range(2):
                    for g in range(2):
                        pbA = ps[which * 2 + g]
                        xx = hid[:, 0, 0:nb]
                        x2 = hid[:, 1, 0:nb]
                        ts(xx, pbA[:, 0:nb], cbias[:, which:which + 1], None, ALU.add)
                        tt(x2, xx, xx, ALU.mult)
                        ts(x2, x2, 0.044715, 1.0, ALU.mult, ALU.add)
                        tt(x2, x2, xx, ALU.mult)
                        act(hid[:, 2, 0:nb], x2, AF.Sigmoid, scale=1.5957691216057308)
                        if which == 0:
                            tt(hidb[:, g, 0:nb], xx, hid[:, 2, 0:nb], ALU.mult)
                        else:
                            tt(hidv[:, g, c_lo:c_lo + nb], xx, hid[:, 2, 0:nb], ALU.mult)
            if SUB < 2:
                continue
            def attend_multi(jobs):
                items = []
                for jb in jobs:
                    jb["npv"] = 0
                    jb["npv_total"] = sum((c1 - c0) // 128 for (kt, c0, c1, extra) in jb["tiles"])
                    for t_ in jb["tiles"]:
                        items.append((jb, t_))

                def emit_scores(idx):
                    jb, (kt, c0, c1, extra) = items[idx]
                    if "q" not in jb:
                        jb["q"] = jb["qfn"]()
                        jb["ops"] = psO.next()
                    sp_ = psS.next()
                    mask_fn = jb.get("mask")
                    n_extra = len(extra) + (1 if mask_fn is not None else 0)
                    mm(sp_[:, c0:c1], jb["kT"](kt), jb["q"][:, c0:c1], True, n_extra == 0)
                    k_ = 0
                    if mask_fn is not None:
                        k_ += 1
                        ml, mr = mask_fn(kt)
                        mm(sp_[:, c0:c1], ml, mr[:, c0:c1], False, k_ == n_extra)
                    for (e0, en, el, er) in extra:
                        k_ += 1
                        mm(sp_[:, e0:e0 + en], el, er, False, k_ == n_extra)
                    return sp_

                DEPTH = 2
                pend = [emit_scores(i) for i in range(min(DEPTH, len(items)))]
                for ti in range(len(items)):
                    jb, (kt, c0, c1, extra) = items[ti]
                    sp_ = pend.pop(0)
                    if ti + DEPTH < len(items):
                        pend.append(emit_scores(ti + DEPTH))
                    pt = ptr.next()
                    if jb["narrow"]:
                        for j in range(c0 // 128, c1 // 128):
                            act(pt[:, j * 128:(j + 1) * 128], sp_[:, j * 128:(j + 1) * 128], AF.Exp,
                                bias=jb["bias"](kt, j), scale=0.125)
                    else:
                        act(pt[:, c0:c1], sp_[:, c0:c1], AF.Exp, bias=jb["bias"](kt, None), scale=0.125)
                    ncols = jb.get("ncols", 65)
                    for j in range(c0 // 128, c1 // 128):
                        jb["npv"] += 1
                        mm(jb["ops"][:, j * 128:j * 128 + ncols], pt[:, j * 128:(j + 1) * 128], jb["v"](kt),
                           jb["npv"] == 1, jb["npv"] == jb["npv_total"])
                    if jb["npv"] == jb["npv_total"]:
                        jb["done"](jb["ops"])

            triA = cb[:, OB["triA"]:OB["triA"] + 128]
            triB = cb[:, OB["triB"]:OB["triB"] + 128]

            def _jhi(kt, tr_, dmax):
                return min(3, (dmax + 126) // 128 + kt - 4 * tr_)

            def causal_tiles(tr_, dmax=1 << 30):
                tl = []
                for kt in range(0, 4 * tr_ + 4):
                    i = kt - 4 * tr_
                    j_lo = max(0, i)
                    j_hi = _jhi(kt, tr_, dmax)
                    if j_hi < j_lo:
                        continue
                    extra = [(128 * i, 128, identb, triA)] if i >= 0 else []
                    tl.append((kt, 128 * j_lo, 128 * (j_hi + 1), extra))
                return tl

            def win_tiles(tr_, dmax=1 << 30):
                tl = []
                for kt in range(4 * tr_ - 4, 4 * tr_ + 4):
                    if kt < 0:
                        continue
                    i = kt - 4 * tr_
                    j_hi = _jhi(kt, tr_, dmax)
                    if i < 0:
                        i2 = i + 4
                        jh = min(i2, j_hi)
                        if jh < 0:
                            continue
                        extra = [(128 * i2, 128, identb, triB)] if jh == i2 else []
                        tl.append((kt, 0, 128 * (jh + 1), extra))
                    else:
                        if j_hi < i:
                            continue
                        tl.append((kt, 128 * i, 128 * (j_hi + 1), [(128 * i, 128, identb, triA)]))
                return tl

            def dmax_of(slope):
                return int(np.ceil(64.0 / float(slope)))

            if not PLAN[0]:
                def bias_head(hidx, narrow):
                    def f(kt, j):
                        if narrow:
                            di = kt - (4 * tr + j) + 15
                            o_ = OF["bn"] + hidx * 16 + di
                        else:
                            di = kt - 4 * tr + 12
                            o_ = OF["bw"] + hidx * 16 + di
                        return cf[:, o_:o_ + 1]
                    return f

                def bias_cmp(h, narrow):
                    def f(kt, j):
                        if narrow:
                            o_ = OF["cn"] + h * 16 + (4 * tr + j)
                        else:
                            o_ = OF["cw"] + h * 4 + tr
                        return cf[:, o_:o_ + 1]
                    return f

                def norm_coef(ob_, gate_ap):
                    ov_ = ob_[:, :].rearrange("p (j c) -> p j c", j=4)
                    ts(smal[:, 0:4], ov_[:, :, 64], TINY, None, ALU.max)
                    S.op("dve", lambda: nc.vector.reciprocal(out=smal[:, 4:8], in_=smal[:, 0:4]),
                         [smal[:, 0:4]], [smal[:, 4:8]])
                    if gate_ap is not None:
                        tt(smal[:, 8:12], smal[:, 4:8], gate_ap, ALU.mult)
                    return ov_

                def qpad(src3, ch, bp, eng):
                    half = bp // 64
                    b = qz[2 * half + (qzc[half] % 2)]
                    qzc[half] += 1
                    cp(b[bp:bp + 64, :], src3[bp:bp + 64, ch, :], e=eng)
                    return b

            if not PLAN[0]:
                for g in range(2):
                    pb2 = psM.next()
                    mm(pb2[:, 0:nb], w2k[:, :], hidb[:, g, 0:nb], True, True)
                    cp(kcT[64 * g:64 * g + 64, c_lo:c_lo + nb], pb2[64 * g:64 * g + 64, 0:nb])
                    pb3 = psM.next()
                    mm(pb3[:, 0:64], hidv[:, g, :], w2v[:, :], True, True)
                    cp(vca[:, g, 0:64], pb3[:, 0:64])
                S.mark("tr%d nsa-cmp" % tr)
                ots = [otk[0], otk[1]]
            if SUB < 3:
                continue
            if not PLAN[0]:
                jobs = []
                for g in range(2):
                    ot = ots[g]
                    for hg in range(4):
                        h = g * 4 + hg
                        ch, bp = h % 4, 64 * (h // 4)
                        nar = bool(sl_n[h] > 0.3)

                        def done(oc, ot=ot, hg=hg, g=g, h=h):
                            ocv = norm_coef(oc, gsig[:, 4 * tr:4 * tr + 4, h * 3 + 0])
                            for j in range(4):
                                ts(ot[:, j, hg, :], ocv[:, j, 0:64], smal[:, 8 + j:9 + j], None, ALU.mult)
                                if tr >= 2:
                                    if hg == 0:
                                        ts(impa[:, j, g, :], ocv[:, j, 65:97], smal[:, 4 + j:5 + j], None, ALU.mult)
                                    else:
                                        stt(impa[:, j, g, :], ocv[:, j, 65:97], smal[:, 4 + j:5 + j],
                                            impa[:, j, g, :], ALU.mult, ALU.add)
                        jobs.append(dict(kT=lambda kt: kcT[:, :],
                                         qfn=lambda ch=ch, bp=bp: qpad(qn, ch, bp, "dve"),
                                         v=lambda kt, g=g: vca[:, g, :],
                                         tiles=[(0, 0, 512, [(0, 512, identb, cmsk[:, :])])],
                                         bias=bias_cmp(h, nar), narrow=nar, mask=None, ncols=97, done=done))
                attend_multi(jobs)
                if tr >= 2:
                    for j in range(4):
                        kp = rtab[:, j * 32:(j + 1) * 32]
                        fo = rtab[:, 128 + j * 32:128 + (j + 1) * 32]
                        for g in range(2):
                            iv = tk32[:, 0:32]
                            tt(iv, impa[:, j, g, :], kp, ALU.mult)
                            tt(iv, iv, fo, ALU.add)
                            S.op("dve", lambda: nc.vector.max(out=m8[:, 0:8], in_=iv), [iv], [m8[:, 0:8]])
                            S.op("dve", lambda: nc.vector.match_replace(out=tk32[:, 32:64], in_to_replace=m8[:, 0:8],
                                                                         in_values=iv, imm_value=-3.0e38),
                                 [iv, m8[:, 0:8]], [tk32[:, 32:64]])
                            S.op("dve", lambda: nc.vector.max(out=m8[:, 8:16], in_=tk32[:, 32:64]),
                                 [tk32[:, 32:64]], [m8[:, 8:16]])
                            ts(tk32[:, 32:64], iv, m8[:, 15:16], None, ALU.is_ge)
                            ts(mbtokN[:, g * 64:g * 64 + 32], tk32[:, 32:64], -1.0, -MASKV, ALU.add, ALU.mult)
                            pb = psM.next()
                            tp(pb[:, 0:128], mbtokN[:, :], identf)
                            cp(mbTn[:, g, j * 128:(j + 1) * 128], pb[:, 0:128])
                            memset(mbtokN[:, g * 64:g * 64 + 32], 0.0)
                S.mark("tr%d moba" % tr)
                jobs = []
                for hp in range(4):
                    ot = otr.next()
                    for h2 in range(2):
                        h = hp * 2 + h2
                        ch, bp = hp, 64 * h2
                        nar = bool(sl_m[h] > 0.3)
                        mfn = None
                        if tr >= 2 and MDBG >= 2:
                            def mfn(kt, h=h):
                                c_ = 8 * h + kt // 2
                                l_ = bass.AP(cb, OB["ident"] + c_, [[NBS, 128], [0, 128]])
                                return l_, mbTm[:, :]

                        def done(om, ot=ot, h2=h2, hp=hp):
                            ov_ = norm_coef(om, None)
                            for j in range(4):
                                ts(ot[:, j, h2, :], ov_[:, j, 0:64], smal[:, 4 + j:5 + j], None, ALU.mult)
                            if h2 == 1:
                                pb = psM.next()
                                for j in range(4):
                                    tp(pb[:, j * 128:(j + 1) * 128], ot[:, j, 0:2, :], identf)
                                cp(omT[:, hp, :], pb[:, :], e="act")
                        jobs.append(dict(kT=lambda kt, ch=ch: kmT[:, ch, kt * 128:(kt + 1) * 128],
                                         qfn=lambda ch=ch, bp=bp: qpad(qm, ch, bp, "dve"),
                                         v=lambda kt, h=h: vm[:, kt, h, :],
                                         tiles=causal_tiles(tr, dmax_of(sl_m[h])), bias=bias_head(8 + h, nar),
                                         narrow=nar, mask=mfn, done=done))
                attend_multi(jobs)

                S.mark("tr%d nsa-selwin" % tr)
                jobs = []
                for g in range(2):
                    ot = ots[g]
                    for hg in range(4):
                        h = g * 4 + hg
                        ch, bp = h % 4, 64 * (h // 4)
                        nar = bool(sl_n[h] > 0.3)
                        qc = {}

                        def qfn(ch=ch, bp=bp, qc=qc):
                            if "q" not in qc:
                                qc["q"] = qpad(qn, ch, bp, "dve")
                            return qc["q"]
                        mfn = None
                        if tr >= 2:
                            def mfn(kt, g=g):
                                return esel[:, kt, :], mbTn[:, g, :]

                        def done_sel(ob_, ot=ot, hg=hg, h=h):
                            ov_ = norm_coef(ob_, gsig[:, 4 * tr:4 * tr + 4, h * 3 + 1])
                            for j in range(4):
                                stt(ot[:, j, hg, :], ov_[:, j, 0:64], smal[:, 8 + j:9 + j], ot[:, j, hg, :],
                                    ALU.mult, ALU.add)

                        def done_win(ob_, ot=ot, hg=hg, h=h, g=g):
                            ov_ = norm_coef(ob_, gsig[:, 4 * tr:4 * tr + 4, h * 3 + 2])
                            for j in range(4):
                                stt(ot[:, j, hg, :], ov_[:, j, 0:64], smal[:, 8 + j:9 + j], ot[:, j, hg, :],
                                    ALU.mult, ALU.add)
                            if hg == 3:
                                for i2 in range(2):
                                    pb = psM.next()
                                    for j in range(4):
                                        tp(pb[:, j * 128:(j + 1) * 128], ot[:, j, 2 * i2:2 * i2 + 2, :], identf)
                                    cp(onT[:, 2 * g + i2, :], pb[:, :], e="act")
                        jobs.append(dict(kT=lambda kt: kslcT[:, kt * 128:(kt + 1) * 128], qfn=qfn,
                                         v=lambda kt, g=g: vslc[:, kt, g, :],
                                         tiles=causal_tiles(tr, dmax_of(sl_n[h])), bias=bias_head(h, nar),
                                         narrow=nar, mask=mfn, done=done_sel))
                        jobs.append(dict(kT=lambda kt: kwinT[:, kt * 128:(kt + 1) * 128], qfn=qfn,
                                         v=lambda kt, g=g: vwin[:, kt, g, :],
                                         tiles=win_tiles(tr, dmax_of(sl_n[h])), bias=bias_head(h, nar),
                                         narrow=nar, mask=None, done=done_win))
                attend_multi(jobs)

            if SUB < 5:
                continue
            if not PLAN[0]:
                S.mark("tr%d merge" % tr)
            for dc in range(8):
                c0 = dc * 128
                MJ = W([(0, [8, 128], win[:, :, 2840 + c0:2840 + c0 + 128]),
                        (1024, [8, 128], win[:, :, 3864 + c0:3864 + c0 + 128]),
                        (2048, [4, 128], wupn[:, :, c0:c0 + 128]),
                        (2560, [4, 128], wupm[:, :, c0:c0 + 128])])
                if PLAN[0]:
                    continue
                ga_ = MJ[:, 0:1024].rearrange("p (c f) -> p c f", c=8)
                gb_ = MJ[:, 1024:2048].rearrange("p (c f) -> p c f", c=8)
                un_ = MJ[:, 2048:2560].rearrange("p (c f) -> p c f", c=4)
                um_ = MJ[:, 2560:3072].rearrange("p (c f) -> p c f", c=4)
                pga = psS.next()
                pgb = psS.next()
                pyn = psO.next()
                pym = psO.next()
                for c in range(8):
                    mm(pga[:, :], ga_[:, c, :], u_tr[:, c, :], c == 0, c == 7)
                for c in range(8):
                    mm(pgb[:, :], gb_[:, c, :], u_tr[:, c, :], c == 0, c == 7)
                for c in range(4):
                    mm(pyn[:, :], un_[:, c, :], onT[:, c, :], c == 0, c == 3)
                for c in range(4):
                    mm(pym[:, :], um_[:, c, :], omT[:, c, :], c == 0, c == 3)
                sA = sgAr.next()
                sB = sgBr.next()
                act(sA[:, :], pga[:, :], AF.Sigmoid)
                act(sB[:, :], pgb[:, :], AF.Sigmoid)
                tt(sA[:, :], sA[:, :], pyn[:, :], ALU.mult)
                tt(sB[:, :], sB[:, :], pym[:, :], ALU.mult)
                tt(mixed[:, dc, :], sA[:, :], sB[:, :], ALU.add)
            for dh in range(2):
                WO = W([(0, [8, 512], wout[:, :, dh * 512:(dh + 1) * 512])])
                if PLAN[0]:
                    continue
                wo_ = WO[:, 0:4096].rearrange("p (c f) -> p c f", c=8)
                for d4 in range(4):
                    dc = dh * 4 + d4
                    pb = psM.next()
                    for c in range(8):
                        mm(pb[:, :], wo_[:, c, d4 * 128:(d4 + 1) * 128], mixed[:, c, :], c == 0, c == 7)
                    tt(hT[:, dc, trs(tr)], hT[:, dc, trs(tr)], pb[:, :], ALU.add)

    def run_all():
        if not PLAN[0]:
            S.mark("ffn1")
        if stage >= 1:
            scoped(phase_ffn, "ffn1_w1", "ffn1_w3", "ffn1_w2", 0, "a")
        if stage >= 2:
            scoped(phase_mixer)
        if stage >= 3:
            if not PLAN[0]:
                S.mark("ffn2")
            scoped(phase_ffn, "ffn2_w1", "ffn2_w3", "ffn2_w2", 2, "b")
        if stage >= 4:
            if not PLAN[0]:
                S.mark("ple")
            scoped(phase_ple)

    PLAN[0] = True
    run_all()
    PLAN[0] = False
    scoped(phase_load)
    run_all()
    S.mark("final")
    scoped(phase_final)
    S.mark("end")
    S.finish()
    _LAST["S"] = S
    return nc


_NC_CACHE = {}
_LAST = {}


def _prep_inputs(inputs):
    cf_np, _, cb_np, _ = _make_consts()
    f = lambda a: np.ascontiguousarray(np.asarray(a, dtype=np.float32))
    sh = {}
    for nm in ("ffn1_w1", "ffn1_w3", "ffn1_w2", "w_in", "cmp_w1_k", "cmp_w2_k", "cmp_w1_v", "cmp_w2_v",
               "w_up_nsa", "w_up_moba", "w_out", "ffn2_w1", "ffn2_w3", "ffn2_w2", "w_ple_gate", "w_ple"):
        sh[nm] = f(inputs[nm])[0]
    wi = sh["w_in"].copy()
    perm = [0, 4, 1, 5, 2, 6, 3, 7]
    wi[:, 0:512] = sh["w_in"][:, 0:512].reshape(D, 8, 64)[:, perm, :].reshape(D, 512)
    sh["w_in"] = np.ascontiguousarray(wi)
    for nm, src in (("pos_kT", "cmp_pos_k"), ("pos_vT", "cmp_pos_v")):
        pt = f(inputs[src])[0].T
        sh[nm] = np.ascontiguousarray(np.concatenate([pt, pt], axis=0))
    g = np.stack([f(inputs[n])[0] for n in ("ffn1_norm", "mix_norm", "ffn2_norm", "ple_norm")], axis=0)
    sh["gains"] = np.ascontiguousarray(g.reshape(4, 8, 128).transpose(2, 0, 1).reshape(128, 32))
    sh["gfin"] = np.ascontiguousarray(np.broadcast_to(f(inputs["final_norm"])[None, :], (128, D)))
    sh["cf32"] = cf_np
    sh["cbf16"] = cb_np
    return sh


def kernel(**inputs):
    stage = int(os.environ.get("MK_STAGE", "99"))
    ncores = int(os.environ.get("MK_CORES", "8"))
    if stage not in _NC_CACHE:
        _NC_CACHE[stage] = build_nc(stage)
    nc = _NC_CACHE[stage]
    sh = _prep_inputs(inputs)
    x = np.asarray(inputs["x"], dtype=np.float32)
    p = np.asarray(inputs["p"], dtype=np.float32)
    in_maps = []
    for b in range(ncores):
        m = dict(sh)
        m["x"] = np.ascontiguousarray(x[b])
        m["p"] = np.ascontiguousarray(p[0, b])
        in_maps.append(m)
    res = run_bass_kernel_spmd(nc, in_maps, core_ids=list(range(ncores)))
    out = np.stack([np.asarray(r["out"], dtype=np.float32) for r in res.results], axis=0)
    return out
```

```python
import os
import numpy as np
import ml_dtypes
import concourse.bass as bass
import concourse.mybir as mybir
from concourse.bass_utils import run_bass_kernel_spmd
from contextlib import ExitStack

F32 = mybir.dt.float32
BF16 = mybir.dt.bfloat16
AF = mybir.ActivationFunctionType
ALU = mybir.AluOpType

T = 2048
D = 1024
FF = 2816
MASKV = -240000.0
EPS = 1e-6
TINY = 1e-30
IN_W = 4888


def _slopes():
    s = 2.0 ** (-(np.arange(16) + 1) / 2.0)
    return s[0::2].copy(), s[1::2].copy()


def _make_consts():
    p = np.arange(128)
    f32 = {}
    b16 = {}
    f32["ident"] = np.eye(128)
    b16["ident"] = np.eye(128)
    b16["ones"] = np.ones((128, 128))
    j = np.arange(128)
    b16["triA"] = np.where(j[None, :] >= p[:, None], 0.0, MASKV)
    b16["triB"] = np.where(j[None, :] < p[:, None], 0.0, MASKV)
    tq = np.arange(T)
    cend = 16 * p + 31
    cm = np.where(tq[None, :] >= cend[:, None], 0.0, MASKV)
    cm[127, :] = MASKV
    ov = np.zeros((128, 32))
    for c in range(127):
        for jj in range(32):
            if 16 * c <= 64 * jj + 63 and 16 * c + 31 >= 64 * jj:
                ov[c, jj] = 1.0
    b16["overlap"] = ov
    b16["cmpmask"] = cm
    es = np.zeros((128, 16, 128))
    for pp in range(128):
        jj = pp % 64
        if jj >= 32:
            continue
        for kt in range(16):
            for m_ in range(128):
                if jj == 2 * kt + m_ // 64:
                    es[pp, kt, m_] = 1.0
    b16["esel"] = es.reshape(128, 2048)
    sl_n, sl_m = _slopes()
    sl16 = np.concatenate([sl_n, sl_m])
    d = np.arange(16) - 12
    f32["bw"] = (sl16[None, :, None] * (128 * d[None, None, :] + p[:, None, None] - 256)).reshape(128, 256)
    d2 = np.arange(16) - 15
    f32["bn"] = (sl16[None, :, None] * (128 * d2[None, None, :] + p[:, None, None] - 64)).reshape(128, 256)
    f32["cw"] = (sl_n[None, :, None] * (cend[:, None, None] - (512 * np.arange(4)[None, None, :] + 256))).reshape(128, 32)
    f32["cn"] = (sl_n[None, :, None] * (cend[:, None, None] - (128 * np.arange(16)[None, None, :] + 64))).reshape(128, 128)
    keep = np.zeros((128, 8, 32))
    force = np.zeros((128, 8, 32))
    for tt in range(8, 16):
        for pp in range(128):
            t = tt * 128 + pp
            cur = t // 64
            for jj in range(32):
                if 64 * jj > t:
                    force[pp, tt - 8, jj] = -1e30 * (1.0 + jj / 64.0)
                elif jj == 0:
                    force[pp, tt - 8, jj] = 3e9
                elif jj == cur:
                    force[pp, tt - 8, jj] = 2e9
                elif jj == cur - 1:
                    force[pp, tt - 8, jj] = 1e9
                else:
                    keep[pp, tt - 8, jj] = 1.0
    padneg = np.zeros((128, 8, 8))
    ownhot = np.zeros((128, 8, 8))
    for tt in range(8, 16):
        cur = tt // 2
        for n in range(8):
            if n >= cur:
                padneg[:, tt - 8, n] = -1e30
            if n == cur:
                ownhot[:, tt - 8, n] = 1.0
    f32["eps"] = np.full((128, 1), EPS)
    rt = []
    for tr in (2, 3):
        a = (tr - 2) * 4
        rt += [keep[:, a:a + 4].reshape(128, 128), force[:, a:a + 4].reshape(128, 128),
               padneg[:, a:a + 4].reshape(128, 32), ownhot[:, a:a + 4].reshape(128, 32)]
    f32["rt"] = np.concatenate(rt, axis=1)
    offs_f = {}
    cols = []
    o = 0
    for k, v in f32.items():
        offs_f[k] = o
        o += v.shape[1]
        cols.append(v.astype(np.float32))
    cf = np.ascontiguousarray(np.concatenate(cols, axis=1))
    offs_b = {}
    cols = []
    o = 0
    for k, v in b16.items():
        offs_b[k] = o
        o += v.shape[1]
        cols.append(v.astype(np.float32))
    cbv = np.ascontiguousarray(np.concatenate(cols, axis=1).astype(ml_dtypes.bfloat16))
    return cf, offs_f, cbv, offs_b


EMBED_WAIT = set(os.environ.get("MK_EMBED", "pe").split(",")) - {""}


def _box(ap):
    t = ap.tensor
    if t.name.startswith("ps") and len(t.name) == 3:
        return (t.name, 0, 128, 0, 512)
    row = 1
    for s in tuple(t.shape)[1:]:
        row *= int(s)
    off = int(ap.offset)
    p0 = off // row
    f0 = off % row
    apl = ap.ap
    npart = apl[0][1]
    ext = 1
    for st, cnt in apl[1:]:
        ext += (cnt - 1) * abs(st)
    return (t.name, p0, p0 + npart, f0, f0 + ext)


class Sched:
    def __init__(self, nc, ndma=24):
        self.nc = nc
        self.engs = {"pe": nc.tensor, "act": nc.scalar, "dve": nc.vector, "pool": nc.gpsimd, "sp": nc.sync}
        self.semh = {}
        for e in self.engs:
            self.semh[e] = nc.alloc_semaphore("sem_" + e)
        self.cnt = {e: 0 for e in self.engs}
        self.ndma = ndma
        for i in range(ndma):
            self.semh[("d", i)] = nc.alloc_semaphore("dsem%d" % i)
        self.dcnt = [0] * ndma
        self.drr = 0
        self.drr_pool = 0
        self.seen = {e: {} for e in self.engs}
        self.acc = {}
        self.out_tickets = []
        self.nwaits = 0
        self.marks = []

    def mark(self, name):
        self.marks.append((name, self.cnt["pe"]))

    def _wait(self, e, sk, val):
        if e == "pe" and sk == "pe":
            return
        if self.seen[e].get(sk, 0) >= val:
            return
        self.engs[e].wait_ge(self.semh[sk], val)
        self.seen[e][sk] = val
        self.nwaits += 1

    def _collect(self, reads, writes):
        waits = {}
        for ap in reads:
            b = _box(ap)
            for ent in self.acc.get(b[0], ()):
                if ent[0] == "w" and ent[3] < b[2] and b[1] < ent[4] and ent[5] < b[4] and b[3] < ent[6]:
                    if waits.get(ent[1], 0) < ent[2]:
                        waits[ent[1]] = ent[2]
        for ap in writes:
            b = _box(ap)
            for ent in self.acc.get(b[0], ()):
                if ent[3] < b[2] and b[1] < ent[4] and ent[5] < b[4] and b[3] < ent[6]:
                    if waits.get(ent[1], 0) < ent[2]:
                        waits[ent[1]] = ent[2]
        return waits

    def _record(self, sk, val, reads, writes):
        for ap in writes:
            b = _box(ap)
            lst = self.acc.setdefault(b[0], [])
            lst[:] = [e for e in lst if not (b[1] <= e[3] and e[4] <= b[2] and b[3] <= e[5] and e[6] <= b[4])]
            lst.append(("w", sk, val, b[1], b[2], b[3], b[4]))
        for ap in reads:
            b = _box(ap)
            lst = self.acc.setdefault(b[0], [])
            lst[:] = [e for e in lst if not (e[0] == "r" and e[1] == sk and b[1] <= e[3] and e[4] <= b[2]
                                             and b[3] <= e[5] and e[6] <= b[4])]
            lst.append(("r", sk, val, b[1], b[2], b[3], b[4]))

    def op(self, e, fn, reads, writes, inc=True):
        assert inc or e == "pe"
        waits = self._collect(reads, writes)
        emb = None
        if EMBED_WAIT and e in EMBED_WAIT:
            need = [(sk, val) for sk, val in waits.items()
                    if not (e == "pe" and sk == "pe") and self.seen[e].get(sk, 0) < val]
            if need:
                emb = need[-1]
                waits = dict(need[:-1])
            else:
                waits = {}
        for sk, val in waits.items():
            self._wait(e, sk, val)
        ins = fn()
        if emb is not None:
            ins._wait_ge(self.semh[emb[0]], emb[1])
            self.seen[e][emb[0]] = emb[1]
        if inc:
            self.cnt[e] += 1
            ins.then_inc(self.semh[e], 1)
            val = self.cnt[e]
        else:
            val = self.cnt[e] + 1
        self._record(e, val, reads, writes)
        return ins

    def dma(self, q, out, in_, reads=(), writes=(), is_out=False, **kw):
        waits = self._collect(reads, writes)
        for sk, val in waits.items():
            self._wait(q, sk, val)
        half = self.ndma // 2
        if q == "pool":
            i = half + (self.drr_pool % half)
            self.drr_pool += 1
        else:
            i = self.drr % half
            self.drr += 1
        sk = ("d", i)
        if self.dcnt[i] > 0:
            self._wait(q, sk, self.dcnt[i])
        ins = self.engs[q].dma_start(out=out, in_=in_, **kw)
        self.dcnt[i] += 16
        ins.then_inc(self.semh[sk], 16)
        self._record(sk, self.dcnt[i], reads, writes)
        if is_out:
            self.out_tickets.append((sk, self.dcnt[i]))

    def barrier(self):
        for e in ("pe", "act", "dve", "pool", "sp"):
            for f in ("pe", "act", "dve", "pool", "sp"):
                if f != e and self.cnt[f] > 0:
                    self._wait(e, f, self.cnt[f])
            for i in range(self.ndma):
                if self.dcnt[i] > 0:
                    self._wait(e, ("d", i), self.dcnt[i])
        self.acc = {}

    def finish(self):
        for sk, val in self.out_tickets:
            self._wait("sp", sk, val)


def build_nc(stage=99):
    nc = bass.Bass("TRN2", target_bir_lowering=False)
    cf_np, OF, cb_np, OB = _make_consts()
    NF = cf_np.shape[1]
    NB = cb_np.shape[1]

    def din(name, shape, dt=F32):
        return nc.dram_tensor(name, list(shape), dt, kind="ExternalInput").ap()

    x_d = din("x", [T, D])
    p_d = din("p", [T, 256])
    wd = {}
    for nm, shp in [("ffn1_w1", [D, FF]), ("ffn1_w3", [D, FF]), ("ffn1_w2", [FF, D]), ("w_in", [D, IN_W]),
                    ("cmp_w1_k", [2048, 128]), ("cmp_w2_k", [128, 64]), ("cmp_w1_v", [2048, 128]),
                    ("cmp_w2_v", [128, 64]), ("pos_kT", [128, 32]), ("pos_vT", [128, 32]),
                    ("w_up_nsa", [512, D]), ("w_up_moba", [512, D]), ("w_out", [D, D]),
                    ("ffn2_w1", [D, FF]), ("ffn2_w3", [D, FF]), ("ffn2_w2", [FF, D]),
                    ("w_ple_gate", [D, D]), ("w_ple", [256, D]), ("gains", [128, 32]), ("gfin", [128, D])]:
        wd[nm] = din(nm, shp)
    cf_d = din("cf32", [128, NF])
    cb_d = din("cbf16", [128, NB], BF16)
    out_d = nc.dram_tensor("out", [T, D], F32, kind="ExternalOutput").ap()

    S = Sched(nc)
    stack = [None]

    def TS(name, shape, dt):
        if stack[0] is None:
            return nc.alloc_sbuf_tensor(name, shape, dt)
        return stack[0].enter_context(nc.sbuf_tensor(name, shape, dt))

    def scoped(fn, *a):
        if PLAN[0]:
            fn(*a)
            return
        S.barrier()
        with ExitStack() as st:
            stack[0] = st
            fn(*a)
            S.barrier()
        stack[0] = None

    hT = TS("hT", [128, 8, T], F32)
    NFS = OF["rt"]
    NBS = OB["cmpmask"]
    cf = TS("cf", [128, NFS], F32)
    cb = TS("cb", [128, NBS], BF16)
    gains = TS("gains_sb", [128, 32], F32)
    slab = [TS("slab%d" % i, [128, 4096], BF16) for i in range(3)]
    ps = [nc.alloc_psum_tensor("ps%d" % i, [128, 512], F32) for i in range(8)]

    identf = cf[:, OF["ident"]:OF["ident"] + 128]
    identb = cb[:, OB["ident"]:OB["ident"] + 128]
    onesb = cb[:, OB["ones"]:OB["ones"] + 128]
    epsc = cf[:, OF["eps"]:OF["eps"] + 1]

    S.dma("sp", cf[:, :], cf_d[:, 0:NFS], writes=[cf[:, :]])
    S.dma("sp", cb[:, :], cb_d[:, 0:NBS], writes=[cb[:, :]])
    S.dma("sp", gains[:, :], wd["gains"][:, :], writes=[gains[:, :]])

    def mm(out, lhsT, rhs, start, stop, inc=None):
        inc = True
        return S.op("pe", lambda: nc.tensor.matmul(out, lhsT, rhs, start=start, stop=stop),
                    [lhsT, rhs], [out], inc=inc)

    def pe_drain():
        if S.cnt["pe"] > 0:
            nc.tensor.wait_ge(S.semh["pe"], S.cnt["pe"])

    def tp(out, in_, ident):
        return S.op("pe", lambda: nc.tensor.transpose(out, in_, ident), [in_, ident], [out])

    def act(out, in_, func, bias=None, scale=None, accum_out=None):
        kw = {}
        rd = [in_]
        wr = [out]
        if bias is not None:
            kw["bias"] = bias
            if not isinstance(bias, (int, float)):
                rd.append(bias)
        if scale is not None:
            kw["scale"] = scale
            if not isinstance(scale, (int, float)):
                rd.append(scale)
        if accum_out is not None:
            kw["accum_out"] = accum_out
            wr.append(accum_out)
        return S.op("act", lambda: nc.scalar.activation(out=out, in_=in_, func=func, **kw), rd, wr)

    def tt(out, in0, in1, op, e="dve"):
        eng = nc.vector if e == "dve" else nc.gpsimd
        return S.op(e, lambda: eng.tensor_tensor(out=out, in0=in0, in1=in1, op=op), [in0, in1], [out])

    def ts(out, in0, s1, s2, op0, op1=None, e="dve"):
        eng = nc.vector if e == "dve" else nc.gpsimd
        rd = [in0]
        if not isinstance(s1, (int, float)):
            rd.append(s1)
        if s2 is not None and not isinstance(s2, (int, float)):
            rd.append(s2)
        if op1 is None:
            return S.op(e, lambda: eng.tensor_scalar(out=out, in0=in0, scalar1=s1, scalar2=None, op0=op0), rd, [out])
        return S.op(e, lambda: eng.tensor_scalar(out=out, in0=in0, scalar1=s1, scalar2=s2, op0=op0, op1=op1),
                    rd, [out])

    def stt(out, in0, scalar, in1, op0, op1):
        rd = [in0, in1]
        if not isinstance(scalar, (int, float)):
            rd.append(scalar)
        return S.op("dve", lambda: nc.vector.scalar_tensor_tensor(out=out, in0=in0, scalar=scalar, in1=in1,
                                                                   op0=op0, op1=op1), rd, [out])

    def cp(out, in_, e="dve"):
        if e == "act":
            return S.op("act", lambda: nc.scalar.copy(out=out, in_=in_), [in_], [out])
        eng = nc.vector if e == "dve" else nc.gpsimd
        return S.op(e, lambda: eng.tensor_copy(out=out, in_=in_), [in_], [out])

    def memset(ap, v, e="dve"):
        eng = nc.vector if e == "dve" else nc.gpsimd
        return S.op(e, lambda: eng.memset(ap, v), [], [ap])

    class RR:
        def __init__(self, items):
            self.items = items
            self.i = 0

        def next(self):
            it = self.items[self.i % len(self.items)]
            self.i += 1
            return it

    class WStream:
        def __init__(self):
            self.jobs = []
            self.issued = 0
            self.k = 0

        def add(self, parts):
            self.jobs.append(parts)

        def _issue(self, k):
            buf = slab[k % 3]
            for (off, shape, src) in self.jobs[k]:
                n = 1
                for s_ in shape:
                    n *= s_
                npart = int(src.shape[0])
                dst = buf[0:npart, off:off + n]
                if len(shape) == 2:
                    dst = dst.rearrange("p (a b) -> p a b", a=shape[0])
                S.dma("pool", dst, src, writes=[buf[0:npart, off:off + n]])

        def get(self):
            k = self.k
            self.k += 1
            while self.issued < min(k + 2, len(self.jobs)):
                self._issue(self.issued)
                self.issued += 1
            return slab[k % 3]

    WS = WStream()
    PLAN = [True]

    def W(parts):
        if PLAN[0]:
            WS.add(parts)
            return None
        return WS.get()

    def trs(tr):
        return slice(tr * 512, (tr + 1) * 512)

    sqb = [TS("sqb%d" % i, [128, 512], BF16) for i in range(2)]
    rstd = TS("rstd", [128, 512], F32)
    sqrr = RR(sqb)
    psM = RR([ps[6], ps[7]])

    def rms_range(tr, gidx, xn3):
        pb = psM.next()
        for c in range(8):
            sq = sqrr.next()
            act(sq[:, :], hT[:, c, trs(tr)], AF.Square)
            mm(pb[:, :], onesb, sq[:, :], c == 0, c == 7)
        act(rstd[:, :], pb[:, :], AF.Ln, bias=epsc, scale=1.0 / D)
        act(rstd[:, :], rstd[:, :], AF.Exp, scale=-0.5)
        for c in range(8):
            stt(xn3[:, c, :], hT[:, c, trs(tr)], gains[:, gidx * 8 + c:gidx * 8 + c + 1], rstd[:, :],
                ALU.mult, ALU.mult)

    def phase_load():
        xtok = [TS("xtok%d" % i, [128, D], F32) for i in range(2)]
        for t_ in range(16):
            xb = xtok[t_ % 2]
            S.dma("sp", xb[:, :], x_d[t_ * 128:(t_ + 1) * 128, :], writes=[xb[:, :]])
            for half in range(2):
                pb = psM.next()
                for c4 in range(4):
                    c = half * 4 + c4
                    tp(pb[:, c4 * 128:(c4 + 1) * 128], xb[:, c * 128:(c + 1) * 128], identf)
                cp(hT[:, half * 4:(half + 1) * 4, t_ * 128:(t_ + 1) * 128],
                   pb[:, :].rearrange("p (c t) -> p c t", c=4), e="act" if half else "dve")

    def phase_ffn(w1n, w3n, w2n, gidx, tag):
        w1 = wd[w1n].rearrange("(c p) f -> p c f", p=128)
        w3 = wd[w3n].rearrange("(c p) f -> p c f", p=128)
        w2 = wd[w2n]
        if not PLAN[0]:
            xnT = TS("xnT" + tag, [128, 8, T], BF16)
            h1 = [TS("h1%s%d" % (tag, i), [128, 2, 512], BF16) for i in range(2)]
            sa = [TS("sa%s%d" % (tag, i), [128, 512], F32) for i in range(2)]
            for tr in range(4):
                rms_range(tr, gidx, xnT[:, :, trs(tr)])
            psAB = RR([ps[0], ps[1], ps[2], ps[3]])
            psY = RR([ps[4], ps[5], ps[6], ps[7]])
            sar = RR(sa)
            h1r = RR(h1)
            ytmp = [TS("ytmp%s%d" % (tag, i), [128, 512], F32) for i in range(2)]
            ytr = RR(ytmp)
        Aj = {}
        Bj = {}

        def getA(s_):
            if s_ not in Aj:
                Aj[s_] = W([(0, [8, 256], w1[:, :, s_ * 256:(s_ + 1) * 256]),
                            (2048, [8, 256], w3[:, :, s_ * 256:(s_ + 1) * 256])])
            return Aj[s_]

        def getB(s_):
            if s_ not in Bj:
                Bj[s_] = W([(0, [2, 1024], w2[s_ * 256:(s_ + 1) * 256, :].rearrange("(j p) d -> p j d", p=128))])
            return Bj[s_]

        def ab_steps(u):
            s_, tr = u
            A = getA(s_)
            if PLAN[0]:
                return
            w1s = A[:, 0:2048].rearrange("p (c f) -> p c f", c=8)
            w3s = A[:, 2048:4096].rearrange("p (c f) -> p c f", c=8)
            hb = h1r.next()
            hbs[u] = hb
            for j in range(2):
                pa = psAB.next()
                pb = psAB.next()
                for c in range(8):
                    mm(pa[:, :], w1s[:, c, j * 128:(j + 1) * 128], xnT[:, c, trs(tr)], c == 0, c == 7)
                sab = sar.next()
                act(sab[:, :], pa[:, :], AF.Silu)
                yield
                for c in range(8):
                    mm(pb[:, :], w3s[:, c, j * 128:(j + 1) * 128], xnT[:, c, trs(tr)], c == 0, c == 7)
                tt(hb[:, j, :], sab[:, :], pb[:, :], ALU.mult)
                yield

        def y_steps(u):
            s_, tr = u
            B = getB(s_)
            if PLAN[0]:
                return
            hb = hbs.pop(u)
            w2s = B[:, 0:2048].rearrange("p (j d) -> p j d", j=2)
            for dc in range(8):
                py = psY.next()
                for j in range(2):
                    mm(py[:, :], w2s[:, j, dc * 128:(dc + 1) * 128], hb[:, j, :], j == 0, j == 1)
                stt(hT[:, dc, trs(tr)], py[:, :], 0.5, hT[:, dc, trs(tr)], ALU.mult, ALU.add)
                yield

        hbs = {}
        units = [(s_, tr) for s_ in range(11) for tr in range(4)]
        prev = None
        for u in units:
            ga = ab_steps(u)
            gy = y_steps(prev) if prev is not None else iter(())
            for _ in ga:
                next(gy, None)
                next(gy, None)
            for _ in gy:
                pass
            prev = u
        for _ in y_steps(prev):
            pass

    def phase_ple():
        wg = wd["w_ple_gate"].rearrange("(c p) f -> p c f", p=128)
        wp = wd["w_ple"].rearrange("(c p) f -> p c f", p=128)
        if not PLAN[0]:
            xnT = TS("xnTple", [128, 8, T], BF16)
            pT = TS("pT", [128, 2, T], BF16)
            ptok = [TS("ptok%d" % i, [128, 256], F32) for i in range(2)]
            sg = [TS("sgple%d" % i, [128, 512], F32) for i in range(2)]
            for t_ in range(16):
                pbuf = ptok[t_ % 2]
                S.dma("sp", pbuf[:, :], p_d[t_ * 128:(t_ + 1) * 128, :], writes=[pbuf[:, :]])
                pb = psM.next()
                for c in range(2):
                    tp(pb[:, c * 128:(c + 1) * 128], pbuf[:, c * 128:(c + 1) * 128], identf)
                cp(pT[:, :, t_ * 128:(t_ + 1) * 128], pb[:, 0:256].rearrange("p (c t) -> p c t", c=2))
            for tr in range(4):
                rms_range(tr, 3, xnT[:, :, trs(tr)])
            psG = RR([ps[0], ps[1], ps[2], ps[3]])
            sgr = RR(sg)
        for dh in range(2):
            A = W([(0, [8, 512], wg[:, :, dh * 512:(dh + 1) * 512])])
            B = W([(0, [2, 512], wp[:, :, dh * 512:(dh + 1) * 512])])
            if PLAN[0]:
                continue
            wgs = A[:, 0:4096].rearrange("p (c f) -> p c f", c=8)
            wps = B[:, 0:1024].rearrange("p (c f) -> p c f", c=2)
            for tr in range(4):
                for d4 in range(4):
                    dc = dh * 4 + d4
                    pg = psG.next()
                    pp = psG.next()
                    for c in range(8):
                        mm(pg[:, :], wgs[:, c, d4 * 128:(d4 + 1) * 128], xnT[:, c, trs(tr)], c == 0, c == 7)
                    for c in range(2):
                        mm(pp[:, :], wps[:, c, d4 * 128:(d4 + 1) * 128], pT[:, c, trs(tr)], c == 0, c == 1)
                    sgb = sgr.next()
                    act(sgb[:, :], pg[:, :], AF.Sigmoid)
                    tt(sgb[:, :], sgb[:, :], pp[:, :], ALU.mult)
                    tt(hT[:, dc, trs(tr)], hT[:, dc, trs(tr)], sgb[:, :], ALU.add)

    def phase_final():
        gfin = TS("gfin_sb", [128, D], F32)
        S.dma("sp", gfin[:, :], wd["gfin"][:, :], writes=[gfin[:, :]])
        otok = [TS("otok%d" % i, [128, D], F32) for i in range(2)]
        junk = TS("junk", [128, 512], F32)
        ssq = TS("ssq", [128, 4], F32)
        psF = RR([ps[0], ps[1], ps[2], ps[3], ps[4], ps[5]])
        for t_ in range(16):
            ob = otok[t_ % 2]
            pbs = [psF.next(), psF.next()]
            for half in range(2):
                for c4 in range(4):
                    c = half * 4 + c4
                    tp(pbs[half][:, c4 * 128:(c4 + 1) * 128], hT[:, c, t_ * 128:(t_ + 1) * 128], identf)
                act(junk[:, :], pbs[half][:, :], AF.Square, accum_out=ssq[:, half:half + 1])
            tt(ssq[:, 2:3], ssq[:, 0:1], ssq[:, 1:2], ALU.add)
            act(ssq[:, 3:4], ssq[:, 2:3], AF.Ln, bias=epsc, scale=1.0 / D)
            act(ssq[:, 3:4], ssq[:, 3:4], AF.Exp, scale=-0.5)
            for half in range(2):
                stt(ob[:, half * 512:(half + 1) * 512], pbs[half][:, :], ssq[:, 3:4],
                    gfin[:, half * 512:(half + 1) * 512], ALU.mult, ALU.mult)
            S.dma("sp", out_d[t_ * 128:(t_ + 1) * 128, :], ob[:, :], reads=[ob[:, :]], is_out=True)

    def phase_mixer():
        win = wd["w_in"].rearrange("(c p) f -> p c f", p=128)
        wupn = wd["w_up_nsa"].rearrange("(c p) f -> p c f", p=128)
        wupm = wd["w_up_moba"].rearrange("(c p) f -> p c f", p=128)
        wout = wd["w_out"].rearrange("(c p) f -> p c f", p=128)
        sl_n, sl_m = _slopes()
        if not PLAN[0]:
            kslcT = TS("kslcT", [128, T], BF16)
            kwinT = TS("kwinT", [128, T], BF16)
            kcmpT = TS("kcmpT", [128, 528], BF16)
            vcmpT = TS("vcmpT", [128, 528], BF16)
            kmT = TS("kmT", [128, 4, T], BF16)
            vslc = TS("vslc", [128, 16, 2, 65], BF16)
            vwin = TS("vwin", [128, 16, 2, 65], BF16)
            vm = TS("vm", [128, 16, 8, 65], BF16)
            gsig = TS("gsig", [128, 16, 24], F32)
            kcT = TS("kcT", [128, 128], BF16)
            vca = TS("vca", [128, 2, 97], BF16)
            kmeanT = TS("kmeanT", [128, 4, 16], BF16)
            kmsum = TS("kmsum", [128, 4, 2], F32)
            w2k = TS("w2k", [128, 128], BF16)
            w2v = TS("w2v", [128, 64], BF16)
            posk = TS("posk", [128, 32], BF16)
            posv = TS("posv", [128, 32], BF16)
            cbias = TS("cbias", [128, 2], F32)
            u_tr = TS("u_tr", [128, 8, 512], BF16)
            qmix = TS("qmix", [128, 8, 512], BF16)

            class _Sub:
                def __init__(self, base, off):
                    self.base, self.off = base, off

                def __getitem__(self, key):
                    a, b, c = key
                    if isinstance(b, int):
                        b = b + self.off
                    else:
                        b = slice((b.start or 0) + self.off, (b.stop if b.stop is not None else 4) + self.off)
                    return self.base[a, b, c]
            qn = _Sub(qmix, 0)
            qm = _Sub(qmix, 4)
            ptl = [TS("ptl%d" % i, [128, 512], BF16) for i in range(3)]
            otk = [TS("otk%d" % i, [128, 4, 4, 64], F32) for i in range(2)]
            omk = [TS("omk%d" % i, [128, 4, 2, 64], F32) for i in range(2)]
            onT = TS("onT", [128, 4, 512], BF16)
            omT = TS("omT", [128, 4, 512], BF16)
            mixed = qmix
            _o0 = otk[0][:, :, :, :].rearrange("p a b c -> p (a b c)")
            sgA = [_o0[:, 0:512]]
            sgB = [_o0[:, 512:1024]]
            mbTn = TS("mbTn", [128, 2, 512], BF16)
            qz = [TS("qz%d" % i, [128, 512], BF16) for i in range(4)]
            qzc = [0, 0]
            mbTm = TS("mbTm", [128, 512], BF16)
            mbtokN = TS("mbtokN", [128, 4, 2, 32], F32)
            mbtok = TS("mbtok", [128, 4, 64], F32)
            impa = TS("impa", [128, 4, 2, 32], F32)
            smal = TS("smal", [128, 16], F32)
            m8 = TS("m8", [128, 16], F32)
            tk32 = TS("tk32", [128, 64], F32)
            cmsk = TS("cmsk", [128, 512], BF16)
            rtab = TS("rtab", [128, 320], F32)
            esel = TS("esel", [128, 16, 128], BF16)
            S.dma("sp", esel[:, :, :], cb_d[:, OB["esel"]:OB["esel"] + 2048].rearrange("p (k m) -> p k m", k=16),
                  writes=[esel[:, :, :]])
            hid = TS("hid", [128, 3, 32], F32)
            hidb = TS("hidb", [128, 2, 32], BF16)
            hidv = TS("hidv", [128, 2, 128], BF16)
            gtmp = TS("gtmp", [64, 528], BF16)
            gsf = TS("gsf", [128, 8, 16], F32)

            memset(kcT[:, :], 0.0)
            for i in range(4):
                memset(qz[i][:, :], 0.0)
            memset(mbTn[:, :, :], 0.0)
            memset(mbTm[:, :], 0.0)
            memset(gsf[:, :, :], -1.0e30)
            memset(kcmpT[:, 0:16], 0.0)
            memset(vcmpT[:, 0:16], 0.0)
            memset(hidv[:, :, :], 0.0)
            memset(vca[:, :, :], 0.0)
            memset(kmeanT[:, :, :], 0.0)
            memset(vslc[:, :, :, 64:65], 1.0)
            memset(vwin[:, :, :, 64:65], 1.0)
            memset(vm[:, :, :, 64:65], 1.0)
            memset(vca[:, :, 64:65], 1.0)
            for g in range(2):
                cp(vca[:, g, 65:97], cb[:, OB["overlap"]:OB["overlap"] + 32])
            for (w2t, w2n_, pt, pn) in ((w2k, "cmp_w2_k", posk, "pos_kT"), (w2v, "cmp_w2_v", posv, "pos_vT")):
                S.dma("pool", pt[:, :], wd[pn][:, :], writes=[pt[:, :]])
                if w2t is w2k:
                    S.dma("pool", w2t[:, 0:64], wd[w2n_][:, :], writes=[w2t[:, 0:64]])
                    S.dma("pool", w2t[:, 64:128], wd[w2n_][:, :], writes=[w2t[:, 64:128]])
                else:
                    S.dma("pool", w2t[:, :], wd[w2n_][:, :], writes=[w2t[:, :]])

            psS = RR([ps[0], ps[1], ps[2]])
            psO = RR([ps[3], ps[4], ps[5]])
            ptr = RR(ptl)
            otr = RR(omk)
            sgAr = RR(sgA)
            sgBr = RR(sgB)

        def proj_fm(ws, c0, dst, tr, base=None, ev="act"):
            pb = psM.next()
            for c in range(8):
                if base is None:
                    l_ = ws[:, c, c0:c0 + 128]
                else:
                    l_ = base(c)
                mm(pb[:, :], l_, u_tr[:, c, :], c == 0, c == 7)
            cp(dst, pb[:, :], e=ev)

        NTRS = int(os.environ.get("MK_NTR", "4"))
        SUB = int(os.environ.get("MK_SUB", "9"))
        for tr in range(NTRS):
            S0 = W([(0, [8, 512], win[:, :, 0:512])])
            if not PLAN[0]:
                S.mark("tr%d proj" % tr)
                rms_range(tr, 1, u_tr)
                S.dma("sp", cmsk[:, :], cb_d[:, OB["cmpmask"] + tr * 512:OB["cmpmask"] + (tr + 1) * 512],
                      writes=[cmsk[:, :]])
                if tr >= 2:
                    S.dma("sp", rtab[:, :], cf_d[:, OF["rt"] + (tr - 2) * 320:OF["rt"] + (tr - 1) * 320],
                          writes=[rtab[:, :]])

                w0 = S0[:, 0:4096].rearrange("p (c f) -> p c f", c=8)
                for i in range(4):
                    proj_fm(w0, i * 128, qn[:, i, :], tr, ev="act" if i % 2 else "dve")
            S1 = W([(0, [8, 512], win[:, :, 512:1024])])
            if not PLAN[0]:
                w1_ = S1[:, 0:4096].rearrange("p (c f) -> p c f", c=8)
                proj_fm(w1_, 0, kcmpT[:, 16:528], tr, ev="act")
                proj_fm(w1_, 128, vcmpT[:, 16:528], tr, ev="dve")
                proj_fm(w1_, 256, kslcT[:, trs(tr)], tr, ev="act")
                for j in range(4):
                    t_ = tr * 4 + j
                    pb = psM.next()
                    for c in range(8):
                        mm(pb[:, 0:128], u_tr[:, c, j * 128:(j + 1) * 128], w1_[:, c, 384:512], c == 0, c == 7)
                    cp(vslc[:, t_, :, 0:64], pb[:, 0:128].rearrange("p (g d) -> p g d", g=2))
            S2 = W([(0, [8, 280], win[:, :, 1024:1304])])
            if not PLAN[0]:
                w2_ = S2[:, 0:2240].rearrange("p (c f) -> p c f", c=8)
                proj_fm(w2_, 0, kwinT[:, trs(tr)], tr, ev="act")
                for j in range(4):
                    t_ = tr * 4 + j
                    pb = psM.next()
                    for c in range(8):
                        mm(pb[:, 0:152], u_tr[:, c, j * 128:(j + 1) * 128], w2_[:, c, 128:280], c == 0, c == 7)
                    cp(vwin[:, t_, :, 0:64], pb[:, 0:128].rearrange("p (g d) -> p g d", g=2))
                    act(gsig[:, t_, :], pb[:, 128:152], AF.Sigmoid)
            S3 = W([(0, [8, 512], win[:, :, 1304:1816])])
            if not PLAN[0]:
                w3_ = S3[:, 0:4096].rearrange("p (c f) -> p c f", c=8)
                for i in range(4):
                    proj_fm(w3_, i * 128, qm[:, i, :], tr, ev="act" if i % 2 else "dve")
            S4 = W([(0, [8, 512], win[:, :, 1816:2328])])
            if not PLAN[0]:
                w4_ = S4[:, 0:4096].rearrange("p (c f) -> p c f", c=8)
                for i in range(4):
                    proj_fm(w4_, i * 128, kmT[:, i, trs(tr)], tr, ev="act" if i % 2 else "dve")
                for i in range(4):
                    S.op("dve", lambda: nc.vector.tensor_reduce(
                        out=kmsum[:, i, :], in_=kmT[:, i, trs(tr)].rearrange("p (n k) -> p n k", n=2),
                        axis=mybir.AxisListType.X, op=ALU.add),
                        [kmT[:, i, trs(tr)]], [kmsum[:, i, :]])
                ts(kmeanT[0:64, :, 2 * tr:2 * tr + 2], kmsum[0:64, :, :], 1.0 / 256.0, None, ALU.mult)
                ts(kmeanT[64:128, :, 8 + 2 * tr:8 + 2 * tr + 2], kmsum[64:128, :, :], 1.0 / 256.0, None, ALU.mult)
            if not PLAN[0]:
                MDBG = int(os.environ.get("MK_MOBA", "3"))
                if tr >= 2 and MDBG >= 1:
                    for j in range(4):
                        pb = psM.next()
                        for ch in range(4):
                            mm(pb[:, ch * 16:(ch + 1) * 16], qm[:, ch, j * 128:(j + 1) * 128],
                               kmeanT[:, ch, :], True, True)
                        GS = int(os.environ.get("MK_GS", "9"))
                        if GS >= 1:
                            tt(gsf[:, :, 0:8], pb[:, 0:64].rearrange("p (h n) -> p h n", h=8),
                               bass.AP(rtab, 256 + j * 8, [[320, 128], [0, 8], [1, 8]]), ALU.add)
                        if GS >= 2:
                            for h in range(8):
                                S.op("dve", lambda: nc.vector.max(out=m8[:, 0:8], in_=gsf[:, h, :]),
                                     [gsf[:, h, :]], [m8[:, 0:8]])
                                ts(tk32[:, h * 8:(h + 1) * 8], gsf[:, h, 0:8], m8[:, 2:3], None, ALU.is_ge)
                        if GS >= 3:
                            tt(tk32[:, :].rearrange("p (h n) -> p h n", h=8), tk32[:, :].rearrange("p (h n) -> p h n", h=8),
                               bass.AP(rtab, 288 + j * 8, [[320, 128], [0, 8], [1, 8]]), ALU.add)
                            ts(mbtok[:, j, :], tk32[:, :], -1.0, -MASKV, ALU.add, ALU.mult)
            S5 = W([(0, [8, 512], win[:, :, 2328:2840])])
            if not PLAN[0]:
                w5_ = S5[:, 0:4096].rearrange("p (c f) -> p c f", c=8)
                for j in range(4):
                    t_ = tr * 4 + j
                    pb = psM.next()
                    for c in range(8):
                        mm(pb[:, :], u_tr[:, c, j * 128:(j + 1) * 128], w5_[:, c, :], c == 0, c == 7)
                    cp(vm[:, t_, :, 0:64], pb[:, :].rearrange("p (h d) -> p h d", h=8), e="act" if j % 2 else "dve")

            if SUB < 1:
                continue
            if not PLAN[0]:
                S.mark("tr%d compress" % tr)
            c_lo = 0 if tr == 0 else 32 * tr - 1
            c_hi = 32 * tr + 30
            nb = c_hi - c_lo + 1
            if not PLAN[0]:
                pe_drain()
            for which in range(2):
                wsrc = wd["cmp_w1_k" if which == 0 else "cmp_w1_v"].rearrange("(l d) h -> d l h", d=64)
                CW = W([(q4 * 1024, [8, 128], wsrc[:, q4 * 8:(q4 + 1) * 8, :]) for q4 in range(4)])
                if PLAN[0]:
                    continue
                srcT = kcmpT if which == 0 else vcmpT
                w1t = CW[0:64, 0:4096].rearrange("p (l h) -> p l h", l=32)
                if tr == 0:
                    pt = posk if which == 0 else posv
                    pb = psM.next()
                    for l in range(32):
                        mm(pb[:, 0:1], w1t[:, l, :], pt[0:64, l:l + 1], l == 0, l == 31)
                    cp(cbias[:, which:which + 1], pb[:, 0:1])
                for g in range(2):
                    if g == 1:
                        cp(gtmp[0:64, 0:528], srcT[64:128, 0:528])
                        src_t = gtmp
                    else:
                        src_t = srcT
                    pbA = ps[which * 2 + g]
                    for l in range(32):
                        col0 = 16 * c_lo + l - 512 * tr + 16
                        rhs_ = bass.AP(src_t, col0, [[528, 64], [16, nb]])
                        mm(pbA[:, 0:nb], w1t[:, l, :], rhs_, l == 0, l == 31)
                cp(srcT[:, 0:16], srcT[:, 512:528])
            if not PLAN[0]:
                pe_drain()
                for which in range(2):
                    for g in range(2):
                        pbA = ps[which * 2 + g]
                        xx = hid[:, 0, 0:nb]
                        x2 = hid[:, 1, 0:nb]
                        ts(xx, pbA[:, 0:nb], cbias[:, which:which + 1], None, ALU.add)
                        tt(x2, xx, xx, ALU.mult)
                        ts(x2, x2, 0.044715, 1.0, ALU.mult, ALU.add)
                        tt(x2, x2, xx, ALU.mult)
                        act(hid[:, 2, 0:nb], x2, AF.Sigmoid, scale=1.5957691216057308)
                        if which == 0:
                            tt(hidb[:, g, 0:nb], xx, hid[:, 2, 0:nb], ALU.mult)
                        else:
                            tt(hidv[:, g, c_lo:c_lo + nb], xx, hid[:, 2, 0:nb], ALU.mult)
            if SUB < 2:
                continue
            def attend_multi(jobs):
                items = []
                for jb in jobs:
                    jb["npv"] = 0
                    jb["npv_total"] = sum((c1 - c0) // 128 for (kt, c0, c1, extra) in jb["tiles"])
                    for t_ in jb["tiles"]:
                        items.append((jb, t_))

                def emit_scores(idx):
                    jb, (kt, c0, c1, extra) = items[idx]
                    if "q" not in jb:
                        jb["q"] = jb["qfn"]()
                        jb["ops"] = psO.next()
                    sp_ = psS.next()
                    mask_fn = jb.get("mask")
                    n_extra = len(extra) + (1 if mask_fn is not None else 0)
                    mm(sp_[:, c0:c1], jb["kT"](kt), jb["q"][:, c0:c1], True, n_extra == 0)
                    k_ = 0
                    if mask_fn is not None:
                        k_ += 1
                        ml, mr = mask_fn(kt)
                        mm(sp_[:, c0:c1], ml, mr[:, c0:c1], False, k_ == n_extra)
                    for (e0, en, el, er) in extra:
                        k_ += 1
                        mm(sp_[:, e0:e0 + en], el, er, False, k_ == n_extra)
                    return sp_

                DEPTH = 2
                pend = [emit_scores(i) for i in range(min(DEPTH, len(items)))]
                for ti in range(len(items)):
                    jb, (kt, c0, c1, extra) = items[ti]
                    sp_ = pend.pop(0)
                    if ti + DEPTH < len(items):
                        pend.append(emit_scores(ti + DEPTH))
                    pt = ptr.next()
                    if jb["narrow"]:
                        for j in range(c0 // 128, c1 // 128):
                            act(pt[:, j * 128:(j + 1) * 128], sp_[:, j * 128:(j + 1) * 128], AF.Exp,
                                bias=jb["bias"](kt, j), scale=0.125)
                    else:
                        act(pt[:, c0:c1], sp_[:, c0:c1], AF.Exp, bias=jb["bias"](kt, None), scale=0.125)
                    ncols = jb.get("ncols", 65)
                    for j in range(c0 // 128, c1 // 128):
                        jb["npv"] += 1
                        mm(jb["ops"][:, j * 128:j * 128 + ncols], pt[:, j * 128:(j + 1) * 128], jb["v"](kt),
                           jb["npv"] == 1, jb["npv"] == jb["npv_total"])
                    if jb["npv"] == jb["npv_total"]:
                        jb["done"](jb["ops"])

            triA = cb[:, OB["triA"]:OB["triA"] + 128]
            triB = cb[:, OB["triB"]:OB["triB"] + 128]

            def _jhi(kt, tr_, dmax):
                return min(3, (dmax + 126) // 128 + kt - 4 * tr_)

            def causal_tiles(tr_, dmax=1 << 30):
                tl = []
                for kt in range(0, 4 * tr_ + 4):
                    i = kt - 4 * tr_
                    j_lo = max(0, i)
                    j_hi = _jhi(kt, tr_, dmax)
                    if j_hi < j_lo:
                        continue
                    extra = [(128 * i, 128, identb, triA)] if i >= 0 else []
                    tl.append((kt, 128 * j_lo, 128 * (j_hi + 1), extra))
                return tl

            def win_tiles(tr_, dmax=1 << 30):
                tl = []
                for kt in range(4 * tr_ - 4, 4 * tr_ + 4):
                    if kt < 0:
                        continue
                    i = kt - 4 * tr_
                    j_hi = _jhi(kt, tr_, dmax)
                    if i < 0:
                        i2 = i + 4
                        jh = min(i2, j_hi)
                        if jh < 0:
                            continue
                        extra = [(128 * i2, 128, identb, triB)] if jh == i2 else []
                        tl.append((kt, 0, 128 * (jh + 1), extra))
                    else:
                        if j_hi < i:
                            continue
                        tl.append((kt, 128 * i, 128 * (j_hi + 1), [(128 * i, 128, identb, triA)]))
                return tl

            def dmax_of(slope):
                return int(np.ceil(64.0 / float(slope)))

            if not PLAN[0]:
                def bias_head(hidx, narrow):
                    def f(kt, j):
                        if narrow:
                            di = kt - (4 * tr + j) + 15
                            o_ = OF["bn"] + hidx * 16 + di
                        else:
                            di = kt - 4 * tr + 12
                            o_ = OF["bw"] + hidx * 16 + di
                        return cf[:, o_:o_ + 1]
                    return f

                def bias_cmp(h, narrow):
                    def f(kt, j):
                        if narrow:
                            o_ = OF["cn"] + h * 16 + (4 * tr + j)
                        else:
                            o_ = OF["cw"] + h * 4 + tr
                        return cf[:, o_:o_ + 1]
                    return f

                def norm_coef(ob_, gate_ap):
                    ov_ = ob_[:, :].rearrange("p (j c) -> p j c", j=4)
                    ts(smal[:, 0:4], ov_[:, :, 64], TINY, None, ALU.max)
                    S.op("dve", lambda: nc.vector.reciprocal(out=smal[:, 4:8], in_=smal[:, 0:4]),
                         [smal[:, 0:4]], [smal[:, 4:8]])
                    if gate_ap is not None:
                        tt(smal[:, 8:12], smal[:, 4:8], gate_ap, ALU.mult)
                    return ov_

                def qpad(src3, ch, bp, eng):
                    half = bp // 64
                    b = qz[2 * half + (qzc[half] % 2)]
                    qzc[half] += 1
                    cp(b[bp:bp + 64, :], src3[bp:bp + 64, ch, :], e=eng)
                    return b

            if not PLAN[0]:
                if tr >= 2 and MDBG >= 1:
                    for j in range(4):
                        pb2 = psM.next()
                        tp(pb2[0:64, 0:128], mbtok[:, j, :], identf)
                        cp(mbTm[0:64, j * 128:(j + 1) * 128], pb2[0:64, 0:128])
            if not PLAN[0]:
                for g in range(2):
                    pb2 = psM.next()
                    mm(pb2[:, 0:nb], w2k[:, :], hidb[:, g, 0:nb], True, True)
                    cp(kcT[64 * g:64 * g + 64, c_lo:c_lo + nb], pb2[64 * g:64 * g + 64, 0:nb])
                    pb3 = psM.next()
                    mm(pb3[:, 0:64], hidv[:, g, :], w2v[:, :], True, True)
                    cp(vca[:, g, 0:64], pb3[:, 0:64])
                S.mark("tr%d nsa-cmp" % tr)
                ots = [otk[0], otk[1]]
            if SUB < 3:
                continue
            if not PLAN[0]:
                jobs = []
                for g in range(2):
                    ot = ots[g]
                    for hg in range(4):
                        h = g * 4 + hg
                        ch, bp = h % 4, 64 * (h // 4)
                        nar = bool(sl_n[h] > 0.3)

                        def done(oc, ot=ot, hg=hg, g=g, h=h):
                            ocv = norm_coef(oc, gsig[:, 4 * tr:4 * tr + 4, h * 3 + 0])
                            for j in range(4):
                                ts(ot[:, j, hg, :], ocv[:, j, 0:64], smal[:, 8 + j:9 + j], None, ALU.mult)
                                if tr >= 2:
                                    if hg == 0:
                                        ts(impa[:, j, g, :], ocv[:, j, 65:97], smal[:, 4 + j:5 + j], None, ALU.mult)
                                    else:
                                        stt(impa[:, j, g, :], ocv[:, j, 65:97], smal[:, 4 + j:5 + j],
                                            impa[:, j, g, :], ALU.mult, ALU.add)
                        jobs.append(dict(kT=lambda kt: kcT[:, :],
                                         qfn=lambda ch=ch, bp=bp: qpad(qn, ch, bp, "dve"),
                                         v=lambda kt, g=g: vca[:, g, :],
                                         tiles=[(0, 0, 512, [(0, 512, identb, cmsk[:, :])])],
                                         bias=bias_cmp(h, nar), narrow=nar, mask=None, ncols=97, done=done))
                attend_multi(jobs)
                if tr >= 2:
                    for j in range(4):
                        kp = rtab[:, j * 32:(j + 1) * 32]
                        fo = rtab[:, 128 + j * 32:128 + (j + 1) * 32]
                        for g in range(2):
                            iv = tk32[:, 0:32]
                            tt(iv, impa[:, j, g, :], kp, ALU.mult)
                            tt(iv, iv, fo, ALU.add)
                            S.op("dve", lambda: nc.vector.max(out=m8[:, 0:8], in_=iv), [iv], [m8[:, 0:8]])
                            S.op("dve", lambda: nc.vector.match_replace(out=tk32[:, 32:64], in_to_replace=m8[:, 0:8],
                                                                         in_values=iv, imm_value=-3.0e38),
                                 [iv, m8[:, 0:8]], [tk32[:, 32:64]])
                            S.op("dve", lambda: nc.vector.max(out=m8[:, 8:16], in_=tk32[:, 32:64]),
                                 [tk32[:, 32:64]], [m8[:, 8:16]])
                            ts(tk32[:, 32:64], iv, m8[:, 15:16], None, ALU.is_ge)
                            ts(mbtokN[:, j, g, :], tk32[:, 32:64], -1.0, -MASKV, ALU.add, ALU.mult)
                S.mark("tr%d moba" % tr)
                jobs = []
                for hp in range(4):
                    ot = otr.next()
                    for h2 in range(2):
                        h = hp * 2 + h2
                        ch, bp = hp, 64 * h2
                        nar = bool(sl_m[h] > 0.3)
                        mfn = None
                        if tr >= 2 and MDBG >= 2:
                            def mfn(kt, h=h):
                                c_ = 8 * h + kt // 2
                                l_ = bass.AP(cb, OB["ident"] + c_, [[NBS, 128], [0, 128]])
                                return l_, mbTm[:, :]

                        def done(om, ot=ot, h2=h2, hp=hp):
                            ov_ = norm_coef(om, None)
                            for j in range(4):
                                ts(ot[:, j, h2, :], ov_[:, j, 0:64], smal[:, 4 + j:5 + j], None, ALU.mult)
                            if h2 == 1:
                                pb = psM.next()
                                for j in range(4):
                                    tp(pb[:, j * 128:(j + 1) * 128], ot[:, j, 0:2, :], identf)
                                cp(omT[:, hp, :], pb[:, :], e="act")
                        jobs.append(dict(kT=lambda kt, ch=ch: kmT[:, ch, kt * 128:(kt + 1) * 128],
                                         qfn=lambda ch=ch, bp=bp: qpad(qm, ch, bp, "dve"),
                                         v=lambda kt, h=h: vm[:, kt, h, :],
                                         tiles=causal_tiles(tr, dmax_of(sl_m[h])), bias=bias_head(8 + h, nar),
                                         narrow=nar, mask=mfn, done=done))
                attend_multi(jobs)

                if tr >= 2:
                    for j in range(4):
                        for g in range(2):
                            pb = psM.next()
                            tp(pb[0:32, 0:128], mbtokN[:, j, g, :], identf)
                            cp(mbTn[64 * g:64 * g + 32, g, j * 128:(j + 1) * 128], pb[0:32, 0:128])
                S.mark("tr%d nsa-selwin" % tr)
                jobs = []
                for g in range(2):
                    ot = ots[g]
                    for hg in range(4):
                        h = g * 4 + hg
                        ch, bp = h % 4, 64 * (h // 4)
                        nar = bool(sl_n[h] > 0.3)
                        qc = {}

                        def qfn(ch=ch, bp=bp, qc=qc):
                            if "q" not in qc:
                                qc["q"] = qpad(qn, ch, bp, "dve")
                            return qc["q"]
                        mfn = None
                        if tr >= 2:
                            def mfn(kt, g=g):
                                return esel[:, kt, :], mbTn[:, g, :]

                        def done_sel(ob_, ot=ot, hg=hg, h=h):
                            ov_ = norm_coef(ob_, gsig[:, 4 * tr:4 * tr + 4, h * 3 + 1])
                            for j in range(4):
                                stt(ot[:, j, hg, :], ov_[:, j, 0:64], smal[:, 8 + j:9 + j], ot[:, j, hg, :],
                                    ALU.mult, ALU.add)

                        def done_win(ob_, ot=ot, hg=hg, h=h, g=g):
                            ov_ = norm_coef(ob_, gsig[:, 4 * tr:4 * tr + 4, h * 3 + 2])
                            for j in range(4):
                                stt(ot[:, j, hg, :], ov_[:, j, 0:64], smal[:, 8 + j:9 + j], ot[:, j, hg, :],
                                    ALU.mult, ALU.add)
                            if hg == 3:
                                for i2 in range(2):
                                    pb = psM.next()
                                    for j in range(4):
                                        tp(pb[:, j * 128:(j + 1) * 128], ot[:, j, 2 * i2:2 * i2 + 2, :], identf)
                                    cp(onT[:, 2 * g + i2, :], pb[:, :], e="act")
                        jobs.append(dict(kT=lambda kt: kslcT[:, kt * 128:(kt + 1) * 128], qfn=qfn,
                                         v=lambda kt, g=g: vslc[:, kt, g, :],
                                         tiles=causal_tiles(tr, dmax_of(sl_n[h])), bias=bias_head(h, nar),
                                         narrow=nar, mask=mfn, done=done_sel))
                        jobs.append(dict(kT=lambda kt: kwinT[:, kt * 128:(kt + 1) * 128], qfn=qfn,
                                         v=lambda kt, g=g: vwin[:, kt, g, :],
                                         tiles=win_tiles(tr, dmax_of(sl_n[h])), bias=bias_head(h, nar),
                                         narrow=nar, mask=None, done=done_win))
                attend_multi(jobs)

            if SUB < 5:
                continue
            if not PLAN[0]:
                S.mark("tr%d merge" % tr)
            for dc in range(8):
                c0 = dc * 128
                MJ = W([(0, [8, 128], win[:, :, 2840 + c0:2840 + c0 + 128]),
                        (1024, [8, 128], win[:, :, 3864 + c0:3864 + c0 + 128]),
                        (2048, [4, 128], wupn[:, :, c0:c0 + 128]),
                        (2560, [4, 128], wupm[:, :, c0:c0 + 128])])
                if PLAN[0]:
                    continue
                ga_ = MJ[:, 0:1024].rearrange("p (c f) -> p c f", c=8)
                gb_ = MJ[:, 1024:2048].rearrange("p (c f) -> p c f", c=8)
                un_ = MJ[:, 2048:2560].rearrange("p (c f) -> p c f", c=4)
                um_ = MJ[:, 2560:3072].rearrange("p (c f) -> p c f", c=4)
                pga = psS.next()
                pgb = psS.next()
                pyn = psO.next()
                pym = psO.next()
                for c in range(8):
                    mm(pga[:, :], ga_[:, c, :], u_tr[:, c, :], c == 0, c == 7)
                for c in range(8):
                    mm(pgb[:, :], gb_[:, c, :], u_tr[:, c, :], c == 0, c == 7)
                for c in range(4):
                    mm(pyn[:, :], un_[:, c, :], onT[:, c, :], c == 0, c == 3)
                for c in range(4):
                    mm(pym[:, :], um_[:, c, :], omT[:, c, :], c == 0, c == 3)
                sA = sgAr.next()
                sB = sgBr.next()
                act(sA[:, :], pga[:, :], AF.Sigmoid)
                act(sB[:, :], pgb[:, :], AF.Sigmoid)
                tt(sA[:, :], sA[:, :], pyn[:, :], ALU.mult)
                tt(sB[:, :], sB[:, :], pym[:, :], ALU.mult)
                tt(mixed[:, dc, :], sA[:, :], sB[:, :], ALU.add)
            for dh in range(2):
                WO = W([(0, [8, 512], wout[:, :, dh * 512:(dh + 1) * 512])])
                if PLAN[0]:
                    continue
                wo_ = WO[:, 0:4096].rearrange("p (c f) -> p c f", c=8)
                for d4 in range(4):
                    dc = dh * 4 + d4
                    pb = psM.next()
                    for c in range(8):
                        mm(pb[:, :], wo_[:, c, d4 * 128:(d4 + 1) * 128], mixed[:, c, :], c == 0, c == 7)
                    tt(hT[:, dc, trs(tr)], hT[:, dc, trs(tr)], pb[:, :], ALU.add)

    def run_all():
        if not PLAN[0]:
            S.mark("ffn1")
        if stage >= 1:
            scoped(phase_ffn, "ffn1_w1", "ffn1_w3", "ffn1_w2", 0, "a")
        if stage >= 2:
            scoped(phase_mixer)
        if stage >= 3:
            if not PLAN[0]:
                S.mark("ffn2")
            scoped(phase_ffn, "ffn2_w1", "ffn2_w3", "ffn2_w2", 2, "b")
        if stage >= 4:
            if not PLAN[0]:
                S.mark("ple")
            scoped(phase_ple)

    PLAN[0] = True
    run_all()
    PLAN[0] = False
    scoped(phase_load)
    run_all()
    S.mark("final")
    scoped(phase_final)
    S.mark("end")
    S.finish()
    _LAST["S"] = S
    return nc


_NC_CACHE = {}
_LAST = {}


def _prep_inputs(inputs):
    cf_np, _, cb_np, _ = _make_consts()
    f = lambda a: np.ascontiguousarray(np.asarray(a, dtype=np.float32))
    sh = {}
    for nm in ("ffn1_w1", "ffn1_w3", "ffn1_w2", "w_in", "cmp_w1_k", "cmp_w2_k", "cmp_w1_v", "cmp_w2_v",
               "w_up_nsa", "w_up_moba", "w_out", "ffn2_w1", "ffn2_w3", "ffn2_w2", "w_ple_gate", "w_ple"):
        sh[nm] = f(inputs[nm])[0]
    wi = sh["w_in"].copy()
    perm = [0, 4, 1, 5, 2, 6, 3, 7]
    wi[:, 0:512] = sh["w_in"][:, 0:512].reshape(D, 8, 64)[:, perm, :].reshape(D, 512)
    sh["w_in"] = np.ascontiguousarray(wi)
    for nm, src in (("pos_kT", "cmp_pos_k"), ("pos_vT", "cmp_pos_v")):
        pt = f(inputs[src])[0].T
        sh[nm] = np.ascontiguousarray(np.concatenate([pt, pt], axis=0))
    g = np.stack([f(inputs[n])[0] for n in ("ffn1_norm", "mix_norm", "ffn2_norm", "ple_norm")], axis=0)
    sh["gains"] = np.ascontiguousarray(g.reshape(4, 8, 128).transpose(2, 0, 1).reshape(128, 32))
    sh["gfin"] = np.ascontiguousarray(np.broadcast_to(f(inputs["final_norm"])[None, :], (128, D)))
    sh["cf32"] = cf_np
    sh["cbf16"] = cb_np
    return sh


def kernel(**inputs):
    stage = int(os.environ.get("MK_STAGE", "99"))
    ncores = int(os.environ.get("MK_CORES", "8"))
    if stage not in _NC_CACHE:
        _NC_CACHE[stage] = build_nc(stage)
    nc = _NC_CACHE[stage]
    sh = _prep_inputs(inputs)
    x = np.asarray(inputs["x"], dtype=np.float32)
    p = np.asarray(inputs["p"], dtype=np.float32)
    in_maps = []
    for b in range(ncores):
        m = dict(sh)
        m["x"] = np.ascontiguousarray(x[b])
        m["p"] = np.ascontiguousarray(p[0, b])
        in_maps.append(m)
    res = run_bass_kernel_spmd(nc, in_maps, core_ids=list(range(ncores)))
    out = np.stack([np.asarray(r["out"], dtype=np.float32) for r in res.results], axis=0)
    return out
```

```python
import os
import numpy as np
import ml_dtypes
import concourse.bass as bass
import concourse.mybir as mybir
from concourse.bass_utils import run_bass_kernel_spmd
from contextlib import ExitStack

F32 = mybir.dt.float32
BF16 = mybir.dt.bfloat16
AF = mybir.ActivationFunctionType
ALU = mybir.AluOpType

T = 2048
D = 1024
FF = 2816
MASKV = -240000.0
EPS = 1e-6
TINY = 1e-30
IN_W = 4888


def _slopes():
    s = 2.0 ** (-(np.arange(16) + 1) / 2.0)
    return s[0::2].copy(), s[1::2].copy()


def _make_consts():
    p = np.arange(128)
    f32 = {}
    b16 = {}
    f32["ident"] = np.eye(128)
    b16["ident"] = np.eye(128)
    b16["ones"] = np.ones((128, 128))
    j = np.arange(128)
    b16["triA"] = np.where(j[None, :] >= p[:, None], 0.0, MASKV)
    b16["triB"] = np.where(j[None, :] < p[:, None], 0.0, MASKV)
    tq = np.arange(T)
    cend = 16 * p + 31
    cm = np.where(tq[None, :] >= cend[:, None], 0.0, MASKV)
    cm[127, :] = MASKV
    ov = np.zeros((128, 32))
    for c in range(127):
        for jj in range(32):
            if 16 * c <= 64 * jj + 63 and 16 * c + 31 >= 64 * jj:
                ov[c, jj] = 1.0
    b16["overlap"] = ov
    b16["cmpmask"] = cm
    es = np.zeros((128, 16, 128))
    for pp in range(128):
        jj = pp % 64
        if jj >= 32:
            continue
        for kt in range(16):
            for m_ in range(128):
                if jj == 2 * kt + m_ // 64:
                    es[pp, kt, m_] = 1.0
    b16["esel"] = es.reshape(128, 2048)
    sl_n, sl_m = _slopes()
    sl16 = np.concatenate([sl_n, sl_m])
    d = np.arange(16) - 12
    f32["bw"] = (sl16[None, :, None] * (128 * d[None, None, :] + p[:, None, None] - 256)).reshape(128, 256)
    d2 = np.arange(16) - 15
    f32["bn"] = (sl16[None, :, None] * (128 * d2[None, None, :] + p[:, None, None] - 64)).reshape(128, 256)
    f32["cw"] = (sl_n[None, :, None] * (cend[:, None, None] - (512 * np.arange(4)[None, None, :] + 256))).reshape(128, 32)
    f32["cn"] = (sl_n[None, :, None] * (cend[:, None, None] - (128 * np.arange(16)[None, None, :] + 64))).reshape(128, 128)
    keep = np.zeros((128, 8, 32))
    force = np.zeros((128, 8, 32))
    for tt in range(8, 16):
        for pp in range(128):
            t = tt * 128 + pp
            cur = t // 64
            for jj in range(32):
                if 64 * jj > t:
                    force[pp, tt - 8, jj] = -1e30 * (1.0 + jj / 64.0)
                elif jj == 0:
                    force[pp, tt - 8, jj] = 3e9
                elif jj == cur:
                    force[pp, tt - 8, jj] = 2e9
                elif jj == cur - 1:
                    force[pp, tt - 8, jj] = 1e9
                else:
                    keep[pp, tt - 8, jj] = 1.0
    padneg = np.zeros((128, 8, 8))
    ownhot = np.zeros((128, 8, 8))
    for tt in range(8, 16):
        cur = tt // 2
        for n in range(8):
            if n >= cur:
                padneg[:, tt - 8, n] = -1e30
            if n == cur:
                ownhot[:, tt - 8, n] = 1.0
    f32["eps"] = np.full((128, 1), EPS)
    rt = []
    for tr in (2, 3):
        a = (tr - 2) * 4
        rt += [keep[:, a:a + 4].reshape(128, 128), force[:, a:a + 4].reshape(128, 128),
               padneg[:, a:a + 4].reshape(128, 32), ownhot[:, a:a + 4].reshape(128, 32)]
    f32["rt"] = np.concatenate(rt, axis=1)
    offs_f = {}
    cols = []
    o = 0
    for k, v in f32.items():
        offs_f[k] = o
        o += v.shape[1]
        cols.append(v.astype(np.float32))
    cf = np.ascontiguousarray(np.concatenate(cols, axis=1))
    offs_b = {}
    cols = []
    o = 0
    for k, v in b16.items():
        offs_b[k] = o
        o += v.shape[1]
        cols.append(v.astype(np.float32))
    cbv = np.ascontiguousarray(np.concatenate(cols, axis=1).astype(ml_dtypes.bfloat16))
    return cf, offs_f, cbv, offs_b


EMBED_WAIT = set(os.environ.get("MK_EMBED", "pe").split(",")) - {""}


def _box(ap):
    t = ap.tensor
    if t.name.startswith("ps") and len(t.name) == 3:
        return (t.name, 0, 128, 0, 512)
    row = 1
    for s in tuple(t.shape)[1:]:
        row *= int(s)
    off = int(ap.offset)
    p0 = off // row
    f0 = off % row
    apl = ap.ap
    npart = apl[0][1]
    ext = 1
    for st, cnt in apl[1:]:
        ext += (cnt - 1) * abs(st)
    return (t.name, p0, p0 + npart, f0, f0 + ext)


class Sched:
    def __init__(self, nc, ndma=24):
        self.nc = nc
        self.engs = {"pe": nc.tensor, "act": nc.scalar, "dve": nc.vector, "pool": nc.gpsimd, "sp": nc.sync}
        self.semh = {}
        for e in self.engs:
            self.semh[e] = nc.alloc_semaphore("sem_" + e)
        self.cnt = {e: 0 for e in self.engs}
        self.ndma = ndma
        for i in range(ndma):
            self.semh[("d", i)] = nc.alloc_semaphore("dsem%d" % i)
        self.dcnt = [0] * ndma
        self.drr = 0
        self.drr_pool = 0
        self.seen = {e: {} for e in self.engs}
        self.acc = {}
        self.out_tickets = []
        self.nwaits = 0
        self.marks = []

    def mark(self, name):
        self.marks.append((name, self.cnt["pe"]))

    def _wait(self, e, sk, val):
        if e == "pe" and sk == "pe":
            return
        if self.seen[e].get(sk, 0) >= val:
            return
        self.engs[e].wait_ge(self.semh[sk], val)
        self.seen[e][sk] = val
        self.nwaits += 1

    def _collect(self, reads, writes):
        waits = {}
        for ap in reads:
            b = _box(ap)
            for ent in self.acc.get(b[0], ()):
                if ent[0] == "w" and ent[3] < b[2] and b[1] < ent[4] and ent[5] < b[4] and b[3] < ent[6]:
                    if waits.get(ent[1], 0) < ent[2]:
                        waits[ent[1]] = ent[2]
        for ap in writes:
            b = _box(ap)
            for ent in self.acc.get(b[0], ()):
                if ent[3] < b[2] and b[1] < ent[4] and ent[5] < b[4] and b[3] < ent[6]:
                    if waits.get(ent[1], 0) < ent[2]:
                        waits[ent[1]] = ent[2]
        return waits

    def _record(self, sk, val, reads, writes):
        for ap in writes:
            b = _box(ap)
            lst = self.acc.setdefault(b[0], [])
            lst[:] = [e for e in lst if not (b[1] <= e[3] and e[4] <= b[2] and b[3] <= e[5] and e[6] <= b[4])]
            lst.append(("w", sk, val, b[1], b[2], b[3], b[4]))
        for ap in reads:
            b = _box(ap)
            lst = self.acc.setdefault(b[0], [])
            lst[:] = [e for e in lst if not (e[0] == "r" and e[1] == sk and b[1] <= e[3] and e[4] <= b[2]
                                             and b[3] <= e[5] and e[6] <= b[4])]
            lst.append(("r", sk, val, b[1], b[2], b[3], b[4]))

    def op(self, e, fn, reads, writes, inc=True):
        assert inc or e == "pe"
        waits = self._collect(reads, writes)
        emb = None
        if EMBED_WAIT and e in EMBED_WAIT:
            need = [(sk, val) for sk, val in waits.items()
                    if not (e == "pe" and sk == "pe") and self.seen[e].get(sk, 0) < val]
            if need:
                emb = need[-1]
                waits = dict(need[:-1])
            else:
                waits = {}
        for sk, val in waits.items():
            self._wait(e, sk, val)
        ins = fn()
        if emb is not None:
            ins._wait_ge(self.semh[emb[0]], emb[1])
            self.seen[e][emb[0]] = emb[1]
        if inc:
            self.cnt[e] += 1
            ins.then_inc(self.semh[e], 1)
            val = self.cnt[e]
        else:
            val = self.cnt[e] + 1
        self._record(e, val, reads, writes)
        return ins

    def dma(self, q, out, in_, reads=(), writes=(), is_out=False, **kw):
        waits = self._collect(reads, writes)
        for sk, val in waits.items():
            self._wait(q, sk, val)
        half = self.ndma // 2
        if q == "pool":
            i = half + (self.drr_pool % half)
            self.drr_pool += 1
        else:
            i = self.drr % half
            self.drr += 1
        sk = ("d", i)
        if self.dcnt[i] > 0:
            self._wait(q, sk, self.dcnt[i])
        ins = self.engs[q].dma_start(out=out, in_=in_, **kw)
        self.dcnt[i] += 16
        ins.then_inc(self.semh[sk], 16)
        self._record(sk, self.dcnt[i], reads, writes)
        if is_out:
            self.out_tickets.append((sk, self.dcnt[i]))

    def barrier(self):
        for e in ("pe", "act", "dve", "pool", "sp"):
            for f in ("pe", "act", "dve", "pool", "sp"):
                if f != e and self.cnt[f] > 0:
                    self._wait(e, f, self.cnt[f])
            for i in range(self.ndma):
                if self.dcnt[i] > 0:
                    self._wait(e, ("d", i), self.dcnt[i])
        self.acc = {}

    def finish(self):
        for sk, val in self.out_tickets:
            self._wait("sp", sk, val)


def build_nc(stage=99):
    nc = bass.Bass("TRN2", target_bir_lowering=False)
    cf_np, OF, cb_np, OB = _make_consts()
    NF = cf_np.shape[1]
    NB = cb_np.shape[1]

    def din(name, shape, dt=F32):
        return nc.dram_tensor(name, list(shape), dt, kind="ExternalInput").ap()

    x_d = din("x", [T, D])
    p_d = din("p", [T, 256])
    wd = {}
    for nm, shp in [("ffn1_w1", [D, FF]), ("ffn1_w3", [D, FF]), ("ffn1_w2", [FF, D]), ("w_in", [D, IN_W]),
                    ("cmp_w1_k", [2048, 128]), ("cmp_w2_k", [128, 64]), ("cmp_w1_v", [2048, 128]),
                    ("cmp_w2_v", [128, 64]), ("pos_kT", [128, 32]), ("pos_vT", [128, 32]),
                    ("w_up_nsa", [512, D]), ("w_up_moba", [512, D]), ("w_out", [D, D]),
                    ("ffn2_w1", [D, FF]), ("ffn2_w3", [D, FF]), ("ffn2_w2", [FF, D]),
                    ("w_ple_gate", [D, D]), ("w_ple", [256, D]), ("gains", [128, 32]), ("gfin", [128, D])]:
        wd[nm] = din(nm, shp)
    cf_d = din("cf32", [128, NF])
    cb_d = din("cbf16", [128, NB], BF16)
    out_d = nc.dram_tensor("out", [T, D], F32, kind="ExternalOutput").ap()

    S = Sched(nc)
    stack = [None]

    def TS(name, shape, dt):
        if stack[0] is None:
            return nc.alloc_sbuf_tensor(name, shape, dt)
        return stack[0].enter_context(nc.sbuf_tensor(name, shape, dt))

    def scoped(fn, *a):
        if PLAN[0]:
            fn(*a)
            return
        S.barrier()
        with ExitStack() as st:
            stack[0] = st
            fn(*a)
            S.barrier()
        stack[0] = None

    hT = TS("hT", [128, 8, T], F32)
    NFS = OF["rt"]
    NBS = OB["cmpmask"]
    cf = TS("cf", [128, NFS], F32)
    cb = TS("cb", [128, NBS], BF16)
    gains = TS("gains_sb", [128, 32], F32)
    slab = [TS("slab%d" % i, [128, 4096], BF16) for i in range(3)]
    ps = [nc.alloc_psum_tensor("ps%d" % i, [128, 512], F32) for i in range(8)]

    identf = cf[:, OF["ident"]:OF["ident"] + 128]
    identb = cb[:, OB["ident"]:OB["ident"] + 128]
    onesb = cb[:, OB["ones"]:OB["ones"] + 128]
    epsc = cf[:, OF["eps"]:OF["eps"] + 1]

    S.dma("sp", cf[:, :], cf_d[:, 0:NFS], writes=[cf[:, :]])
    S.dma("sp", cb[:, :], cb_d[:, 0:NBS], writes=[cb[:, :]])
    S.dma("sp", gains[:, :], wd["gains"][:, :], writes=[gains[:, :]])

    def mm(out, lhsT, rhs, start, stop, inc=None):
        inc = True
        return S.op("pe", lambda: nc.tensor.matmul(out, lhsT, rhs, start=start, stop=stop),
                    [lhsT, rhs], [out], inc=inc)

    def pe_drain():
        if S.cnt["pe"] > 0:
            nc.tensor.wait_ge(S.semh["pe"], S.cnt["pe"])

    def tp(out, in_, ident):
        return S.op("pe", lambda: nc.tensor.transpose(out, in_, ident), [in_, ident], [out])

    def act(out, in_, func, bias=None, scale=None, accum_out=None):
        kw = {}
        rd = [in_]
        wr = [out]
        if bias is not None:
            kw["bias"] = bias
            if not isinstance(bias, (int, float)):
                rd.append(bias)
        if scale is not None:
            kw["scale"] = scale
            if not isinstance(scale, (int, float)):
                rd.append(scale)
        if accum_out is not None:
            kw["accum_out"] = accum_out
            wr.append(accum_out)
        return S.op("act", lambda: nc.scalar.activation(out=out, in_=in_, func=func, **kw), rd, wr)

    def tt(out, in0, in1, op, e="dve"):
        eng = nc.vector if e == "dve" else nc.gpsimd
        return S.op(e, lambda: eng.tensor_tensor(out=out, in0=in0, in1=in1, op=op), [in0, in1], [out])

    def ts(out, in0, s1, s2, op0, op1=None, e="dve"):
        eng = nc.vector if e == "dve" else nc.gpsimd
        rd = [in0]
        if not isinstance(s1, (int, float)):
            rd.append(s1)
        if s2 is not None and not isinstance(s2, (int, float)):
            rd.append(s2)
        if op1 is None:
            return S.op(e, lambda: eng.tensor_scalar(out=out, in0=in0, scalar1=s1, scalar2=None, op0=op0), rd, [out])
        return S.op(e, lambda: eng.tensor_scalar(out=out, in0=in0, scalar1=s1, scalar2=s2, op0=op0, op1=op1),
                    rd, [out])

    def stt(out, in0, scalar, in1, op0, op1):
        rd = [in0, in1]
        if not isinstance(scalar, (int, float)):
            rd.append(scalar)
        return S.op("dve", lambda: nc.vector.scalar_tensor_tensor(out=out, in0=in0, scalar=scalar, in1=in1,
                                                                   op0=op0, op1=op1), rd, [out])

    def cp(out, in_, e="dve"):
        if e == "act":
            return S.op("act", lambda: nc.scalar.copy(out=out, in_=in_), [in_], [out])
        eng = nc.vector if e == "dve" else nc.gpsimd
        return S.op(e, lambda: eng.tensor_copy(out=out, in_=in_), [in_], [out])

    def memset(ap, v, e="dve"):
        eng = nc.vector if e == "dve" else nc.gpsimd
        return S.op(e, lambda: eng.memset(ap, v), [], [ap])

    class RR:
        def __init__(self, items):
            self.items = items
            self.i = 0

        def next(self):
            it = self.items[self.i % len(self.items)]
            self.i += 1
            return it

    class WStream:
        def __init__(self):
            self.jobs = []
            self.issued = 0
            self.k = 0

        def add(self, parts):
            self.jobs.append(parts)

        def _issue(self, k):
            buf = slab[k % 3]
            for (off, shape, src) in self.jobs[k]:
                n = 1
                for s_ in shape:
                    n *= s_
                npart = int(src.shape[0])
                dst = buf[0:npart, off:off + n]
                if len(shape) == 2:
                    dst = dst.rearrange("p (a b) -> p a b", a=shape[0])
                S.dma("pool", dst, src, writes=[buf[0:npart, off:off + n]])

        def get(self):
            k = self.k
            self.k += 1
            while self.issued < min(k + 2, len(self.jobs)):
                self._issue(self.issued)
                self.issued += 1
            return slab[k % 3]

    WS = WStream()
    PLAN = [True]

    def W(parts):
        if PLAN[0]:
            WS.add(parts)
            return None
        return WS.get()

    def trs(tr):
        return slice(tr * 512, (tr + 1) * 512)

    sqb = [TS("sqb%d" % i, [128, 512], BF16) for i in range(2)]
    rstd = TS("rstd", [128, 512], F32)
    sqrr = RR(sqb)
    psM = RR([ps[6], ps[7]])
    psL = RR([ps[0], ps[1], ps[2], ps[3]])

    def rms_range(tr, gidx, xn3):
        pb = psM.next()
        for c in range(8):
            sq = sqrr.next()
            act(sq[:, :], hT[:, c, trs(tr)], AF.Square)
            mm(pb[:, :], onesb, sq[:, :], c == 0, c == 7)
        act(rstd[:, :], pb[:, :], AF.Ln, bias=epsc, scale=1.0 / D)
        act(rstd[:, :], rstd[:, :], AF.Exp, scale=-0.5)
        for c in range(8):
            stt(xn3[:, c, :], hT[:, c, trs(tr)], gains[:, gidx * 8 + c:gidx * 8 + c + 1], rstd[:, :],
                ALU.mult, ALU.mult)

    def phase_load():
        NB_ = 4
        xtok = [TS("xtok%d" % i, [128, D], F32) for i in range(NB_)]

        def ld(t_):
            xb = xtok[t_ % NB_]
            S.dma("sp", xb[:, :], x_d[t_ * 128:(t_ + 1) * 128, :], writes=[xb[:, :]])
        for t_ in range(NB_ - 1):
            ld(t_)
        while WS.issued < min(2, len(WS.jobs)):
            WS._issue(WS.issued)
            WS.issued += 1
        for t_ in range(16):
            if t_ + NB_ - 1 < 16:
                ld(t_ + NB_ - 1)
            xb = xtok[t_ % NB_]
            for half in range(2):
                pb = psL.next()
                for c4 in range(4):
                    c = half * 4 + c4
                    tp(pb[:, c4 * 128:(c4 + 1) * 128], xb[:, c * 128:(c + 1) * 128], identf)
                cp(hT[:, half * 4:(half + 1) * 4, t_ * 128:(t_ + 1) * 128],
                   pb[:, :].rearrange("p (c t) -> p c t", c=4), e="act" if half else "dve")

    def phase_ffn(w1n, w3n, w2n, gidx, tag):
        w1 = wd[w1n].rearrange("(c p) f -> p c f", p=128)
        w3 = wd[w3n].rearrange("(c p) f -> p c f", p=128)
        w2 = wd[w2n]
        if not PLAN[0]:
            xnT = TS("xnT" + tag, [128, 8, T], BF16)
            h1 = [TS("h1%s%d" % (tag, i), [128, 2, 512], BF16) for i in range(2)]
            sa = [TS("sa%s%d" % (tag, i), [128, 512], F32) for i in range(2)]
            for tr in range(4):
                rms_range(tr, gidx, xnT[:, :, trs(tr)])
            psAB = RR([ps[0], ps[1], ps[2], ps[3]])
            psY = RR([ps[4], ps[5], ps[6], ps[7]])
            sar = RR(sa)
            h1r = RR(h1)
            ytmp = [TS("ytmp%s%d" % (tag, i), [128, 512], F32) for i in range(2)]
            ytr = RR(ytmp)
        Aj = {}
        Bj = {}

        def getA(s_):
            if s_ not in Aj:
                Aj[s_] = W([(0, [8, 256], w1[:, :, s_ * 256:(s_ + 1) * 256]),
                            (2048, [8, 256], w3[:, :, s_ * 256:(s_ + 1) * 256])])
            return Aj[s_]

        def getB(s_):
            if s_ not in Bj:
                Bj[s_] = W([(0, [2, 1024], w2[s_ * 256:(s_ + 1) * 256, :].rearrange("(j p) d -> p j d", p=128))])
            return Bj[s_]

        def ab_steps(u):
            s_, tr = u
            A = getA(s_)
            if PLAN[0]:
                return
            w1s = A[:, 0:2048].rearrange("p (c f) -> p c f", c=8)
            w3s = A[:, 2048:4096].rearrange("p (c f) -> p c f", c=8)
            hb = h1r.next()
            hbs[u] = hb
            for j in range(2):
                pa = psAB.next()
                pb = psAB.next()
                for c in range(8):
                    mm(pa[:, :], w1s[:, c, j * 128:(j + 1) * 128], xnT[:, c, trs(tr)], c == 0, c == 7)
                sab = sar.next()
                act(sab[:, :], pa[:, :], AF.Silu)
                yield
                for c in range(8):
                    mm(pb[:, :], w3s[:, c, j * 128:(j + 1) * 128], xnT[:, c, trs(tr)], c == 0, c == 7)
                tt(hb[:, j, :], sab[:, :], pb[:, :], ALU.mult)
                yield

        def y_steps(u):
            s_, tr = u
            B = getB(s_)
            if PLAN[0]:
                return
            hb = hbs.pop(u)
            w2s = B[:, 0:2048].rearrange("p (j d) -> p j d", j=2)
            for dc in range(8):
                py = psY.next()
                for j in range(2):
                    mm(py[:, :], w2s[:, j, dc * 128:(dc + 1) * 128], hb[:, j, :], j == 0, j == 1)
                stt(hT[:, dc, trs(tr)], py[:, :], 0.5, hT[:, dc, trs(tr)], ALU.mult, ALU.add)
                yield

        hbs = {}
        units = [(s_, tr) for s_ in range(11) for tr in range(4)]
        prev = None
        for u in units:
            ga = ab_steps(u)
            gy = y_steps(prev) if prev is not None else iter(())
            for _ in ga:
                next(gy, None)
                next(gy, None)
            for _ in gy:
                pass
            prev = u
        for _ in y_steps(prev):
            pass

    def phase_ple():
        wg = wd["w_ple_gate"].rearrange("(c p) f -> p c f", p=128)
        wp = wd["w_ple"].rearrange("(c p) f -> p c f", p=128)
        if not PLAN[0]:
            xnT = TS("xnTple", [128, 8, T], BF16)
            pT = TS("pT", [128, 2, T], BF16)
            ptok = [TS("ptok%d" % i, [128, 256], F32) for i in range(2)]
            sg = [TS("sgple%d" % i, [128, 512], F32) for i in range(2)]
            for t_ in range(16):
                pbuf = ptok[t_ % 2]
                S.dma("sp", pbuf[:, :], p_d[t_ * 128:(t_ + 1) * 128, :], writes=[pbuf[:, :]])
                pb = psM.next()
                for c in range(2):
                    tp(pb[:, c * 128:(c + 1) * 128], pbuf[:, c * 128:(c + 1) * 128], identf)
                cp(pT[:, :, t_ * 128:(t_ + 1) * 128], pb[:, 0:256].rearrange("p (c t) -> p c t", c=2))
            for tr in range(4):
                rms_range(tr, 3, xnT[:, :, trs(tr)])
            psG = RR([ps[0], ps[1], ps[2], ps[3]])
            sgr = RR(sg)
        for dh in range(2):
            A = W([(0, [8, 512], wg[:, :, dh * 512:(dh + 1) * 512])])
            B = W([(0, [2, 512], wp[:, :, dh * 512:(dh + 1) * 512])])
            if PLAN[0]:
                continue
            wgs = A[:, 0:4096].rearrange("p (c f) -> p c f", c=8)
            wps = B[:, 0:1024].rearrange("p (c f) -> p c f", c=2)
            for tr in range(4):
                for d4 in range(4):
                    dc = dh * 4 + d4
                    pg = psG.next()
                    pp = psG.next()
                    for c in range(8):
                        mm(pg[:, :], wgs[:, c, d4 * 128:(d4 + 1) * 128], xnT[:, c, trs(tr)], c == 0, c == 7)
                    for c in range(2):
                        mm(pp[:, :], wps[:, c, d4 * 128:(d4 + 1) * 128], pT[:, c, trs(tr)], c == 0, c == 1)
                    sgb = sgr.next()
                    act(sgb[:, :], pg[:, :], AF.Sigmoid)
                    tt(sgb[:, :], sgb[:, :], pp[:, :], ALU.mult)
                    tt(hT[:, dc, trs(tr)], hT[:, dc, trs(tr)], sgb[:, :], ALU.add)

    def phase_final():
        gfin = TS("gfin_sb", [128, D], F32)
        S.dma("sp", gfin[:, :], wd["gfin"][:, :], writes=[gfin[:, :]])
        otok = [TS("otok%d" % i, [128, D], F32) for i in range(4)]
        junk = TS("junk", [128, 512], F32)
        ssq = TS("ssq", [128, 4], F32)
        psF = RR([ps[0], ps[1], ps[2], ps[3], ps[4], ps[5]])
        for t_ in range(16):
            ob = otok[t_ % 4]
            pbs = [psF.next(), psF.next()]
            for half in range(2):
                for c4 in range(4):
                    c = half * 4 + c4
                    tp(pbs[half][:, c4 * 128:(c4 + 1) * 128], hT[:, c, t_ * 128:(t_ + 1) * 128], identf)
                act(junk[:, :], pbs[half][:, :], AF.Square, accum_out=ssq[:, half:half + 1])
            tt(ssq[:, 2:3], ssq[:, 0:1], ssq[:, 1:2], ALU.add)
            act(ssq[:, 3:4], ssq[:, 2:3], AF.Ln, bias=epsc, scale=1.0 / D)
            act(ssq[:, 3:4], ssq[:, 3:4], AF.Exp, scale=-0.5)
            for half in range(2):
                stt(ob[:, half * 512:(half + 1) * 512], pbs[half][:, :], ssq[:, 3:4],
                    gfin[:, half * 512:(half + 1) * 512], ALU.mult, ALU.mult)
            S.dma("sp", out_d[t_ * 128:(t_ + 1) * 128, :], ob[:, :], reads=[ob[:, :]], is_out=True)

    def phase_mixer():
        win = wd["w_in"].rearrange("(c p) f -> p c f", p=128)
        wupn = wd["w_up_nsa"].rearrange("(c p) f -> p c f", p=128)
        wupm = wd["w_up_moba"].rearrange("(c p) f -> p c f", p=128)
        wout = wd["w_out"].rearrange("(c p) f -> p c f", p=128)
        sl_n, sl_m = _slopes()
        if not PLAN[0]:
            kslcT = TS("kslcT", [128, T], BF16)
            kwinT = TS("kwinT", [128, T], BF16)
            kcmpT = TS("kcmpT", [128, 528], BF16)
            vcmpT = TS("vcmpT", [128, 528], BF16)
            kmT = TS("kmT", [128, 4, T], BF16)
            vslc = TS("vslc", [128, 16, 2, 65], BF16)
            vwin = TS("vwin", [128, 16, 2, 65], BF16)
            vm = TS("vm", [128, 16, 8, 65], BF16)
            gsig = TS("gsig", [128, 16, 24], F32)
            kcT = TS("kcT", [128, 128], BF16)
            vca = TS("vca", [128, 2, 97], BF16)
            kmeanT = TS("kmeanT", [128, 4, 16], BF16)
            kmsum = TS("kmsum", [128, 4, 2], F32)
            w2k = TS("w2k", [128, 128], BF16)
            w2v = TS("w2v", [128, 64], BF16)
            posk = TS("posk", [128, 32], BF16)
            posv = TS("posv", [128, 32], BF16)
            cbias = TS("cbias", [128, 2], F32)
            u_tr = TS("u_tr", [128, 8, 512], BF16)
            qmix = TS("qmix", [128, 8, 512], BF16)

            class _Sub:
                def __init__(self, base, off):
                    self.base, self.off = base, off

                def __getitem__(self, key):
                    a, b, c = key
                    if isinstance(b, int):
                        b = b + self.off
                    else:
                        b = slice((b.start or 0) + self.off, (b.stop if b.stop is not None else 4) + self.off)
                    return self.base[a, b, c]
            qn = _Sub(qmix, 0)
            qm = _Sub(qmix, 4)
            ptl = [TS("ptl%d" % i, [128, 512], BF16) for i in range(3)]
            otk = [TS("otk%d" % i, [128, 4, 4, 64], F32) for i in range(2)]
            omk = [TS("omk%d" % i, [128, 4, 2, 64], F32) for i in range(2)]
            onT = TS("onT", [128, 4, 512], BF16)
            omT = TS("omT", [128, 4, 512], BF16)
            mixed = qmix
            _o0 = otk[0][:, :, :, :].rearrange("p a b c -> p (a b c)")
            sgA = [_o0[:, 0:512]]
            sgB = [_o0[:, 512:1024]]
            mbTn = TS("mbTn", [128, 2, 512], BF16)
            qz = [TS("qz%d" % i, [128, 512], BF16) for i in range(4)]
            qzc = [0, 0]
            mbTm = TS("mbTm", [128, 512], BF16)
            mbtokN = TS("mbtokN", [128, 4, 2, 32], F32)
            mbtok = TS("mbtok", [128, 4, 64], F32)
            impa = TS("impa", [128, 4, 2, 32], F32)
            smal = TS("smal", [128, 16], F32)
            m8 = TS("m8", [128, 16], F32)
            tk32 = TS("tk32", [128, 64], F32)
            cmsk = TS("cmsk", [128, 512], BF16)
            rtab = TS("rtab", [128, 320], F32)
            esel = TS("esel", [128, 16, 128], BF16)
            S.dma("sp", esel[:, :, :], cb_d[:, OB["esel"]:OB["esel"] + 2048].rearrange("p (k m) -> p k m", k=16),
                  writes=[esel[:, :, :]])
            hid = TS("hid", [128, 3, 32], F32)
            hidb = TS("hidb", [128, 2, 32], BF16)
            hidv = TS("hidv", [128, 2, 128], BF16)
            gtmp = TS("gtmp", [64, 528], BF16)
            gsf = TS("gsf", [128, 8, 16], F32)

            memset(kcT[:, :], 0.0)
            for i in range(4):
                memset(qz[i][:, :], 0.0)
            memset(mbTn[:, :, :], 0.0)
            memset(mbTm[:, :], 0.0)
            memset(gsf[:, :, :], -1.0e30)
            memset(kcmpT[:, 0:16], 0.0)
            memset(vcmpT[:, 0:16], 0.0)
            memset(hidv[:, :, :], 0.0)
            memset(vca[:, :, :], 0.0)
            memset(kmeanT[:, :, :], 0.0)
            memset(vslc[:, :, :, 64:65], 1.0)
            memset(vwin[:, :, :, 64:65], 1.0)
            memset(vm[:, :, :, 64:65], 1.0)
            memset(vca[:, :, 64:65], 1.0)
            for g in range(2):
                cp(vca[:, g, 65:97], cb[:, OB["overlap"]:OB["overlap"] + 32])
            for (w2t, w2n_, pt, pn) in ((w2k, "cmp_w2_k", posk, "pos_kT"), (w2v, "cmp_w2_v", posv, "pos_vT")):
                S.dma("pool", pt[:, :], wd[pn][:, :], writes=[pt[:, :]])
                if w2t is w2k:
                    S.dma("pool", w2t[:, 0:64], wd[w2n_][:, :], writes=[w2t[:, 0:64]])
                    S.dma("pool", w2t[:, 64:128], wd[w2n_][:, :], writes=[w2t[:, 64:128]])
                else:
                    S.dma("pool", w2t[:, :], wd[w2n_][:, :], writes=[w2t[:, :]])

            psS = RR([ps[0], ps[1], ps[2]])
            psO = RR([ps[3], ps[4], ps[5]])
            ptr = RR(ptl)
            otr = RR(omk)
            sgAr = RR(sgA)
            sgBr = RR(sgB)

        def proj_fm(ws, c0, dst, tr, base=None, ev="act"):
            pb = psM.next()
            for c in range(8):
                if base is None:
                    l_ = ws[:, c, c0:c0 + 128]
                else:
                    l_ = base(c)
                mm(pb[:, :], l_, u_tr[:, c, :], c == 0, c == 7)
            cp(dst, pb[:, :], e=ev)

        NTRS = int(os.environ.get("MK_NTR", "4"))
        SUB = int(os.environ.get("MK_SUB", "9"))
        for tr in range(NTRS):
            S0 = W([(0, [8, 512], win[:, :, 0:512])])
            if not PLAN[0]:
                S.mark("tr%d proj" % tr)
                rms_range(tr, 1, u_tr)
                S.dma("sp", cmsk[:, :], cb_d[:, OB["cmpmask"] + tr * 512:OB["cmpmask"] + (tr + 1) * 512],
                      writes=[cmsk[:, :]])
                if tr >= 2:
                    S.dma("sp", rtab[:, :], cf_d[:, OF["rt"] + (tr - 2) * 320:OF["rt"] + (tr - 1) * 320],
                          writes=[rtab[:, :]])

                w0 = S0[:, 0:4096].rearrange("p (c f) -> p c f", c=8)
                for i in range(4):
                    proj_fm(w0, i * 128, qn[:, i, :], tr, ev="act" if i % 2 else "dve")
            S1 = W([(0, [8, 512], win[:, :, 512:1024])])
            if not PLAN[0]:
                w1_ = S1[:, 0:4096].rearrange("p (c f) -> p c f", c=8)
                proj_fm(w1_, 0, kcmpT[:, 16:528], tr, ev="act")
                proj_fm(w1_, 128, vcmpT[:, 16:528], tr, ev="dve")
                proj_fm(w1_, 256, kslcT[:, trs(tr)], tr, ev="act")
                for j in range(4):
                    t_ = tr * 4 + j
                    pb = psM.next()
                    for c in range(8):
                        mm(pb[:, 0:128], u_tr[:, c, j * 128:(j + 1) * 128], w1_[:, c, 384:512], c == 0, c == 7)
                    cp(vslc[:, t_, :, 0:64], pb[:, 0:128].rearrange("p (g d) -> p g d", g=2))
            S2 = W([(0, [8, 280], win[:, :, 1024:1304])])
            if not PLAN[0]:
                w2_ = S2[:, 0:2240].rearrange("p (c f) -> p c f", c=8)
                proj_fm(w2_, 0, kwinT[:, trs(tr)], tr, ev="act")
                for j in range(4):
                    t_ = tr * 4 + j
                    pb = psM.next()
                    for c in range(8):
                        mm(pb[:, 0:152], u_tr[:, c, j * 128:(j + 1) * 128], w2_[:, c, 128:280], c == 0, c == 7)
                    cp(vwin[:, t_, :, 0:64], pb[:, 0:128].rearrange("p (g d) -> p g d", g=2))
                    act(gsig[:, t_, :], pb[:, 128:152], AF.Sigmoid)
            S3 = W([(0, [8, 512], win[:, :, 1304:1816])])
            if not PLAN[0]:
                w3_ = S3[:, 0:4096].rearrange("p (c f) -> p c f", c=8)
                for i in range(4):
                    proj_fm(w3_, i * 128, qm[:, i, :], tr, ev="act" if i % 2 else "dve")
            S4 = W([(0, [8, 512], win[:, :, 1816:2328])])
            if not PLAN[0]:
                w4_ = S4[:, 0:4096].rearrange("p (c f) -> p c f", c=8)
                for i in range(4):
                    proj_fm(w4_, i * 128, kmT[:, i, trs(tr)], tr, ev="act" if i % 2 else "dve")
                for i in range(4):
                    S.op("dve", lambda: nc.vector.tensor_reduce(
                        out=kmsum[:, i, :], in_=kmT[:, i, trs(tr)].rearrange("p (n k) -> p n k", n=2),
                        axis=mybir.AxisListType.X, op=ALU.add),
                        [kmT[:, i, trs(tr)]], [kmsum[:, i, :]])
                ts(kmeanT[0:64, :, 2 * tr:2 * tr + 2], kmsum[0:64, :, :], 1.0 / 256.0, None, ALU.mult)
                ts(kmeanT[64:128, :, 8 + 2 * tr:8 + 2 * tr + 2], kmsum[64:128, :, :], 1.0 / 256.0, None, ALU.mult)
            if not PLAN[0]:
                MDBG = int(os.environ.get("MK_MOBA", "3"))
                if tr >= 2 and MDBG >= 1:
                    for j in range(4):
                        pb = psM.next()
                        for ch in range(4):
                            mm(pb[:, ch * 16:(ch + 1) * 16], qm[:, ch, j * 128:(j + 1) * 128],
                               kmeanT[:, ch, :], True, True)
                        GS = int(os.environ.get("MK_GS", "9"))
                        if GS >= 1:
                            tt(gsf[:, :, 0:8], pb[:, 0:64].rearrange("p (h n) -> p h n", h=8),
                               bass.AP(rtab, 256 + j * 8, [[320, 128], [0, 8], [1, 8]]), ALU.add)
                        if GS >= 2:
                            for h in range(8):
                                S.op("dve", lambda: nc.vector.max(out=m8[:, 0:8], in_=gsf[:, h, :]),
                                     [gsf[:, h, :]], [m8[:, 0:8]])
                                ts(tk32[:, h * 8:(h + 1) * 8], gsf[:, h, 0:8], m8[:, 2:3], None, ALU.is_ge)
                        if GS >= 3:
                            tt(tk32[:, :].rearrange("p (h n) -> p h n", h=8), tk32[:, :].rearrange("p (h n) -> p h n", h=8),
                               bass.AP(rtab, 288 + j * 8, [[320, 128], [0, 8], [1, 8]]), ALU.add)
                            ts(mbtok[:, j, :], tk32[:, :], -1.0, -MASKV, ALU.add, ALU.mult)
            S5 = W([(0, [8, 512], win[:, :, 2328:2840])])
            if not PLAN[0]:
                w5_ = S5[:, 0:4096].rearrange("p (c f) -> p c f", c=8)
                for j in range(4):
                    t_ = tr * 4 + j
                    pb = psM.next()
                    for c in range(8):
                        mm(pb[:, :], u_tr[:, c, j * 128:(j + 1) * 128], w5_[:, c, :], c == 0, c == 7)
                    cp(vm[:, t_, :, 0:64], pb[:, :].rearrange("p (h d) -> p h d", h=8), e="act" if j % 2 else "dve")

            if SUB < 1:
                continue
            if not PLAN[0]:
                S.mark("tr%d compress" % tr)
            c_lo = 0 if tr == 0 else 32 * tr - 1
            c_hi = 32 * tr + 30
            nb = c_hi - c_lo + 1
            if not PLAN[0]:
                pe_drain()
            for which in range(2):
                wsrc = wd["cmp_w1_k" if which == 0 else "cmp_w1_v"].rearrange("(l d) h -> d l h", d=64)
                CW = W([(q4 * 1024, [8, 128], wsrc[:, q4 * 8:(q4 + 1) * 8, :]) for q4 in range(4)])
                if PLAN[0]:
                    continue
                srcT = kcmpT if which == 0 else vcmpT
                w1t = CW[0:64, 0:4096].rearrange("p (l h) -> p l h", l=32)
                if tr == 0:
                    pt = posk if which == 0 else posv
                    pb = psM.next()
                    for l in range(32):
                        mm(pb[:, 0:1], w1t[:, l, :], pt[0:64, l:l + 1], l == 0, l == 31)
                    cp(cbias[:, which:which + 1], pb[:, 0:1])
                for g in range(2):
                    if g == 1:
                        cp(gtmp[0:64, 0:528], srcT[64:128, 0:528])
                        src_t = gtmp
                    else:
                        src_t = srcT
                    pbA = ps[which * 2 + g]
                    for l in range(32):
                        col0 = 16 * c_lo + l - 512 * tr + 16
                        rhs_ = bass.AP(src_t, col0, [[528, 64], [16, nb]])
                        mm(pbA[:, 0:nb], w1t[:, l, :], rhs_, l == 0, l == 31)
                cp(srcT[:, 0:16], srcT[:, 512:528])
            if not PLAN[0]:
                pe_drain()
                for which in range(2):
                    for g in range(2):
                        pbA = ps[which * 2 + g]
                        xx = hid[:, 0, 0:nb]
                        x2 = hid[:, 1, 0:nb]
                        ts(xx, pbA[:, 0:nb], cbias[:, which:which + 1], None, ALU.add)
                        tt(x2, xx, xx, ALU.mult)
                        ts(x2, x2, 0.044715, 1.0, ALU.mult, ALU.add)
                        tt(x2, x2, xx, ALU.mult)
                        act(hid[:, 2, 0:nb], x2, AF.Sigmoid, scale=1.5957691216057308)
                        if which == 0:
                            tt(hidb[:, g, 0:nb], xx, hid[:, 2, 0:nb], ALU.mult)
                        else:
                            tt(hidv[:, g, c_lo:c_lo + nb], xx, hid[:, 2, 0:nb], ALU.mult)
            if SUB < 2:
                continue
            def attend_multi(jobs):
                items = []
                for jb in jobs:
                    jb["npv"] = 0
                    jb["npv_total"] = sum((c1 - c0) // 128 for (kt, c0, c1, extra) in jb["tiles"])
                    for t_ in jb["tiles"]:
                        items.append((jb, t_))

                def emit_scores(idx):
                    jb, (kt, c0, c1, extra) = items[idx]
                    if "q" not in jb:
                        jb["q"] = jb["qfn"]()
                        jb["ops"] = psO.next()
                    sp_ = psS.next()
                    mask_fn = jb.get("mask")
                    n_extra = len(extra) + (1 if mask_fn is not None else 0)
                    mm(sp_[:, c0:c1], jb["kT"](kt), jb["q"][:, c0:c1], True, n_extra == 0)
                    k_ = 0
                    if mask_fn is not None:
                        k_ += 1
                        ml, mr = mask_fn(kt)
                        mm(sp_[:, c0:c1], ml, mr[:, c0:c1], False, k_ == n_extra)
                    for (e0, en, el, er) in extra:
                        k_ += 1
                        mm(sp_[:, e0:e0 + en], el, er, False, k_ == n_extra)
                    return sp_

                DEPTH = 2
                pend = [emit_scores(i) for i in range(min(DEPTH, len(items)))]
                for ti in range(len(items)):
                    jb, (kt, c0, c1, extra) = items[ti]
                    sp_ = pend.pop(0)
                    if ti + DEPTH < len(items):
                        pend.append(emit_scores(ti + DEPTH))
                    pt = ptr.next()
                    if jb["narrow"]:
                        for j in range(c0 // 128, c1 // 128):
                            act(pt[:, j * 128:(j + 1) * 128], sp_[:, j * 128:(j + 1) * 128], AF.Exp,
                                bias=jb["bias"](kt, j), scale=0.125)
                    else:
                        act(pt[:, c0:c1], sp_[:, c0:c1], AF.Exp, bias=jb["bias"](kt, None), scale=0.125)
                    ncols = jb.get("ncols", 65)
                    for j in range(c0 // 128, c1 // 128):
                        jb["npv"] += 1
                        mm(jb["ops"][:, j * 128:j * 128 + ncols], pt[:, j * 128:(j + 1) * 128], jb["v"](kt),
                           jb["npv"] == 1, jb["npv"] == jb["npv_total"])
                    if jb["npv"] == jb["npv_total"]:
                        jb["done"](jb["ops"])

            triA = cb[:, OB["triA"]:OB["triA"] + 128]
            triB = cb[:, OB["triB"]:OB["triB"] + 128]

            def _jhi(kt, tr_, dmax):
                return min(3, (dmax + 126) // 128 + kt - 4 * tr_)

            def causal_tiles(tr_, dmax=1 << 30):
                tl = []
                for kt in range(0, 4 * tr_ + 4):
                    i = kt - 4 * tr_
                    j_lo = max(0, i)
                    j_hi = _jhi(kt, tr_, dmax)
                    if j_hi < j_lo:
                        continue
                    extra = [(128 * i, 128, identb, triA)] if i >= 0 else []
                    tl.append((kt, 128 * j_lo, 128 * (j_hi + 1), extra))
                return tl

            def win_tiles(tr_, dmax=1 << 30):
                tl = []
                for kt in range(4 * tr_ - 4, 4 * tr_ + 4):
                    if kt < 0:
                        continue
                    i = kt - 4 * tr_
                    j_hi = _jhi(kt, tr_, dmax)
                    if i < 0:
                        i2 = i + 4
                        jh = min(i2, j_hi)
                        if jh < 0:
                            continue
                        extra = [(128 * i2, 128, identb, triB)] if jh == i2 else []
                        tl.append((kt, 0, 128 * (jh + 1), extra))
                    else:
                        if j_hi < i:
                            continue
                        tl.append((kt, 128 * i, 128 * (j_hi + 1), [(128 * i, 128, identb, triA)]))
                return tl

            def dmax_of(slope):
                return int(np.ceil(64.0 / float(slope)))

            if not PLAN[0]:
                def bias_head(hidx, narrow):
                    def f(kt, j):
                        if narrow:
                            di = kt - (4 * tr + j) + 15
                            o_ = OF["bn"] + hidx * 16 + di
                        else:
                            di = kt - 4 * tr + 12
                            o_ = OF["bw"] + hidx * 16 + di
                        return cf[:, o_:o_ + 1]
                    return f

                def bias_cmp(h, narrow):
                    def f(kt, j):
                        if narrow:
                            o_ = OF["cn"] + h * 16 + (4 * tr + j)
                        else:
                            o_ = OF["cw"] + h * 4 + tr
                        return cf[:, o_:o_ + 1]
                    return f

                def norm_coef(ob_, gate_ap):
                    ov_ = ob_[:, :].rearrange("p (j c) -> p j c", j=4)
                    ts(smal[:, 0:4], ov_[:, :, 64], TINY, None, ALU.max)
                    S.op("dve", lambda: nc.vector.reciprocal(out=smal[:, 4:8], in_=smal[:, 0:4]),
                         [smal[:, 0:4]], [smal[:, 4:8]])
                    if gate_ap is not None:
                        tt(smal[:, 8:12], smal[:, 4:8], gate_ap, ALU.mult)
                    return ov_

                def qpad(src3, ch, bp, eng):
                    half = bp // 64
                    b = qz[2 * half + (qzc[half] % 2)]
                    qzc[half] += 1
                    cp(b[bp:bp + 64, :], src3[bp:bp + 64, ch, :], e=eng)
                    return b

            if not PLAN[0]:
                if tr >= 2 and MDBG >= 1:
                    for j in range(4):
                        pb2 = psM.next()
                        tp(pb2[0:64, 0:128], mbtok[:, j, :], identf)
                        cp(mbTm[0:64, j * 128:(j + 1) * 128], pb2[0:64, 0:128])
            if not PLAN[0]:
                for g in range(2):
                    pb2 = psM.next()
                    mm(pb2[:, 0:nb], w2k[:, :], hidb[:, g, 0:nb], True, True)
                    cp(kcT[64 * g:64 * g + 64, c_lo:c_lo + nb], pb2[64 * g:64 * g + 64, 0:nb])
                    pb3 = psM.next()
                    mm(pb3[:, 0:64], hidv[:, g, :], w2v[:, :], True, True)
                    cp(vca[:, g, 0:64], pb3[:, 0:64])
                S.mark("tr%d nsa-cmp" % tr)
                ots = [otk[0], otk[1]]
            if SUB < 3:
                continue
            if not PLAN[0]:
                jobs = []
                for g in range(2):
                    ot = ots[g]
                    for hg in range(4):
                        h = g * 4 + hg
                        ch, bp = h % 4, 64 * (h // 4)
                        nar = bool(sl_n[h] > 0.3)

                        def done(oc, ot=ot, hg=hg, g=g, h=h):
                            ocv = norm_coef(oc, gsig[:, 4 * tr:4 * tr + 4, h * 3 + 0])
                            for j in range(4):
                                ts(ot[:, j, hg, :], ocv[:, j, 0:64], smal[:, 8 + j:9 + j], None, ALU.mult)
                                if tr >= 2:
                                    if hg == 0:
                                        ts(impa[:, j, g, :], ocv[:, j, 65:97], smal[:, 4 + j:5 + j], None, ALU.mult)
                                    else:
                                        stt(impa[:, j, g, :], ocv[:, j, 65:97], smal[:, 4 + j:5 + j],
                                            impa[:, j, g, :], ALU.mult, ALU.add)
                        jobs.append(dict(kT=lambda kt: kcT[:, :],
                                         qfn=lambda ch=ch, bp=bp: qpad(qn, ch, bp, "dve"),
                                         v=lambda kt, g=g: vca[:, g, :],
                                         tiles=[(0, 0, 512, [(0, 512, identb, cmsk[:, :])])],
                                         bias=bias_cmp(h, nar), narrow=nar, mask=None, ncols=97, done=done))
                attend_multi(jobs)
                if tr >= 2:
                    for j in range(4):
                        kp = rtab[:, j * 32:(j + 1) * 32]
                        fo = rtab[:, 128 + j * 32:128 + (j + 1) * 32]
                        for g in range(2):
                            iv = tk32[:, 0:32]
                            tt(iv, impa[:, j, g, :], kp, ALU.mult)
                            tt(iv, iv, fo, ALU.add)
                            S.op("dve", lambda: nc.vector.max(out=m8[:, 0:8], in_=iv), [iv], [m8[:, 0:8]])
                            S.op("dve", lambda: nc.vector.match_replace(out=tk32[:, 32:64], in_to_replace=m8[:, 0:8],
                                                                         in_values=iv, imm_value=-3.0e38),
                                 [iv, m8[:, 0:8]], [tk32[:, 32:64]])
                            S.op("dve", lambda: nc.vector.max(out=m8[:, 8:16], in_=tk32[:, 32:64]),
                                 [tk32[:, 32:64]], [m8[:, 8:16]])
                            ts(tk32[:, 32:64], iv, m8[:, 15:16], None, ALU.is_ge)
                            ts(mbtokN[:, j, g, :], tk32[:, 32:64], -1.0, -MASKV, ALU.add, ALU.mult)
                S.mark("tr%d moba" % tr)
                jobs = []
                for hp in range(4):
                    ot = otr.next()
                    for h2 in range(2):
                        h = hp * 2 + h2
                        ch, bp = hp, 64 * h2
                        nar = bool(sl_m[h] > 0.3)
                        mfn = None
                        if tr >= 2 and MDBG >= 2:
                            def mfn(kt, h=h):
                                c_ = 8 * h + kt // 2
                                l_ = bass.AP(cb, OB["ident"] + c_, [[NBS, 128], [0, 128]])
                                return l_, mbTm[:, :]

                        def done(om, ot=ot, h2=h2, hp=hp):
                            ov_ = norm_coef(om, None)
                            for j in range(4):
                                ts(ot[:, j, h2, :], ov_[:, j, 0:64], smal[:, 4 + j:5 + j], None, ALU.mult)
                            if h2 == 1:
                                pb = psM.next()
                                for j in range(4):
                                    tp(pb[:, j * 128:(j + 1) * 128], ot[:, j, 0:2, :], identf)
                                cp(omT[:, hp, :], pb[:, :], e="act")
                        jobs.append(dict(kT=lambda kt, ch=ch: kmT[:, ch, kt * 128:(kt + 1) * 128],
                                         qfn=lambda ch=ch, bp=bp: qpad(qm, ch, bp, "dve"),
                                         v=lambda kt, h=h: vm[:, kt, h, :],
                                         tiles=causal_tiles(tr, dmax_of(sl_m[h])), bias=bias_head(8 + h, nar),
                                         narrow=nar, mask=mfn, done=done))
                attend_multi(jobs)

                if tr >= 2:
                    for j in range(4):
                        for g in range(2):
                            pb = psM.next()
                            tp(pb[0:32, 0:128], mbtokN[:, j, g, :], identf)
                            cp(mbTn[64 * g:64 * g + 32, g, j * 128:(j + 1) * 128], pb[0:32, 0:128])
                S.mark("tr%d nsa-selwin" % tr)
                jobs = []
                for g in range(2):
                    ot = ots[g]
                    for hg in range(4):
                        h = g * 4 + hg
                        ch, bp = h % 4, 64 * (h // 4)
                        nar = bool(sl_n[h] > 0.3)
                        qc = {}

                        def qfn(ch=ch, bp=bp, qc=qc):
                            if "q" not in qc:
                                qc["q"] = qpad(qn, ch, bp, "dve")
                            return qc["q"]
                        mfn = None
                        if tr >= 2:
                            def mfn(kt, g=g):
                                return esel[:, kt, :], mbTn[:, g, :]

                        def done_sel(ob_, ot=ot, hg=hg, h=h):
                            ov_ = norm_coef(ob_, gsig[:, 4 * tr:4 * tr + 4, h * 3 + 1])
                            for j in range(4):
                                stt(ot[:, j, hg, :], ov_[:, j, 0:64], smal[:, 8 + j:9 + j], ot[:, j, hg, :],
                                    ALU.mult, ALU.add)

                        def done_win(ob_, ot=ot, hg=hg, h=h, g=g):
                            ov_ = norm_coef(ob_, gsig[:, 4 * tr:4 * tr + 4, h * 3 + 2])
                            for j in range(4):
                                stt(ot[:, j, hg, :], ov_[:, j, 0:64], smal[:, 8 + j:9 + j], ot[:, j, hg, :],
                                    ALU.mult, ALU.add)
                            if hg == 3:
                                for i2 in range(2):
                                    pb = psM.next()
                                    for j in range(4):
                                        tp(pb[:, j * 128:(j + 1) * 128], ot[:, j, 2 * i2:2 * i2 + 2, :], identf)
                                    cp(onT[:, 2 * g + i2, :], pb[:, :], e="act")
                        jobs.append(dict(kT=lambda kt: kslcT[:, kt * 128:(kt + 1) * 128], qfn=qfn,
                                         v=lambda kt, g=g: vslc[:, kt, g, :],
                                         tiles=causal_tiles(tr, dmax_of(sl_n[h])), bias=bias_head(h, nar),
                                         narrow=nar, mask=mfn, done=done_sel))
                        jobs.append(dict(kT=lambda kt: kwinT[:, kt * 128:(kt + 1) * 128], qfn=qfn,
                                         v=lambda kt, g=g: vwin[:, kt, g, :],
                                         tiles=win_tiles(tr, dmax_of(sl_n[h])), bias=bias_head(h, nar),
                                         narrow=nar, mask=None, done=done_win))
                attend_multi(jobs)

            if SUB < 5:
                continue
            if not PLAN[0]:
                S.mark("tr%d merge" % tr)
            for dc in range(8):
                c0 = dc * 128
                MJ = W([(0, [8, 128], win[:, :, 2840 + c0:2840 + c0 + 128]),
                        (1024, [8, 128], win[:, :, 3864 + c0:3864 + c0 + 128]),
                        (2048, [4, 128], wupn[:, :, c0:c0 + 128]),
                        (2560, [4, 128], wupm[:, :, c0:c0 + 128])])
                if PLAN[0]:
                    continue
                ga_ = MJ[:, 0:1024].rearrange("p (c f) -> p c f", c=8)
                gb_ = MJ[:, 1024:2048].rearrange("p (c f) -> p c f", c=8)
                un_ = MJ[:, 2048:2560].rearrange("p (c f) -> p c f", c=4)
                um_ = MJ[:, 2560:3072].rearrange("p (c f) -> p c f", c=4)
                pga = psS.next()
                pgb = psS.next()
                pyn = psO.next()
                pym = psO.next()
                for c in range(8):
                    mm(pga[:, :], ga_[:, c, :], u_tr[:, c, :], c == 0, c == 7)
                for c in range(8):
                    mm(pgb[:, :], gb_[:, c, :], u_tr[:, c, :], c == 0, c == 7)
                for c in range(4):
                    mm(pyn[:, :], un_[:, c, :], onT[:, c, :], c == 0, c == 3)
                for c in range(4):
                    mm(pym[:, :], um_[:, c, :], omT[:, c, :], c == 0, c == 3)
                sA = sgAr.next()
                sB = sgBr.next()
                act(sA[:, :], pga[:, :], AF.Sigmoid)
                act(sB[:, :], pgb[:, :], AF.Sigmoid)
                tt(sA[:, :], sA[:, :], pyn[:, :], ALU.mult)
                tt(sB[:, :], sB[:, :], pym[:, :], ALU.mult)
                tt(mixed[:, dc, :], sA[:, :], sB[:, :], ALU.add)
            for dh in range(2):
                WO = W([(0, [8, 512], wout[:, :, dh * 512:(dh + 1) * 512])])
                if PLAN[0]:
                    continue
                wo_ = WO[:, 0:4096].rearrange("p (c f) -> p c f", c=8)
                for d4 in range(4):
                    dc = dh * 4 + d4
                    pb = psM.next()
                    for c in range(8):
                        mm(pb[:, :], wo_[:, c, d4 * 128:(d4 + 1) * 128], mixed[:, c, :], c == 0, c == 7)
                    tt(hT[:, dc, trs(tr)], hT[:, dc, trs(tr)], pb[:, :], ALU.add)

    def run_all():
        if not PLAN[0]:
            S.mark("ffn1")
        if stage >= 1:
            scoped(phase_ffn, "ffn1_w1", "ffn1_w3", "ffn1_w2", 0, "a")
        if stage >= 2:
            scoped(phase_mixer)
        if stage >= 3:
            if not PLAN[0]:
                S.mark("ffn2")
            scoped(phase_ffn, "ffn2_w1", "ffn2_w3", "ffn2_w2", 2, "b")
        if stage >= 4:
            if not PLAN[0]:
                S.mark("ple")
            scoped(phase_ple)

    PLAN[0] = True
    run_all()
    PLAN[0] = False
    scoped(phase_load)
    run_all()
    S.mark("final")
    scoped(phase_final)
    S.mark("end")
    S.finish()
    _LAST["S"] = S
    return nc


_NC_CACHE = {}
_LAST = {}


def _prep_inputs(inputs):
    cf_np, _, cb_np, _ = _make_consts()
    f = lambda a: np.ascontiguousarray(np.asarray(a, dtype=np.float32))
    sh = {}
    for nm in ("ffn1_w1", "ffn1_w3", "ffn1_w2", "w_in", "cmp_w1_k", "cmp_w2_k", "cmp_w1_v", "cmp_w2_v",
               "w_up_nsa", "w_up_moba", "w_out", "ffn2_w1", "ffn2_w3", "ffn2_w2", "w_ple_gate", "w_ple"):
        sh[nm] = f(inputs[nm])[0]
    wi = sh["w_in"].copy()
    perm = [0, 4, 1, 5, 2, 6, 3, 7]
    wi[:, 0:512] = sh["w_in"][:, 0:512].reshape(D, 8, 64)[:, perm, :].reshape(D, 512)
    sh["w_in"] = np.ascontiguousarray(wi)
    for nm, src in (("pos_kT", "cmp_pos_k"), ("pos_vT", "cmp_pos_v")):
        pt = f(inputs[src])[0].T
        sh[nm] = np.ascontiguousarray(np.concatenate([pt, pt], axis=0))
    g = np.stack([f(inputs[n])[0] for n in ("ffn1_norm", "mix_norm", "ffn2_norm", "ple_norm")], axis=0)
    sh["gains"] = np.ascontiguousarray(g.reshape(4, 8, 128).transpose(2, 0, 1).reshape(128, 32))
    sh["gfin"] = np.ascontiguousarray(np.broadcast_to(f(inputs["final_norm"])[None, :], (128, D)))
    sh["cf32"] = cf_np
    sh["cbf16"] = cb_np
    return sh


def kernel(**inputs):
    stage = int(os.environ.get("MK_STAGE", "99"))
    ncores = int(os.environ.get("MK_CORES", "8"))
    if stage not in _NC_CACHE:
        _NC_CACHE[stage] = build_nc(stage)
    nc = _NC_CACHE[stage]
    sh = _prep_inputs(inputs)
    x = np.asarray(inputs["x"], dtype=np.float32)
    p = np.asarray(inputs["p"], dtype=np.float32)
    in_maps = []
    for b in range(ncores):
        m = dict(sh)
        m["x"] = np.ascontiguousarray(x[b])
        m["p"] = np.ascontiguousarray(p[0, b])
        in_maps.append(m)
    res = run_bass_kernel_spmd(nc, in_maps, core_ids=list(range(ncores)))
    out = np.stack([np.asarray(r["out"], dtype=np.float32) for r in res.results], axis=0)
    return out
```

```python
import os
import numpy as np
import ml_dtypes
import concourse.bass as bass
import concourse.mybir as mybir
from concourse.bass_utils import run_bass_kernel_spmd
from contextlib import ExitStack

F32 = mybir.dt.float32
BF16 = mybir.dt.bfloat16
AF = mybir.ActivationFunctionType
ALU = mybir.AluOpType

T = 2048
D = 1024
FF = 2816
MASKV = -240000.0
EPS = 1e-6
TINY = 1e-30
IN_W = 4888


def _slopes():
    s = 2.0 ** (-(np.arange(16) + 1) / 2.0)
    return s[0::2].copy(), s[1::2].copy()


def _make_consts():
    p = np.arange(128)
    f32 = {}
    b16 = {}
    f32["ident"] = np.eye(128)
    b16["ident"] = np.eye(128)
    b16["ones"] = np.ones((128, 128))
    j = np.arange(128)
    b16["triA"] = np.where(j[None, :] >= p[:, None], 0.0, MASKV)
    b16["triB"] = np.where(j[None, :] < p[:, None], 0.0, MASKV)
    tq = np.arange(T)
    cend = 16 * p + 31
    cm = np.where(tq[None, :] >= cend[:, None], 0.0, MASKV)
    cm[127, :] = MASKV
    ov = np.zeros((128, 32))
    for c in range(127):
        for jj in range(32):
            if 16 * c <= 64 * jj + 63 and 16 * c + 31 >= 64 * jj:
                ov[c, jj] = 1.0
    b16["overlap"] = ov
    b16["cmpmask"] = cm
    es = np.zeros((128, 16, 128))
    for pp in range(128):
        jj = pp % 64
        if jj >= 32:
            continue
        for kt in range(16):
            for m_ in range(128):
                if jj == 2 * kt + m_ // 64:
                    es[pp, kt, m_] = 1.0
    b16["esel"] = es.reshape(128, 2048)
    sl_n, sl_m = _slopes()
    sl16 = np.concatenate([sl_n, sl_m])
    d = np.arange(16) - 12
    f32["bw"] = (sl16[None, :, None] * (128 * d[None, None, :] + p[:, None, None] - 256)).reshape(128, 256)
    d2 = np.arange(16) - 15
    f32["bn"] = (sl16[None, :, None] * (128 * d2[None, None, :] + p[:, None, None] - 64)).reshape(128, 256)
    f32["cw"] = (sl_n[None, :, None] * (cend[:, None, None] - (512 * np.arange(4)[None, None, :] + 256))).reshape(128, 32)
    f32["cn"] = (sl_n[None, :, None] * (cend[:, None, None] - (128 * np.arange(16)[None, None, :] + 64))).reshape(128, 128)
    keep = np.zeros((128, 8, 32))
    force = np.zeros((128, 8, 32))
    for tt in range(8, 16):
        for pp in range(128):
            t = tt * 128 + pp
            cur = t // 64
            for jj in range(32):
                if 64 * jj > t:
                    force[pp, tt - 8, jj] = -1e30 * (1.0 + jj / 64.0)
                elif jj == 0:
                    force[pp, tt - 8, jj] = 3e9
                elif jj == cur:
                    force[pp, tt - 8, jj] = 2e9
                elif jj == cur - 1:
                    force[pp, tt - 8, jj] = 1e9
                else:
                    keep[pp, tt - 8, jj] = 1.0
    padneg = np.zeros((128, 8, 8))
    ownhot = np.zeros((128, 8, 8))
    for tt in range(8, 16):
        cur = tt // 2
        for n in range(8):
            if n >= cur:
                padneg[:, tt - 8, n] = -1e30
            if n == cur:
                ownhot[:, tt - 8, n] = 1.0
    f32["eps"] = np.full((128, 1), EPS)
    rt = []
    for tr in (2, 3):
        a = (tr - 2) * 4
        rt += [keep[:, a:a + 4].reshape(128, 128), force[:, a:a + 4].reshape(128, 128),
               padneg[:, a:a + 4].reshape(128, 32), ownhot[:, a:a + 4].reshape(128, 32)]
    f32["rt"] = np.concatenate(rt, axis=1)
    offs_f = {}
    cols = []
    o = 0
    for k, v in f32.items():
        offs_f[k] = o
        o += v.shape[1]
        cols.append(v.astype(np.float32))
    cf = np.ascontiguousarray(np.concatenate(cols, axis=1))
    offs_b = {}
    cols = []
    o = 0
    for k, v in b16.items():
        offs_b[k] = o
        o += v.shape[1]
        cols.append(v.astype(np.float32))
    cbv = np.ascontiguousarray(np.concatenate(cols, axis=1).astype(ml_dtypes.bfloat16))
    return cf, offs_f, cbv, offs_b


EMBED_WAIT = set(os.environ.get("MK_EMBED", "pe").split(",")) - {""}


def _box(ap):
    t = ap.tensor
    if t.name.startswith("ps") and len(t.name) == 3:
        return (t.name, 0, 128, 0, 512)
    row = 1
    for s in tuple(t.shape)[1:]:
        row *= int(s)
    off = int(ap.offset)
    p0 = off // row
    f0 = off % row
    apl = ap.ap
    npart = apl[0][1]
    ext = 1
    for st, cnt in apl[1:]:
        ext += (cnt - 1) * abs(st)
    return (t.name, p0, p0 + npart, f0, f0 + ext)


class Sched:
    def __init__(self, nc, ndma=24):
        self.nc = nc
        self.engs = {"pe": nc.tensor, "act": nc.scalar, "dve": nc.vector, "pool": nc.gpsimd, "sp": nc.sync}
        self.semh = {}
        for e in self.engs:
            self.semh[e] = nc.alloc_semaphore("sem_" + e)
        self.cnt = {e: 0 for e in self.engs}
        self.ndma = ndma
        for i in range(ndma):
            self.semh[("d", i)] = nc.alloc_semaphore("dsem%d" % i)
        self.dcnt = [0] * ndma
        self.drr = 0
        self.drr_pool = 0
        self.seen = {e: {} for e in self.engs}
        self.acc = {}
        self.out_tickets = []
        self.nwaits = 0
        self.marks = []

    def mark(self, name):
        self.marks.append((name, self.cnt["pe"]))

    def _wait(self, e, sk, val):
        if e == "pe" and sk == "pe":
            return
        if self.seen[e].get(sk, 0) >= val:
            return
        self.engs[e].wait_ge(self.semh[sk], val)
        self.seen[e][sk] = val
        self.nwaits += 1

    def _collect(self, reads, writes):
        waits = {}
        for ap in reads:
            b = _box(ap)
            for ent in self.acc.get(b[0], ()):
                if ent[0] == "w" and ent[3] < b[2] and b[1] < ent[4] and ent[5] < b[4] and b[3] < ent[6]:
                    if waits.get(ent[1], 0) < ent[2]:
                        waits[ent[1]] = ent[2]
        for ap in writes:
            b = _box(ap)
            for ent in self.acc.get(b[0], ()):
                if ent[3] < b[2] and b[1] < ent[4] and ent[5] < b[4] and b[3] < ent[6]:
                    if waits.get(ent[1], 0) < ent[2]:
                        waits[ent[1]] = ent[2]
        return waits

    def _record(self, sk, val, reads, writes):
        for ap in writes:
            b = _box(ap)
            lst = self.acc.setdefault(b[0], [])
            lst[:] = [e for e in lst if not (b[1] <= e[3] and e[4] <= b[2] and b[3] <= e[5] and e[6] <= b[4])]
            lst.append(("w", sk, val, b[1], b[2], b[3], b[4]))
        for ap in reads:
            b = _box(ap)
            lst = self.acc.setdefault(b[0], [])
            lst[:] = [e for e in lst if not (e[0] == "r" and e[1] == sk and b[1] <= e[3] and e[4] <= b[2]
                                             and b[3] <= e[5] and e[6] <= b[4])]
            lst.append(("r", sk, val, b[1], b[2], b[3], b[4]))

    def op(self, e, fn, reads, writes, inc=True):
        assert inc or e == "pe"
        waits = self._collect(reads, writes)
        emb = None
        if EMBED_WAIT and e in EMBED_WAIT:
            need = [(sk, val) for sk, val in waits.items()
                    if not (e == "pe" and sk == "pe") and self.seen[e].get(sk, 0) < val]
            if need:
                emb = need[-1]
                waits = dict(need[:-1])
            else:
                waits = {}
        for sk, val in waits.items():
            self._wait(e, sk, val)
        ins = fn()
        if emb is not None:
            ins._wait_ge(self.semh[emb[0]], emb[1])
            self.seen[e][emb[0]] = emb[1]
        if inc:
            self.cnt[e] += 1
            ins.then_inc(self.semh[e], 1)
            val = self.cnt[e]
        else:
            val = self.cnt[e] + 1
        self._record(e, val, reads, writes)
        return ins

    def dma(self, q, out, in_, reads=(), writes=(), is_out=False, **kw):
        waits = self._collect(reads, writes)
        for sk, val in waits.items():
            self._wait(q, sk, val)
        half = self.ndma // 2
        if q == "pool":
            i = half + (self.drr_pool % half)
            self.drr_pool += 1
        else:
            i = self.drr % half
            self.drr += 1
        sk = ("d", i)
        if self.dcnt[i] > 0:
            self._wait(q, sk, self.dcnt[i])
        ins = self.engs[q].dma_start(out=out, in_=in_, **kw)
        self.dcnt[i] += 16
        ins.then_inc(self.semh[sk], 16)
        self._record(sk, self.dcnt[i], reads, writes)
        if is_out:
            self.out_tickets.append((sk, self.dcnt[i]))

    def barrier(self):
        for e in ("pe", "act", "dve", "pool", "sp"):
            for f in ("pe", "act", "dve", "pool", "sp"):
                if f != e and self.cnt[f] > 0:
                    self._wait(e, f, self.cnt[f])
            for i in range(self.ndma):
                if self.dcnt[i] > 0:
                    self._wait(e, ("d", i), self.dcnt[i])
        self.acc = {}

    def finish(self):
        for sk, val in self.out_tickets:
            self._wait("sp", sk, val)


def build_nc(stage=99):
    nc = bass.Bass("TRN2", target_bir_lowering=False)
    cf_np, OF, cb_np, OB = _make_consts()
    NF = cf_np.shape[1]
    NB = cb_np.shape[1]

    def din(name, shape, dt=F32):
        return nc.dram_tensor(name, list(shape), dt, kind="ExternalInput").ap()

    x_d = din("x", [T, D])
    p_d = din("p", [T, 256])
    wd = {}
    for nm, shp in [("ffn1_w1", [D, FF]), ("ffn1_w3", [D, FF]), ("ffn1_w2", [FF, D]), ("w_in", [D, IN_W]),
                    ("cmp_w1_k", [2048, 128]), ("cmp_w2_k", [128, 64]), ("cmp_w1_v", [2048, 128]),
                    ("cmp_w2_v", [128, 64]), ("pos_kT", [128, 32]), ("pos_vT", [128, 32]),
                    ("w_up_nsa", [512, D]), ("w_up_moba", [512, D]), ("w_out", [D, D]),
                    ("ffn2_w1", [D, FF]), ("ffn2_w3", [D, FF]), ("ffn2_w2", [FF, D]),
                    ("w_ple_gate", [D, D]), ("w_ple", [256, D]), ("gains", [128, 32]), ("gfin", [128, D])]:
        wd[nm] = din(nm, shp)
    cf_d = din("cf32", [128, NF])
    cb_d = din("cbf16", [128, NB], BF16)
    out_d = nc.dram_tensor("out", [T, D], F32, kind="ExternalOutput").ap()

    S = Sched(nc)
    stack = [None]

    def TS(name, shape, dt):
        if stack[0] is None:
            return nc.alloc_sbuf_tensor(name, shape, dt)
        return stack[0].enter_context(nc.sbuf_tensor(name, shape, dt))

    def scoped(fn, *a):
        if PLAN[0]:
            fn(*a)
            return
        S.barrier()
        with ExitStack() as st:
            stack[0] = st
            fn(*a)
            S.barrier()
        stack[0] = None

    hT = TS("hT", [128, 8, T], F32)
    NFS = OF["rt"]
    NBS = OB["cmpmask"]
    cf = TS("cf", [128, NFS], F32)
    cb = TS("cb", [128, NBS], BF16)
    gains = TS("gains_sb", [128, 32], F32)
    slab = [TS("slab%d" % i, [128, 4096], BF16) for i in range(3)]
    ps = [nc.alloc_psum_tensor("ps%d" % i, [128, 512], F32) for i in range(8)]

    identf = cf[:, OF["ident"]:OF["ident"] + 128]
    identb = cb[:, OB["ident"]:OB["ident"] + 128]
    onesb = cb[:, OB["ones"]:OB["ones"] + 128]
    epsc = cf[:, OF["eps"]:OF["eps"] + 1]

    S.dma("sp", cf[:, :], cf_d[:, 0:NFS], writes=[cf[:, :]])
    S.dma("sp", cb[:, :], cb_d[:, 0:NBS], writes=[cb[:, :]])
    S.dma("sp", gains[:, :], wd["gains"][:, :], writes=[gains[:, :]])

    def mm(out, lhsT, rhs, start, stop, inc=None):
        inc = True
        return S.op("pe", lambda: nc.tensor.matmul(out, lhsT, rhs, start=start, stop=stop),
                    [lhsT, rhs], [out], inc=inc)

    def pe_drain():
        if S.cnt["pe"] > 0:
            nc.tensor.wait_ge(S.semh["pe"], S.cnt["pe"])

    def tp(out, in_, ident):
        return S.op("pe", lambda: nc.tensor.transpose(out, in_, ident), [in_, ident], [out])

    def act(out, in_, func, bias=None, scale=None, accum_out=None):
        kw = {}
        rd = [in_]
        wr = [out]
        if bias is not None:
            kw["bias"] = bias
            if not isinstance(bias, (int, float)):
                rd.append(bias)
        if scale is not None:
            kw["scale"] = scale
            if not isinstance(scale, (int, float)):
                rd.append(scale)
        if accum_out is not None:
            kw["accum_out"] = accum_out
            wr.append(accum_out)
        return S.op("act", lambda: nc.scalar.activation(out=out, in_=in_, func=func, **kw), rd, wr)

    def tt(out, in0, in1, op, e="dve"):
        eng = nc.vector if e == "dve" else nc.gpsimd
        return S.op(e, lambda: eng.tensor_tensor(out=out, in0=in0, in1=in1, op=op), [in0, in1], [out])

    def ts(out, in0, s1, s2, op0, op1=None, e="dve"):
        eng = nc.vector if e == "dve" else nc.gpsimd
        rd = [in0]
        if not isinstance(s1, (int, float)):
            rd.append(s1)
        if s2 is not None and not isinstance(s2, (int, float)):
            rd.append(s2)
        if op1 is None:
            return S.op(e, lambda: eng.tensor_scalar(out=out, in0=in0, scalar1=s1, scalar2=None, op0=op0), rd, [out])
        return S.op(e, lambda: eng.tensor_scalar(out=out, in0=in0, scalar1=s1, scalar2=s2, op0=op0, op1=op1),
                    rd, [out])

    def stt(out, in0, scalar, in1, op0, op1):
        rd = [in0, in1]
        if not isinstance(scalar, (int, float)):
            rd.append(scalar)
        return S.op("dve", lambda: nc.vector.scalar_tensor_tensor(out=out, in0=in0, scalar=scalar, in1=in1,
                                                                   op0=op0, op1=op1), rd, [out])

    def cp(out, in_, e="dve"):
        if e == "act":
            return S.op("act", lambda: nc.scalar.copy(out=out, in_=in_), [in_], [out])
        eng = nc.vector if e == "dve" else nc.gpsimd
        return S.op(e, lambda: eng.tensor_copy(out=out, in_=in_), [in_], [out])

    def memset(ap, v, e="dve"):
        eng = nc.vector if e == "dve" else nc.gpsimd
        return S.op(e, lambda: eng.memset(ap, v), [], [ap])

    class RR:
        def __init__(self, items):
            self.items = items
            self.i = 0

        def next(self):
            it = self.items[self.i % len(self.items)]
            self.i += 1
            return it

    class WStream:
        def __init__(self):
            self.jobs = []
            self.issued = 0
            self.k = 0

        def add(self, parts):
            self.jobs.append(parts)

        def _issue(self, k):
            buf = slab[k % 3]
            for (off, shape, src) in self.jobs[k]:
                n = 1
                for s_ in shape:
                    n *= s_
                npart = int(src.shape[0])
                dst = buf[0:npart, off:off + n]
                if len(shape) == 2:
                    dst = dst.rearrange("p (a b) -> p a b", a=shape[0])
                S.dma("pool", dst, src, writes=[buf[0:npart, off:off + n]])

        def get(self):
            k = self.k
            self.k += 1
            while self.issued < min(k + 2, len(self.jobs)):
                self._issue(self.issued)
                self.issued += 1
            return slab[k % 3]

    WS = WStream()
    PLAN = [True]

    def W(parts):
        if PLAN[0]:
            WS.add(parts)
            return None
        return WS.get()

    def trs(tr):
        return slice(tr * 512, (tr + 1) * 512)

    sqb = [TS("sqb%d" % i, [128, 512], BF16) for i in range(2)]
    rstd = TS("rstd", [128, 512], F32)
    sqrr = RR(sqb)
    psM = RR([ps[6], ps[7]])
    psL = RR([ps[0], ps[1], ps[2], ps[3]])

    def rms_range(tr, gidx, xn3):
        pb = psM.next()
        for c in range(8):
            sq = sqrr.next()
            act(sq[:, :], hT[:, c, trs(tr)], AF.Square)
            mm(pb[:, :], onesb, sq[:, :], c == 0, c == 7)
        act(rstd[:, :], pb[:, :], AF.Ln, bias=epsc, scale=1.0 / D)
        act(rstd[:, :], rstd[:, :], AF.Exp, scale=-0.5)
        for c in range(8):
            stt(xn3[:, c, :], hT[:, c, trs(tr)], gains[:, gidx * 8 + c:gidx * 8 + c + 1], rstd[:, :],
                ALU.mult, ALU.mult)

    def phase_load():
        NB_ = 4
        xtok = [TS("xtok%d" % i, [128, D], F32) for i in range(NB_)]

        def ld(t_):
            xb = xtok[t_ % NB_]
            S.dma("sp", xb[:, :], x_d[t_ * 128:(t_ + 1) * 128, :], writes=[xb[:, :]])
        for t_ in range(NB_ - 1):
            ld(t_)
        while WS.issued < min(2, len(WS.jobs)):
            WS._issue(WS.issued)
            WS.issued += 1
        for t_ in range(16):
            if t_ + NB_ - 1 < 16:
                ld(t_ + NB_ - 1)
            xb = xtok[t_ % NB_]
            for half in range(2):
                pb = psL.next()
                for c4 in range(4):
                    c = half * 4 + c4
                    tp(pb[:, c4 * 128:(c4 + 1) * 128], xb[:, c * 128:(c + 1) * 128], identf)
                cp(hT[:, half * 4:(half + 1) * 4, t_ * 128:(t_ + 1) * 128],
                   pb[:, :].rearrange("p (c t) -> p c t", c=4), e="act" if half else "dve")

    def phase_ffn(w1n, w3n, w2n, gidx, tag):
        w1 = wd[w1n].rearrange("(c p) f -> p c f", p=128)
        w3 = wd[w3n].rearrange("(c p) f -> p c f", p=128)
        w2 = wd[w2n]
        if not PLAN[0]:
            xnT = TS("xnT" + tag, [128, 8, T], BF16)
            h1 = [TS("h1%s%d" % (tag, i), [128, 2, 512], BF16) for i in range(3)]
            sa = [TS("sa%s%d" % (tag, i), [128, 512], F32) for i in range(4)]
            for tr in range(4):
                rms_range(tr, gidx, xnT[:, :, trs(tr)])
            psAB = RR([ps[0], ps[1], ps[2], ps[3]])
            psY = RR([ps[4], ps[5], ps[6], ps[7]])
            sar = RR(sa)
            h1r = RR(h1)
            ytmp = [TS("ytmp%s%d" % (tag, i), [128, 512], F32) for i in range(2)]
            ytr = RR(ytmp)
        Aj = {}
        Bj = {}

        def getA(s_):
            if s_ not in Aj:
                Aj[s_] = W([(0, [8, 256], w1[:, :, s_ * 256:(s_ + 1) * 256]),
                            (2048, [8, 256], w3[:, :, s_ * 256:(s_ + 1) * 256])])
            return Aj[s_]

        def getB(s_):
            if s_ not in Bj:
                Bj[s_] = W([(0, [2, 1024], w2[s_ * 256:(s_ + 1) * 256, :].rearrange("(j p) d -> p j d", p=128))])
            return Bj[s_]

        def ab_steps(u):
            s_, tr = u
            A = getA(s_)
            if PLAN[0]:
                return
            w1s = A[:, 0:2048].rearrange("p (c f) -> p c f", c=8)
            w3s = A[:, 2048:4096].rearrange("p (c f) -> p c f", c=8)
            hb = h1r.next()
            hbs[u] = hb
            for j in range(2):
                pa = psAB.next()
                pb = psAB.next()
                for c in range(8):
                    mm(pa[:, :], w1s[:, c, j * 128:(j + 1) * 128], xnT[:, c, trs(tr)], c == 0, c == 7)
                sab = sar.next()
                act(sab[:, :], pa[:, :], AF.Silu)
                yield
                for c in range(8):
                    mm(pb[:, :], w3s[:, c, j * 128:(j + 1) * 128], xnT[:, c, trs(tr)], c == 0, c == 7)
                tt(hb[:, j, :], sab[:, :], pb[:, :], ALU.mult)
                yield

        def y_steps(u):
            s_, tr = u
            B = getB(s_)
            if PLAN[0]:
                return
            hb = hbs.pop(u)
            w2s = B[:, 0:2048].rearrange("p (j d) -> p j d", j=2)
            for dc in range(8):
                py = psY.next()
                for j in range(2):
                    mm(py[:, :], w2s[:, j, dc * 128:(dc + 1) * 128], hb[:, j, :], j == 0, j == 1)
                stt(hT[:, dc, trs(tr)], py[:, :], 0.5, hT[:, dc, trs(tr)], ALU.mult, ALU.add)
                yield

        hbs = {}
        units = [(s_, tr) for s_ in range(11) for tr in range(4)]
        prev = None
        for u in units:
            ga = ab_steps(u)
            gy = y_steps(prev) if prev is not None else iter(())
            for _ in ga:
                next(gy, None)
                next(gy, None)
            for _ in gy:
                pass
            prev = u
        for _ in y_steps(prev):
            pass

    def phase_ple():
        wg = wd["w_ple_gate"].rearrange("(c p) f -> p c f", p=128)
        wp = wd["w_ple"].rearrange("(c p) f -> p c f", p=128)
        if not PLAN[0]:
            xnT = TS("xnTple", [128, 8, T], BF16)
            pT = TS("pT", [128, 2, T], BF16)
            ptok = [TS("ptok%d" % i, [128, 256], F32) for i in range(2)]
            sg = [TS("sgple%d" % i, [128, 512], F32) for i in range(2)]
            for t_ in range(16):
                pbuf = ptok[t_ % 2]
                S.dma("sp", pbuf[:, :], p_d[t_ * 128:(t_ + 1) * 128, :], writes=[pbuf[:, :]])
                pb = psM.next()
                for c in range(2):
                    tp(pb[:, c * 128:(c + 1) * 128], pbuf[:, c * 128:(c + 1) * 128], identf)
                cp(pT[:, :, t_ * 128:(t_ + 1) * 128], pb[:, 0:256].rearrange("p (c t) -> p c t", c=2))
            for tr in range(4):
                rms_range(tr, 3, xnT[:, :, trs(tr)])
            psG = RR([ps[0], ps[1], ps[2], ps[3]])
            sgr = RR(sg)
        for dh in range(2):
            A = W([(0, [8, 512], wg[:, :, dh * 512:(dh + 1) * 512])])
            B = W([(0, [2, 512], wp[:, :, dh * 512:(dh + 1) * 512])])
            if PLAN[0]:
                continue
            wgs = A[:, 0:4096].rearrange("p (c f) -> p c f", c=8)
            wps = B[:, 0:1024].rearrange("p (c f) -> p c f", c=2)
            for tr in range(4):
                for d4 in range(4):
                    dc = dh * 4 + d4
                    pg = psG.next()
                    pp = psG.next()
                    for c in range(8):
                        mm(pg[:, :], wgs[:, c, d4 * 128:(d4 + 1) * 128], xnT[:, c, trs(tr)], c == 0, c == 7)
                    for c in range(2):
                        mm(pp[:, :], wps[:, c, d4 * 128:(d4 + 1) * 128], pT[:, c, trs(tr)], c == 0, c == 1)
                    sgb = sgr.next()
                    act(sgb[:, :], pg[:, :], AF.Sigmoid)
                    tt(sgb[:, :], sgb[:, :], pp[:, :], ALU.mult)
                    tt(hT[:, dc, trs(tr)], hT[:, dc, trs(tr)], sgb[:, :], ALU.add)

    def phase_final():
        gfin = TS("gfin_sb", [128, D], F32)
        S.dma("sp", gfin[:, :], wd["gfin"][:, :], writes=[gfin[:, :]])
        otok = [TS("otok%d" % i, [128, D], F32) for i in range(4)]
        junk = TS("junk", [128, 512], F32)
        ssq = TS("ssq", [128, 4], F32)
        psF = RR([ps[0], ps[1], ps[2], ps[3], ps[4], ps[5]])
        for t_ in range(16):
            ob = otok[t_ % 4]
            pbs = [psF.next(), psF.next()]
            for half in range(2):
                for c4 in range(4):
                    c = half * 4 + c4
                    tp(pbs[half][:, c4 * 128:(c4 + 1) * 128], hT[:, c, t_ * 128:(t_ + 1) * 128], identf)
                act(junk[:, :], pbs[half][:, :], AF.Square, accum_out=ssq[:, half:half + 1])
            tt(ssq[:, 2:3], ssq[:, 0:1], ssq[:, 1:2], ALU.add)
            act(ssq[:, 3:4], ssq[:, 2:3], AF.Ln, bias=epsc, scale=1.0 / D)
            act(ssq[:, 3:4], ssq[:, 3:4], AF.Exp, scale=-0.5)
            for half in range(2):
                stt(ob[:, half * 512:(half + 1) * 512], pbs[half][:, :], ssq[:, 3:4],
                    gfin[:, half * 512:(half + 1) * 512], ALU.mult, ALU.mult)
            S.dma("sp", out_d[t_ * 128:(t_ + 1) * 128, :], ob[:, :], reads=[ob[:, :]], is_out=True)

    def phase_mixer():
        win = wd["w_in"].rearrange("(c p) f -> p c f", p=128)
        wupn = wd["w_up_nsa"].rearrange("(c p) f -> p c f", p=128)
        wupm = wd["w_up_moba"].rearrange("(c p) f -> p c f", p=128)
        wout = wd["w_out"].rearrange("(c p) f -> p c f", p=128)
        sl_n, sl_m = _slopes()
        if not PLAN[0]:
            kslcT = TS("kslcT", [128, T], BF16)
            kwinT = TS("kwinT", [128, T], BF16)
            kcmpT = TS("kcmpT", [128, 528], BF16)
            vcmpT = TS("vcmpT", [128, 528], BF16)
            kmT = TS("kmT", [128, 4, T], BF16)
            vslc = TS("vslc", [128, 16, 2, 65], BF16)
            vwin = TS("vwin", [128, 16, 2, 65], BF16)
            vm = TS("vm", [128, 16, 8, 65], BF16)
            gsig = TS("gsig", [128, 16, 24], F32)
            kcT = TS("kcT", [128, 128], BF16)
            vca = TS("vca", [128, 2, 97], BF16)
            kmeanT = TS("kmeanT", [128, 4, 16], BF16)
            kmsum = TS("kmsum", [128, 4, 2], F32)
            w2k = TS("w2k", [128, 128], BF16)
            w2v = TS("w2v", [128, 64], BF16)
            posk = TS("posk", [128, 32], BF16)
            posv = TS("posv", [128, 32], BF16)
            cbias = TS("cbias", [128, 2], F32)
            u_tr = TS("u_tr", [128, 8, 512], BF16)
            qmix = TS("qmix", [128, 8, 512], BF16)

            class _Sub:
                def __init__(self, base, off):
                    self.base, self.off = base, off

                def __getitem__(self, key):
                    a, b, c = key
                    if isinstance(b, int):
                        b = b + self.off
                    else:
                        b = slice((b.start or 0) + self.off, (b.stop if b.stop is not None else 4) + self.off)
                    return self.base[a, b, c]
            qn = _Sub(qmix, 0)
            qm = _Sub(qmix, 4)
            ptl = [TS("ptl%d" % i, [128, 512], BF16) for i in range(3)]
            otk = [TS("otk%d" % i, [128, 4, 4, 64], F32) for i in range(2)]
            omk = [TS("omk%d" % i, [128, 4, 2, 64], F32) for i in range(2)]
            onT = TS("onT", [128, 4, 512], BF16)
            omT = TS("omT", [128, 4, 512], BF16)
            mixed = qmix
            _o0 = otk[0][:, :, :, :].rearrange("p a b c -> p (a b c)")
            sgA = [_o0[:, 0:512]]
            sgB = [_o0[:, 512:1024]]
            mbTn = TS("mbTn", [128, 2, 512], BF16)
            qz = [TS("qz%d" % i, [128, 512], BF16) for i in range(4)]
            qzc = [0, 0]
            mbTm = TS("mbTm", [128, 512], BF16)
            mbtokN = TS("mbtokN", [128, 4, 2, 32], F32)
            mbtok = TS("mbtok", [128, 4, 64], F32)
            impa = TS("impa", [128, 4, 2, 32], F32)
            smal = TS("smal", [128, 16], F32)
            m8 = TS("m8", [128, 16], F32)
            tk32 = TS("tk32", [128, 64], F32)
            cmsk = TS("cmsk", [128, 512], BF16)
            rtab = TS("rtab", [128, 320], F32)
            esel = TS("esel", [128, 16, 128], BF16)
            S.dma("sp", esel[:, :, :], cb_d[:, OB["esel"]:OB["esel"] + 2048].rearrange("p (k m) -> p k m", k=16),
                  writes=[esel[:, :, :]])
            hid = TS("hid", [128, 3, 32], F32)
            hidb = TS("hidb", [128, 2, 32], BF16)
            hidv = TS("hidv", [128, 2, 128], BF16)
            gtmp = TS("gtmp", [64, 528], BF16)
            gsf = TS("gsf", [128, 8, 16], F32)

            memset(kcT[:, :], 0.0)
            for i in range(4):
                memset(qz[i][:, :], 0.0)
            memset(mbTn[:, :, :], 0.0)
            memset(mbTm[:, :], 0.0)
            memset(gsf[:, :, :], -1.0e30)
            memset(kcmpT[:, 0:16], 0.0)
            memset(vcmpT[:, 0:16], 0.0)
            memset(hidv[:, :, :], 0.0)
            memset(vca[:, :, :], 0.0)
            memset(kmeanT[:, :, :], 0.0)
            memset(vslc[:, :, :, 64:65], 1.0)
            memset(vwin[:, :, :, 64:65], 1.0)
            memset(vm[:, :, :, 64:65], 1.0)
            memset(vca[:, :, 64:65], 1.0)
            for g in range(2):
                cp(vca[:, g, 65:97], cb[:, OB["overlap"]:OB["overlap"] + 32])
            for (w2t, w2n_, pt, pn) in ((w2k, "cmp_w2_k", posk, "pos_kT"), (w2v, "cmp_w2_v", posv, "pos_vT")):
                S.dma("pool", pt[:, :], wd[pn][:, :], writes=[pt[:, :]])
                if w2t is w2k:
                    S.dma("pool", w2t[:, 0:64], wd[w2n_][:, :], writes=[w2t[:, 0:64]])
                    S.dma("pool", w2t[:, 64:128], wd[w2n_][:, :], writes=[w2t[:, 64:128]])
                else:
                    S.dma("pool", w2t[:, :], wd[w2n_][:, :], writes=[w2t[:, :]])

            psS = RR([ps[0], ps[1], ps[2]])
            psO = RR([ps[3], ps[4], ps[5]])
            psG2 = RR([ps[0], ps[1], ps[2], ps[6], ps[7]])
            ptr = RR(ptl)
            otr = RR(omk)
            sgAr = RR(sgA)
            sgBr = RR(sgB)

        def proj_fm(ws, c0, dst, tr, base=None, ev="act"):
            pb = psM.next()
            for c in range(8):
                if base is None:
                    l_ = ws[:, c, c0:c0 + 128]
                else:
                    l_ = base(c)
                mm(pb[:, :], l_, u_tr[:, c, :], c == 0, c == 7)
            cp(dst, pb[:, :], e=ev)

        NTRS = int(os.environ.get("MK_NTR", "4"))
        SUB = int(os.environ.get("MK_SUB", "9"))
        for tr in range(NTRS):
            S0 = W([(0, [8, 512], win[:, :, 0:512])])
            if not PLAN[0]:
                S.mark("tr%d proj" % tr)
                rms_range(tr, 1, u_tr)
                S.dma("sp", cmsk[:, :], cb_d[:, OB["cmpmask"] + tr * 512:OB["cmpmask"] + (tr + 1) * 512],
                      writes=[cmsk[:, :]])
                if tr >= 2:
                    S.dma("sp", rtab[:, :], cf_d[:, OF["rt"] + (tr - 2) * 320:OF["rt"] + (tr - 1) * 320],
                          writes=[rtab[:, :]])

                w0 = S0[:, 0:4096].rearrange("p (c f) -> p c f", c=8)
                for i in range(4):
                    proj_fm(w0, i * 128, qn[:, i, :], tr, ev="act" if i % 2 else "dve")
            S1 = W([(0, [8, 512], win[:, :, 512:1024])])
            if not PLAN[0]:
                w1_ = S1[:, 0:4096].rearrange("p (c f) -> p c f", c=8)
                proj_fm(w1_, 0, kcmpT[:, 16:528], tr, ev="act")
                proj_fm(w1_, 128, vcmpT[:, 16:528], tr, ev="dve")
                proj_fm(w1_, 256, kslcT[:, trs(tr)], tr, ev="act")
                for j in range(4):
                    t_ = tr * 4 + j
                    pb = psM.next()
                    for c in range(8):
                        mm(pb[:, 0:128], u_tr[:, c, j * 128:(j + 1) * 128], w1_[:, c, 384:512], c == 0, c == 7)
                    cp(vslc[:, t_, :, 0:64], pb[:, 0:128].rearrange("p (g d) -> p g d", g=2))
            S2 = W([(0, [8, 280], win[:, :, 1024:1304])])
            if not PLAN[0]:
                w2_ = S2[:, 0:2240].rearrange("p (c f) -> p c f", c=8)
                proj_fm(w2_, 0, kwinT[:, trs(tr)], tr, ev="act")
                for j in range(4):
                    t_ = tr * 4 + j
                    pb = psM.next()
                    for c in range(8):
                        mm(pb[:, 0:152], u_tr[:, c, j * 128:(j + 1) * 128], w2_[:, c, 128:280], c == 0, c == 7)
                    cp(vwin[:, t_, :, 0:64], pb[:, 0:128].rearrange("p (g d) -> p g d", g=2))
                    act(gsig[:, t_, :], pb[:, 128:152], AF.Sigmoid)
            S3 = W([(0, [8, 512], win[:, :, 1304:1816])])
            if not PLAN[0]:
                w3_ = S3[:, 0:4096].rearrange("p (c f) -> p c f", c=8)
                for i in range(4):
                    proj_fm(w3_, i * 128, qm[:, i, :], tr, ev="act" if i % 2 else "dve")
            S4 = W([(0, [8, 512], win[:, :, 1816:2328])])
            if not PLAN[0]:
                w4_ = S4[:, 0:4096].rearrange("p (c f) -> p c f", c=8)
                for i in range(4):
                    proj_fm(w4_, i * 128, kmT[:, i, trs(tr)], tr, ev="act" if i % 2 else "dve")
                for i in range(4):
                    S.op("dve", lambda: nc.vector.tensor_reduce(
                        out=kmsum[:, i, :], in_=kmT[:, i, trs(tr)].rearrange("p (n k) -> p n k", n=2),
                        axis=mybir.AxisListType.X, op=ALU.add),
                        [kmT[:, i, trs(tr)]], [kmsum[:, i, :]])
                ts(kmeanT[0:64, :, 2 * tr:2 * tr + 2], kmsum[0:64, :, :], 1.0 / 256.0, None, ALU.mult)
                ts(kmeanT[64:128, :, 8 + 2 * tr:8 + 2 * tr + 2], kmsum[64:128, :, :], 1.0 / 256.0, None, ALU.mult)
            if not PLAN[0]:
                MDBG = int(os.environ.get("MK_MOBA", "3"))
                if tr >= 2 and MDBG >= 1:
                    for j in range(4):
                        pb = psM.next()
                        for ch in range(4):
                            mm(pb[:, ch * 16:(ch + 1) * 16], qm[:, ch, j * 128:(j + 1) * 128],
                               kmeanT[:, ch, :], True, True)
                        GS = int(os.environ.get("MK_GS", "9"))
                        if GS >= 1:
                            tt(gsf[:, :, 0:8], pb[:, 0:64].rearrange("p (h n) -> p h n", h=8),
                               bass.AP(rtab, 256 + j * 8, [[320, 128], [0, 8], [1, 8]]), ALU.add)
                        if GS >= 2:
                            for h in range(8):
                                S.op("dve", lambda: nc.vector.max(out=m8[:, 0:8], in_=gsf[:, h, :]),
                                     [gsf[:, h, :]], [m8[:, 0:8]])
                                ts(tk32[:, h * 8:(h + 1) * 8], gsf[:, h, 0:8], m8[:, 2:3], None, ALU.is_ge)
                        if GS >= 3:
                            tt(tk32[:, :].rearrange("p (h n) -> p h n", h=8), tk32[:, :].rearrange("p (h n) -> p h n", h=8),
                               bass.AP(rtab, 288 + j * 8, [[320, 128], [0, 8], [1, 8]]), ALU.add)
                            ts(mbtok[:, j, :], tk32[:, :], -1.0, -MASKV, ALU.add, ALU.mult)
            S5 = W([(0, [8, 512], win[:, :, 2328:2840])])
            if not PLAN[0]:
                w5_ = S5[:, 0:4096].rearrange("p (c f) -> p c f", c=8)
                for j in range(4):
                    t_ = tr * 4 + j
                    pb = psM.next()
                    for c in range(8):
                        mm(pb[:, :], u_tr[:, c, j * 128:(j + 1) * 128], w5_[:, c, :], c == 0, c == 7)
                    cp(vm[:, t_, :, 0:64], pb[:, :].rearrange("p (h d) -> p h d", h=8), e="act" if j % 2 else "dve")

            if SUB < 1:
                continue
            if not PLAN[0]:
                S.mark("tr%d compress" % tr)
            c_lo = 0 if tr == 0 else 32 * tr - 1
            c_hi = 32 * tr + 30
            nb = c_hi - c_lo + 1
            if not PLAN[0]:
                pe_drain()
            for which in range(2):
                wsrc = wd["cmp_w1_k" if which == 0 else "cmp_w1_v"].rearrange("(l d) h -> d l h", d=64)
                CW = W([(q4 * 1024, [8, 128], wsrc[:, q4 * 8:(q4 + 1) * 8, :]) for q4 in range(4)])
                if PLAN[0]:
                    continue
                srcT = kcmpT if which == 0 else vcmpT
                w1t = CW[0:64, 0:4096].rearrange("p (l h) -> p l h", l=32)
                if tr == 0:
                    pt = posk if which == 0 else posv
                    pb = psM.next()
                    for l in range(32):
                        mm(pb[:, 0:1], w1t[:, l, :], pt[0:64, l:l + 1], l == 0, l == 31)
                    cp(cbias[:, which:which + 1], pb[:, 0:1])
                for g in range(2):
                    if g == 1:
                        cp(gtmp[0:64, 0:528], srcT[64:128, 0:528])
                        src_t = gtmp
                    else:
                        src_t = srcT
                    pbA = ps[which * 2 + g]
                    for l in range(32):
                        col0 = 16 * c_lo + l - 512 * tr + 16
                        rhs_ = bass.AP(src_t, col0, [[528, 64], [16, nb]])
                        mm(pbA[:, 0:nb], w1t[:, l, :], rhs_, l == 0, l == 31)
                cp(srcT[:, 0:16], srcT[:, 512:528])
            if not PLAN[0]:
                pe_drain()
                for which in range(2):
                    for g in range(2):
                        pbA = ps[which * 2 + g]
                        xx = hid[:, 0, 0:nb]
                        x2 = hid[:, 1, 0:nb]
                        ts(xx, pbA[:, 0:nb], cbias[:, which:which + 1], None, ALU.add)
                        tt(x2, xx, xx, ALU.mult)
                        ts(x2, x2, 0.044715, 1.0, ALU.mult, ALU.add)
                        tt(x2, x2, xx, ALU.mult)
                        act(hid[:, 2, 0:nb], x2, AF.Sigmoid, scale=1.5957691216057308)
                        if which == 0:
                            tt(hidb[:, g, 0:nb], xx, hid[:, 2, 0:nb], ALU.mult)
                        else:
                            tt(hidv[:, g, c_lo:c_lo + nb], xx, hid[:, 2, 0:nb], ALU.mult)
            if SUB < 2:
                continue
            def attend_multi(jobs):
                items = []
                for jb in jobs:
                    jb["npv"] = 0
                    jb["npv_total"] = sum((c1 - c0) // 128 for (kt, c0, c1, extra) in jb["tiles"])
                    for t_ in jb["tiles"]:
                        items.append((jb, t_))

                def emit_scores(idx):
                    jb, (kt, c0, c1, extra) = items[idx]
                    if "q" not in jb:
                        jb["q"] = jb["qfn"]()
                        jb["ops"] = psO.next()
                    sp_ = psS.next()
                    mask_fn = jb.get("mask")
                    n_extra = len(extra) + (1 if mask_fn is not None else 0)
                    mm(sp_[:, c0:c1], jb["kT"](kt), jb["q"][:, c0:c1], True, n_extra == 0)
                    k_ = 0
                    if mask_fn is not None:
                        k_ += 1
                        ml, mr = mask_fn(kt)
                        mm(sp_[:, c0:c1], ml, mr[:, c0:c1], False, k_ == n_extra)
                    for (e0, en, el, er) in extra:
                        k_ += 1
                        mm(sp_[:, e0:e0 + en], el, er, False, k_ == n_extra)
                    return sp_

                DEPTH = 2
                pend = [emit_scores(i) for i in range(min(DEPTH, len(items)))]
                for ti in range(len(items)):
                    jb, (kt, c0, c1, extra) = items[ti]
                    sp_ = pend.pop(0)
                    if ti + DEPTH < len(items):
                        pend.append(emit_scores(ti + DEPTH))
                    pt = ptr.next()
                    if jb["narrow"]:
                        for j in range(c0 // 128, c1 // 128):
                            act(pt[:, j * 128:(j + 1) * 128], sp_[:, j * 128:(j + 1) * 128], AF.Exp,
                                bias=jb["bias"](kt, j), scale=0.125)
                    else:
                        act(pt[:, c0:c1], sp_[:, c0:c1], AF.Exp, bias=jb["bias"](kt, None), scale=0.125)
                    ncols = jb.get("ncols", 65)
                    for j in range(c0 // 128, c1 // 128):
                        jb["npv"] += 1
                        mm(jb["ops"][:, j * 128:j * 128 + ncols], pt[:, j * 128:(j + 1) * 128], jb["v"](kt),
                           jb["npv"] == 1, jb["npv"] == jb["npv_total"])
                    if jb["npv"] == jb["npv_total"]:
                        jb["done"](jb["ops"])

            triA = cb[:, OB["triA"]:OB["triA"] + 128]
            triB = cb[:, OB["triB"]:OB["triB"] + 128]

            def _jhi(kt, tr_, dmax):
                return min(3, (dmax + 126) // 128 + kt - 4 * tr_)

            def causal_tiles(tr_, dmax=1 << 30):
                tl = []
                for kt in range(0, 4 * tr_ + 4):
                    i = kt - 4 * tr_
                    j_lo = max(0, i)
                    j_hi = _jhi(kt, tr_, dmax)
                    if j_hi < j_lo:
                        continue
                    extra = [(128 * i, 128, identb, triA)] if i >= 0 else []
                    tl.append((kt, 128 * j_lo, 128 * (j_hi + 1), extra))
                return tl

            def win_tiles(tr_, dmax=1 << 30):
                tl = []
                for kt in range(4 * tr_ - 4, 4 * tr_ + 4):
                    if kt < 0:
                        continue
                    i = kt - 4 * tr_
                    j_hi = _jhi(kt, tr_, dmax)
                    if i < 0:
                        i2 = i + 4
                        jh = min(i2, j_hi)
                        if jh < 0:
                            continue
                        extra = [(128 * i2, 128, identb, triB)] if jh == i2 else []
                        tl.append((kt, 0, 128 * (jh + 1), extra))
                    else:
                        if j_hi < i:
                            continue
                        tl.append((kt, 128 * i, 128 * (j_hi + 1), [(128 * i, 128, identb, triA)]))
                return tl

            def dmax_of(slope):
                return int(np.ceil(64.0 / float(slope)))

            if not PLAN[0]:
                def bias_head(hidx, narrow):
                    def f(kt, j):
                        if narrow:
                            di = kt - (4 * tr + j) + 15
                            o_ = OF["bn"] + hidx * 16 + di
                        else:
                            di = kt - 4 * tr + 12
                            o_ = OF["bw"] + hidx * 16 + di
                        return cf[:, o_:o_ + 1]
                    return f

                def bias_cmp(h, narrow):
                    def f(kt, j):
                        if narrow:
                            o_ = OF["cn"] + h * 16 + (4 * tr + j)
                        else:
                            o_ = OF["cw"] + h * 4 + tr
                        return cf[:, o_:o_ + 1]
                    return f

                def norm_coef(ob_, gate_ap):
                    ov_ = ob_[:, :].rearrange("p (j c) -> p j c", j=4)
                    ts(smal[:, 0:4], ov_[:, :, 64], TINY, None, ALU.max)
                    S.op("dve", lambda: nc.vector.reciprocal(out=smal[:, 4:8], in_=smal[:, 0:4]),
                         [smal[:, 0:4]], [smal[:, 4:8]])
                    if gate_ap is not None:
                        tt(smal[:, 8:12], smal[:, 4:8], gate_ap, ALU.mult)
                    return ov_

                def qpad(src3, ch, bp, eng):
                    half = bp // 64
                    b = qz[2 * half + (qzc[half] % 2)]
                    qzc[half] += 1
                    cp(b[bp:bp + 64, :], src3[bp:bp + 64, ch, :], e=eng)
                    return b

            if not PLAN[0]:
                if tr >= 2 and MDBG >= 1:
                    for j in range(4):
                        pb2 = psM.next()
                        tp(pb2[0:64, 0:128], mbtok[:, j, :], identf)
                        cp(mbTm[0:64, j * 128:(j + 1) * 128], pb2[0:64, 0:128])
            if not PLAN[0]:
                for g in range(2):
                    pb2 = psM.next()
                    mm(pb2[:, 0:nb], w2k[:, :], hidb[:, g, 0:nb], True, True)
                    cp(kcT[64 * g:64 * g + 64, c_lo:c_lo + nb], pb2[64 * g:64 * g + 64, 0:nb])
                    pb3 = psM.next()
                    mm(pb3[:, 0:64], hidv[:, g, :], w2v[:, :], True, True)
                    cp(vca[:, g, 0:64], pb3[:, 0:64])
                S.mark("tr%d nsa-cmp" % tr)
                ots = [otk[0], otk[1]]
            if SUB < 3:
                continue
            if not PLAN[0]:
                jobs = []
                for g in range(2):
                    ot = ots[g]
                    for hg in range(4):
                        h = g * 4 + hg
                        ch, bp = h % 4, 64 * (h // 4)
                        nar = bool(sl_n[h] > 0.3)

                        def done(oc, ot=ot, hg=hg, g=g, h=h):
                            ocv = norm_coef(oc, gsig[:, 4 * tr:4 * tr + 4, h * 3 + 0])
                            for j in range(4):
                                ts(ot[:, j, hg, :], ocv[:, j, 0:64], smal[:, 8 + j:9 + j], None, ALU.mult)
                                if tr >= 2:
                                    if hg == 0:
                                        ts(impa[:, j, g, :], ocv[:, j, 65:97], smal[:, 4 + j:5 + j], None, ALU.mult)
                                    else:
                                        stt(impa[:, j, g, :], ocv[:, j, 65:97], smal[:, 4 + j:5 + j],
                                            impa[:, j, g, :], ALU.mult, ALU.add)
                        jobs.append(dict(kT=lambda kt: kcT[:, :],
                                         qfn=lambda ch=ch, bp=bp: qpad(qn, ch, bp, "dve"),
                                         v=lambda kt, g=g: vca[:, g, :],
                                         tiles=[(0, 0, 512, [(0, 512, identb, cmsk[:, :])])],
                                         bias=bias_cmp(h, nar), narrow=nar, mask=None, ncols=97, done=done))
                attend_multi(jobs)
                if tr >= 2:
                    for j in range(4):
                        kp = rtab[:, j * 32:(j + 1) * 32]
                        fo = rtab[:, 128 + j * 32:128 + (j + 1) * 32]
                        for g in range(2):
                            iv = tk32[:, 0:32]
                            tt(iv, impa[:, j, g, :], kp, ALU.mult)
                            tt(iv, iv, fo, ALU.add)
                            S.op("dve", lambda: nc.vector.max(out=m8[:, 0:8], in_=iv), [iv], [m8[:, 0:8]])
                            S.op("dve", lambda: nc.vector.match_replace(out=tk32[:, 32:64], in_to_replace=m8[:, 0:8],
                                                                         in_values=iv, imm_value=-3.0e38),
                                 [iv, m8[:, 0:8]], [tk32[:, 32:64]])
                            S.op("dve", lambda: nc.vector.max(out=m8[:, 8:16], in_=tk32[:, 32:64]),
                                 [tk32[:, 32:64]], [m8[:, 8:16]])
                            ts(tk32[:, 32:64], iv, m8[:, 15:16], None, ALU.is_ge)
                            ts(mbtokN[:, j, g, :], tk32[:, 32:64], -1.0, -MASKV, ALU.add, ALU.mult)
                S.mark("tr%d moba" % tr)
                jobs = []
                for hp in range(4):
                    ot = otr.next()
                    for h2 in range(2):
                        h = hp * 2 + h2
                        ch, bp = hp, 64 * h2
                        nar = bool(sl_m[h] > 0.3)
                        mfn = None
                        if tr >= 2 and MDBG >= 2:
                            def mfn(kt, h=h):
                                c_ = 8 * h + kt // 2
                                l_ = bass.AP(cb, OB["ident"] + c_, [[NBS, 128], [0, 128]])
                                return l_, mbTm[:, :]

                        def done(om, ot=ot, h2=h2, hp=hp):
                            ov_ = norm_coef(om, None)
                            for j in range(4):
                                ts(ot[:, j, h2, :], ov_[:, j, 0:64], smal[:, 4 + j:5 + j], None, ALU.mult)
                            if h2 == 1:
                                pb = psM.next()
                                for j in range(4):
                                    tp(pb[:, j * 128:(j + 1) * 128], ot[:, j, 0:2, :], identf)
                                cp(omT[:, hp, :], pb[:, :], e="act")
                        jobs.append(dict(kT=lambda kt, ch=ch: kmT[:, ch, kt * 128:(kt + 1) * 128],
                                         qfn=lambda ch=ch, bp=bp: qpad(qm, ch, bp, "dve"),
                                         v=lambda kt, h=h: vm[:, kt, h, :],
                                         tiles=causal_tiles(tr, dmax_of(sl_m[h])), bias=bias_head(8 + h, nar),
                                         narrow=nar, mask=mfn, done=done))
                attend_multi(jobs)

                if tr >= 2:
                    for j in range(4):
                        for g in range(2):
                            pb = psM.next()
                            tp(pb[0:32, 0:128], mbtokN[:, j, g, :], identf)
                            cp(mbTn[64 * g:64 * g + 32, g, j * 128:(j + 1) * 128], pb[0:32, 0:128])
                S.mark("tr%d nsa-selwin" % tr)
                jobs = []
                for g in range(2):
                    ot = ots[g]
                    for hg in range(4):
                        h = g * 4 + hg
                        ch, bp = h % 4, 64 * (h // 4)
                        nar = bool(sl_n[h] > 0.3)
                        qc = {}

                        def qfn(ch=ch, bp=bp, qc=qc):
                            if "q" not in qc:
                                qc["q"] = qpad(qn, ch, bp, "dve")
                            return qc["q"]
                        mfn = None
                        if tr >= 2:
                            def mfn(kt, g=g):
                                return esel[:, kt, :], mbTn[:, g, :]

                        def done_sel(ob_, ot=ot, hg=hg, h=h):
                            ov_ = norm_coef(ob_, gsig[:, 4 * tr:4 * tr + 4, h * 3 + 1])
                            for j in range(4):
                                stt(ot[:, j, hg, :], ov_[:, j, 0:64], smal[:, 8 + j:9 + j], ot[:, j, hg, :],
                                    ALU.mult, ALU.add)

                        def done_win(ob_, ot=ot, hg=hg, h=h, g=g):
                            ov_ = norm_coef(ob_, gsig[:, 4 * tr:4 * tr + 4, h * 3 + 2])
                            for j in range(4):
                                stt(ot[:, j, hg, :], ov_[:, j, 0:64], smal[:, 8 + j:9 + j], ot[:, j, hg, :],
                                    ALU.mult, ALU.add)
                            if hg == 3:
                                for i2 in range(2):
                                    pb = psM.next()
                                    for j in range(4):
                                        tp(pb[:, j * 128:(j + 1) * 128], ot[:, j, 2 * i2:2 * i2 + 2, :], identf)
                                    cp(onT[:, 2 * g + i2, :], pb[:, :], e="act")
                        jobs.append(dict(kT=lambda kt: kslcT[:, kt * 128:(kt + 1) * 128], qfn=qfn,
                                         v=lambda kt, g=g: vslc[:, kt, g, :],
                                         tiles=causal_tiles(tr, dmax_of(sl_n[h])), bias=bias_head(h, nar),
                                         narrow=nar, mask=mfn, done=done_sel))
                        jobs.append(dict(kT=lambda kt: kwinT[:, kt * 128:(kt + 1) * 128], qfn=qfn,
                                         v=lambda kt, g=g: vwin[:, kt, g, :],
                                         tiles=win_tiles(tr, dmax_of(sl_n[h])), bias=bias_head(h, nar),
                                         narrow=nar, mask=None, done=done_win))
                attend_multi(jobs)

            if SUB < 5:
                continue
            if not PLAN[0]:
                S.mark("tr%d merge" % tr)
            for dc in range(8):
                c0 = dc * 128
                MJ = W([(0, [8, 128], win[:, :, 2840 + c0:2840 + c0 + 128]),
                        (1024, [8, 128], win[:, :, 3864 + c0:3864 + c0 + 128]),
                        (2048, [4, 128], wupn[:, :, c0:c0 + 128]),
                        (2560, [4, 128], wupm[:, :, c0:c0 + 128])])
                if PLAN[0]:
                    continue
                ga_ = MJ[:, 0:1024].rearrange("p (c f) -> p c f", c=8)
                gb_ = MJ[:, 1024:2048].rearrange("p (c f) -> p c f", c=8)
                un_ = MJ[:, 2048:2560].rearrange("p (c f) -> p c f", c=4)
                um_ = MJ[:, 2560:3072].rearrange("p (c f) -> p c f", c=4)
                pga = psG2.next()
                pgb = psG2.next()
                pyn = psO.next()
                pym = psO.next()
                for c in range(8):
                    mm(pga[:, :], ga_[:, c, :], u_tr[:, c, :], c == 0, c == 7)
                for c in range(8):
                    mm(pgb[:, :], gb_[:, c, :], u_tr[:, c, :], c == 0, c == 7)
                for c in range(4):
                    mm(pyn[:, :], un_[:, c, :], onT[:, c, :], c == 0, c == 3)
                for c in range(4):
                    mm(pym[:, :], um_[:, c, :], omT[:, c, :], c == 0, c == 3)
                sA = sgAr.next()
                sB = sgBr.next()
                act(sA[:, :], pga[:, :], AF.Sigmoid)
                act(sB[:, :], pgb[:, :], AF.Sigmoid)
                tt(sA[:, :], sA[:, :], pyn[:, :], ALU.mult)
                tt(sB[:, :], sB[:, :], pym[:, :], ALU.mult)
                tt(mixed[:, dc, :], sA[:, :], sB[:, :], ALU.add)
            for dh in range(2):
                WO = W([(0, [8, 512], wout[:, :, dh * 512:(dh + 1) * 512])])
                if PLAN[0]:
                    continue
                wo_ = WO[:, 0:4096].rearrange("p (c f) -> p c f", c=8)
                for d4 in range(4):
                    dc = dh * 4 + d4
                    pb = psM.next()
                    for c in range(8):
                        mm(pb[:, :], wo_[:, c, d4 * 128:(d4 + 1) * 128], mixed[:, c, :], c == 0, c == 7)
                    tt(hT[:, dc, trs(tr)], hT[:, dc, trs(tr)], pb[:, :], ALU.add)

    def run_all():
        if not PLAN[0]:
            S.mark("ffn1")
        if stage >= 1:
            scoped(phase_ffn, "ffn1_w1", "ffn1_w3", "ffn1_w2", 0, "a")
        if stage >= 2:
            scoped(phase_mixer)
        if stage >= 3:
            if not PLAN[0]:
                S.mark("ffn2")
            scoped(phase_ffn, "ffn2_w1", "ffn2_w3", "ffn2_w2", 2, "b")
        if stage >= 4:
            if not PLAN[0]:
                S.mark("ple")
            scoped(phase_ple)

    PLAN[0] = True
    run_all()
    PLAN[0] = False
    scoped(phase_load)
    run_all()
    S.mark("final")
    scoped(phase_final)
    S.mark("end")
    S.finish()
    _LAST["S"] = S
    return nc


_NC_CACHE = {}
_LAST = {}


def _prep_inputs(inputs):
    cf_np, _, cb_np, _ = _make_consts()
    f = lambda a: np.ascontiguousarray(np.asarray(a, dtype=np.float32))
    sh = {}
    for nm in ("ffn1_w1", "ffn1_w3", "ffn1_w2", "w_in", "cmp_w1_k", "cmp_w2_k", "cmp_w1_v", "cmp_w2_v",
               "w_up_nsa", "w_up_moba", "w_out", "ffn2_w1", "ffn2_w3", "ffn2_w2", "w_ple_gate", "w_ple"):
        sh[nm] = f(inputs[nm])[0]
    wi = sh["w_in"].copy()
    perm = [0, 4, 1, 5, 2, 6, 3, 7]
    wi[:, 0:512] = sh["w_in"][:, 0:512].reshape(D, 8, 64)[:, perm, :].reshape(D, 512)
    sh["w_in"] = np.ascontiguousarray(wi)
    for nm, src in (("pos_kT", "cmp_pos_k"), ("pos_vT", "cmp_pos_v")):
        pt = f(inputs[src])[0].T
        sh[nm] = np.ascontiguousarray(np.concatenate([pt, pt], axis=0))
    g = np.stack([f(inputs[n])[0] for n in ("ffn1_norm", "mix_norm", "ffn2_norm", "ple_norm")], axis=0)
    sh["gains"] = np.ascontiguousarray(g.reshape(4, 8, 128).transpose(2, 0, 1).reshape(128, 32))
    sh["gfin"] = np.ascontiguousarray(np.broadcast_to(f(inputs["final_norm"])[None, :], (128, D)))
    sh["cf32"] = cf_np
    sh["cbf16"] = cb_np
    return sh


def kernel(**inputs):
    stage = int(os.environ.get("MK_STAGE", "99"))
    ncores = int(os.environ.get("MK_CORES", "8"))
    if stage not in _NC_CACHE:
        _NC_CACHE[stage] = build_nc(stage)
    nc = _NC_CACHE[stage]
    sh = _prep_inputs(inputs)
    x = np.asarray(inputs["x"], dtype=np.float32)
    p = np.asarray(inputs["p"], dtype=np.float32)
    in_maps = []
    for b in range(ncores):
        m = dict(sh)
        m["x"] = np.ascontiguousarray(x[b])
        m["p"] = np.ascontiguousarray(p[0, b])
        in_maps.append(m)
    res = run_bass_kernel_spmd(nc, in_maps, core_ids=list(range(ncores)))
    out = np.stack([np.asarray(r["out"], dtype=np.float32) for r in res.results], axis=0)
    return out
```
